# Optimizing a Trainium2 kernel written in Bass

```python
import math
import jax, jax.numpy as jnp
from jax import lax
import numpy as np

D_MODEL = 2048
BATCH = 16
SEQ = 2048
DEPTH = 2

GRID_W = 64
CTX_LEN = 256

D_MIX = D_MODEL
D_SSD = D_MIX // 2
SSD_HEAD_DIM = 64
SSD_HEADS = D_SSD // SSD_HEAD_DIM
SSD_GROUPS = 2
SSD_HPG = SSD_HEADS // SSD_GROUPS
SSD_STATE = 128
SSD_CONV = 3
SSD_CHUNK = 128
SSD_CONV_DIM = D_SSD + 2 * SSD_GROUPS * SSD_STATE
MLA_HEADS = 8
MLA_NOPE = 128
MLA_ROPE = 64
MLA_V = 128
D_MLA = MLA_HEADS * MLA_V
Q_LORA = 512
KV_LORA = 256
ROPE_THETA = 10000.0
ATTN_SCALE = (MLA_NOPE + MLA_ROPE) ** -0.5
ATTN_BLOCK = 128
IN_SPLITS = (D_SSD,
             D_SSD + SSD_CONV_DIM,
             D_SSD + SSD_CONV_DIM + 2 * SSD_HEADS,
             D_SSD + SSD_CONV_DIM + 2 * SSD_HEADS + Q_LORA)
N_IN = IN_SPLITS[-1] + KV_LORA + MLA_ROPE
N_EXPERTS = 16
N_EXPERT_GROUPS = 4
EXPERTS_PER_GROUP = N_EXPERTS // N_EXPERT_GROUPS
TOP_K = 2
D_EXPERT = 512
N_MOD = 6
NORM_EPS = 1e-6

kernel_name = "hymba_ssd_mla_grouped_moe_dit"


def rmsnorm(x, g):
    xf = x.astype(jnp.float32)
    y = xf * lax.rsqrt(jnp.mean(xf * xf, axis=-1, keepdims=True) + NORM_EPS)
    return (y * g.astype(jnp.float32)).astype(x.dtype)


def modulate(h, shift, scale):
    return h * (1 + scale) + shift


def adaln(cond, w, b):
    return jnp.split(jax.nn.silu(cond) @ w + b, N_MOD, axis=-1)


def depthwise_conv(u, w, b):
    out = lax.conv_general_dilated(
        u, w[:, None, :].astype(u.dtype), window_strides=(1,),
        padding=[((SSD_CONV - 1) // 2, SSD_CONV // 2)],
        dimension_numbers=("NWC", "WIO", "NWC"),
        feature_group_count=u.shape[-1])
    return jax.nn.silu(out + b)


def ssd_inputs(xbc_raw, dt_raw, conv_w, conv_b, dt_bias):
    b, l = xbc_raw.shape[:2]
    xbc = depthwise_conv(xbc_raw, conv_w, conv_b)
    xs, bm, cm = jnp.split(xbc, (D_SSD, D_SSD + SSD_GROUPS * SSD_STATE), axis=-1)
    dt = jax.nn.softplus(dt_raw.astype(jnp.float32).reshape(b, l, 2, SSD_HEADS)
                         + dt_bias.astype(jnp.float32))
    return xs, bm, cm, dt


def _ssd_prepare(xs, dt, a, bm):
    f32 = jnp.float32
    b, l = xs.shape[:2]
    nc = l // SSD_CHUNK
    x = xs.astype(f32).reshape(b, nc, SSD_CHUNK, SSD_GROUPS, SSD_HPG, SSD_HEAD_DIM)
    dtc = dt.astype(f32).reshape(b, nc, SSD_CHUNK, SSD_GROUPS, SSD_HPG)
    bc = bm.astype(f32).reshape(b, nc, SSD_CHUNK, SSD_GROUPS, SSD_STATE)
    a_cum = jnp.cumsum(dtc * a.astype(f32).reshape(SSD_GROUPS, SSD_HPG), axis=2)
    return x * dtc[..., None], bc, a_cum


def _ssd_states(xdt, bc, a_cum, init):
    decay = jnp.exp(a_cum[:, :, -1:] - a_cum)
    states = jnp.einsum("bcqgn,bcqge,bcqgep->bcgepn", bc, decay, xdt)
    chunk_decay = jnp.exp(a_cum[:, :, -1])

    def step(s, inp):
        st, dc = inp
        return s * dc[..., None, None] + st, s

    final, prev = lax.scan(step, init, (jnp.moveaxis(states, 1, 0),
                                        jnp.moveaxis(chunk_decay, 1, 0)))
    return jnp.moveaxis(prev, 0, 1), final


def _ssd_outputs(xdt, bc, cm, a_cum, prev):
    b, nc = xdt.shape[:2]
    cc = cm.astype(jnp.float32).reshape(b, nc, SSD_CHUNK, SSD_GROUPS, SSD_STATE)
    mask = jnp.tril(jnp.ones((SSD_CHUNK, SSD_CHUNK), dtype=bool))[:, :, None, None]
    seg = a_cum[:, :, :, None] - a_cum[:, :, None, :]
    lmat = jnp.exp(jnp.where(mask, seg, -jnp.inf))
    cb = jnp.einsum("bcign,bcjgn->bcgij", cc, bc)
    y_diag = jnp.einsum("bcgij,bcijge,bcjgep->bcigep", cb, lmat, xdt)
    y_off = jnp.einsum("bcign,bcgepn,bcige->bcigep", cc, prev, jnp.exp(a_cum))
    return (y_diag + y_off).reshape(b, nc * SSD_CHUNK, D_SSD)


def ssd_scan(xs, dt, a, bm, cm, init):
    xdt, bc, a_cum = _ssd_prepare(xs, dt, a, bm)
    prev, final = _ssd_states(xdt, bc, a_cum, init)
    return _ssd_outputs(xdt, bc, cm, a_cum, prev), final


def ssd_final_state(xs, dt, a, bm, init):
    xdt, bc, a_cum = _ssd_prepare(xs, dt, a, bm)
    _, final = _ssd_states(xdt, bc, a_cum, init)
    return final


def ssd_merge(y_fwd, y_bwd, xs, z, d_skip, norm_g):
    b, l = xs.shape[:2]
    skip = (xs.astype(jnp.float32).reshape(b, l, SSD_HEADS, SSD_HEAD_DIM)
            * d_skip.astype(jnp.float32)[:, None]).reshape(b, l, D_SSD)
    y = y_fwd + y_bwd + skip
    return rmsnorm(y * jax.nn.silu(z.astype(jnp.float32)), norm_g).astype(z.dtype)


def rope_2d_tables(seq_len):
    n_rows = seq_len // GRID_W
    rows = jnp.repeat(jnp.arange(n_rows, dtype=jnp.float32), GRID_W)
    cols = jnp.tile(jnp.arange(GRID_W, dtype=jnp.float32), n_rows)
    nf = MLA_ROPE // 4
    inv = ROPE_THETA ** (-jnp.arange(nf, dtype=jnp.float32) / nf)
    ang = jnp.stack([rows[:, None] * inv, cols[:, None] * inv], axis=1)
    return jnp.cos(ang), jnp.sin(ang)


def apply_rope_2d(x, cos, sin):
    shp = x.shape
    xr = x.reshape(shp[:-1] + (2, 2, MLA_ROPE // 4))
    x1, x2 = xr[..., 0, :], xr[..., 1, :]
    out = jnp.stack([x1 * cos - x2 * sin, x2 * cos + x1 * sin], axis=-2)
    return out.reshape(shp).astype(x.dtype)


def mla_queries(q_a, norm_g, w_q_b):
    b, l = q_a.shape[:2]
    q = (rmsnorm(q_a, norm_g) @ w_q_b).reshape(b, l, MLA_HEADS, MLA_NOPE + MLA_ROPE)
    return q[..., :MLA_NOPE], q[..., MLA_NOPE:]


def mla_keys(kv_a, norm_g, w_kv_b):
    b, l = kv_a.shape[:2]
    kv_c, k_pe = kv_a[..., :KV_LORA], kv_a[..., KV_LORA:]
    kv = (rmsnorm(kv_c, norm_g) @ w_kv_b).reshape(b, l, MLA_HEADS, MLA_NOPE + MLA_V)
    return kv[..., :MLA_NOPE], k_pe, kv[..., MLA_NOPE:]


def attend_block(q_nope, q_pe, k_nope, k_pe, v):
    s = (jnp.einsum("bqhd,bkhd->bhqk", q_nope, k_nope)
         + jnp.einsum("bqhr,bkr->bhqk", q_pe, k_pe)).astype(jnp.float32) * ATTN_SCALE
    p = jax.nn.softmax(s, axis=-1)
    return jnp.einsum("bhqk,bkhd->bqhd", p.astype(v.dtype), v)


def blocked_attention(q_nope, q_pe, k_nope, k_pe, v):
    b, l = q_nope.shape[:2]
    nb = l // ATTN_BLOCK
    qn = q_nope.reshape(b, nb, ATTN_BLOCK, MLA_HEADS, MLA_NOPE).swapaxes(0, 1)
    qp = q_pe.reshape(b, nb, ATTN_BLOCK, MLA_HEADS, MLA_ROPE).swapaxes(0, 1)
    out = lax.map(lambda qs: attend_block(qs[0], qs[1], k_nope, k_pe, v), (qn, qp))
    return out.swapaxes(0, 1).reshape(b, l, D_MLA)


def moe(h, router_w, router_b, w_gate, w_up, w_down):
    f32 = jnp.float32
    scores = jax.nn.sigmoid(jnp.einsum("bld,de->ble", h.astype(f32), router_w.astype(f32)))
    sel = scores + router_b.astype(f32)
    grouped = sel.reshape(h.shape[:-1] + (N_EXPERT_GROUPS, EXPERTS_PER_GROUP))
    group_score = lax.top_k(grouped, TOP_K)[0].sum(-1)
    best = jnp.argmax(group_score, axis=-1)
    in_group = jnp.arange(N_EXPERT_GROUPS) == best[..., None]
    masked = jnp.where(in_group[..., None], grouped, -jnp.inf).reshape(h.shape[:-1] + (N_EXPERTS,))
    _, top_idx = lax.top_k(masked, TOP_K)
    top_w = jnp.take_along_axis(scores, top_idx, axis=-1)
    top_w = top_w / jnp.sum(top_w, axis=-1, keepdims=True)
    combine = jnp.sum(jax.nn.one_hot(top_idx, N_EXPERTS, dtype=f32) * top_w[..., None], axis=-2)
    out = jnp.zeros(h.shape, f32)
    for e in range(N_EXPERTS):
        y = (jax.nn.silu(h @ w_gate[e]) * (h @ w_up[e])) @ w_down[e]
        out = out + combine[..., e:e + 1] * y.astype(f32)
    return out.astype(h.dtype)


def hybrid_layer(x, ctx, c, c_ctx, p, router_w, router_b, cos, sin, update_ctx):
    b = x.shape[0]
    m_x = [m[:, None, :] for m in adaln(c, p["ada_w"], p["ada_b"])]
    m_c = adaln(c_ctx, p["ada_w"], p["ada_b"])
    hx = modulate(rmsnorm(x, p["norm1_g"]), m_x[0], m_x[1])
    hc = modulate(rmsnorm(ctx, p["norm1_g"]), m_c[0], m_c[1])
    z_x, xbc_x, dt_x, qa_x, kva_x = jnp.split(hx @ p["w_in"], IN_SPLITS, axis=-1)
    z_c, xbc_c, dt_c, qa_c, kva_c = jnp.split(hc @ p["w_in"], IN_SPLITS, axis=-1)

    a = -jnp.exp(p["a_log"].astype(jnp.float32))
    xs_c, b_c, cm_c, dtv_c = ssd_inputs(xbc_c, dt_c, p["conv_w"], p["conv_b"], p["dt_bias"])
    xs_x, b_x, cm_x, dtv_x = ssd_inputs(xbc_x, dt_x, p["conv_w"], p["conv_b"], p["dt_bias"])
    zero = jnp.zeros((b, SSD_GROUPS, SSD_HPG, SSD_HEAD_DIM, SSD_STATE), jnp.float32)
    rev = lambda t: jnp.flip(t, axis=1)
    if update_ctx:
        yf_c, s_f = ssd_scan(xs_c, dtv_c[:, :, 0], a[0], b_c, cm_c, zero)
        yb_c, s_b = ssd_scan(rev(xs_c), rev(dtv_c[:, :, 1]), a[1], rev(b_c), rev(cm_c), zero)
        ssd_c = ssd_merge(yf_c, rev(yb_c), xs_c, z_c, p["d_skip"], p["ssd_norm_g"])
    else:
        s_f = ssd_final_state(xs_c, dtv_c[:, :, 0], a[0], b_c, zero)
        s_b = ssd_final_state(rev(xs_c), rev(dtv_c[:, :, 1]), a[1], rev(b_c), zero)
    yf_x, _ = ssd_scan(xs_x, dtv_x[:, :, 0], a[0], b_x, cm_x, s_f)
    yb_x, _ = ssd_scan(rev(xs_x), rev(dtv_x[:, :, 1]), a[1], rev(b_x), rev(cm_x), s_b)
    ssd_x = ssd_merge(yf_x, rev(yb_x), xs_x, z_x, p["d_skip"], p["ssd_norm_g"])

    kn_c, kp_c, v_c = mla_keys(kva_c, p["kv_norm_g"], p["w_kv_b"])
    kn_x, kp_x, v_x = mla_keys(kva_x, p["kv_norm_g"], p["w_kv_b"])
    kp_x = apply_rope_2d(kp_x, cos, sin)
    qn_x, qp_x = mla_queries(qa_x, p["q_norm_g"], p["w_q_b"])
    qp_x = apply_rope_2d(qp_x, cos[:, None], sin[:, None])
    att_x = blocked_attention(qn_x, qp_x,
                              jnp.concatenate([kn_c, kn_x], axis=1),
                              jnp.concatenate([kp_c, kp_x], axis=1),
                              jnp.concatenate([v_c, v_x], axis=1))

    x = x + m_x[2] * (jnp.concatenate([ssd_x, att_x.astype(x.dtype)], axis=-1) @ p["w_o"])
    h2 = modulate(rmsnorm(x, p["norm2_g"]), m_x[3], m_x[4])
    x = x + m_x[5] * moe(h2, router_w, router_b, p["w_gate"], p["w_up"], p["w_down"])

    if update_ctx:
        qn_c, qp_c = mla_queries(qa_c, p["q_norm_g"], p["w_q_b"])
        att_c = blocked_attention(qn_c, qp_c, kn_c, kp_c, v_c)
        ctx = ctx + m_c[2] * (jnp.concatenate([ssd_c, att_c.astype(ctx.dtype)], axis=-1) @ p["w_o"])
        h2c = modulate(rmsnorm(ctx, p["norm2_g"]), m_c[3], m_c[4])
        ctx = ctx + m_c[5] * moe(h2c, router_w, router_b, p["w_gate"], p["w_up"], p["w_down"])
    return x, ctx


def setup_inputs(seed: int = 0) -> dict:
    key = jax.random.key(seed)
    ks = jax.random.split(key, 32)
    f32 = jnp.float32

    def nrm(k, shape, s):
        return jax.random.normal(k, shape, f32) * s

    x = nrm(ks[0], (BATCH, SEQ, D_MODEL), 1.0)
    c = nrm(ks[1], (BATCH, D_MODEL), 1.0)
    ctx = nrm(ks[2], (BATCH, CTX_LEN, D_MODEL), 1.0)
    c_ctx = nrm(ks[3], (D_MODEL,), 1.0)
    ada_w = nrm(ks[4], (DEPTH, D_MODEL, N_MOD * D_MODEL), 0.5 * D_MODEL ** -0.5)
    ada_b = nrm(ks[5], (DEPTH, N_MOD * D_MODEL), 0.02)
    norm1_g = 1.0 + nrm(ks[6], (DEPTH, D_MODEL), 0.05)
    norm2_g = 1.0 + nrm(ks[7], (DEPTH, D_MODEL), 0.05)
    w_in = nrm(ks[8], (DEPTH, D_MODEL, N_IN), D_MODEL ** -0.5)
    conv_w = nrm(ks[9], (DEPTH, SSD_CONV, SSD_CONV_DIM), SSD_CONV ** -0.5)
    conv_b = nrm(ks[10], (DEPTH, SSD_CONV_DIM), 0.02)
    dt0 = jnp.exp(jax.random.uniform(ks[11], (DEPTH, 2, SSD_HEADS), f32,
                                     minval=math.log(1e-3), maxval=math.log(1e-1)))
    dt_bias = dt0 + jnp.log(-jnp.expm1(-dt0))
    a_log = jnp.log(jax.random.uniform(ks[12], (DEPTH, 2, SSD_HEADS), f32, minval=1.0, maxval=16.0))
    d_skip = 1.0 + nrm(ks[13], (DEPTH, SSD_HEADS), 0.05)
    ssd_norm_g = 1.0 + nrm(ks[14], (DEPTH, D_SSD), 0.05)
    q_norm_g = 1.0 + nrm(ks[15], (DEPTH, Q_LORA), 0.05)
    w_q_b = nrm(ks[16], (DEPTH, Q_LORA, MLA_HEADS * (MLA_NOPE + MLA_ROPE)), Q_LORA ** -0.5)
    kv_norm_g = 1.0 + nrm(ks[17], (DEPTH, KV_LORA), 0.05)
    w_kv_b = nrm(ks[18], (DEPTH, KV_LORA, MLA_HEADS * (MLA_NOPE + MLA_V)), KV_LORA ** -0.5)
    w_o = nrm(ks[19], (DEPTH, D_MIX, D_MODEL), D_MIX ** -0.5)
    router_w = nrm(ks[20], (D_MODEL, N_EXPERTS), D_MODEL ** -0.5)
    router_b = nrm(ks[21], (N_EXPERTS,), 0.01)
    w_gate = nrm(ks[22], (DEPTH, N_EXPERTS, D_MODEL, D_EXPERT), D_MODEL ** -0.5)
    w_up = nrm(ks[23], (DEPTH, N_EXPERTS, D_MODEL, D_EXPERT), D_MODEL ** -0.5)
    w_down = nrm(ks[24], (DEPTH, N_EXPERTS, D_EXPERT, D_MODEL), D_EXPERT ** -0.5)
    final_norm_g = 1.0 + nrm(ks[25], (D_MODEL,), 0.05)
    return {"x": x, "c": c, "ctx": ctx, "c_ctx": c_ctx,
            "ada_w": ada_w, "ada_b": ada_b, "norm1_g": norm1_g, "norm2_g": norm2_g,
            "w_in": w_in, "conv_w": conv_w, "conv_b": conv_b, "dt_bias": dt_bias,
            "a_log": a_log, "d_skip": d_skip, "ssd_norm_g": ssd_norm_g,
            "q_norm_g": q_norm_g, "w_q_b": w_q_b, "kv_norm_g": kv_norm_g, "w_kv_b": w_kv_b,
            "w_o": w_o, "router_w": router_w, "router_b": router_b,
            "w_gate": w_gate, "w_up": w_up, "w_down": w_down, "final_norm_g": final_norm_g}


def reference(x, c, ctx, c_ctx, ada_w, ada_b, norm1_g, norm2_g, w_in, conv_w, conv_b,
              dt_bias, a_log, d_skip, ssd_norm_g, q_norm_g, w_q_b, kv_norm_g, w_kv_b,
              w_o, router_w, router_b, w_gate, w_up, w_down, final_norm_g):
    cos, sin = rope_2d_tables(x.shape[1])
    for i in range(DEPTH):
        p = {"ada_w": ada_w[i], "ada_b": ada_b[i], "norm1_g": norm1_g[i], "norm2_g": norm2_g[i],
             "w_in": w_in[i], "conv_w": conv_w[i], "conv_b": conv_b[i], "dt_bias": dt_bias[i],
             "a_log": a_log[i], "d_skip": d_skip[i], "ssd_norm_g": ssd_norm_g[i],
             "q_norm_g": q_norm_g[i], "w_q_b": w_q_b[i], "kv_norm_g": kv_norm_g[i],
             "w_kv_b": w_kv_b[i], "w_o": w_o[i], "w_gate": w_gate[i], "w_up": w_up[i],
             "w_down": w_down[i]}
        x, ctx = hybrid_layer(x, ctx, c, c_ctx, p, router_w, router_b, cos, sin,
                              update_ctx=(i < DEPTH - 1))
    return rmsnorm(x, final_norm_g)
```

```python
import numpy as np
from contextlib import ExitStack
import concourse.bass as bass
import concourse.mybir as mybir
from concourse.bass_utils import run_bass_kernel_spmd

F32 = mybir.dt.float32
BF16 = mybir.dt.bfloat16
AF = mybir.ActivationFunctionType
ALU = mybir.AluOpType
AX = mybir.AxisListType

L = 2
D = 2048
TB = 2304
T = 2 * TB
EPS = 1e-6
NWIN = 3616
ATTN_SCALE = 192.0 ** -0.5
NEG = -30000.0


class R:
    __slots__ = ("lw", "rd")

    def __init__(self):
        self.lw = None
        self.rd = {}


class Prog:
    ENGS = ("pe", "act", "dve", "pool", "sp")
    NDS = 48
    NHW = 36

    def __init__(self, nc, es):
        self.nc = nc
        self.q = {e: [] for e in self.ENGS}
        self.cnt = {e: 0 for e in self.ENGS}
        self.waited = {e: {} for e in self.ENGS}
        self.sem = {e: es.enter_context(nc.semaphore("s_" + e)) for e in self.ENGS}
        self.dsem = [es.enter_context(nc.semaphore("d%d" % i)) for i in range(self.NDS)]
        self.dcnt = [0] * self.NDS
        self.dnext = 0
        self.dnext_sw = 0

    def _semof(self, key):
        return self.sem[key[1]] if key[0] == "e" else self.dsem[key[1]]

    def _deps(self, eng, reads, writes, extra=()):
        deps = {}

        def add(d):
            if d is None:
                return
            k, v = d
            if deps.get(k, 0) < v:
                deps[k] = v

        for r in reads:
            add(r.lw)
        for w in writes:
            add(w.lw)
            for kv in w.rd.items():
                add(kv)
        for d in extra:
            add(d)
        waits = []
        wd = self.waited[eng]
        for k, v in deps.items():
            if eng == "pe" and k == ("e", "pe"):
                continue
            if wd.get(k, 0) >= v:
                continue
            wd[k] = v
            waits.append((self._semof(k), v))
        return waits

    def emit(self, eng, fn, reads=(), writes=()):
        waits = self._deps(eng, reads, writes)
        self.cnt[eng] += 1
        key = ("e", eng)
        val = self.cnt[eng]
        sem = self.sem[eng]

        def thunk(e):
            for s, v in waits:
                e.wait_ge(s, v)
            fn(e).then_inc(sem, 1)

        self.q[eng].append(thunk)
        for r in reads:
            r.rd[key] = val
        for w in writes:
            w.lw = (key, val)
            w.rd = {}

    def dma(self, queue, out, in_, reads=(), writes=(), **kw):
        if queue == "pool":
            i = self.NHW + self.dnext_sw
            self.dnext_sw = (self.dnext_sw + 1) % (self.NDS - self.NHW)
        else:
            i = self.dnext
            self.dnext = (i + 1) % self.NHW
        prev = self.dcnt[i]
        self.dcnt[i] += 16
        val = self.dcnt[i]
        key = ("d", i)
        extra = [(key, prev)] if prev > 0 else []
        waits = self._deps(queue, reads, writes, extra)
        sem = self.dsem[i]

        def thunk(e):
            for s, v in waits:
                e.wait_ge(s, v)
            e.dma_start(out=out, in_=in_, **kw).then_inc(sem, 16)

        self.q[queue].append(thunk)
        for r in reads:
            r.rd[key] = val
        for w in writes:
            w.lw = (key, val)
            w.rd = {}

    def finish(self):
        waits = []
        for i in range(self.NDS):
            if self.dcnt[i] > 0:
                waits.append((self.dsem[i], self.dcnt[i]))
        for en in self.ENGS:
            if en != "sp" and self.cnt[en] > 0:
                waits.append((self.sem[en], self.cnt[en]))

        def thunk(e):
            for s, v in waits:
                e.wait_ge(s, v)

        self.q["sp"].append(thunk)

    def barrier(self):
        for en in self.ENGS:
            waits = []
            wd = self.waited[en]
            for i in range(self.NDS):
                k = ("d", i)
                if self.dcnt[i] > wd.get(k, 0):
                    wd[k] = self.dcnt[i]
                    waits.append((self.dsem[i], self.dcnt[i]))
            for e2 in self.ENGS:
                k = ("e", e2)
                if e2 != en and self.cnt[e2] > wd.get(k, 0):
                    wd[k] = self.cnt[e2]
                    waits.append((self.sem[e2], self.cnt[e2]))

            def thunk(e, waits=waits):
                for s_, v in waits:
                    e.wait_ge(s_, v)
            self.q[en].append(thunk)

    def flush(self):
        self.barrier()
        nc = self.nc
        q = self.q
        with nc.Block() as block:
            @block.tensor
            def _(e):
                for t in q["pe"]:
                    t(e)

            @block.scalar
            def _(e):
                for t in q["act"]:
                    t(e)

            @block.vector
            def _(e):
                for t in q["dve"]:
                    t(e)

            @block.gpsimd
            def _(e):
                for t in q["pool"]:
                    t(e)

            @block.sync
            def _(e):
                for t in q["sp"]:
                    t(e)
        self.q = {e: [] for e in self.ENGS}


def build(dbg=False, nlayers=L, phases=None):
    nc = bass.Bass("TRN2", target_bir_lowering=False)

    def din(name, shape, dt=F32):
        return nc.dram_tensor(name, list(shape), dt, kind="ExternalInput").ap()

    def dscr(name, shape, dt):
        return nc.dram_tensor(name, list(shape), dt, kind=("ExternalOutput" if dbg else "Internal")).ap()

    xin = din("xin", [2, 2048, D])
    cin = din("cin", [2, 256, D])
    ccT = din("ccT", [128, 16, 3])
    ada_w = din("ada_w", [L, D, 12288])
    ada_bT = din("ada_bT", [L, 128, 96])
    g1T = din("g1T", [L, 128, 16])
    g2T = din("g2T", [L, 128, 16])
    win = din("win", [L, D, NWIN])
    convw = din("convw", [L, 128, 36])
    convb = din("convb", [L, 128, 12])
    dtb = din("dtb", [L, 128, 32])
    alog = din("alog", [L, 128, 32])
    dskip = din("dskip", [L, 128, 16])
    ssdg = din("ssdg", [L, 128, 1024])
    qgT = din("qgT", [L, 128, 4])
    wq = din("wq", [L, 512, 2048])
    kvgT = din("kvgT", [L, 128, 2])
    wkv = din("wkv", [L, 256, 2048])
    wo = din("wo", [L, D, D])
    rwT = din("rwT", [128, 16, 16])
    rb = din("rb", [128, 16])
    wg = din("wg", [L, 16, D, 512])
    wu = din("wu", [L, 16, D, 512])
    wd = din("wd", [L, 16, 512, D])
    fng = din("fng", [128, D])
    c_ident = din("c_ident", [128, 128])
    c_tri = din("c_tri", [2, 128, 128])
    c_mask = din("c_mask", [2, 128, 512])
    c_cos = din("c_cos", [128, 2048])
    c_sin = din("c_sin", [128, 2048])
    out = nc.dram_tensor("out", [2, 2048, D], F32, kind="ExternalOutput").ap()

    RES = dscr("RES", [T, D], F32)
    ZT = dscr("ZT", [T, 1024], BF16)
    DTR = dscr("DTR", [T, 32], F32)
    XBC = dscr("XBC", [1536, T], BF16)
    QN = dscr("QN", [8, 128, T], BF16)
    QR = dscr("QR", [4, 128, T], BF16)
    KN = dscr("KN", [8, 128, T], BF16)
    KR = dscr("KR", [128, T], BF16)
    VV = dscr("VV", [T, 1024], BF16)
    YF = dscr("YF", [T, 1024], F32)
    YB = dscr("YB", [T, 1024], F32)
    MIX = dscr("MIX", [D, T], BF16)
    HT = dscr("HT", [D, T], BF16)
    COMB = dscr("COMB", [T, 16], F32)
    DBGM = dscr("DBGM", [128, 96, 3], F32)
    rRES = [R() for _ in range(36)]
    rZT = [R() for _ in range(36)]
    rDTR = [R() for _ in range(36)]
    rXBC = [R() for _ in range(2)]
    rQK = [R() for _ in range(2)]
    rYF = [R() for _ in range(36)]
    rYB = [R() for _ in range(36)]
    rMIX = [R() for _ in range(36)]
    rHT = [R() for _ in range(36)]
    rCOMB = [R() for _ in range(36)]

    def resid_src(l, b, ti):
        if l == 0:
            if ti < 2:
                return cin[b, ti * 128:(ti + 1) * 128, :]
            return xin[b, (ti - 2) * 128:(ti - 1) * 128, :]
        r0 = b * TB + ti * 128
        return RES[r0:r0 + 128, :]

    with ExitStack() as es:
        P = Prog(nc, es)

        uid = [0]

        def sbt(st, name, shape, dt):
            uid[0] += 1
            return st.enter_context(nc.sbuf_tensor("%s_%d" % (name, uid[0]), list(shape), dt))

        def pst(st, name, shape, dt):
            uid[0] += 1
            return st.enter_context(nc.psum_tensor("%s_%d" % (name, uid[0]), list(shape), dt))

        identf = sbt(es, "identf", [128, 128], F32)
        identb = sbt(es, "identb", [128, 128], BF16)
        onesf = sbt(es, "onesf", [128, 128], F32)
        onesb = sbt(es, "onesb", [128, 128], BF16)
        modS = sbt(es, "modS", [128, 96, 3], F32)
        gm1 = sbt(es, "gm1", [128, 16, 3], F32)
        gm2 = sbt(es, "gm2", [128, 16, 3], F32)
        rC = R()
        rmod = R()
        P.dma("sp", identf[:], c_ident, writes=[rC])
        P.emit("dve", lambda e: e.tensor_copy(identb[:], identf[:]), reads=[rC], writes=[rC])
        P.emit("dve", lambda e: e.memset(onesf[:], 1.0), writes=[rC])
        P.emit("dve", lambda e: e.memset(onesb[:], 1.0), writes=[rC])

        def rms_rstd(eng_src, src_ap, n, ss, rstd, junk, rsrc, rtmp):
            P.emit("act", lambda e: e.activation(junk, src_ap, AF.Square, accum_out=ss), reads=[rsrc], writes=[rtmp])
            P.emit("act", lambda e: e.activation(rstd, ss, AF.Sqrt, bias=EPS, scale=1.0 / n), reads=[rtmp], writes=[rtmp])
            P.emit("dve", lambda e: e.reciprocal(rstd, rstd), reads=[rtmp], writes=[rtmp])

        for l in range(nlayers):
            last = (l == L - 1)
            tiles_l = [(b, ti) for b in range(2) for ti in range(18) if not (last and ti < 2)]

            if phases is None or 0 in phases:
              with ExitStack() as st:
                scf = sbt(st, "scf", [128, 16, 3], F32)
                scb = sbt(st, "scb", [128, 16, 3], BF16)
                abT = sbt(st, "abT", [128, 96], F32)
                g1s = sbt(st, "g1s", [128, 16], F32)
                g2s = sbt(st, "g2s", [128, 16], F32)
                awf = [sbt(st, "awf%d" % i, [128, 16, 512], F32) for i in range(2)]
                rawf = [R(), R()]
                aw = [sbt(st, "aw%d" % i, [128, 16, 512], BF16) for i in range(2)]
                raw = [R(), R()]
                pm = pst(st, "pm", [128, 96, 3], F32)
                rpm = R()
                rs = R()
                P.dma("sp", scf[:], ccT, writes=[rs])
                P.dma("sp", abT[:], ada_bT[l], writes=[rs])
                P.dma("sp", g1s[:], g1T[l], writes=[rs])
                P.dma("sp", g2s[:], g2T[l], writes=[rs])
                P.emit("act", lambda e: e.activation(scb[:], scf[:], AF.Silu), reads=[rs], writes=[rs])
                for pc in range(24):
                    af, raf = awf[pc % 2], rawf[pc % 2]
                    a, ra = aw[pc % 2], raw[pc % 2]
                    for hj in range(2):
                        P.dma("sp" if hj == 0 else "act", af[:, hj * 8:(hj + 1) * 8, :], ada_w[l, hj * 1024:(hj + 1) * 1024, pc * 512:(pc + 1) * 512].rearrange("(j p) m -> p j m", p=128), writes=[raf])
                    P.emit("dve", lambda e, a=a, af=af: e.tensor_copy(a[:, 0:6, :], af[:, 0:6, :]), reads=[raf], writes=[ra])
                    P.emit("pool", lambda e, a=a, af=af: e.tensor_copy(a[:, 6:10, :], af[:, 6:10, :]), reads=[raf], writes=[ra])
                    P.emit("act", lambda e, a=a, af=af: e.activation(a[:, 10:16, :], af[:, 10:16, :], AF.Copy), reads=[raf], writes=[ra])
                    for mc in range(4):
                        m = pc * 4 + mc

                        def f(e, a=a, m=m, mc=mc):
                            for j in range(16):
                                ins = e.matmul(pm[:, m, :], a[:, j, mc * 128:(mc + 1) * 128], scb[:, j, :], start=(j == 0), stop=(j == 15))
                            return ins
                        P.emit("pe", f, reads=[ra, rs], writes=[rpm])
                P.emit("dve", lambda e: e.tensor_tensor(modS[:], pm[:], abT[:].unsqueeze(2).broadcast_to([128, 96, 3]), ALU.add), reads=[rpm, rs, rmod], writes=[rmod])
                P.emit("dve", lambda e: e.tensor_scalar(gm1[:], modS[:, 16:32, :], 1.0, None, ALU.add), reads=[rmod], writes=[rmod])
                P.emit("dve", lambda e: e.tensor_tensor(gm1[:], gm1[:], g1s[:].unsqueeze(2).broadcast_to([128, 16, 3]), ALU.mult), reads=[rmod, rs], writes=[rmod])
                P.emit("dve", lambda e: e.tensor_scalar(gm2[:], modS[:, 64:80, :], 1.0, None, ALU.add), reads=[rmod], writes=[rmod])
                P.emit("dve", lambda e: e.tensor_tensor(gm2[:], gm2[:], g2s[:].unsqueeze(2).broadcast_to([128, 16, 3]), ALU.mult), reads=[rmod, rs], writes=[rmod])
                if dbg:
                    P.dma("sp", DBGM, modS[:], reads=[rmod])
                P.flush()

            def build_gate(st, name, j0, conds, pg=None, rpg=None, gbuf=None):
                G = {}
                dg = gbuf[2] if gbuf else sbt(st, name + "dg", [128, 128], F32)
                if pg is None:
                    pg = pst(st, name + "pg", [128, 512], F32)
                    rpg = R()
                rdg = gbuf[3] if gbuf else R()
                for c in conds:
                    if gbuf:
                        g, rg = gbuf[0], gbuf[1]
                    else:
                        g = sbt(st, name + "G%d" % c, [128, D], F32)
                        rg = R()
                    for j in range(16):
                        P.emit("dve", lambda e, j=j, c=c: e.tensor_scalar(dg[:], identf[:], modS[:, j0 + j, c:c + 1], None, ALU.mult), reads=[rmod, rC], writes=[rdg])
                        P.emit("pe", lambda e, j=j: e.matmul(pg[:, (j % 4) * 128:(j % 4 + 1) * 128], onesf[:], dg[:], start=True, stop=True), reads=[rdg, rC], writes=[rpg])
                        if j % 4 == 3:
                            P.emit("act", lambda e, j=j, g=g: e.activation(g[:, (j - 3) * 128:(j + 1) * 128], pg[:, 0:512], AF.Copy), reads=[rpg], writes=[rg])
                    G[c] = (g, rg)
                return G

            if phases is None or 1 in phases:
              with ExitStack() as st:
                NA = 2592
                W = sbt(st, "W", [128, 16, NA], BF16)
                rW = R()
                for j in range(16):
                    P.dma("pool", W[:, j, :], win[l, j * 128:(j + 1) * 128, 0:NA], writes=[rW], max_dma_last_dim=4096)
                xt = [sbt(st, "xt%d" % i, [128, D], F32) for i in range(2)]
                rxt = [R(), R()]
                junk = sbt(st, "junk", [128, D], BF16)
                ss = sbt(st, "ss", [128, 1], F32)
                rstd = sbt(st, "rstd", [128, 1], F32)
                rtmp = R()
                xns = [sbt(st, "xn%d" % i, [128, D], BF16) for i in range(2)]
                rxns = [R(), R()]
                hTs = [sbt(st, "hT%d" % i, [128, 16, 512], BF16) for i in range(2)]
                rhTs = [R(), R()]
                gcnt = 0
                pT = [pst(st, "pT%d" % i, [128, 8, 128], BF16) for i in range(2)]
                rpT = [R(), R()]
                pO = [pst(st, "pO%d" % i, [128, 512], F32) for i in range(4)]
                rpO = [R() for _ in range(4)]
                ob = [sbt(st, "ob%d" % i, [128, 512], BF16) for i in range(4)]
                rob = [R() for _ in range(4)]
                dto = sbt(st, "dto", [128, 32], F32)
                rdto = R()
                cnt = 0
                a1_tiles = [(b, t0 + i) for b in range(2) for (t0, nt) in ((0, 2), (2, 4), (6, 4), (10, 4), (14, 4)) for i in range(nt)]

                def a1_norm(k_):
                    b_, ti_ = a1_tiles[k_]
                    x_, rx = xt[k_ % 2], rxt[k_ % 2]
                    xn_, rxn_ = xns[k_ % 2], rxns[k_ % 2]
                    P.dma("act", x_[:], resid_src(l, b_, ti_), reads=[rRES[b_ * 18 + ti_]], writes=[rx])
                    rms_rstd("act", x_[:], D, ss[:], rstd[:], junk[:], rx, rtmp)
                    P.emit("act", lambda e: e.activation(xn_[:], x_[:], AF.Copy, scale=rstd[:]), reads=[rx, rtmp], writes=[rxn_])
                for b in range(2):
                    for (t0, nt) in ((0, 2), (2, 4), (6, 4), (10, 4), (14, 4)):
                        c = 2 if t0 == 0 else b
                        n = nt * 128
                        col0 = b * TB + t0 * 128
                        hT, rhT = hTs[gcnt % 2], rhTs[gcnt % 2]
                        gcnt += 1
                        for i in range(nt):
                            ti = t0 + i
                            gi = b * 18 + ti
                            xn, rxn = xns[cnt % 2], rxns[cnt % 2]
                            if cnt == 0:
                                a1_norm(0)
                            if cnt + 1 < len(a1_tiles):
                                a1_norm(cnt + 1)
                            cnt += 1
                            for hh in range(2):
                                def f(e, hh=hh, xn=xn):
                                    for jj in range(8):
                                        j = hh * 8 + jj
                                        ins = e.transpose(pT[hh][:, jj, :], xn[:, j * 128:(j + 1) * 128], identb[:])
                                    return ins
                                P.emit("pe", f, reads=[rxn, rC], writes=[rpT[hh]])
                                for jj in range(8):
                                    j = hh * 8 + jj
                                    if jj % 2 == 0:
                                        P.emit("dve", lambda e, hh=hh, jj=jj, j=j, i=i, c=c, hT=hT: e.tensor_scalar(hT[:, j, i * 128:(i + 1) * 128], pT[hh][:, jj, :], gm1[:, j, c:c + 1], modS[:, j, c:c + 1], ALU.mult, ALU.add), reads=[rpT[hh], rmod], writes=[rhT])
                                    else:
                                        P.emit("act", lambda e, hh=hh, jj=jj, j=j, i=i, c=c, hT=hT: e.activation(hT[:, j, i * 128:(i + 1) * 128], pT[hh][:, jj, :], AF.Identity, scale=gm1[:, j, c:c + 1], bias=modS[:, j, c:c + 1]), reads=[rpT[hh], rmod], writes=[rhT])
                        P.dma("sp", HT[:, col0:col0 + n].rearrange("(j p) t -> p j t", p=128), hT[:, :, 0:n], reads=[rhT], writes=[rHT[b * 18 + t0 + i] for i in range(nt)])
                        k = 0
                        for i in range(nt):
                            ti = t0 + i
                            gi = b * 18 + ti
                            r0 = b * TB + ti * 128
                            for zb in range(2):
                                po, rpo, o_, ro = pO[k % 4], rpO[k % 4], ob[k % 4], rob[k % 4]
                                k += 1

                                def f(e, po=po, zb=zb, i=i, hT=hT):
                                    for j in range(16):
                                        ins = e.matmul(po[:], hT[:, j, i * 128:(i + 1) * 128], W[:, j, zb * 512:(zb + 1) * 512], start=(j == 0), stop=(j == 15))
                                    return ins
                                P.emit("pe", f, reads=[rhT, rW], writes=[rpo])
                                P.emit("act" if k % 2 else "dve", (lambda e, po=po, o_=o_: e.activation(o_[:], po[:], AF.Copy)) if k % 2 else (lambda e, po=po, o_=o_: e.tensor_copy(o_[:], po[:])), reads=[rpo], writes=[ro])
                                P.dma("sp", ZT[r0:r0 + 128, zb * 512:(zb + 1) * 512], o_[:], reads=[ro], writes=[rZT[gi]])
                            po, rpo = pO[k % 4], rpO[k % 4]
                            k += 1

                            def f(e, po=po, i=i, hT=hT):
                                for j in range(16):
                                    ins = e.matmul(po[:, 0:32], hT[:, j, i * 128:(i + 1) * 128], W[:, j, 2560:2592], start=(j == 0), stop=(j == 15))
                                return ins
                            P.emit("pe", f, reads=[rhT, rW], writes=[rpo])
                            P.emit("dve", lambda e, po=po: e.tensor_copy(dto[:], po[:, 0:32]), reads=[rpo], writes=[rdto])
                            P.dma("sp", DTR[r0:r0 + 128, :], dto[:], reads=[rdto], writes=[rDTR[gi]])
                        for m in range(12):
                            po, rpo, o_, ro = pO[k % 4], rpO[k % 4], ob[k % 4], rob[k % 4]
                            k += 1

                            def f(e, po=po, m=m, n=n, hT=hT):
                                for j in range(16):
                                    ins = e.matmul(po[:, 0:n], W[:, j, 1024 + m * 128:1024 + (m + 1) * 128], hT[:, j, 0:n], start=(j == 0), stop=(j == 15))
                                return ins
                            P.emit("pe", f, reads=[rhT, rW], writes=[rpo])
                            P.emit("act" if k % 2 else "dve", (lambda e, po=po, o_=o_, n=n: e.activation(o_[:, 0:n], po[:, 0:n], AF.Copy)) if k % 2 else (lambda e, po=po, o_=o_, n=n: e.tensor_copy(o_[:, 0:n], po[:, 0:n])), reads=[rpo], writes=[ro])
                            P.dma("sp", XBC[m * 128:(m + 1) * 128, col0:col0 + n], o_[:, 0:n], reads=[ro], writes=[rXBC[b]])
                P.flush()

            if phases is None or 2 in phases:
              with ExitStack() as st:
                NB2 = NWIN - 2592
                W = sbt(st, "W2", [128, 16, NB2], BF16)
                rW = R()
                for j in range(16):
                    P.dma("pool", W[:, j, :], win[l, j * 128:(j + 1) * 128, 2592:NWIN], writes=[rW])
                WQ = sbt(st, "WQ", [128, 4, 2048], BF16)
                WKV = sbt(st, "WKV", [128, 2, 2048], BF16)
                for j in range(4):
                    P.dma("pool", WQ[:, j, :], wq[l, j * 128:(j + 1) * 128, :], writes=[rW], max_dma_last_dim=4096)
                for j in range(2):
                    P.dma("pool", WKV[:, j, :], wkv[l, j * 128:(j + 1) * 128, :], writes=[rW], max_dma_last_dim=4096)
                cosT = sbt(st, "cosT", [128, 2048], F32)
                sinT = sbt(st, "sinT", [128, 2048], F32)
                qg = sbt(st, "qg", [128, 4], F32)
                kvg = sbt(st, "kvg", [128, 2], F32)
                P.dma("sp", cosT[:], c_cos, writes=[rW])
                P.dma("sp", sinT[:], c_sin, writes=[rW])
                P.dma("sp", qg[:], qgT[l], writes=[rW])
                P.dma("sp", kvg[:], kvgT[l], writes=[rW])
                hT = [sbt(st, "hT2%d" % i, [128, 16, 512], BF16) for i in range(2)]
                rhT = [R(), R()]
                pO = [pst(st, "pO%d" % i, [128, 512], F32) for i in range(6)]
                rpO = [R() for _ in range(6)]
                pSq = [pst(st, "pSq%d" % i, [128, 512], F32) for i in range(2)]
                rpSq = [R(), R()]

                class NS:
                    pass
                nsets = {}
                for nm, nch in (("q", 4), ("kv", 2)):
                    for i in range(2):
                        s_ = NS()
                        s_.sq = sbt(st, "sq%s%d" % (nm, i), [128, nch, 512], BF16)
                        s_.qa = sbt(st, "qa%s%d" % (nm, i), [128, nch, 512], F32)
                        s_.qab = sbt(st, "qab%s%d" % (nm, i), [128, nch, 512], BF16)
                        s_.rbc = sbt(st, "rbc%s%d" % (nm, i), [128, 512], F32)
                        s_.rsq, s_.rqa, s_.rqab, s_.rrbc = R(), R(), R(), R()
                        nsets[(nm, i)] = s_
                ob = [sbt(st, "ob%d" % i, [128, 512], BF16) for i in range(6)]
                rob = [R() for _ in range(6)]
                ta = [sbt(st, "ta%d" % i, [128, 512], F32) for i in range(2)]
                tb_ = [sbt(st, "tb%d" % i, [128, 512], F32) for i in range(2)]
                rta, rtb = [R(), R()], [R(), R()]
                ov = [sbt(st, "ov%d" % i, [128, 1024], BF16) for i in range(2)]
                rov = [R(), R()]
                kst = [0, 0, 0, 0]

                def group(b, t0, nt, gidx):
                    isx = t0 > 0
                    n = nt * 128
                    col0 = b * TB + t0 * 128
                    p0 = (t0 - 2) * 128
                    h_ = hT[gidx % 2]
                    rh = rhT[gidx % 2]
                    Nq = nsets[("q", gidx % 2)]
                    Nk = nsets[("kv", gidx % 2)]
                    P.dma("act", h_[:, :, 0:n], HT[:, col0:col0 + n].rearrange("(j p) t -> p j t", p=128), reads=[rHT[b * 18 + t0 + i] for i in range(nt)], writes=[rh])

                    def nps():
                        kst[0] += 1
                        return pO[kst[0] % 6], rpO[kst[0] % 6]

                    def nob():
                        kst[1] += 1
                        return ob[kst[1] % 6], rob[kst[1] % 6]

                    def store(dst, src, rsrc):
                        kst[3] += 1
                        P.dma("sp", dst, src, reads=[rsrc], writes=[rQK[b]])

                    def proj(po, c0):
                        def f(e):
                            for j in range(16):
                                ins = e.matmul(po[:, 0:n], W[:, j, c0:c0 + 128], h_[:, j, 0:n], start=(j == 0), stop=(j == 15))
                            return ins
                        return f

                    def stage1(N_, nchunk, c0, gcol):
                        for m in range(nchunk):
                            po, rpo = nps()
                            P.emit("pe", proj(po, c0 + m * 128), reads=[rh, rW], writes=[rpo])
                            P.emit("act", lambda e, po=po, m=m: e.activation(N_.qa[:, m, 0:n], po[:, 0:n], AF.Copy), reads=[rpo], writes=[N_.rqa])
                            P.emit("dve", lambda e, m=m: e.tensor_tensor(N_.sq[:, m, 0:n], N_.qa[:, m, 0:n], N_.qa[:, m, 0:n], ALU.mult), reads=[N_.rqa], writes=[N_.rsq])
                            P.emit("pool", lambda e, m=m: e.tensor_scalar(N_.qa[:, m, 0:n], N_.qa[:, m, 0:n], gcol[:, m:m + 1], 1.0, ALU.mult, ALU.mult), reads=[N_.rqa, rW, N_.rsq], writes=[N_.rqa])

                    def stage2(N_, nchunk, pS, rpS):
                        def f(e):
                            for m in range(nchunk):
                                ins = e.matmul(pS[:, 0:n], onesb[:], N_.sq[:, m, 0:n], start=(m == 0), stop=(m == nchunk - 1))
                            return ins
                        P.emit("pe", f, reads=[N_.rsq, rC], writes=[rpS])
                        P.emit("act", lambda e: e.activation(N_.rbc[:, 0:n], pS[:, 0:n], AF.Sqrt, bias=EPS, scale=1.0 / (nchunk * 128)), reads=[rpS], writes=[N_.rrbc])
                        P.emit("dve", lambda e: e.reciprocal(N_.rbc[:, 0:n], N_.rbc[:, 0:n]), reads=[N_.rrbc], writes=[N_.rrbc])
                        for m in range(nchunk):
                            P.emit("dve", lambda e, m=m: e.tensor_tensor(N_.qab[:, m, 0:n], N_.qa[:, m, 0:n], N_.rbc[:, 0:n], ALU.mult), reads=[N_.rqa, N_.rrbc], writes=[N_.rqab])

                    def rope_combine(pa, rpa, pb, rpb, o_, ro):
                        if not isx:
                            P.emit("act", lambda e: e.activation(o_[:, 0:n], pa[:, 0:n], AF.Copy), reads=[rpa], writes=[ro])
                            return
                        kst[2] += 1
                        ta_, tb2, rta_, rtb_ = ta[kst[2] % 2], tb_[kst[2] % 2], rta[kst[2] % 2], rtb[kst[2] % 2]
                        P.emit("dve", lambda e: e.tensor_tensor(ta_[:, 0:n], pa[:, 0:n], cosT[:, p0:p0 + n], ALU.mult), reads=[rpa, rW], writes=[rta_])
                        P.emit("dve", lambda e: e.tensor_tensor(tb2[:, 0:n], pb[:, 0:n], sinT[:, p0:p0 + n], ALU.mult), reads=[rpb, rW], writes=[rtb_])
                        P.emit("pool", lambda e: e.tensor_tensor(o_[:, 0:n], ta_[:, 0:n], tb2[:, 0:n], ALU.add), reads=[rta_, rtb_], writes=[ro])

                    stage1(Nq, 4, 0, qg)
                    stage1(Nk, 2, 512, kvg)
                    pa, rpa = nps()
                    pb, rpb = nps()
                    o_, ro = nob()
                    P.emit("pe", proj(pa, 768), reads=[rh, rW], writes=[rpa])
                    P.emit("pe", proj(pb, 896), reads=[rh, rW], writes=[rpb])
                    rope_combine(pa, rpa, pb, rpb, o_, ro)
                    store(KR[:, col0:col0 + n], o_[:, 0:n], ro)
                    stage2(Nq, 4, pSq[0], rpSq[0])
                    stage2(Nk, 2, pSq[1], rpSq[1])

                    def qproj(po, mcol):
                        def f(e):
                            for j in range(4):
                                ins = e.matmul(po[:, 0:n], WQ[:, j, mcol:mcol + 128], Nq.qab[:, j, 0:n], start=(j == 0), stop=(j == 3))
                            return ins
                        return f
                    for h in range(8):
                        po, rpo = nps()
                        o_, ro = nob()
                        P.emit("pe", qproj(po, h * 128), reads=[Nq.rqab, rW], writes=[rpo])
                        P.emit("act", lambda e, po=po, o_=o_: e.activation(o_[:, 0:n], po[:, 0:n], AF.Copy), reads=[rpo], writes=[ro])
                        store(QN[h, :, col0:col0 + n], o_[:, 0:n], ro)
                    for h in range(8):
                        po, rpo = nps()
                        o_, ro = nob()

                        def f(e, po=po, h=h):
                            for j in range(2):
                                ins = e.matmul(po[:, 0:n], WKV[:, j, h * 128:(h + 1) * 128], Nk.qab[:, j, 0:n], start=(j == 0), stop=(j == 1))
                            return ins
                        P.emit("pe", f, reads=[Nk.rqab, rW], writes=[rpo])
                        P.emit("dve", lambda e, po=po, o_=o_: e.tensor_copy(o_[:, 0:n], po[:, 0:n]), reads=[rpo], writes=[ro])
                        store(KN[h, :, col0:col0 + n], o_[:, 0:n], ro)
                    for pr in range(4):
                        pa, rpa = nps()
                        pb, rpb = nps()
                        o_, ro = nob()
                        P.emit("pe", qproj(pa, 1024 + pr * 128), reads=[Nq.rqab, rW], writes=[rpa])
                        P.emit("pe", qproj(pb, 1536 + pr * 128), reads=[Nq.rqab, rW], writes=[rpb])
                        rope_combine(pa, rpa, pb, rpb, o_, ro)
                        store(QR[pr, :, col0:col0 + n], o_[:, 0:n], ro)
                    for i in range(nt):
                        r0 = col0 + i * 128
                        ov_, rov_ = ov[i % 2], rov[i % 2]
                        for vb in range(2):
                            po, rpo = nps()

                            def f(e, po=po, vb=vb, i=i):
                                for j in range(2):
                                    ins = e.matmul(po[:], Nk.qab[:, j, i * 128:(i + 1) * 128], WKV[:, j, 1024 + vb * 512:1024 + (vb + 1) * 512], start=(j == 0), stop=(j == 1))
                                return ins
                            P.emit("pe", f, reads=[Nk.rqab, rW], writes=[rpo])
                            P.emit("act", lambda e, po=po, ov_=ov_, vb=vb: e.activation(ov_[:, vb * 512:(vb + 1) * 512], po[:], AF.Copy), reads=[rpo], writes=[rov_])
                        store(VV[r0:r0 + 128, :], ov_[:], rov_)

                gidx = 0
                for b in range(2):
                    for (t0, nt) in ((0, 2), (2, 4), (6, 4), (10, 4), (14, 4)):
                        group(b, t0, nt, gidx)
                        gidx += 1
                P.flush()

            if phases is None or 3 in phases:
              with ExitStack() as st:
                cw = sbt(st, "cw", [128, 36], F32)
                cb = sbt(st, "cb", [128, 12], F32)
                dtbs = sbt(st, "dtbs", [128, 32], F32)
                abc = sbt(st, "abc", [128, 32], F32)
                dsk = sbt(st, "dsk", [128, 16], F32)
                tri = [sbt(st, "tri%d" % d, [128, 128], F32) for d in range(2)]
                msk = [sbt(st, "msk%d" % d, [128, 512], F32) for d in range(2)]
                rK = R()
                P.dma("sp", cw[:], convw[l], writes=[rK])
                P.dma("sp", cb[:], convb[l], writes=[rK])
                P.dma("sp", dtbs[:], dtb[l], writes=[rK])
                P.dma("sp", abc[:], alog[l], writes=[rK])
                P.dma("sp", dsk[:], dskip[l], writes=[rK])
                for d in range(2):
                    P.dma("sp", tri[d][:], c_tri[d], writes=[rK])
                    P.dma("sp", msk[d][:], c_mask[d], writes=[rK])
                P.emit("act", lambda e: e.activation(abc[:], abc[:], AF.Exp), reads=[rK], writes=[rK])
                P.emit("dve", lambda e: e.tensor_scalar(abc[:], abc[:], -1.0, None, ALU.mult), reads=[rK], writes=[rK])
                XC = sbt(st, "XC", [128, 12, TB], BF16)
                rXC = R()
                u = [sbt(st, "u%d" % i, [128, 2048], BF16) for i in range(2)]
                ru = [R(), R()]
                acc = [sbt(st, "acc%d" % i, [128, 2048], F32) for i in range(2)]
                racc = [R(), R()]
                zs = sbt(st, "zsB", [128, 1024], F32)
                rzs = R()

                class BS:
                    pass
                sets = []
                for d in range(2):
                    s_ = BS()
                    for nm, shp, dt_t in (("S", [128, 1024], F32), ("Sb", [128, 1024], BF16), ("XS", [128, 1024], BF16), ("BT", [128, 256], BF16),
                                          ("dtr", [128, 32], F32), ("dt_", [128, 32], F32), ("dtA", [128, 32], F32),
                                          ("xdt", [128, 16, 64], BF16), ("xdd", [128, 16, 64], BF16),
                                          ("nac", [128, 16], F32), ("expA", [128, 16], F32), ("dec", [128, 16], F32), ("cd", [128, 16], F32),
                                          ("Rt", [128, 8, 128], F32), ("LM", [128, 16, 128], BF16), ("CBs", [128, 2, 128], BF16),
                                          ("MT", [128, 16, 128], BF16), ("yo", [128, 16, 64], F32), ("y", [128, 1024], F32)):
                        setattr(s_, nm, sbt(st, "%s_d%d" % (nm, d), shp, dt_t))
                    for nm in ("rS", "rSb", "rXS", "rBT", "rdtr", "rdt", "rxdt", "rxdd", "rsm", "rRt", "rLM", "rCBs", "rMT", "ryo", "ry"):
                        setattr(s_, nm, R())
                    sets.append(s_)
                bT = pst(st, "bT", [128, 8, 128], BF16)
                rbT = R()
                bA = pst(st, "bA", [128, 512], F32)
                rbA = R()
                pSs = pst(st, "pSs", [128, 1024], F32)
                rpSs = R()
                pY = pst(st, "pY", [128, 1024], F32)
                rpY = R()
                pL = pst(st, "pL", [128, 8, 128], F32)
                rpL = R()

                def chunk_pass(b, d, ti, B_):
                    base = b * TB
                    gi = b * 18 + ti
                    r0 = base + ti * 128
                    cs = slice(ti * 128, (ti + 1) * 128)
                    do_y = not (last and ti < 2)
                    dsl = slice(d * 16, (d + 1) * 16)

                    def f(e):
                        for j in range(8):
                            ins = e.transpose(bT[:, j, :], XC[:, j, cs], identb[:])
                        return ins
                    P.emit("pe", f, reads=[rXC, rC], writes=[rbT])
                    P.emit("act", lambda e: e.activation(B_.XS[:], bT[:].rearrange("p a b -> p (a b)"), AF.Copy), reads=[rbT], writes=[B_.rXS])

                    def f(e):
                        for j in range(2):
                            ins = e.transpose(bT[:, j, :], XC[:, 8 + j, cs], identb[:])
                        return ins
                    P.emit("pe", f, reads=[rXC, rC], writes=[rbT])
                    P.emit("act", lambda e: e.activation(B_.BT[:], bT[:, 0:2, :].rearrange("p a b -> p (a b)"), AF.Copy), reads=[rbT], writes=[B_.rBT])
                    P.dma("act", B_.dtr[:], DTR[r0:r0 + 128, :], reads=[rDTR[gi]], writes=[B_.rdtr])
                    P.emit("dve", lambda e: e.tensor_tensor(B_.dt_[:], B_.dtr[:], dtbs[:], ALU.add), reads=[B_.rdtr, rK], writes=[B_.rdt])
                    P.emit("act", lambda e: e.activation(B_.dt_[:], B_.dt_[:], AF.Exp), reads=[B_.rdt], writes=[B_.rdt])
                    P.emit("act", lambda e: e.activation(B_.dt_[:], B_.dt_[:], AF.Ln, bias=1.0), reads=[B_.rdt], writes=[B_.rdt])
                    P.emit("dve", lambda e: e.tensor_tensor(B_.dtA[:], B_.dt_[:], abc[:], ALU.mult), reads=[B_.rdt, rK], writes=[B_.rdt])
                    P.emit("dve", lambda e: e.tensor_tensor(B_.xdt[:], B_.XS[:].rearrange("p (h q) -> p h q", h=16), B_.dt_[:, dsl].unsqueeze(2).broadcast_to([128, 16, 64]), ALU.mult), reads=[B_.rXS, B_.rdt], writes=[B_.rxdt])
                    yield

                    def f(e):
                        e.matmul(bA[:, 0:16], tri[d][:], B_.dtA[:, dsl], start=True, stop=True)
                        return e.matmul(bA[:, 16:32], onesf[:], B_.dtA[:, dsl], start=True, stop=True)
                    P.emit("pe", f, reads=[B_.rdt, rK, rC], writes=[rbA])
                    P.emit("dve", lambda e: e.tensor_scalar(B_.nac[:], bA[:, 0:16], -1.0, None, ALU.mult), reads=[rbA], writes=[B_.rsm])
                    P.emit("act", lambda e: e.activation(B_.expA[:], bA[:, 0:16], AF.Exp), reads=[rbA], writes=[B_.rsm])
                    P.emit("act", lambda e: e.activation(B_.cd[:], bA[:, 16:32], AF.Exp), reads=[rbA], writes=[B_.rsm])
                    P.emit("dve", lambda e: e.tensor_tensor(B_.dec[:], bA[:, 16:32], B_.nac[:], ALU.add), reads=[rbA, B_.rsm], writes=[B_.rsm])
                    P.emit("act", lambda e: e.activation(B_.dec[:], B_.dec[:], AF.Exp), reads=[B_.rsm], writes=[B_.rsm])
                    P.emit("dve", lambda e: e.tensor_tensor(B_.xdd[:], B_.xdt[:], B_.dec[:].unsqueeze(2).broadcast_to([128, 16, 64]), ALU.mult), reads=[B_.rxdt, B_.rsm], writes=[B_.rxdd])
                    yield

                    def f(e):
                        for g in range(2):
                            ins = e.matmul(pSs[:, g * 512:(g + 1) * 512], B_.BT[:, g * 128:(g + 1) * 128], B_.xdd[:, g * 8:(g + 1) * 8, :].rearrange("p h q -> p (h q)"), start=True, stop=True)
                        return ins
                    P.emit("pe", f, reads=[B_.rBT, B_.rxdd], writes=[rpSs])
                    if do_y:
                        P.emit("act", lambda e: e.activation(B_.Sb[:], B_.S[:], AF.Copy), reads=[B_.rS], writes=[B_.rSb])

                        def f(e):
                            for g in range(2):
                                ins = e.matmul(pY[:, g * 512:(g + 1) * 512], XC[:, 10 + g, cs], B_.Sb[:, g * 512:(g + 1) * 512], start=True, stop=True)
                            return ins
                        P.emit("pe", f, reads=[rXC, B_.rSb], writes=[rpY])
                        P.emit("dve", lambda e: e.tensor_tensor(B_.yo[:], pY[:].rearrange("p (h q) -> p h q", h=16), B_.expA[:].unsqueeze(2).broadcast_to([128, 16, 64]), ALU.mult), reads=[rpY, B_.rsm], writes=[B_.ryo])
                    P.emit("dve", lambda e: e.tensor_tensor(B_.S[:].rearrange("p (h q) -> p h q", h=16), B_.S[:].rearrange("p (h q) -> p h q", h=16), B_.cd[:].unsqueeze(2).broadcast_to([128, 16, 64]), ALU.mult), reads=[B_.rS, B_.rsm, B_.rSb], writes=[B_.rS])
                    P.emit("dve", lambda e: e.tensor_tensor(B_.S[:], B_.S[:], pSs[:], ALU.add), reads=[B_.rS, rpSs], writes=[B_.rS])
                    yield
                    if not do_y:
                        return

                    def f(e):
                        for g in range(2):
                            ins = e.matmul(bA[:, 256 + g * 128:256 + (g + 1) * 128], XC[:, 8 + g, cs], XC[:, 10 + g, cs], start=True, stop=True)
                        return ins
                    P.emit("pe", f, reads=[rXC], writes=[rbA])
                    P.emit("act", lambda e: e.activation(B_.CBs[:].rearrange("p a b -> p (a b)"), bA[:, 256:512], AF.Copy), reads=[rbA], writes=[B_.rCBs])
                    yield
                    for hf in range(2):
                        P.emit("pool", lambda e, hf=hf: e.tensor_tensor(B_.Rt[:], tri[d][:].unsqueeze(1).broadcast_to([128, 8, 128]), B_.dtA[:, d * 16 + hf * 8:d * 16 + hf * 8 + 8].unsqueeze(2).broadcast_to([128, 8, 128]), ALU.mult), reads=[rK, B_.rdt], writes=[B_.rRt])

                        def f(e):
                            for kb in range(2):
                                e.matmul(pL[:, kb * 4:(kb + 1) * 4, :].rearrange("p a b -> p (a b)"), onesf[:], B_.Rt[:, kb * 4:(kb + 1) * 4, :].rearrange("p a b -> p (a b)"), start=True, stop=False)
                                ins = e.matmul(pL[:, kb * 4:(kb + 1) * 4, :].rearrange("p a b -> p (a b)"), identf[:], msk[d][:], start=False, stop=True)
                            return ins
                        P.emit("pe", f, reads=[B_.rRt, rK, rC], writes=[rpL])
                        for hh in range(8):
                            h = hf * 8 + hh
                            P.emit("act", lambda e, h=h, hh=hh: e.activation(B_.LM[:, h, :], pL[:, hh, :], AF.Exp, bias=B_.nac[:, h:h + 1]), reads=[rpL, B_.rsm], writes=[B_.rLM])
                        yield
                    P.emit("dve", lambda e: e.tensor_tensor(B_.MT[:].rearrange("p (g h) i -> p g h i", g=2), B_.LM[:].rearrange("p (g h) i -> p g h i", g=2), B_.CBs[:].unsqueeze(2).broadcast_to([128, 2, 8, 128]), ALU.mult), reads=[B_.rLM, B_.rCBs], writes=[B_.rMT])

                    def f(e):
                        for h in range(16):
                            ins = e.matmul(pY[:, h * 64:(h + 1) * 64], B_.MT[:, h, :], B_.xdt[:, h, :], start=True, stop=True)
                        return ins
                    P.emit("pe", f, reads=[B_.rMT, B_.rxdt, B_.ryo], writes=[rpY])
                    P.emit("dve", lambda e: e.tensor_tensor(B_.y[:], B_.yo[:].rearrange("p h q -> p (h q)"), pY[:], ALU.add), reads=[B_.ryo, rpY], writes=[B_.ry])
                    if d == 0:
                        P.emit("pool", lambda e: e.tensor_tensor(zs[:].rearrange("p (h q) -> p h q", h=16), B_.XS[:].rearrange("p (h q) -> p h q", h=16), dsk[:].unsqueeze(2).broadcast_to([128, 16, 64]), ALU.mult), reads=[B_.rXS, rK], writes=[rzs])
                        P.emit("dve", lambda e: e.tensor_tensor(B_.y[:], B_.y[:], zs[:], ALU.add), reads=[B_.ry, rzs], writes=[B_.ry])
                        P.dma("sp", YF[r0:r0 + 128, :], B_.y[:], reads=[B_.ry], writes=[rYF[gi]])
                    else:
                        P.dma("sp", YB[r0:r0 + 128, :], B_.y[:], reads=[B_.ry], writes=[rYB[gi]])

                for b in range(2):
                    base = b * TB
                    cc_ = 0
                    for j in range(12):
                        for (s0, Ls) in ((0, 256), (256, 2048)):
                            u_, ru_, a_, ra_ = u[cc_ % 2], ru[cc_ % 2], acc[cc_ % 2], racc[cc_ % 2]
                            cc_ += 1
                            P.dma("sp", u_[:, 0:Ls], XBC[j * 128:(j + 1) * 128, base + s0:base + s0 + Ls], reads=[rXBC[b]], writes=[ru_])
                            P.emit("dve", lambda e, j=j, Ls=Ls, u_=u_, a_=a_: e.tensor_scalar(a_[:, 0:Ls], u_[:, 0:Ls], cw[:, j * 3 + 1:j * 3 + 2], None, ALU.mult), reads=[ru_, rK], writes=[ra_])
                            P.emit("dve", lambda e, j=j, Ls=Ls, u_=u_, a_=a_: e.scalar_tensor_tensor(a_[:, 1:Ls], u_[:, 0:Ls - 1], cw[:, j * 3:j * 3 + 1], a_[:, 1:Ls], ALU.mult, ALU.add), reads=[ru_, rK, ra_], writes=[ra_])
                            P.emit("dve", lambda e, j=j, Ls=Ls, u_=u_, a_=a_: e.scalar_tensor_tensor(a_[:, 0:Ls - 1], u_[:, 1:Ls], cw[:, j * 3 + 2:j * 3 + 3], a_[:, 0:Ls - 1], ALU.mult, ALU.add), reads=[ru_, rK, ra_], writes=[ra_])
                            P.emit("act", lambda e, j=j, Ls=Ls, s0=s0, a_=a_: e.activation(XC[:, j, s0:s0 + Ls], a_[:, 0:Ls], AF.Silu, bias=cb[:, j:j + 1]), reads=[ra_, rK], writes=[rXC])
                    orders = [list(range(18)), [1, 0] + list(range(17, 1, -1))]
                    for d in range(2):
                        P.emit("dve", lambda e, d=d: e.memset(sets[d].S[:], 0.0), writes=[sets[d].rS])
                    for step in range(18):
                        gens = [chunk_pass(b, d, orders[d][step], sets[d]) for d in range(2)]
                        while gens:
                            for g_ in list(gens):
                                try:
                                    next(g_)
                                except StopIteration:
                                    gens.remove(g_)
                P.flush()
              with ExitStack() as st:
                sg_ = sbt(st, "ssdgs", [128, 1024], F32)
                rK = R()
                P.dma("sp", sg_[:], ssdg[l], writes=[rK])

                class MS:
                    pass
                ms = []
                for i in range(2):
                    m_ = MS()
                    for nm, shp, dt_t in (("yf", [128, 1024], F32), ("yb", [128, 1024], F32), ("zt", [128, 1024], BF16), ("zs", [128, 1024], F32),
                                          ("y16", [128, 1024], BF16), ("yT", [128, 8, 128], BF16), ("junk", [128, 1024], BF16),
                                          ("ss", [128, 1], F32), ("rstd", [128, 1], F32)):
                        setattr(m_, nm, sbt(st, "%s_m%d" % (nm, i), shp, dt_t))
                    m_.bT = pst(st, "bTm%d" % i, [128, 8, 128], BF16)
                    for nm in ("ryf", "ryb", "rzt", "rzs", "ry16", "ryT", "rtmp", "rbT"):
                        setattr(m_, nm, R())
                    ms.append(m_)
                for n_, (b, ti) in enumerate(tiles_l):
                    M_ = ms[n_ % 2]
                    gi = b * 18 + ti
                    r0 = b * TB + ti * 128

                    def mrg(M_=M_, gi=gi, r0=r0):
                        P.dma("act", M_.yf[:], YF[r0:r0 + 128, :], reads=[rYF[gi]], writes=[M_.ryf])
                        P.dma("act", M_.yb[:], YB[r0:r0 + 128, :], reads=[rYB[gi]], writes=[M_.ryb])
                        P.dma("act", M_.zt[:], ZT[r0:r0 + 128, :], reads=[rZT[gi]], writes=[M_.rzt])
                        P.emit("dve", lambda e: e.tensor_tensor(M_.yf[:], M_.yf[:], M_.yb[:], ALU.add), reads=[M_.ryf, M_.ryb], writes=[M_.ryf])
                        P.emit("act", lambda e: e.activation(M_.zs[:], M_.zt[:], AF.Silu), reads=[M_.rzt], writes=[M_.rzs])
                        P.emit("dve", lambda e: e.tensor_tensor(M_.yf[:], M_.yf[:], M_.zs[:], ALU.mult), reads=[M_.ryf, M_.rzs], writes=[M_.ryf])
                        rms_rstd("act", M_.yf[:], 1024, M_.ss[:], M_.rstd[:], M_.junk[:], M_.ryf, M_.rtmp)
                        P.emit("act", lambda e: e.activation(M_.yf[:], M_.yf[:], AF.Copy, scale=M_.rstd[:]), reads=[M_.ryf, M_.rtmp], writes=[M_.ryf])
                        P.emit("dve", lambda e: e.tensor_tensor(M_.y16[:], M_.yf[:], sg_[:], ALU.mult), reads=[M_.ryf, rK], writes=[M_.ry16])

                        def f(e):
                            for j in range(8):
                                ins = e.transpose(M_.bT[:, j, :], M_.y16[:, j * 128:(j + 1) * 128], identb[:])
                            return ins
                        P.emit("pe", f, reads=[M_.ry16, rC], writes=[M_.rbT])
                        P.emit("act", lambda e: e.activation(M_.yT[:], M_.bT[:], AF.Copy), reads=[M_.rbT], writes=[M_.ryT])
                        P.dma("sp", MIX[0:1024, r0:r0 + 128].rearrange("(j p) t -> p j t", p=128), M_.yT[:], reads=[M_.ryT], writes=[rMIX[gi]])
                    mrg()
                P.flush()

            if phases is None or 4 in phases:
              with ExitStack() as st:
                KNs = sbt(st, "KNs", [128, 8, TB], BF16)
                KRs = [sbt(st, "KRs%d" % i, [128, TB], BF16) for i in range(2)]
                Vs = sbt(st, "Vs", [128, 18, 1024], BF16)
                rKV = R()
                QNs = [sbt(st, "QNs%d" % i, [128, 8, 512], BF16) for i in range(2)]
                QRs = [sbt(st, "QRs%d" % i, [128, 4, 512], BF16) for i in range(2)]
                rQ = [R(), R()]
                pS = [pst(st, "pSc%d" % i, [128, 512], F32) for i in range(4)]
                rpS = [R() for _ in range(4)]
                pO = [pst(st, "pOc%d" % i, [128, 512], F32) for i in range(2)]
                rpO = [R(), R()]
                pL = [pst(st, "pLc%d" % i, [128, 512], F32) for i in range(2)]
                rpL = [R(), R()]
                PT = [sbt(st, "PTc%d" % i, [128, 512], BF16) for i in range(6)]
                rPT = [R() for _ in range(6)]
                rl = [sbt(st, "rlc%d" % i, [128, 512], F32) for i in range(2)]
                rrl = [R(), R()]
                ot = [sbt(st, "otc%d" % i, [128, 512], BF16) for i in range(2)]
                rot = [R(), R()]
                cnt = [0, 0]

                def block_head(b, Qn, Qr, rq, h, nq, nkt, c0, gis):
                    u = cnt[1]
                    cnt[1] += 1
                    po, rpo, pl, rpl = pO[u % 2], rpO[u % 2], pL[u % 2], rpL[u % 2]
                    tiles = []

                    def score(kt):
                        i = cnt[0]
                        cnt[0] += 1
                        ps, rps = pS[i % 4], rpS[i % 4]
                        pt, rpt = PT[i % 6], rPT[i % 6]

                        def f(e):
                            e.matmul(ps[:, 0:nq], KNs[:, h, kt * 128:(kt + 1) * 128], Qn[:, h, 0:nq], start=True, stop=False)
                            return e.matmul(ps[:, 0:nq], KRs[h % 2][:, kt * 128:(kt + 1) * 128], Qr[:, h // 2, 0:nq], start=False, stop=True)
                        P.emit("pe", f, reads=[rq, rKV], writes=[rps])
                        P.emit("act", lambda e: e.activation(pt[:, 0:nq], ps[:, 0:nq], AF.Exp, scale=ATTN_SCALE), reads=[rps], writes=[rpt])
                        tiles.append((kt, pt, rpt))

                    def pv(idx):
                        kt, pt, rpt = tiles[idx]

                        def f(e):
                            e.matmul(po[:, 0:nq], Vs[:, kt, h * 128:(h + 1) * 128], pt[:, 0:nq], start=(idx == 0), stop=(idx == nkt - 1))
                            return e.matmul(pl[:, 0:nq], onesb[:], pt[:, 0:nq], start=(idx == 0), stop=(idx == nkt - 1))
                        P.emit("pe", f, reads=[rpt, rKV, rC], writes=[rpo, rpl])
                    DEPTH = 2
                    for kt in range(nkt):
                        score(kt)
                        if kt >= DEPTH:
                            pv(kt - DEPTH)
                    for idx in range(max(0, nkt - DEPTH), nkt):
                        pv(idx)
                    r_, rr_, o_, ro_ = rl[u % 2], rrl[u % 2], ot[u % 2], rot[u % 2]
                    P.emit("dve", lambda e: e.reciprocal(r_[:, 0:nq], pl[:, 0:nq]), reads=[rpl], writes=[rr_])
                    P.emit("dve", lambda e: e.tensor_tensor(o_[:, 0:nq], po[:, 0:nq], r_[:, 0:nq], ALU.mult), reads=[rpo, rr_], writes=[ro_])
                    P.dma("sp", MIX[1024 + h * 128:1024 + (h + 1) * 128, c0:c0 + nq], o_[:, 0:nq], reads=[ro_], writes=[rMIX[g] for g in gis])

                for b in range(2):
                    base = b * TB
                    P.dma("sp", KNs[:], KN[:, :, base:base + TB].rearrange("h p t -> p h t"), reads=[rQK[b]], writes=[rKV])
                    for i_ in range(2):
                        P.emit("dve", lambda e, i_=i_: e.memset(KRs[i_][:], 0.0), writes=[rKV])
                        P.dma("act", KRs[i_][i_ * 64:(i_ + 1) * 64, :], KR[i_ * 64:(i_ + 1) * 64, base:base + TB], reads=[rQK[b]], writes=[rKV])
                    for kt in range(18):
                        P.dma("sp" if kt % 2 else "act", Vs[:, kt, :], VV[base + kt * 128:base + (kt + 1) * 128, :], reads=[rQK[b]], writes=[rKV])
                    blocks = [(2, 4), (6, 4), (10, 4), (14, 4)]
                    if not last:
                        blocks = [(0, 2)] + blocks
                    for bi, (t0, nt) in enumerate(blocks):
                        nq = nt * 128
                        nkt = 2 if t0 == 0 else 18
                        c0 = base + t0 * 128
                        Qn, Qr, rq = QNs[bi % 2], QRs[bi % 2], rQ[bi % 2]
                        P.dma("act", Qn[:, :, 0:nq], QN[:, :, c0:c0 + nq].rearrange("h p t -> p h t"), reads=[rQK[b]], writes=[rq])
                        P.dma("act", Qr[:, :, 0:nq], QR[:, :, c0:c0 + nq].rearrange("h p t -> p h t"), reads=[rQK[b]], writes=[rq])
                        gis = [b * 18 + t0 + i for i in range(nt)]
                        for h in range(8):
                            block_head(b, Qn, Qr, rq, h, nq, nkt, c0, gis)
                P.flush()

            if phases is None or 5 in phases:
              with ExitStack() as st:
                WO = sbt(st, "WO", [128, 16, D], BF16)
                rW = R()
                for j in range(16):
                    P.dma("pool", WO[:, j, :], wo[l, j * 128:(j + 1) * 128, :], writes=[rW], max_dma_last_dim=4096)
                RW = sbt(st, "RW", [128, 16, 16], F32)
                rbs = sbt(st, "rbs", [128, 16], F32)
                P.dma("sp", RW[:], rwT, writes=[rW])
                P.dma("sp", rbs[:], rb, writes=[rW])
                G1 = build_gate(st, "g1", 32, [0, 1] if last else [0, 1, 2])
                pO = pst(st, "pOd", [128, D], F32)
                rpO = R()
                pT = [pst(st, "pTd%d" % i, [128, 4, 128], F32) for i in range(2)]
                rpT = [R(), R()]
                pR = pst(st, "pRd", [128, 16], F32)
                rpR = R()

                class DS:
                    pass
                dsets = []
                for i in range(2):
                    s_ = DS()
                    for nm, shp, dt_t in (("mixT", [128, 16, 128], BF16), ("xt", [128, D], F32), ("tt", [128, D], F32), ("xs", [128, D], F32),
                                          ("junk", [128, D], BF16), ("ss", [128, 1], F32), ("rstd", [128, 1], F32),
                                          ("h2f", [128, 16, 128], F32), ("h2b", [128, 16, 128], BF16),
                                          ("sc", [128, 16], F32), ("sel", [128, 16], F32), ("pr6", [128, 4, 6], F32), ("gs", [128, 4], F32),
                                          ("gmx", [128, 1], F32), ("gmk", [128, 4], F32), ("mk", [128, 16], F32), ("m1", [128, 16], F32),
                                          ("m2", [128, 16], F32), ("t1", [128, 1], F32), ("cmb", [128, 16], F32)):
                        setattr(s_, nm, sbt(st, "%s_D%d" % (nm, i), shp, dt_t))
                    for nm in ("rmixT", "rxt", "rtt", "rxs", "rtmp", "rh2f", "rh2b", "rr"):
                        setattr(s_, nm, R())
                    dsets.append(s_)

                def dtile(S_, b, ti):
                    gi = b * 18 + ti
                    r0 = b * TB + ti * 128
                    c = 2 if ti < 2 else b
                    g1, rg1 = G1[c]
                    P.dma("act", S_.mixT[:], MIX[:, r0:r0 + 128].rearrange("(j p) t -> p j t", p=128), reads=[rMIX[gi]], writes=[S_.rmixT])
                    P.dma("act", S_.xt[:], resid_src(l, b, ti), reads=[rRES[gi]], writes=[S_.rxt])

                    def f(e):
                        for nb in range(4):
                            for j in range(16):
                                ins = e.matmul(pO[:, nb * 512:(nb + 1) * 512], S_.mixT[:, j, :], WO[:, j, nb * 512:(nb + 1) * 512], start=(j == 0), stop=(j == 15))
                        return ins
                    P.emit("pe", f, reads=[S_.rmixT, rW], writes=[rpO])

                def dtile1b(S_, b, ti):
                    gi = b * 18 + ti
                    r0 = b * TB + ti * 128
                    c = 2 if ti < 2 else b
                    g1, rg1 = G1[c]
                    P.emit("dve", lambda e: e.tensor_tensor(S_.tt[:], pO[:], g1[:], ALU.mult), reads=[rpO, rg1], writes=[S_.rtt])
                    P.emit("dve", lambda e: e.tensor_tensor(S_.xt[:], S_.xt[:], S_.tt[:], ALU.add), reads=[S_.rxt, S_.rtt], writes=[S_.rxt])
                    P.dma("sp", RES[r0:r0 + 128, :], S_.xt[:], reads=[S_.rxt], writes=[rRES[gi]])
                    rms_rstd("act", S_.xt[:], D, S_.ss[:], S_.rstd[:], S_.junk[:], S_.rxt, S_.rtmp)
                    P.emit("act", lambda e: e.activation(S_.xs[:], S_.xt[:], AF.Copy, scale=S_.rstd[:]), reads=[S_.rxt, S_.rtmp], writes=[S_.rxs])

                def dtile2(S_, b, ti):
                    gi = b * 18 + ti
                    r0 = b * TB + ti * 128
                    c = 2 if ti < 2 else b
                    for r4 in range(4):
                        p_, rp_ = pT[r4 % 2], rpT[r4 % 2]

                        def f(e, r4=r4, p_=p_):
                            for jj in range(4):
                                j = r4 * 4 + jj
                                ins = e.transpose(p_[:, jj, :], S_.xs[:, j * 128:(j + 1) * 128], identf[:])
                            return ins
                        P.emit("pe", f, reads=[S_.rxs, rC], writes=[rp_])
                        for jj in range(4):
                            j = r4 * 4 + jj
                            if jj % 2 == 0:
                                P.emit("dve", lambda e, p_=p_, jj=jj, j=j: e.tensor_scalar(S_.h2f[:, j, :], p_[:, jj, :], gm2[:, j, c:c + 1], modS[:, 48 + j, c:c + 1], ALU.mult, ALU.add), reads=[rp_, rmod], writes=[S_.rh2f])
                            else:
                                P.emit("act", lambda e, p_=p_, jj=jj, j=j: e.activation(S_.h2f[:, j, :], p_[:, jj, :], AF.Identity, scale=gm2[:, j, c:c + 1], bias=modS[:, 48 + j, c:c + 1]), reads=[rp_, rmod], writes=[S_.rh2f])
                    P.emit("dve", lambda e: e.tensor_copy(S_.h2b[:], S_.h2f[:]), reads=[S_.rh2f], writes=[S_.rh2b])
                    P.dma("sp", HT[:, r0:r0 + 128].rearrange("(j p) t -> p j t", p=128), S_.h2b[:], reads=[S_.rh2b], writes=[rHT[gi]])

                    def f(e):
                        for j in range(16):
                            ins = e.matmul(pR[:], S_.h2f[:, j, :], RW[:, j, :], start=(j == 0), stop=(j == 15))
                        return ins
                    P.emit("pe", f, reads=[S_.rh2f, rW], writes=[rpR])

                def dtile2b(S_, b, ti):
                    gi = b * 18 + ti
                    r0 = b * TB + ti * 128
                    P.emit("act", lambda e: e.activation(S_.sc[:], pR[:], AF.Sigmoid), reads=[rpR], writes=[S_.rr])
                    V = lambda fn, rd=(): P.emit("dve", fn, reads=[S_.rr] + list(rd), writes=[S_.rr])
                    sc, sel, pr6, gs, gmx, gmk, mk, m1, m2, t1, cmb = S_.sc, S_.sel, S_.pr6, S_.gs, S_.gmx, S_.gmk, S_.mk, S_.m1, S_.m2, S_.t1, S_.cmb
                    V(lambda e: e.tensor_tensor(sel[:], sc[:], rbs[:], ALU.add), [rW])
                    s4 = sel[:].rearrange("p (g k) -> p g k", g=4)
                    V(lambda e: e.tensor_tensor(pr6[:, :, 0:3], s4[:, :, 0:3], s4[:, :, 1:4], ALU.add))
                    V(lambda e: e.tensor_tensor(pr6[:, :, 3:5], s4[:, :, 0:2], s4[:, :, 2:4], ALU.add))
                    V(lambda e: e.tensor_tensor(pr6[:, :, 5:6], s4[:, :, 0:1], s4[:, :, 3:4], ALU.add))
                    V(lambda e: e.tensor_reduce(gs[:], pr6[:], AX.X, ALU.max))
                    V(lambda e: e.tensor_reduce(gmx[:], gs[:], AX.X, ALU.max))
                    V(lambda e: e.tensor_scalar(gmk[:], gs[:], gmx[:], None, ALU.is_ge))
                    V(lambda e: e.tensor_tensor(mk[:].rearrange("p (g k) -> p g k", g=4), s4, gmk[:].unsqueeze(2).broadcast_to([128, 4, 4]), ALU.mult))
                    V(lambda e: e.tensor_scalar(gmk[:], gmk[:], -1.0, 10.0, ALU.add, ALU.mult))
                    V(lambda e: e.tensor_tensor(mk[:].rearrange("p (g k) -> p g k", g=4), mk[:].rearrange("p (g k) -> p g k", g=4), gmk[:].unsqueeze(2).broadcast_to([128, 4, 4]), ALU.add))
                    V(lambda e: e.tensor_reduce(t1[:], mk[:], AX.X, ALU.max))
                    V(lambda e: e.tensor_scalar(m1[:], mk[:], t1[:], None, ALU.is_ge))
                    V(lambda e: e.scalar_tensor_tensor(mk[:], m1[:], -20.0, mk[:], ALU.mult, ALU.add))
                    V(lambda e: e.tensor_reduce(t1[:], mk[:], AX.X, ALU.max))
                    V(lambda e: e.tensor_scalar(m2[:], mk[:], t1[:], None, ALU.is_ge))
                    V(lambda e: e.tensor_tensor(m1[:], m1[:], m2[:], ALU.add))
                    V(lambda e: e.tensor_tensor(m1[:], m1[:], sc[:], ALU.mult))
                    V(lambda e: e.tensor_reduce(t1[:], m1[:], AX.X, ALU.add))
                    V(lambda e: e.reciprocal(t1[:], t1[:]))
                    V(lambda e: e.tensor_scalar(cmb[:], m1[:], t1[:], None, ALU.mult))
                    P.dma("sp", COMB[r0:r0 + 128, :], cmb[:], reads=[S_.rr], writes=[rCOMB[gi]])

                prev = None
                for n_, (b, ti) in enumerate(tiles_l):
                    cur = (dsets[n_ % 2], b, ti)
                    dtile(*cur)
                    if prev is not None:
                        dtile2(*prev)
                    dtile1b(*cur)
                    if prev is not None:
                        dtile2b(*prev)
                    prev = cur
                dtile2(*prev)
                dtile2b(*prev)
                P.flush()

            if phases is None or 6 in phases:
              with ExitStack() as st:
                xt_tiles = [(b, ti) for b in range(2) for ti in range(2, 18)]
                sblocks = [xt_tiles[i * 8:(i + 1) * 8] for i in range(4)]
                if not last:
                    sblocks.append([(0, 0), (0, 1), (1, 0), (1, 1)])
                WG = sbt(st, "WG", [128, 16, 512], BF16)
                WU = sbt(st, "WU", [128, 16, 512], BF16)
                WD = sbt(st, "WD", [128, 4, D], BF16)
                rWG, rWD = R(), R()
                h2 = sbt(st, "h2", [128, 16, 1024], BF16)
                rh2 = R()
                accm = sbt(st, "accm", [128, 8, D], F32)
                racc = [R() for _ in range(8)]
                cmb = sbt(st, "cmbm", [128, 8, 16], F32)
                rcmb = R()
                actT = sbt(st, "actT", [128, 4, 1024], BF16)
                ract = R()
                sgl = [sbt(st, "sgl%d" % i, [128, 512], F32) for i in range(2)]
                rsgl = [R(), R()]
                pGU = [pst(st, "pGU%d" % i, [128, 2, 512], F32) for i in range(2)]
                rpGU = [R(), R()]
                pY = [pst(st, "pYm%d" % i, [128, 1024], F32) for i in range(2)]
                rpY = [R(), R()]
                xt = sbt(st, "xtm", [128, D], F32)
                rxt = R()
                junk = sbt(st, "junkm", [128, D], BF16)
                ss = sbt(st, "ssm", [128, 1], F32)
                rstd = sbt(st, "rstdm", [128, 1], F32)
                rtmp = R()
                fg = sbt(st, "fg", [128, D], F32)
                rfg = R()
                if last:
                    P.dma("sp", fg[:], fng, writes=[rfg])
                g2buf = (sbt(st, "g2b", [128, D], F32), R(), sbt(st, "g2dg", [128, 128], F32), R())
                g2cond = None
                kq = 0
                for sbk in sblocks:
                    nt = len(sbk)
                    nh = nt // 4
                    for i, (b, ti) in enumerate(sbk):
                        gi = b * 18 + ti
                        r0 = b * TB + ti * 128
                        P.dma("sp", h2[:, :, i * 128:(i + 1) * 128], HT[:, r0:r0 + 128].rearrange("(j p) t -> p j t", p=128), reads=[rHT[gi]], writes=[rh2])
                        P.dma("sp", cmb[:, i, :], COMB[r0:r0 + 128, :], reads=[rCOMB[gi]], writes=[rcmb])
                    for ex in range(16):
                        P.dma("pool", WG[:], wg[l, ex].rearrange("(j p) f -> p j f", p=128), writes=[rWG])
                        P.dma("pool", WU[:], wu[l, ex].rearrange("(j p) f -> p j f", p=128), writes=[rWG])
                        for j in range(4):
                            P.dma("pool", WD[:, j, :], wd[l, ex, j * 128:(j + 1) * 128, :], writes=[rWD], max_dma_last_dim=4096)
                        for hb in range(nh):
                            for fc in range(4):
                                pg, rpg = pGU[kq % 2], rpGU[kq % 2]
                                sg, rsg = sgl[kq % 2], rsgl[kq % 2]
                                kq += 1

                                def f(e, pg=pg, fc=fc, hb=hb):
                                    for j in range(16):
                                        e.matmul(pg[:, 0, :], WG[:, j, fc * 128:(fc + 1) * 128], h2[:, j, hb * 512:(hb + 1) * 512], start=(j == 0), stop=(j == 15))
                                    for j in range(16):
                                        ins = e.matmul(pg[:, 1, :], WU[:, j, fc * 128:(fc + 1) * 128], h2[:, j, hb * 512:(hb + 1) * 512], start=(j == 0), stop=(j == 15))
                                    return ins
                                P.emit("pe", f, reads=[rWG, rh2], writes=[rpg])
                                P.emit("act", lambda e, pg=pg, sg=sg: e.activation(sg[:], pg[:, 0, :], AF.Silu), reads=[rpg], writes=[rsg])
                                P.emit("dve", lambda e, pg=pg, sg=sg, fc=fc, hb=hb: e.tensor_tensor(actT[:, fc, hb * 512:(hb + 1) * 512], sg[:], pg[:, 1, :], ALU.mult), reads=[rpg, rsg], writes=[ract])
                        for i in range(nt):
                            for dh in range(2):
                                py, rpy = pY[kq % 2], rpY[kq % 2]
                                kq += 1

                                def f(e, py=py, i=i, dh=dh):
                                    for nb in range(2):
                                        for fc in range(4):
                                            ins = e.matmul(py[:, nb * 512:(nb + 1) * 512], actT[:, fc, i * 128:(i + 1) * 128], WD[:, fc, dh * 1024 + nb * 512:dh * 1024 + (nb + 1) * 512], start=(fc == 0), stop=(fc == 3))
                                    return ins
                                P.emit("pe", f, reads=[ract, rWD], writes=[rpy])
                                a_ = accm[:, i, dh * 1024:(dh + 1) * 1024]
                                if ex == 0:
                                    P.emit("dve", lambda e, py=py, a_=a_, i=i, ex=ex: e.tensor_scalar(a_, py[:], cmb[:, i, ex:ex + 1], None, ALU.mult), reads=[rpy, rcmb], writes=[racc[i]])
                                else:
                                    P.emit("dve", lambda e, py=py, a_=a_, i=i, ex=ex: e.scalar_tensor_tensor(a_, py[:], cmb[:, i, ex:ex + 1], a_, ALU.mult, ALU.add), reads=[rpy, rcmb, racc[i]], writes=[racc[i]])
                    for i, (b, ti) in enumerate(sbk):
                        gi = b * 18 + ti
                        r0 = b * TB + ti * 128
                        c = 2 if ti < 2 else b
                        if g2cond != c:
                            build_gate(st, "g2", 80, [c], pg=pY[0], rpg=rpY[0], gbuf=g2buf)
                            g2cond = c
                        g2, rg2 = g2buf[0], g2buf[1]
                        P.dma("sp", xt[:], RES[r0:r0 + 128, :], reads=[rRES[gi]], writes=[rxt])
                        P.emit("pool", lambda e, i=i, g2=g2: e.tensor_tensor(accm[:, i, :], accm[:, i, :], g2[:], ALU.mult), reads=[racc[i], rg2], writes=[racc[i]])
                        P.emit("pool", lambda e, i=i: e.tensor_tensor(xt[:], xt[:], accm[:, i, :], ALU.add), reads=[racc[i], rxt], writes=[rxt])
                        if not last:
                            P.dma("sp", RES[r0:r0 + 128, :], xt[:], reads=[rxt], writes=[rRES[gi]])
                        else:
                            rms_rstd("act", xt[:], D, ss[:], rstd[:], junk[:], rxt, rtmp)
                            P.emit("act", lambda e: e.activation(xt[:], xt[:], AF.Copy, scale=rstd[:]), reads=[rxt, rtmp], writes=[rxt])
                            P.emit("dve", lambda e: e.tensor_tensor(xt[:], xt[:], fg[:], ALU.mult), reads=[rxt, rfg], writes=[rxt])
                            P.dma("sp", out[b, (ti - 2) * 128:(ti - 1) * 128, :], xt[:], reads=[rxt], writes=[rRES[gi]])
                P.flush()

        P.finish()
        P.flush()
    return nc


def _rope_tables():
    t = np.arange(2048)
    rows = (t // 64).astype(np.float32)
    cols = (t % 64).astype(np.float32)
    nf = 16
    inv = (np.float32(10000.0) ** (-np.arange(nf, dtype=np.float32) / nf)).astype(np.float32)
    ang = np.stack([rows[:, None] * inv, cols[:, None] * inv], axis=1)
    cos = np.cos(ang).astype(np.float32)
    sin = np.sin(ang).astype(np.float32)
    C = np.zeros((64, 2048), np.float32)
    S = np.zeros((64, 2048), np.float32)
    for a in range(2):
        for b in range(2):
            for f in range(16):
                idx = a * 32 + b * 16 + f
                C[idx] = cos[:, a, f]
                S[idx] = sin[:, a, f] * (-1.0 if b == 0 else 1.0)
    return np.concatenate([C, C], 0), np.concatenate([S, S], 0)


def _swap_perm():
    perm = np.zeros(64, np.int64)
    for a in range(2):
        for b in range(2):
            for f in range(16):
                perm[a * 32 + b * 16 + f] = a * 32 + (1 - b) * 16 + f
    return perm


def prep_shared(inp):
    f = lambda a: np.ascontiguousarray(np.asarray(a, dtype=np.float32))
    perm = _swap_perm()
    w_in = f(inp["w_in"])
    kpe = w_in[:, :, 3360:3424]
    kpes = kpe[:, :, perm]
    win = np.concatenate([w_in[:, :, :3360], kpe, kpe, kpes, kpes], axis=2)
    assert win.shape[2] == NWIN
    wqb = f(inp["w_q_b"]).reshape(L, 512, 8, 192)
    nope = wqb[:, :, :, :128].reshape(L, 512, 1024)
    rope = wqb[:, :, :, 128:]
    wq = np.concatenate([nope, rope.reshape(L, 512, 512), rope[:, :, :, perm].reshape(L, 512, 512)], axis=2)
    wkvb = f(inp["w_kv_b"]).reshape(L, 256, 8, 256)
    wkv = np.concatenate([wkvb[:, :, :, :128].reshape(L, 256, 1024), wkvb[:, :, :, 128:].reshape(L, 256, 1024)], axis=2)
    colT = lambda v, n: np.ascontiguousarray(f(v).reshape(L, n, 128).transpose(0, 2, 1))
    bc = lambda v: np.ascontiguousarray(np.broadcast_to(f(v).reshape(L, 1, -1), (L, 128, f(v).reshape(L, -1).shape[1])))
    convw = f(inp["conv_w"])
    convw_l = np.ascontiguousarray(convw.reshape(L, 3, 12, 128).transpose(0, 3, 2, 1).reshape(L, 128, 36))
    cosT, sinT = _rope_tables()
    i_ = np.arange(128)
    tri_f = (i_[:, None] <= i_[None, :]).astype(np.float32)
    tri_b = (i_[:, None] >= i_[None, :]).astype(np.float32)
    mask_f = np.where(i_[None, :] >= i_[:, None], 0.0, NEG).astype(np.float32)
    mask_b = np.where(i_[None, :] <= i_[:, None], 0.0, NEG).astype(np.float32)
    sh = {
        "ada_w": f(inp["ada_w"]),
        "ada_bT": colT(inp["ada_b"], 96),
        "g1T": colT(inp["norm1_g"], 16), "g2T": colT(inp["norm2_g"], 16),
        "win": np.ascontiguousarray(win),
        "convw": convw_l, "convb": colT(inp["conv_b"], 12),
        "dtb": bc(inp["dt_bias"]), "alog": bc(inp["a_log"]), "dskip": bc(inp["d_skip"]),
        "ssdg": bc(inp["ssd_norm_g"]),
        "qgT": colT(inp["q_norm_g"], 4), "wq": np.ascontiguousarray(wq),
        "kvgT": colT(inp["kv_norm_g"], 2), "wkv": np.ascontiguousarray(wkv),
        "wo": f(inp["w_o"]),
        "rwT": np.ascontiguousarray(f(inp["router_w"]).reshape(16, 128, 16).transpose(1, 0, 2)),
        "rb": np.ascontiguousarray(np.broadcast_to(f(inp["router_b"]).reshape(1, 16), (128, 16))),
        "wg": f(inp["w_gate"]), "wu": f(inp["w_up"]), "wd": f(inp["w_down"]),
        "fng": np.ascontiguousarray(np.broadcast_to(f(inp["final_norm_g"]).reshape(1, D), (128, D))),
        "c_ident": np.eye(128, dtype=np.float32),
        "c_tri": np.stack([tri_f, tri_b]),
        "c_mask": np.stack([np.tile(mask_f, (1, 4)), np.tile(mask_b, (1, 4))]),
        "c_cos": cosT, "c_sin": sinT,
    }
    return sh


def core_inputs(inp, sh, core):
    f = lambda a: np.ascontiguousarray(np.asarray(a, dtype=np.float32))
    b0 = core * 2
    cc = np.stack([f(inp["c"])[b0], f(inp["c"])[b0 + 1], f(inp["c_ctx"])], axis=1)
    m = dict(sh)
    m["xin"] = f(inp["x"][b0:b0 + 2])
    m["cin"] = f(inp["ctx"][b0:b0 + 2])
    m["ccT"] = np.ascontiguousarray(cc.reshape(16, 128, 3).transpose(1, 0, 2))
    return m


_NC = None
_SKIP = set()


def kernel(**inputs):
    global _NC
    if _NC is None:
        _NC = build()
    sh = prep_shared(inputs)
    in_maps = [core_inputs(inputs, sh, c) for c in range(8)]
    res = run_bass_kernel_spmd(_NC, in_maps, core_ids=list(range(8)))
    return np.concatenate([np.asarray(r["out"]) for r in res.results], axis=0).astype(np.float32)
```

```python
import numpy as np
from contextlib import ExitStack
import concourse.bass as bass
import concourse.mybir as mybir
from concourse.bass_utils import run_bass_kernel_spmd

F32 = mybir.dt.float32
BF16 = mybir.dt.bfloat16
AF = mybir.ActivationFunctionType
ALU = mybir.AluOpType
AX = mybir.AxisListType

L = 2
D = 2048
TB = 2304
T = 2 * TB
EPS = 1e-6
NWIN = 3616
ATTN_SCALE = 192.0 ** -0.5
NEG = -30000.0


class R:
    __slots__ = ("lw", "rd")

    def __init__(self):
        self.lw = None
        self.rd = {}


class Prog:
    ENGS = ("pe", "act", "dve", "pool", "sp")
    NDS = 48
    NHW = 36

    def __init__(self, nc, es):
        self.nc = nc
        self.q = {e: [] for e in self.ENGS}
        self.cnt = {e: 0 for e in self.ENGS}
        self.waited = {e: {} for e in self.ENGS}
        self.sem = {e: es.enter_context(nc.semaphore("s_" + e)) for e in self.ENGS}
        self.dsem = [es.enter_context(nc.semaphore("d%d" % i)) for i in range(self.NDS)]
        self.dcnt = [0] * self.NDS
        self.dnext = 0
        self.dnext_sw = 0

    def _semof(self, key):
        return self.sem[key[1]] if key[0] == "e" else self.dsem[key[1]]

    def _deps(self, eng, reads, writes, extra=()):
        deps = {}

        def add(d):
            if d is None:
                return
            k, v = d
            if deps.get(k, 0) < v:
                deps[k] = v

        for r in reads:
            add(r.lw)
        for w in writes:
            add(w.lw)
            for kv in w.rd.items():
                add(kv)
        for d in extra:
            add(d)
        waits = []
        wd = self.waited[eng]
        for k, v in deps.items():
            if eng == "pe" and k == ("e", "pe"):
                continue
            if wd.get(k, 0) >= v:
                continue
            wd[k] = v
            waits.append((self._semof(k), v))
        return waits

    def emit(self, eng, fn, reads=(), writes=()):
        waits = self._deps(eng, reads, writes)
        self.cnt[eng] += 1
        key = ("e", eng)
        val = self.cnt[eng]
        sem = self.sem[eng]

        def thunk(e):
            for s, v in waits:
                e.wait_ge(s, v)
            fn(e).then_inc(sem, 1)

        self.q[eng].append(thunk)
        for r in reads:
            r.rd[key] = val
        for w in writes:
            w.lw = (key, val)
            w.rd = {}

    def dma(self, queue, out, in_, reads=(), writes=(), **kw):
        if queue == "pool":
            i = self.NHW + self.dnext_sw
            self.dnext_sw = (self.dnext_sw + 1) % (self.NDS - self.NHW)
        else:
            i = self.dnext
            self.dnext = (i + 1) % self.NHW
        prev = self.dcnt[i]
        self.dcnt[i] += 16
        val = self.dcnt[i]
        key = ("d", i)
        extra = [(key, prev)] if prev > 0 else []
        waits = self._deps(queue, reads, writes, extra)
        sem = self.dsem[i]

        def thunk(e):
            for s, v in waits:
                e.wait_ge(s, v)
            e.dma_start(out=out, in_=in_, **kw).then_inc(sem, 16)

        self.q[queue].append(thunk)
        for r in reads:
            r.rd[key] = val
        for w in writes:
            w.lw = (key, val)
            w.rd = {}

    def finish(self):
        waits = []
        for i in range(self.NDS):
            if self.dcnt[i] > 0:
                waits.append((self.dsem[i], self.dcnt[i]))
        for en in self.ENGS:
            if en != "sp" and self.cnt[en] > 0:
                waits.append((self.sem[en], self.cnt[en]))

        def thunk(e):
            for s, v in waits:
                e.wait_ge(s, v)

        self.q["sp"].append(thunk)

    def barrier(self):
        for en in self.ENGS:
            waits = []
            wd = self.waited[en]
            for i in range(self.NDS):
                k = ("d", i)
                if self.dcnt[i] > wd.get(k, 0):
                    wd[k] = self.dcnt[i]
                    waits.append((self.dsem[i], self.dcnt[i]))
            for e2 in self.ENGS:
                k = ("e", e2)
                if e2 != en and self.cnt[e2] > wd.get(k, 0):
                    wd[k] = self.cnt[e2]
                    waits.append((self.sem[e2], self.cnt[e2]))

            def thunk(e, waits=waits):
                for s_, v in waits:
                    e.wait_ge(s_, v)
            self.q[en].append(thunk)

    def flush(self):
        self.barrier()
        nc = self.nc
        q = self.q
        with nc.Block() as block:
            @block.tensor
            def _(e):
                for t in q["pe"]:
                    t(e)

            @block.scalar
            def _(e):
                for t in q["act"]:
                    t(e)

            @block.vector
            def _(e):
                for t in q["dve"]:
                    t(e)

            @block.gpsimd
            def _(e):
                for t in q["pool"]:
                    t(e)

            @block.sync
            def _(e):
                for t in q["sp"]:
                    t(e)
        self.q = {e: [] for e in self.ENGS}


def build(dbg=False, nlayers=L, phases=None):
    nc = bass.Bass("TRN2", target_bir_lowering=False)

    def din(name, shape, dt=F32):
        return nc.dram_tensor(name, list(shape), dt, kind="ExternalInput").ap()

    def dscr(name, shape, dt):
        return nc.dram_tensor(name, list(shape), dt, kind=("ExternalOutput" if dbg else "Internal")).ap()

    xin = din("xin", [2, 2048, D])
    cin = din("cin", [2, 256, D])
    ccT = din("ccT", [128, 16, 3])
    ada_w = din("ada_w", [L, D, 12288])
    ada_bT = din("ada_bT", [L, 128, 96])
    g1T = din("g1T", [L, 128, 16])
    g2T = din("g2T", [L, 128, 16])
    win = din("win", [L, D, NWIN])
    convw = din("convw", [L, 128, 36])
    convb = din("convb", [L, 128, 12])
    dtb = din("dtb", [L, 128, 32])
    alog = din("alog", [L, 128, 32])
    dskip = din("dskip", [L, 128, 16])
    ssdg = din("ssdg", [L, 128, 1024])
    qgT = din("qgT", [L, 128, 4])
    wq = din("wq", [L, 512, 2048])
    kvgT = din("kvgT", [L, 128, 2])
    wkv = din("wkv", [L, 256, 2048])
    wo = din("wo", [L, D, D])
    rwT = din("rwT", [128, 16, 16])
    rb = din("rb", [128, 16])
    wg = din("wg", [L, 16, D, 512])
    wu = din("wu", [L, 16, D, 512])
    wd = din("wd", [L, 16, 512, D])
    fng = din("fng", [128, D])
    c_ident = din("c_ident", [128, 128])
    c_tri = din("c_tri", [2, 128, 128])
    c_mask = din("c_mask", [2, 128, 512])
    c_cos = din("c_cos", [128, 2048])
    c_sin = din("c_sin", [128, 2048])
    out = nc.dram_tensor("out", [2, 2048, D], F32, kind="ExternalOutput").ap()

    RES = dscr("RES", [T, D], F32)
    ZT = dscr("ZT", [T, 1024], BF16)
    DTR = dscr("DTR", [T, 32], F32)
    XBC = dscr("XBC", [1536, T], BF16)
    QN = dscr("QN", [8, 128, T], BF16)
    QR = dscr("QR", [4, 128, T], BF16)
    KN = dscr("KN", [8, 128, T], BF16)
    KR = dscr("KR", [128, T], BF16)
    VV = dscr("VV", [T, 1024], BF16)
    YF = dscr("YF", [T, 1024], F32)
    YB = dscr("YB", [T, 1024], F32)
    MIX = dscr("MIX", [D, T], BF16)
    HT = dscr("HT", [D, T], BF16)
    COMB = dscr("COMB", [T, 16], F32)
    DBGM = dscr("DBGM", [128, 96, 3], F32)
    rRES = [R() for _ in range(36)]
    rZT = [R() for _ in range(36)]
    rDTR = [R() for _ in range(36)]
    rXBC = [R() for _ in range(2)]
    rQK = [R() for _ in range(2)]
    rYF = [R() for _ in range(36)]
    rYB = [R() for _ in range(36)]
    rMIX = [R() for _ in range(36)]
    rHT = [R() for _ in range(36)]
    rCOMB = [R() for _ in range(36)]

    def resid_src(l, b, ti):
        if l == 0:
            if ti < 2:
                return cin[b, ti * 128:(ti + 1) * 128, :]
            return xin[b, (ti - 2) * 128:(ti - 1) * 128, :]
        r0 = b * TB + ti * 128
        return RES[r0:r0 + 128, :]

    with ExitStack() as es:
        P = Prog(nc, es)

        uid = [0]

        def sbt(st, name, shape, dt):
            uid[0] += 1
            return st.enter_context(nc.sbuf_tensor("%s_%d" % (name, uid[0]), list(shape), dt))

        def pst(st, name, shape, dt):
            uid[0] += 1
            return st.enter_context(nc.psum_tensor("%s_%d" % (name, uid[0]), list(shape), dt))

        identf = sbt(es, "identf", [128, 128], F32)
        identb = sbt(es, "identb", [128, 128], BF16)
        onesf = sbt(es, "onesf", [128, 128], F32)
        onesb = sbt(es, "onesb", [128, 128], BF16)
        modS = sbt(es, "modS", [128, 96, 3], F32)
        gm1 = sbt(es, "gm1", [128, 16, 3], F32)
        gm2 = sbt(es, "gm2", [128, 16, 3], F32)
        rC = R()
        rmod = R()
        P.dma("sp", identf[:], c_ident, writes=[rC])
        P.emit("dve", lambda e: e.tensor_copy(identb[:], identf[:]), reads=[rC], writes=[rC])
        P.emit("dve", lambda e: e.memset(onesf[:], 1.0), writes=[rC])
        P.emit("dve", lambda e: e.memset(onesb[:], 1.0), writes=[rC])

        def rms_rstd(eng_src, src_ap, n, ss, rstd, junk, rsrc, rtmp):
            P.emit("act", lambda e: e.activation(junk, src_ap, AF.Square, accum_out=ss), reads=[rsrc], writes=[rtmp])
            P.emit("act", lambda e: e.activation(rstd, ss, AF.Sqrt, bias=EPS, scale=1.0 / n), reads=[rtmp], writes=[rtmp])
            P.emit("dve", lambda e: e.reciprocal(rstd, rstd), reads=[rtmp], writes=[rtmp])

        for l in range(nlayers):
            last = (l == L - 1)
            tiles_l = [(b, ti) for b in range(2) for ti in range(18) if not (last and ti < 2)]

            if phases is None or 0 in phases:
              with ExitStack() as st:
                scf = sbt(st, "scf", [128, 16, 3], F32)
                scb = sbt(st, "scb", [128, 16, 3], BF16)
                abT = sbt(st, "abT", [128, 96], F32)
                g1s = sbt(st, "g1s", [128, 16], F32)
                g2s = sbt(st, "g2s", [128, 16], F32)
                awf = [sbt(st, "awf%d" % i, [128, 16, 512], F32) for i in range(2)]
                rawf = [R(), R()]
                aw = [sbt(st, "aw%d" % i, [128, 16, 512], BF16) for i in range(2)]
                raw = [R(), R()]
                pm = pst(st, "pm", [128, 96, 3], F32)
                rpm = R()
                rs = R()
                P.dma("sp", scf[:], ccT, writes=[rs])
                P.dma("sp", abT[:], ada_bT[l], writes=[rs])
                P.dma("sp", g1s[:], g1T[l], writes=[rs])
                P.dma("sp", g2s[:], g2T[l], writes=[rs])
                P.emit("act", lambda e: e.activation(scb[:], scf[:], AF.Silu), reads=[rs], writes=[rs])
                for pc in range(24):
                    af, raf = awf[pc % 2], rawf[pc % 2]
                    a, ra = aw[pc % 2], raw[pc % 2]
                    for hj in range(2):
                        P.dma("sp" if hj == 0 else "act", af[:, hj * 8:(hj + 1) * 8, :], ada_w[l, hj * 1024:(hj + 1) * 1024, pc * 512:(pc + 1) * 512].rearrange("(j p) m -> p j m", p=128), writes=[raf])
                    P.emit("dve", lambda e, a=a, af=af: e.tensor_copy(a[:, 0:6, :], af[:, 0:6, :]), reads=[raf], writes=[ra])
                    P.emit("pool", lambda e, a=a, af=af: e.tensor_copy(a[:, 6:10, :], af[:, 6:10, :]), reads=[raf], writes=[ra])
                    P.emit("act", lambda e, a=a, af=af: e.activation(a[:, 10:16, :], af[:, 10:16, :], AF.Copy), reads=[raf], writes=[ra])
                    for mc in range(4):
                        m = pc * 4 + mc

                        def f(e, a=a, m=m, mc=mc):
                            for j in range(16):
                                ins = e.matmul(pm[:, m, :], a[:, j, mc * 128:(mc + 1) * 128], scb[:, j, :], start=(j == 0), stop=(j == 15))
                            return ins
                        P.emit("pe", f, reads=[ra, rs], writes=[rpm])
                P.emit("dve", lambda e: e.tensor_tensor(modS[:], pm[:], abT[:].unsqueeze(2).broadcast_to([128, 96, 3]), ALU.add), reads=[rpm, rs, rmod], writes=[rmod])
                P.emit("dve", lambda e: e.tensor_scalar(gm1[:], modS[:, 16:32, :], 1.0, None, ALU.add), reads=[rmod], writes=[rmod])
                P.emit("dve", lambda e: e.tensor_tensor(gm1[:], gm1[:], g1s[:].unsqueeze(2).broadcast_to([128, 16, 3]), ALU.mult), reads=[rmod, rs], writes=[rmod])
                P.emit("dve", lambda e: e.tensor_scalar(gm2[:], modS[:, 64:80, :], 1.0, None, ALU.add), reads=[rmod], writes=[rmod])
                P.emit("dve", lambda e: e.tensor_tensor(gm2[:], gm2[:], g2s[:].unsqueeze(2).broadcast_to([128, 16, 3]), ALU.mult), reads=[rmod, rs], writes=[rmod])
                if dbg:
                    P.dma("sp", DBGM, modS[:], reads=[rmod])
                P.flush()

            def build_gate(st, name, j0, conds, pg=None, rpg=None, gbuf=None):
                G = {}
                dg = gbuf[2] if gbuf else sbt(st, name + "dg", [128, 128], F32)
                if pg is None:
                    pg = pst(st, name + "pg", [128, 512], F32)
                    rpg = R()
                rdg = gbuf[3] if gbuf else R()
                for c in conds:
                    if gbuf:
                        g, rg = gbuf[0], gbuf[1]
                    else:
                        g = sbt(st, name + "G%d" % c, [128, D], F32)
                        rg = R()
                    for j in range(16):
                        P.emit("dve", lambda e, j=j, c=c: e.tensor_scalar(dg[:], identf[:], modS[:, j0 + j, c:c + 1], None, ALU.mult), reads=[rmod, rC], writes=[rdg])
                        P.emit("pe", lambda e, j=j: e.matmul(pg[:, (j % 4) * 128:(j % 4 + 1) * 128], onesf[:], dg[:], start=True, stop=True), reads=[rdg, rC], writes=[rpg])
                        if j % 4 == 3:
                            P.emit("act", lambda e, j=j, g=g: e.activation(g[:, (j - 3) * 128:(j + 1) * 128], pg[:, 0:512], AF.Copy), reads=[rpg], writes=[rg])
                    G[c] = (g, rg)
                return G

            if phases is None or 1 in phases:
              with ExitStack() as st:
                NA = 2592
                W = sbt(st, "W", [128, 16, NA], BF16)
                rW = R()
                for j in range(16):
                    P.dma("pool", W[:, j, :], win[l, j * 128:(j + 1) * 128, 0:NA], writes=[rW], max_dma_last_dim=4096)
                xt = [sbt(st, "xt%d" % i, [128, D], F32) for i in range(2)]
                rxt = [R(), R()]
                junk = sbt(st, "junk", [128, D], BF16)
                ss = sbt(st, "ss", [128, 1], F32)
                rstd = sbt(st, "rstd", [128, 1], F32)
                rtmp = R()
                xns = [sbt(st, "xn%d" % i, [128, D], BF16) for i in range(2)]
                rxns = [R(), R()]
                hTs = [sbt(st, "hT%d" % i, [128, 16, 512], BF16) for i in range(2)]
                rhTs = [R(), R()]
                gcnt = 0
                pT = [pst(st, "pT%d" % i, [128, 8, 128], BF16) for i in range(2)]
                rpT = [R(), R()]
                pO = [pst(st, "pO%d" % i, [128, 512], F32) for i in range(4)]
                rpO = [R() for _ in range(4)]
                ob = [sbt(st, "ob%d" % i, [128, 512], BF16) for i in range(4)]
                rob = [R() for _ in range(4)]
                dto = sbt(st, "dto", [128, 32], F32)
                rdto = R()
                cnt = 0
                a1_tiles = [(b, t0 + i) for b in range(2) for (t0, nt) in ((0, 2), (2, 4), (6, 4), (10, 4), (14, 4)) for i in range(nt)]

                def a1_norm(k_):
                    b_, ti_ = a1_tiles[k_]
                    x_, rx = xt[k_ % 2], rxt[k_ % 2]
                    xn_, rxn_ = xns[k_ % 2], rxns[k_ % 2]
                    P.dma("sp", x_[:], resid_src(l, b_, ti_), reads=[rRES[b_ * 18 + ti_]], writes=[rx])
                    rms_rstd("act", x_[:], D, ss[:], rstd[:], junk[:], rx, rtmp)
                    P.emit("act", lambda e: e.activation(xn_[:], x_[:], AF.Copy, scale=rstd[:]), reads=[rx, rtmp], writes=[rxn_])
                for b in range(2):
                    for (t0, nt) in ((0, 2), (2, 4), (6, 4), (10, 4), (14, 4)):
                        c = 2 if t0 == 0 else b
                        n = nt * 128
                        col0 = b * TB + t0 * 128
                        hT, rhT = hTs[gcnt % 2], rhTs[gcnt % 2]
                        gcnt += 1
                        for i in range(nt):
                            ti = t0 + i
                            gi = b * 18 + ti
                            xn, rxn = xns[cnt % 2], rxns[cnt % 2]
                            if cnt == 0:
                                a1_norm(0)
                            if cnt + 1 < len(a1_tiles):
                                a1_norm(cnt + 1)
                            cnt += 1
                            for hh in range(2):
                                def f(e, hh=hh, xn=xn):
                                    for jj in range(8):
                                        j = hh * 8 + jj
                                        ins = e.transpose(pT[hh][:, jj, :], xn[:, j * 128:(j + 1) * 128], identb[:])
                                    return ins
                                P.emit("pe", f, reads=[rxn, rC], writes=[rpT[hh]])
                                for jj in range(8):
                                    j = hh * 8 + jj
                                    if jj % 2 == 0:
                                        P.emit("dve", lambda e, hh=hh, jj=jj, j=j, i=i, c=c, hT=hT: e.tensor_scalar(hT[:, j, i * 128:(i + 1) * 128], pT[hh][:, jj, :], gm1[:, j, c:c + 1], modS[:, j, c:c + 1], ALU.mult, ALU.add), reads=[rpT[hh], rmod], writes=[rhT])
                                    else:
                                        P.emit("act", lambda e, hh=hh, jj=jj, j=j, i=i, c=c, hT=hT: e.activation(hT[:, j, i * 128:(i + 1) * 128], pT[hh][:, jj, :], AF.Identity, scale=gm1[:, j, c:c + 1], bias=modS[:, j, c:c + 1]), reads=[rpT[hh], rmod], writes=[rhT])
                        P.dma("sp", HT[:, col0:col0 + n].rearrange("(j p) t -> p j t", p=128), hT[:, :, 0:n], reads=[rhT], writes=[rHT[b * 18 + t0 + i] for i in range(nt)])
                        k = 0
                        for i in range(nt):
                            ti = t0 + i
                            gi = b * 18 + ti
                            r0 = b * TB + ti * 128
                            for zb in range(2):
                                po, rpo, o_, ro = pO[k % 4], rpO[k % 4], ob[k % 4], rob[k % 4]
                                k += 1

                                def f(e, po=po, zb=zb, i=i, hT=hT):
                                    for j in range(16):
                                        ins = e.matmul(po[:], hT[:, j, i * 128:(i + 1) * 128], W[:, j, zb * 512:(zb + 1) * 512], start=(j == 0), stop=(j == 15))
                                    return ins
                                P.emit("pe", f, reads=[rhT, rW], writes=[rpo])
                                P.emit("act" if k % 2 else "dve", (lambda e, po=po, o_=o_: e.activation(o_[:], po[:], AF.Copy)) if k % 2 else (lambda e, po=po, o_=o_: e.tensor_copy(o_[:], po[:])), reads=[rpo], writes=[ro])
                                P.dma("sp", ZT[r0:r0 + 128, zb * 512:(zb + 1) * 512], o_[:], reads=[ro], writes=[rZT[gi]])
                            po, rpo = pO[k % 4], rpO[k % 4]
                            k += 1

                            def f(e, po=po, i=i, hT=hT):
                                for j in range(16):
                                    ins = e.matmul(po[:, 0:32], hT[:, j, i * 128:(i + 1) * 128], W[:, j, 2560:2592], start=(j == 0), stop=(j == 15))
                                return ins
                            P.emit("pe", f, reads=[rhT, rW], writes=[rpo])
                            P.emit("dve", lambda e, po=po: e.tensor_copy(dto[:], po[:, 0:32]), reads=[rpo], writes=[rdto])
                            P.dma("sp", DTR[r0:r0 + 128, :], dto[:], reads=[rdto], writes=[rDTR[gi]])
                        for m in range(12):
                            po, rpo, o_, ro = pO[k % 4], rpO[k % 4], ob[k % 4], rob[k % 4]
                            k += 1

                            def f(e, po=po, m=m, n=n, hT=hT):
                                for j in range(16):
                                    ins = e.matmul(po[:, 0:n], W[:, j, 1024 + m * 128:1024 + (m + 1) * 128], hT[:, j, 0:n], start=(j == 0), stop=(j == 15))
                                return ins
                            P.emit("pe", f, reads=[rhT, rW], writes=[rpo])
                            P.emit("act" if k % 2 else "dve", (lambda e, po=po, o_=o_, n=n: e.activation(o_[:, 0:n], po[:, 0:n], AF.Copy)) if k % 2 else (lambda e, po=po, o_=o_, n=n: e.tensor_copy(o_[:, 0:n], po[:, 0:n])), reads=[rpo], writes=[ro])
                            P.dma("sp", XBC[m * 128:(m + 1) * 128, col0:col0 + n], o_[:, 0:n], reads=[ro], writes=[rXBC[b]])
                P.flush()

            if phases is None or 2 in phases:
              with ExitStack() as st:
                NB2 = NWIN - 2592
                W = sbt(st, "W2", [128, 16, NB2], BF16)
                rW = R()
                for j in range(16):
                    P.dma("pool", W[:, j, :], win[l, j * 128:(j + 1) * 128, 2592:NWIN], writes=[rW])
                WQ = sbt(st, "WQ", [128, 4, 2048], BF16)
                WKV = sbt(st, "WKV", [128, 2, 2048], BF16)
                for j in range(4):
                    P.dma("pool", WQ[:, j, :], wq[l, j * 128:(j + 1) * 128, :], writes=[rW], max_dma_last_dim=4096)
                for j in range(2):
                    P.dma("pool", WKV[:, j, :], wkv[l, j * 128:(j + 1) * 128, :], writes=[rW], max_dma_last_dim=4096)
                cosT = sbt(st, "cosT", [128, 2048], F32)
                sinT = sbt(st, "sinT", [128, 2048], F32)
                qg = sbt(st, "qg", [128, 4], F32)
                kvg = sbt(st, "kvg", [128, 2], F32)
                P.dma("sp", cosT[:], c_cos, writes=[rW])
                P.dma("sp", sinT[:], c_sin, writes=[rW])
                P.dma("sp", qg[:], qgT[l], writes=[rW])
                P.dma("sp", kvg[:], kvgT[l], writes=[rW])
                hT = [sbt(st, "hT2%d" % i, [128, 16, 512], BF16) for i in range(2)]
                rhT = [R(), R()]
                pO = [pst(st, "pO%d" % i, [128, 512], F32) for i in range(6)]
                rpO = [R() for _ in range(6)]
                pSq = [pst(st, "pSq%d" % i, [128, 512], F32) for i in range(2)]
                rpSq = [R(), R()]

                class NS:
                    pass
                nsets = {}
                for nm, nch in (("q", 4), ("kv", 2)):
                    for i in range(2):
                        s_ = NS()
                        s_.sq = sbt(st, "sq%s%d" % (nm, i), [128, nch, 512], BF16)
                        s_.qa = sbt(st, "qa%s%d" % (nm, i), [128, nch, 512], F32)
                        s_.qab = sbt(st, "qab%s%d" % (nm, i), [128, nch, 512], BF16)
                        s_.rbc = sbt(st, "rbc%s%d" % (nm, i), [128, 512], F32)
                        s_.rsq, s_.rqa, s_.rqab, s_.rrbc = R(), R(), R(), R()
                        nsets[(nm, i)] = s_
                ob = [sbt(st, "ob%d" % i, [128, 512], BF16) for i in range(6)]
                rob = [R() for _ in range(6)]
                ta = [sbt(st, "ta%d" % i, [128, 512], F32) for i in range(2)]
                tb_ = [sbt(st, "tb%d" % i, [128, 512], F32) for i in range(2)]
                rta, rtb = [R(), R()], [R(), R()]
                ov = [sbt(st, "ov%d" % i, [128, 1024], BF16) for i in range(2)]
                rov = [R(), R()]
                kst = [0, 0, 0, 0]

                def group(b, t0, nt, gidx):
                    isx = t0 > 0
                    n = nt * 128
                    col0 = b * TB + t0 * 128
                    p0 = (t0 - 2) * 128
                    h_ = hT[gidx % 2]
                    rh = rhT[gidx % 2]
                    Nq = nsets[("q", gidx % 2)]
                    Nk = nsets[("kv", gidx % 2)]
                    P.dma("sp", h_[:, :, 0:n], HT[:, col0:col0 + n].rearrange("(j p) t -> p j t", p=128), reads=[rHT[b * 18 + t0 + i] for i in range(nt)], writes=[rh])

                    def nps():
                        kst[0] += 1
                        return pO[kst[0] % 6], rpO[kst[0] % 6]

                    def nob():
                        kst[1] += 1
                        return ob[kst[1] % 6], rob[kst[1] % 6]

                    def store(dst, src, rsrc):
                        kst[3] += 1
                        P.dma("sp" if kst[3] % 2 else "act", dst, src, reads=[rsrc], writes=[rQK[b]])

                    def proj(po, c0):
                        def f(e):
                            for j in range(16):
                                ins = e.matmul(po[:, 0:n], W[:, j, c0:c0 + 128], h_[:, j, 0:n], start=(j == 0), stop=(j == 15))
                            return ins
                        return f

                    def stage1(N_, nchunk, c0, gcol):
                        for m in range(nchunk):
                            po, rpo = nps()
                            P.emit("pe", proj(po, c0 + m * 128), reads=[rh, rW], writes=[rpo])
                            P.emit("act", lambda e, po=po, m=m: e.activation(N_.qa[:, m, 0:n], po[:, 0:n], AF.Copy), reads=[rpo], writes=[N_.rqa])
                            P.emit("dve", lambda e, m=m: e.tensor_tensor(N_.sq[:, m, 0:n], N_.qa[:, m, 0:n], N_.qa[:, m, 0:n], ALU.mult), reads=[N_.rqa], writes=[N_.rsq])
                            P.emit("pool", lambda e, m=m: e.tensor_scalar(N_.qa[:, m, 0:n], N_.qa[:, m, 0:n], gcol[:, m:m + 1], 1.0, ALU.mult, ALU.mult), reads=[N_.rqa, rW, N_.rsq], writes=[N_.rqa])

                    def stage2(N_, nchunk, pS, rpS):
                        def f(e):
                            for m in range(nchunk):
                                ins = e.matmul(pS[:, 0:n], onesb[:], N_.sq[:, m, 0:n], start=(m == 0), stop=(m == nchunk - 1))
                            return ins
                        P.emit("pe", f, reads=[N_.rsq, rC], writes=[rpS])
                        P.emit("act", lambda e: e.activation(N_.rbc[:, 0:n], pS[:, 0:n], AF.Sqrt, bias=EPS, scale=1.0 / (nchunk * 128)), reads=[rpS], writes=[N_.rrbc])
                        P.emit("dve", lambda e: e.reciprocal(N_.rbc[:, 0:n], N_.rbc[:, 0:n]), reads=[N_.rrbc], writes=[N_.rrbc])
                        for m in range(nchunk):
                            P.emit("dve", lambda e, m=m: e.tensor_tensor(N_.qab[:, m, 0:n], N_.qa[:, m, 0:n], N_.rbc[:, 0:n], ALU.mult), reads=[N_.rqa, N_.rrbc], writes=[N_.rqab])

                    def rope_combine(pa, rpa, pb, rpb, o_, ro):
                        if not isx:
                            P.emit("act", lambda e: e.activation(o_[:, 0:n], pa[:, 0:n], AF.Copy), reads=[rpa], writes=[ro])
                            return
                        kst[2] += 1
                        ta_, tb2, rta_, rtb_ = ta[kst[2] % 2], tb_[kst[2] % 2], rta[kst[2] % 2], rtb[kst[2] % 2]
                        P.emit("dve", lambda e: e.tensor_tensor(ta_[:, 0:n], pa[:, 0:n], cosT[:, p0:p0 + n], ALU.mult), reads=[rpa, rW], writes=[rta_])
                        P.emit("dve", lambda e: e.tensor_tensor(tb2[:, 0:n], pb[:, 0:n], sinT[:, p0:p0 + n], ALU.mult), reads=[rpb, rW], writes=[rtb_])
                        P.emit("pool", lambda e: e.tensor_tensor(o_[:, 0:n], ta_[:, 0:n], tb2[:, 0:n], ALU.add), reads=[rta_, rtb_], writes=[ro])

                    stage1(Nq, 4, 0, qg)
                    stage1(Nk, 2, 512, kvg)
                    pa, rpa = nps()
                    pb, rpb = nps()
                    o_, ro = nob()
                    P.emit("pe", proj(pa, 768), reads=[rh, rW], writes=[rpa])
                    P.emit("pe", proj(pb, 896), reads=[rh, rW], writes=[rpb])
                    rope_combine(pa, rpa, pb, rpb, o_, ro)
                    store(KR[:, col0:col0 + n], o_[:, 0:n], ro)
                    stage2(Nq, 4, pSq[0], rpSq[0])
                    stage2(Nk, 2, pSq[1], rpSq[1])

                    def qproj(po, mcol):
                        def f(e):
                            for j in range(4):
                                ins = e.matmul(po[:, 0:n], WQ[:, j, mcol:mcol + 128], Nq.qab[:, j, 0:n], start=(j == 0), stop=(j == 3))
                            return ins
                        return f
                    for h in range(8):
                        po, rpo = nps()
                        o_, ro = nob()
                        P.emit("pe", qproj(po, h * 128), reads=[Nq.rqab, rW], writes=[rpo])
                        P.emit("act", lambda e, po=po, o_=o_: e.activation(o_[:, 0:n], po[:, 0:n], AF.Copy), reads=[rpo], writes=[ro])
                        store(QN[h, :, col0:col0 + n], o_[:, 0:n], ro)
                    for h in range(8):
                        po, rpo = nps()
                        o_, ro = nob()

                        def f(e, po=po, h=h):
                            for j in range(2):
                                ins = e.matmul(po[:, 0:n], WKV[:, j, h * 128:(h + 1) * 128], Nk.qab[:, j, 0:n], start=(j == 0), stop=(j == 1))
                            return ins
                        P.emit("pe", f, reads=[Nk.rqab, rW], writes=[rpo])
                        P.emit("dve", lambda e, po=po, o_=o_: e.tensor_copy(o_[:, 0:n], po[:, 0:n]), reads=[rpo], writes=[ro])
                        store(KN[h, :, col0:col0 + n], o_[:, 0:n], ro)
                    for pr in range(4):
                        pa, rpa = nps()
                        pb, rpb = nps()
                        o_, ro = nob()
                        P.emit("pe", qproj(pa, 1024 + pr * 128), reads=[Nq.rqab, rW], writes=[rpa])
                        P.emit("pe", qproj(pb, 1536 + pr * 128), reads=[Nq.rqab, rW], writes=[rpb])
                        rope_combine(pa, rpa, pb, rpb, o_, ro)
                        store(QR[pr, :, col0:col0 + n], o_[:, 0:n], ro)
                    for i in range(nt):
                        r0 = col0 + i * 128
                        ov_, rov_ = ov[i % 2], rov[i % 2]
                        for vb in range(2):
                            po, rpo = nps()

                            def f(e, po=po, vb=vb, i=i):
                                for j in range(2):
                                    ins = e.matmul(po[:], Nk.qab[:, j, i * 128:(i + 1) * 128], WKV[:, j, 1024 + vb * 512:1024 + (vb + 1) * 512], start=(j == 0), stop=(j == 1))
                                return ins
                            P.emit("pe", f, reads=[Nk.rqab, rW], writes=[rpo])
                            P.emit("act", lambda e, po=po, ov_=ov_, vb=vb: e.activation(ov_[:, vb * 512:(vb + 1) * 512], po[:], AF.Copy), reads=[rpo], writes=[rov_])
                        store(VV[r0:r0 + 128, :], ov_[:], rov_)

                gidx = 0
                for b in range(2):
                    for (t0, nt) in ((0, 2), (2, 4), (6, 4), (10, 4), (14, 4)):
                        group(b, t0, nt, gidx)
                        gidx += 1
                P.flush()

            if phases is None or 3 in phases:
              with ExitStack() as st:
                cw = sbt(st, "cw", [128, 36], F32)
                cb = sbt(st, "cb", [128, 12], F32)
                dtbs = sbt(st, "dtbs", [128, 32], F32)
                abc = sbt(st, "abc", [128, 32], F32)
                dsk = sbt(st, "dsk", [128, 16], F32)
                tri = [sbt(st, "tri%d" % d, [128, 128], F32) for d in range(2)]
                msk = [sbt(st, "msk%d" % d, [128, 512], F32) for d in range(2)]
                rK = R()
                P.dma("sp", cw[:], convw[l], writes=[rK])
                P.dma("sp", cb[:], convb[l], writes=[rK])
                P.dma("sp", dtbs[:], dtb[l], writes=[rK])
                P.dma("sp", abc[:], alog[l], writes=[rK])
                P.dma("sp", dsk[:], dskip[l], writes=[rK])
                for d in range(2):
                    P.dma("sp", tri[d][:], c_tri[d], writes=[rK])
                    P.dma("sp", msk[d][:], c_mask[d], writes=[rK])
                P.emit("act", lambda e: e.activation(abc[:], abc[:], AF.Exp), reads=[rK], writes=[rK])
                P.emit("dve", lambda e: e.tensor_scalar(abc[:], abc[:], -1.0, None, ALU.mult), reads=[rK], writes=[rK])
                XC = sbt(st, "XC", [128, 12, TB], BF16)
                rXC = R()
                u = [sbt(st, "u%d" % i, [128, 2048], BF16) for i in range(2)]
                ru = [R(), R()]
                acc = [sbt(st, "acc%d" % i, [128, 2048], F32) for i in range(2)]
                racc = [R(), R()]
                zs = sbt(st, "zsB", [128, 1024], F32)
                rzs = R()

                class BS:
                    pass
                sets = []
                for d in range(2):
                    s_ = BS()
                    for nm, shp, dt_t in (("S", [128, 1024], F32), ("Sb", [128, 1024], BF16), ("XS", [128, 1024], BF16), ("BT", [128, 256], BF16),
                                          ("dtr", [128, 32], F32), ("dt_", [128, 32], F32), ("dtA", [128, 32], F32),
                                          ("xdt", [128, 16, 64], BF16), ("xdd", [128, 16, 64], BF16),
                                          ("nac", [128, 16], F32), ("expA", [128, 16], F32), ("dec", [128, 16], F32), ("cd", [128, 16], F32),
                                          ("Rt", [128, 8, 128], F32), ("LM", [128, 16, 128], BF16), ("CBs", [128, 2, 128], BF16),
                                          ("MT", [128, 16, 128], BF16), ("yo", [128, 16, 64], F32), ("y", [128, 1024], F32)):
                        setattr(s_, nm, sbt(st, "%s_d%d" % (nm, d), shp, dt_t))
                    for nm in ("rS", "rSb", "rXS", "rBT", "rdtr", "rdt", "rxdt", "rxdd", "rsm", "rRt", "rLM", "rCBs", "rMT", "ryo", "ry"):
                        setattr(s_, nm, R())
                    sets.append(s_)
                bT = pst(st, "bT", [128, 8, 128], BF16)
                rbT = R()
                bA = pst(st, "bA", [128, 512], F32)
                rbA = R()
                pSs = pst(st, "pSs", [128, 1024], F32)
                rpSs = R()
                pY = pst(st, "pY", [128, 1024], F32)
                rpY = R()
                pL = pst(st, "pL", [128, 8, 128], F32)
                rpL = R()

                def chunk_pass(b, d, ti, B_):
                    base = b * TB
                    gi = b * 18 + ti
                    r0 = base + ti * 128
                    cs = slice(ti * 128, (ti + 1) * 128)
                    do_y = not (last and ti < 2)
                    dsl = slice(d * 16, (d + 1) * 16)

                    def f(e):
                        for j in range(8):
                            ins = e.transpose(bT[:, j, :], XC[:, j, cs], identb[:])
                        return ins
                    P.emit("pe", f, reads=[rXC, rC], writes=[rbT])
                    P.emit("act", lambda e: e.activation(B_.XS[:], bT[:].rearrange("p a b -> p (a b)"), AF.Copy), reads=[rbT], writes=[B_.rXS])

                    def f(e):
                        for j in range(2):
                            ins = e.transpose(bT[:, j, :], XC[:, 8 + j, cs], identb[:])
                        return ins
                    P.emit("pe", f, reads=[rXC, rC], writes=[rbT])
                    P.emit("act", lambda e: e.activation(B_.BT[:], bT[:, 0:2, :].rearrange("p a b -> p (a b)"), AF.Copy), reads=[rbT], writes=[B_.rBT])
                    P.dma("act", B_.dtr[:], DTR[r0:r0 + 128, :], reads=[rDTR[gi]], writes=[B_.rdtr])
                    P.emit("dve", lambda e: e.tensor_tensor(B_.dt_[:], B_.dtr[:], dtbs[:], ALU.add), reads=[B_.rdtr, rK], writes=[B_.rdt])
                    P.emit("act", lambda e: e.activation(B_.dt_[:], B_.dt_[:], AF.Exp), reads=[B_.rdt], writes=[B_.rdt])
                    P.emit("act", lambda e: e.activation(B_.dt_[:], B_.dt_[:], AF.Ln, bias=1.0), reads=[B_.rdt], writes=[B_.rdt])
                    P.emit("dve", lambda e: e.tensor_tensor(B_.dtA[:], B_.dt_[:], abc[:], ALU.mult), reads=[B_.rdt, rK], writes=[B_.rdt])
                    P.emit("dve", lambda e: e.tensor_tensor(B_.xdt[:], B_.XS[:].rearrange("p (h q) -> p h q", h=16), B_.dt_[:, dsl].unsqueeze(2).broadcast_to([128, 16, 64]), ALU.mult), reads=[B_.rXS, B_.rdt], writes=[B_.rxdt])
                    yield

                    def f(e):
                        e.matmul(bA[:, 0:16], tri[d][:], B_.dtA[:, dsl], start=True, stop=True)
                        return e.matmul(bA[:, 16:32], onesf[:], B_.dtA[:, dsl], start=True, stop=True)
                    P.emit("pe", f, reads=[B_.rdt, rK, rC], writes=[rbA])
                    P.emit("dve", lambda e: e.tensor_scalar(B_.nac[:], bA[:, 0:16], -1.0, None, ALU.mult), reads=[rbA], writes=[B_.rsm])
                    P.emit("act", lambda e: e.activation(B_.expA[:], bA[:, 0:16], AF.Exp), reads=[rbA], writes=[B_.rsm])
                    P.emit("act", lambda e: e.activation(B_.cd[:], bA[:, 16:32], AF.Exp), reads=[rbA], writes=[B_.rsm])
                    P.emit("dve", lambda e: e.tensor_tensor(B_.dec[:], bA[:, 16:32], B_.nac[:], ALU.add), reads=[rbA, B_.rsm], writes=[B_.rsm])
                    P.emit("act", lambda e: e.activation(B_.dec[:], B_.dec[:], AF.Exp), reads=[B_.rsm], writes=[B_.rsm])
                    P.emit("dve", lambda e: e.tensor_tensor(B_.xdd[:], B_.xdt[:], B_.dec[:].unsqueeze(2).broadcast_to([128, 16, 64]), ALU.mult), reads=[B_.rxdt, B_.rsm], writes=[B_.rxdd])
                    yield

                    def f(e):
                        for g in range(2):
                            ins = e.matmul(pSs[:, g * 512:(g + 1) * 512], B_.BT[:, g * 128:(g + 1) * 128], B_.xdd[:, g * 8:(g + 1) * 8, :].rearrange("p h q -> p (h q)"), start=True, stop=True)
                        return ins
                    P.emit("pe", f, reads=[B_.rBT, B_.rxdd], writes=[rpSs])
                    if do_y:
                        P.emit("act", lambda e: e.activation(B_.Sb[:], B_.S[:], AF.Copy), reads=[B_.rS], writes=[B_.rSb])

                        def f(e):
                            for g in range(2):
                                ins = e.matmul(pY[:, g * 512:(g + 1) * 512], XC[:, 10 + g, cs], B_.Sb[:, g * 512:(g + 1) * 512], start=True, stop=True)
                            return ins
                        P.emit("pe", f, reads=[rXC, B_.rSb], writes=[rpY])
                        P.emit("dve", lambda e: e.tensor_tensor(B_.yo[:], pY[:].rearrange("p (h q) -> p h q", h=16), B_.expA[:].unsqueeze(2).broadcast_to([128, 16, 64]), ALU.mult), reads=[rpY, B_.rsm], writes=[B_.ryo])
                    P.emit("dve", lambda e: e.tensor_tensor(B_.S[:].rearrange("p (h q) -> p h q", h=16), B_.S[:].rearrange("p (h q) -> p h q", h=16), B_.cd[:].unsqueeze(2).broadcast_to([128, 16, 64]), ALU.mult), reads=[B_.rS, B_.rsm, B_.rSb], writes=[B_.rS])
                    P.emit("dve", lambda e: e.tensor_tensor(B_.S[:], B_.S[:], pSs[:], ALU.add), reads=[B_.rS, rpSs], writes=[B_.rS])
                    yield
                    if not do_y:
                        return

                    def f(e):
                        for g in range(2):
                            ins = e.matmul(bA[:, 256 + g * 128:256 + (g + 1) * 128], XC[:, 8 + g, cs], XC[:, 10 + g, cs], start=True, stop=True)
                        return ins
                    P.emit("pe", f, reads=[rXC], writes=[rbA])
                    P.emit("act", lambda e: e.activation(B_.CBs[:].rearrange("p a b -> p (a b)"), bA[:, 256:512], AF.Copy), reads=[rbA], writes=[B_.rCBs])
                    yield
                    for hf in range(2):
                        P.emit("pool", lambda e, hf=hf: e.tensor_tensor(B_.Rt[:], tri[d][:].unsqueeze(1).broadcast_to([128, 8, 128]), B_.dtA[:, d * 16 + hf * 8:d * 16 + hf * 8 + 8].unsqueeze(2).broadcast_to([128, 8, 128]), ALU.mult), reads=[rK, B_.rdt], writes=[B_.rRt])

                        def f(e):
                            for kb in range(2):
                                e.matmul(pL[:, kb * 4:(kb + 1) * 4, :].rearrange("p a b -> p (a b)"), onesf[:], B_.Rt[:, kb * 4:(kb + 1) * 4, :].rearrange("p a b -> p (a b)"), start=True, stop=False)
                                ins = e.matmul(pL[:, kb * 4:(kb + 1) * 4, :].rearrange("p a b -> p (a b)"), identf[:], msk[d][:], start=False, stop=True)
                            return ins
                        P.emit("pe", f, reads=[B_.rRt, rK, rC], writes=[rpL])
                        for hh in range(8):
                            h = hf * 8 + hh
                            P.emit("act", lambda e, h=h, hh=hh: e.activation(B_.LM[:, h, :], pL[:, hh, :], AF.Exp, bias=B_.nac[:, h:h + 1]), reads=[rpL, B_.rsm], writes=[B_.rLM])
                        yield
                    P.emit("dve", lambda e: e.tensor_tensor(B_.MT[:].rearrange("p (g h) i -> p g h i", g=2), B_.LM[:].rearrange("p (g h) i -> p g h i", g=2), B_.CBs[:].unsqueeze(2).broadcast_to([128, 2, 8, 128]), ALU.mult), reads=[B_.rLM, B_.rCBs], writes=[B_.rMT])

                    def f(e):
                        for h in range(16):
                            ins = e.matmul(pY[:, h * 64:(h + 1) * 64], B_.MT[:, h, :], B_.xdt[:, h, :], start=True, stop=True)
                        return ins
                    P.emit("pe", f, reads=[B_.rMT, B_.rxdt, B_.ryo], writes=[rpY])
                    P.emit("dve", lambda e: e.tensor_tensor(B_.y[:], B_.yo[:].rearrange("p h q -> p (h q)"), pY[:], ALU.add), reads=[B_.ryo, rpY], writes=[B_.ry])
                    if d == 0:
                        P.emit("pool", lambda e: e.tensor_tensor(zs[:].rearrange("p (h q) -> p h q", h=16), B_.XS[:].rearrange("p (h q) -> p h q", h=16), dsk[:].unsqueeze(2).broadcast_to([128, 16, 64]), ALU.mult), reads=[B_.rXS, rK], writes=[rzs])
                        P.emit("dve", lambda e: e.tensor_tensor(B_.y[:], B_.y[:], zs[:], ALU.add), reads=[B_.ry, rzs], writes=[B_.ry])
                        P.dma("sp", YF[r0:r0 + 128, :], B_.y[:], reads=[B_.ry], writes=[rYF[gi]])
                    else:
                        P.dma("sp", YB[r0:r0 + 128, :], B_.y[:], reads=[B_.ry], writes=[rYB[gi]])

                for b in range(2):
                    base = b * TB
                    cc_ = 0
                    for j in range(12):
                        for (s0, Ls) in ((0, 256), (256, 2048)):
                            u_, ru_, a_, ra_ = u[cc_ % 2], ru[cc_ % 2], acc[cc_ % 2], racc[cc_ % 2]
                            cc_ += 1
                            P.dma("sp", u_[:, 0:Ls], XBC[j * 128:(j + 1) * 128, base + s0:base + s0 + Ls], reads=[rXBC[b]], writes=[ru_])
                            P.emit("dve", lambda e, j=j, Ls=Ls, u_=u_, a_=a_: e.tensor_scalar(a_[:, 0:Ls], u_[:, 0:Ls], cw[:, j * 3 + 1:j * 3 + 2], None, ALU.mult), reads=[ru_, rK], writes=[ra_])
                            P.emit("dve", lambda e, j=j, Ls=Ls, u_=u_, a_=a_: e.scalar_tensor_tensor(a_[:, 1:Ls], u_[:, 0:Ls - 1], cw[:, j * 3:j * 3 + 1], a_[:, 1:Ls], ALU.mult, ALU.add), reads=[ru_, rK, ra_], writes=[ra_])
                            P.emit("dve", lambda e, j=j, Ls=Ls, u_=u_, a_=a_: e.scalar_tensor_tensor(a_[:, 0:Ls - 1], u_[:, 1:Ls], cw[:, j * 3 + 2:j * 3 + 3], a_[:, 0:Ls - 1], ALU.mult, ALU.add), reads=[ru_, rK, ra_], writes=[ra_])
                            P.emit("act", lambda e, j=j, Ls=Ls, s0=s0, a_=a_: e.activation(XC[:, j, s0:s0 + Ls], a_[:, 0:Ls], AF.Silu, bias=cb[:, j:j + 1]), reads=[ra_, rK], writes=[rXC])
                    orders = [list(range(18)), [1, 0] + list(range(17, 1, -1))]
                    for d in range(2):
                        P.emit("dve", lambda e, d=d: e.memset(sets[d].S[:], 0.0), writes=[sets[d].rS])
                    for step in range(18):
                        gens = [chunk_pass(b, d, orders[d][step], sets[d]) for d in range(2)]
                        while gens:
                            for g_ in list(gens):
                                try:
                                    next(g_)
                                except StopIteration:
                                    gens.remove(g_)
                P.flush()
              with ExitStack() as st:
                sg_ = sbt(st, "ssdgs", [128, 1024], F32)
                rK = R()
                P.dma("sp", sg_[:], ssdg[l], writes=[rK])

                class MS:
                    pass
                ms = []
                for i in range(2):
                    m_ = MS()
                    for nm, shp, dt_t in (("yf", [128, 1024], F32), ("yb", [128, 1024], F32), ("zt", [128, 1024], BF16), ("zs", [128, 1024], F32),
                                          ("y16", [128, 1024], BF16), ("yT", [128, 8, 128], BF16), ("junk", [128, 1024], BF16),
                                          ("ss", [128, 1], F32), ("rstd", [128, 1], F32)):
                        setattr(m_, nm, sbt(st, "%s_m%d" % (nm, i), shp, dt_t))
                    m_.bT = pst(st, "bTm%d" % i, [128, 8, 128], BF16)
                    for nm in ("ryf", "ryb", "rzt", "rzs", "ry16", "ryT", "rtmp", "rbT"):
                        setattr(m_, nm, R())
                    ms.append(m_)
                for n_, (b, ti) in enumerate(tiles_l):
                    M_ = ms[n_ % 2]
                    gi = b * 18 + ti
                    r0 = b * TB + ti * 128

                    def mrg(M_=M_, gi=gi, r0=r0):
                        P.dma("act", M_.yf[:], YF[r0:r0 + 128, :], reads=[rYF[gi]], writes=[M_.ryf])
                        P.dma("act", M_.yb[:], YB[r0:r0 + 128, :], reads=[rYB[gi]], writes=[M_.ryb])
                        P.dma("act", M_.zt[:], ZT[r0:r0 + 128, :], reads=[rZT[gi]], writes=[M_.rzt])
                        P.emit("dve", lambda e: e.tensor_tensor(M_.yf[:], M_.yf[:], M_.yb[:], ALU.add), reads=[M_.ryf, M_.ryb], writes=[M_.ryf])
                        P.emit("act", lambda e: e.activation(M_.zs[:], M_.zt[:], AF.Silu), reads=[M_.rzt], writes=[M_.rzs])
                        P.emit("dve", lambda e: e.tensor_tensor(M_.yf[:], M_.yf[:], M_.zs[:], ALU.mult), reads=[M_.ryf, M_.rzs], writes=[M_.ryf])
                        rms_rstd("act", M_.yf[:], 1024, M_.ss[:], M_.rstd[:], M_.junk[:], M_.ryf, M_.rtmp)
                        P.emit("act", lambda e: e.activation(M_.yf[:], M_.yf[:], AF.Copy, scale=M_.rstd[:]), reads=[M_.ryf, M_.rtmp], writes=[M_.ryf])
                        P.emit("dve", lambda e: e.tensor_tensor(M_.y16[:], M_.yf[:], sg_[:], ALU.mult), reads=[M_.ryf, rK], writes=[M_.ry16])

                        def f(e):
                            for j in range(8):
                                ins = e.transpose(M_.bT[:, j, :], M_.y16[:, j * 128:(j + 1) * 128], identb[:])
                            return ins
                        P.emit("pe", f, reads=[M_.ry16, rC], writes=[M_.rbT])
                        P.emit("act", lambda e: e.activation(M_.yT[:], M_.bT[:], AF.Copy), reads=[M_.rbT], writes=[M_.ryT])
                        P.dma("sp", MIX[0:1024, r0:r0 + 128].rearrange("(j p) t -> p j t", p=128), M_.yT[:], reads=[M_.ryT], writes=[rMIX[gi]])
                    mrg()
                P.flush()

            if phases is None or 4 in phases:
              with ExitStack() as st:
                KNs = sbt(st, "KNs", [128, 8, TB], BF16)
                KRs = [sbt(st, "KRs%d" % i, [128, TB], BF16) for i in range(2)]
                Vs = sbt(st, "Vs", [128, 18, 1024], BF16)
                rKV = R()
                QNs = [sbt(st, "QNs%d" % i, [128, 8, 512], BF16) for i in range(2)]
                QRs = [sbt(st, "QRs%d" % i, [128, 4, 512], BF16) for i in range(2)]
                rQ = [R(), R()]
                pS = [pst(st, "pSc%d" % i, [128, 512], F32) for i in range(4)]
                rpS = [R() for _ in range(4)]
                pO = [pst(st, "pOc%d" % i, [128, 512], F32) for i in range(2)]
                rpO = [R(), R()]
                pL = [pst(st, "pLc%d" % i, [128, 512], F32) for i in range(2)]
                rpL = [R(), R()]
                PT = [sbt(st, "PTc%d" % i, [128, 512], BF16) for i in range(6)]
                rPT = [R() for _ in range(6)]
                rl = [sbt(st, "rlc%d" % i, [128, 512], F32) for i in range(2)]
                rrl = [R(), R()]
                ot = [sbt(st, "otc%d" % i, [128, 512], BF16) for i in range(2)]
                rot = [R(), R()]
                cnt = [0, 0]

                def block_head(b, Qn, Qr, rq, h, nq, nkt, c0, gis):
                    u = cnt[1]
                    cnt[1] += 1
                    po, rpo, pl, rpl = pO[u % 2], rpO[u % 2], pL[u % 2], rpL[u % 2]
                    tiles = []

                    def score(kt):
                        i = cnt[0]
                        cnt[0] += 1
                        ps, rps = pS[i % 4], rpS[i % 4]
                        pt, rpt = PT[i % 6], rPT[i % 6]

                        def f(e):
                            e.matmul(ps[:, 0:nq], KNs[:, h, kt * 128:(kt + 1) * 128], Qn[:, h, 0:nq], start=True, stop=False)
                            return e.matmul(ps[:, 0:nq], KRs[h % 2][:, kt * 128:(kt + 1) * 128], Qr[:, h // 2, 0:nq], start=False, stop=True)
                        P.emit("pe", f, reads=[rq, rKV], writes=[rps])
                        P.emit("act", lambda e: e.activation(pt[:, 0:nq], ps[:, 0:nq], AF.Exp, scale=ATTN_SCALE), reads=[rps], writes=[rpt])
                        tiles.append((kt, pt, rpt))

                    def pv(idx):
                        kt, pt, rpt = tiles[idx]

                        def f(e):
                            e.matmul(po[:, 0:nq], Vs[:, kt, h * 128:(h + 1) * 128], pt[:, 0:nq], start=(idx == 0), stop=(idx == nkt - 1))
                            return e.matmul(pl[:, 0:nq], onesb[:], pt[:, 0:nq], start=(idx == 0), stop=(idx == nkt - 1))
                        P.emit("pe", f, reads=[rpt, rKV, rC], writes=[rpo, rpl])
                    DEPTH = 2
                    for kt in range(nkt):
                        score(kt)
                        if kt >= DEPTH:
                            pv(kt - DEPTH)
                    for idx in range(max(0, nkt - DEPTH), nkt):
                        pv(idx)
                    r_, rr_, o_, ro_ = rl[u % 2], rrl[u % 2], ot[u % 2], rot[u % 2]
                    P.emit("dve", lambda e: e.reciprocal(r_[:, 0:nq], pl[:, 0:nq]), reads=[rpl], writes=[rr_])
                    P.emit("dve", lambda e: e.tensor_tensor(o_[:, 0:nq], po[:, 0:nq], r_[:, 0:nq], ALU.mult), reads=[rpo, rr_], writes=[ro_])
                    P.dma("sp", MIX[1024 + h * 128:1024 + (h + 1) * 128, c0:c0 + nq], o_[:, 0:nq], reads=[ro_], writes=[rMIX[g] for g in gis])

                for b in range(2):
                    base = b * TB
                    P.dma("sp", KNs[:], KN[:, :, base:base + TB].rearrange("h p t -> p h t"), reads=[rQK[b]], writes=[rKV])
                    for i_ in range(2):
                        P.emit("dve", lambda e, i_=i_: e.memset(KRs[i_][:], 0.0), writes=[rKV])
                        P.dma("act", KRs[i_][i_ * 64:(i_ + 1) * 64, :], KR[i_ * 64:(i_ + 1) * 64, base:base + TB], reads=[rQK[b]], writes=[rKV])
                    for kt in range(18):
                        P.dma("sp" if kt % 2 else "act", Vs[:, kt, :], VV[base + kt * 128:base + (kt + 1) * 128, :], reads=[rQK[b]], writes=[rKV])
                    blocks = [(2, 4), (6, 4), (10, 4), (14, 4)]
                    if not last:
                        blocks = [(0, 2)] + blocks
                    for bi, (t0, nt) in enumerate(blocks):
                        nq = nt * 128
                        nkt = 2 if t0 == 0 else 18
                        c0 = base + t0 * 128
                        Qn, Qr, rq = QNs[bi % 2], QRs[bi % 2], rQ[bi % 2]
                        P.dma("act", Qn[:, :, 0:nq], QN[:, :, c0:c0 + nq].rearrange("h p t -> p h t"), reads=[rQK[b]], writes=[rq])
                        P.dma("act", Qr[:, :, 0:nq], QR[:, :, c0:c0 + nq].rearrange("h p t -> p h t"), reads=[rQK[b]], writes=[rq])
                        gis = [b * 18 + t0 + i for i in range(nt)]
                        for h in range(8):
                            block_head(b, Qn, Qr, rq, h, nq, nkt, c0, gis)
                P.flush()

            if phases is None or 5 in phases:
              with ExitStack() as st:
                WO = sbt(st, "WO", [128, 16, D], BF16)
                rW = R()
                for j in range(16):
                    P.dma("pool", WO[:, j, :], wo[l, j * 128:(j + 1) * 128, :], writes=[rW], max_dma_last_dim=4096)
                RW = sbt(st, "RW", [128, 16, 16], F32)
                rbs = sbt(st, "rbs", [128, 16], F32)
                P.dma("sp", RW[:], rwT, writes=[rW])
                P.dma("sp", rbs[:], rb, writes=[rW])
                G1 = build_gate(st, "g1", 32, [0, 1] if last else [0, 1, 2])
                pO = pst(st, "pOd", [128, D], F32)
                rpO = R()
                pT = [pst(st, "pTd%d" % i, [128, 4, 128], F32) for i in range(2)]
                rpT = [R(), R()]
                pR = pst(st, "pRd", [128, 16], F32)
                rpR = R()

                class DS:
                    pass
                dsets = []
                for i in range(2):
                    s_ = DS()
                    for nm, shp, dt_t in (("mixT", [128, 16, 128], BF16), ("xt", [128, D], F32), ("tt", [128, D], F32), ("xs", [128, D], F32),
                                          ("junk", [128, D], BF16), ("ss", [128, 1], F32), ("rstd", [128, 1], F32),
                                          ("h2f", [128, 16, 128], F32), ("h2b", [128, 16, 128], BF16),
                                          ("sc", [128, 16], F32), ("sel", [128, 16], F32), ("pr6", [128, 4, 6], F32), ("gs", [128, 4], F32),
                                          ("gmx", [128, 1], F32), ("gmk", [128, 4], F32), ("mk", [128, 16], F32), ("m1", [128, 16], F32),
                                          ("m2", [128, 16], F32), ("t1", [128, 1], F32), ("cmb", [128, 16], F32)):
                        setattr(s_, nm, sbt(st, "%s_D%d" % (nm, i), shp, dt_t))
                    for nm in ("rmixT", "rxt", "rtt", "rxs", "rtmp", "rh2f", "rh2b", "rr"):
                        setattr(s_, nm, R())
                    dsets.append(s_)

                def dtile(S_, b, ti):
                    gi = b * 18 + ti
                    r0 = b * TB + ti * 128
                    c = 2 if ti < 2 else b
                    g1, rg1 = G1[c]
                    P.dma("act", S_.mixT[:], MIX[:, r0:r0 + 128].rearrange("(j p) t -> p j t", p=128), reads=[rMIX[gi]], writes=[S_.rmixT])
                    P.dma("act", S_.xt[:], resid_src(l, b, ti), reads=[rRES[gi]], writes=[S_.rxt])

                    def f(e):
                        for nb in range(4):
                            for j in range(16):
                                ins = e.matmul(pO[:, nb * 512:(nb + 1) * 512], S_.mixT[:, j, :], WO[:, j, nb * 512:(nb + 1) * 512], start=(j == 0), stop=(j == 15))
                        return ins
                    P.emit("pe", f, reads=[S_.rmixT, rW], writes=[rpO])

                def dtile1b(S_, b, ti):
                    gi = b * 18 + ti
                    r0 = b * TB + ti * 128
                    c = 2 if ti < 2 else b
                    g1, rg1 = G1[c]
                    P.emit("dve", lambda e: e.tensor_tensor(S_.tt[:], pO[:], g1[:], ALU.mult), reads=[rpO, rg1], writes=[S_.rtt])
                    P.emit("dve", lambda e: e.tensor_tensor(S_.xt[:], S_.xt[:], S_.tt[:], ALU.add), reads=[S_.rxt, S_.rtt], writes=[S_.rxt])
                    P.dma("sp", RES[r0:r0 + 128, :], S_.xt[:], reads=[S_.rxt], writes=[rRES[gi]])
                    rms_rstd("act", S_.xt[:], D, S_.ss[:], S_.rstd[:], S_.junk[:], S_.rxt, S_.rtmp)
                    P.emit("act", lambda e: e.activation(S_.xs[:], S_.xt[:], AF.Copy, scale=S_.rstd[:]), reads=[S_.rxt, S_.rtmp], writes=[S_.rxs])

                def dtile2(S_, b, ti):
                    gi = b * 18 + ti
                    r0 = b * TB + ti * 128
                    c = 2 if ti < 2 else b
                    for r4 in range(4):
                        p_, rp_ = pT[r4 % 2], rpT[r4 % 2]

                        def f(e, r4=r4, p_=p_):
                            for jj in range(4):
                                j = r4 * 4 + jj
                                ins = e.transpose(p_[:, jj, :], S_.xs[:, j * 128:(j + 1) * 128], identf[:])
                            return ins
                        P.emit("pe", f, reads=[S_.rxs, rC], writes=[rp_])
                        for jj in range(4):
                            j = r4 * 4 + jj
                            if jj % 2 == 0:
                                P.emit("dve", lambda e, p_=p_, jj=jj, j=j: e.tensor_scalar(S_.h2f[:, j, :], p_[:, jj, :], gm2[:, j, c:c + 1], modS[:, 48 + j, c:c + 1], ALU.mult, ALU.add), reads=[rp_, rmod], writes=[S_.rh2f])
                            else:
                                P.emit("act", lambda e, p_=p_, jj=jj, j=j: e.activation(S_.h2f[:, j, :], p_[:, jj, :], AF.Identity, scale=gm2[:, j, c:c + 1], bias=modS[:, 48 + j, c:c + 1]), reads=[rp_, rmod], writes=[S_.rh2f])
                    P.emit("dve", lambda e: e.tensor_copy(S_.h2b[:], S_.h2f[:]), reads=[S_.rh2f], writes=[S_.rh2b])
                    P.dma("sp", HT[:, r0:r0 + 128].rearrange("(j p) t -> p j t", p=128), S_.h2b[:], reads=[S_.rh2b], writes=[rHT[gi]])

                    def f(e):
                        for j in range(16):
                            ins = e.matmul(pR[:], S_.h2f[:, j, :], RW[:, j, :], start=(j == 0), stop=(j == 15))
                        return ins
                    P.emit("pe", f, reads=[S_.rh2f, rW], writes=[rpR])

                def dtile2b(S_, b, ti):
                    gi = b * 18 + ti
                    r0 = b * TB + ti * 128
                    P.emit("act", lambda e: e.activation(S_.sc[:], pR[:], AF.Sigmoid), reads=[rpR], writes=[S_.rr])
                    V = lambda fn, rd=(): P.emit("dve", fn, reads=[S_.rr] + list(rd), writes=[S_.rr])
                    sc, sel, pr6, gs, gmx, gmk, mk, m1, m2, t1, cmb = S_.sc, S_.sel, S_.pr6, S_.gs, S_.gmx, S_.gmk, S_.mk, S_.m1, S_.m2, S_.t1, S_.cmb
                    V(lambda e: e.tensor_tensor(sel[:], sc[:], rbs[:], ALU.add), [rW])
                    s4 = sel[:].rearrange("p (g k) -> p g k", g=4)
                    V(lambda e: e.tensor_tensor(pr6[:, :, 0:3], s4[:, :, 0:3], s4[:, :, 1:4], ALU.add))
                    V(lambda e: e.tensor_tensor(pr6[:, :, 3:5], s4[:, :, 0:2], s4[:, :, 2:4], ALU.add))
                    V(lambda e: e.tensor_tensor(pr6[:, :, 5:6], s4[:, :, 0:1], s4[:, :, 3:4], ALU.add))
                    V(lambda e: e.tensor_reduce(gs[:], pr6[:], AX.X, ALU.max))
                    V(lambda e: e.tensor_reduce(gmx[:], gs[:], AX.X, ALU.max))
                    V(lambda e: e.tensor_scalar(gmk[:], gs[:], gmx[:], None, ALU.is_ge))
                    V(lambda e: e.tensor_tensor(mk[:].rearrange("p (g k) -> p g k", g=4), s4, gmk[:].unsqueeze(2).broadcast_to([128, 4, 4]), ALU.mult))
                    V(lambda e: e.tensor_scalar(gmk[:], gmk[:], -1.0, 10.0, ALU.add, ALU.mult))
                    V(lambda e: e.tensor_tensor(mk[:].rearrange("p (g k) -> p g k", g=4), mk[:].rearrange("p (g k) -> p g k", g=4), gmk[:].unsqueeze(2).broadcast_to([128, 4, 4]), ALU.add))
                    V(lambda e: e.tensor_reduce(t1[:], mk[:], AX.X, ALU.max))
                    V(lambda e: e.tensor_scalar(m1[:], mk[:], t1[:], None, ALU.is_ge))
                    V(lambda e: e.scalar_tensor_tensor(mk[:], m1[:], -20.0, mk[:], ALU.mult, ALU.add))
                    V(lambda e: e.tensor_reduce(t1[:], mk[:], AX.X, ALU.max))
                    V(lambda e: e.tensor_scalar(m2[:], mk[:], t1[:], None, ALU.is_ge))
                    V(lambda e: e.tensor_tensor(m1[:], m1[:], m2[:], ALU.add))
                    V(lambda e: e.tensor_tensor(m1[:], m1[:], sc[:], ALU.mult))
                    V(lambda e: e.tensor_reduce(t1[:], m1[:], AX.X, ALU.add))
                    V(lambda e: e.reciprocal(t1[:], t1[:]))
                    V(lambda e: e.tensor_scalar(cmb[:], m1[:], t1[:], None, ALU.mult))
                    P.dma("sp", COMB[r0:r0 + 128, :], cmb[:], reads=[S_.rr], writes=[rCOMB[gi]])

                prev = None
                for n_, (b, ti) in enumerate(tiles_l):
                    cur = (dsets[n_ % 2], b, ti)
                    dtile(*cur)
                    if prev is not None:
                        dtile2(*prev)
                    dtile1b(*cur)
                    if prev is not None:
                        dtile2b(*prev)
                    prev = cur
                dtile2(*prev)
                dtile2b(*prev)
                P.flush()

            if phases is None or 6 in phases:
              with ExitStack() as st:
                xt_tiles = [(b, ti) for b in range(2) for ti in range(2, 18)]
                sblocks = [xt_tiles[i * 8:(i + 1) * 8] for i in range(4)]
                if not last:
                    sblocks.append([(0, 0), (0, 1), (1, 0), (1, 1)])
                WG = sbt(st, "WG", [128, 16, 512], BF16)
                WU = sbt(st, "WU", [128, 16, 512], BF16)
                WD = sbt(st, "WD", [128, 4, D], BF16)
                rWG, rWD = R(), R()
                h2 = sbt(st, "h2", [128, 16, 1024], BF16)
                rh2 = R()
                accm = sbt(st, "accm", [128, 8, D], F32)
                racc = [R() for _ in range(8)]
                cmb = sbt(st, "cmbm", [128, 8, 16], F32)
                rcmb = R()
                actT = sbt(st, "actT", [128, 4, 1024], BF16)
                ract = R()
                sgl = [sbt(st, "sgl%d" % i, [128, 512], F32) for i in range(2)]
                rsgl = [R(), R()]
                pGU = [pst(st, "pGU%d" % i, [128, 2, 512], F32) for i in range(2)]
                rpGU = [R(), R()]
                pY = [pst(st, "pYm%d" % i, [128, 1024], F32) for i in range(2)]
                rpY = [R(), R()]
                xts = [sbt(st, "xtm%d" % i, [128, D], F32) for i in range(2)]
                rxts = [R(), R()]
                xcnt = 0
                junk = sbt(st, "junkm", [128, D], BF16)
                ss = sbt(st, "ssm", [128, 1], F32)
                rstd = sbt(st, "rstdm", [128, 1], F32)
                rtmp = R()
                fg = sbt(st, "fg", [128, D], F32)
                rfg = R()
                if last:
                    P.dma("sp", fg[:], fng, writes=[rfg])
                g2buf = (sbt(st, "g2b", [128, D], F32), R(), sbt(st, "g2dg", [128, 128], F32), R())
                g2cond = None
                kq = 0
                for sbk in sblocks:
                    nt = len(sbk)
                    nh = nt // 4
                    for i, (b, ti) in enumerate(sbk):
                        gi = b * 18 + ti
                        r0 = b * TB + ti * 128
                        P.dma("sp", h2[:, :, i * 128:(i + 1) * 128], HT[:, r0:r0 + 128].rearrange("(j p) t -> p j t", p=128), reads=[rHT[gi]], writes=[rh2])
                        P.dma("sp", cmb[:, i, :], COMB[r0:r0 + 128, :], reads=[rCOMB[gi]], writes=[rcmb])
                    for ex in range(16):
                        P.dma("pool", WG[:], wg[l, ex].rearrange("(j p) f -> p j f", p=128), writes=[rWG])
                        P.dma("pool", WU[:], wu[l, ex].rearrange("(j p) f -> p j f", p=128), writes=[rWG])
                        for j in range(4):
                            P.dma("pool", WD[:, j, :], wd[l, ex, j * 128:(j + 1) * 128, :], writes=[rWD], max_dma_last_dim=4096)
                        for hb in range(nh):
                            for fc in range(4):
                                pg, rpg = pGU[kq % 2], rpGU[kq % 2]
                                sg, rsg = sgl[kq % 2], rsgl[kq % 2]
                                kq += 1

                                def f(e, pg=pg, fc=fc, hb=hb):
                                    for j in range(16):
                                        e.matmul(pg[:, 0, :], WG[:, j, fc * 128:(fc + 1) * 128], h2[:, j, hb * 512:(hb + 1) * 512], start=(j == 0), stop=(j == 15))
                                    for j in range(16):
                                        ins = e.matmul(pg[:, 1, :], WU[:, j, fc * 128:(fc + 1) * 128], h2[:, j, hb * 512:(hb + 1) * 512], start=(j == 0), stop=(j == 15))
                                    return ins
                                P.emit("pe", f, reads=[rWG, rh2], writes=[rpg])
                                P.emit("act", lambda e, pg=pg, sg=sg: e.activation(sg[:], pg[:, 0, :], AF.Silu), reads=[rpg], writes=[rsg])
                                P.emit("dve", lambda e, pg=pg, sg=sg, fc=fc, hb=hb: e.tensor_tensor(actT[:, fc, hb * 512:(hb + 1) * 512], sg[:], pg[:, 1, :], ALU.mult), reads=[rpg, rsg], writes=[ract])
                        for i in range(nt):
                            for dh in range(2):
                                py, rpy = pY[kq % 2], rpY[kq % 2]
                                kq += 1

                                def f(e, py=py, i=i, dh=dh):
                                    for nb in range(2):
                                        for fc in range(4):
                                            ins = e.matmul(py[:, nb * 512:(nb + 1) * 512], actT[:, fc, i * 128:(i + 1) * 128], WD[:, fc, dh * 1024 + nb * 512:dh * 1024 + (nb + 1) * 512], start=(fc == 0), stop=(fc == 3))
                                    return ins
                                P.emit("pe", f, reads=[ract, rWD], writes=[rpy])
                                a_ = accm[:, i, dh * 1024:(dh + 1) * 1024]
                                if ex == 0:
                                    P.emit("dve", lambda e, py=py, a_=a_, i=i, ex=ex: e.tensor_scalar(a_, py[:], cmb[:, i, ex:ex + 1], None, ALU.mult), reads=[rpy, rcmb], writes=[racc[i]])
                                else:
                                    P.emit("dve", lambda e, py=py, a_=a_, i=i, ex=ex: e.scalar_tensor_tensor(a_, py[:], cmb[:, i, ex:ex + 1], a_, ALU.mult, ALU.add), reads=[rpy, rcmb, racc[i]], writes=[racc[i]])
                    for i, (b, ti) in enumerate(sbk):
                        gi = b * 18 + ti
                        r0 = b * TB + ti * 128
                        c = 2 if ti < 2 else b
                        if g2cond != c:
                            build_gate(st, "g2", 80, [c], pg=pY[0], rpg=rpY[0], gbuf=g2buf)
                            g2cond = c
                        g2, rg2 = g2buf[0], g2buf[1]
                        xt, rxt = xts[xcnt % 2], rxts[xcnt % 2]
                        xcnt += 1
                        P.dma("act", xt[:], RES[r0:r0 + 128, :], reads=[rRES[gi]], writes=[rxt])
                        P.emit("dve", lambda e, i=i, g2=g2: e.tensor_tensor(accm[:, i, :], accm[:, i, :], g2[:], ALU.mult), reads=[racc[i], rg2], writes=[racc[i]])
                        P.emit("dve", lambda e, i=i, xt=xt: e.tensor_tensor(xt[:], xt[:], accm[:, i, :], ALU.add), reads=[racc[i], rxt], writes=[rxt])
                        if not last:
                            P.dma("sp", RES[r0:r0 + 128, :], xt[:], reads=[rxt], writes=[rRES[gi]])
                        else:
                            rms_rstd("act", xt[:], D, ss[:], rstd[:], junk[:], rxt, rtmp)
                            P.emit("act", lambda e, xt=xt: e.activation(xt[:], xt[:], AF.Copy, scale=rstd[:]), reads=[rxt, rtmp], writes=[rxt])
                            P.emit("dve", lambda e, xt=xt: e.tensor_tensor(xt[:], xt[:], fg[:], ALU.mult), reads=[rxt, rfg], writes=[rxt])
                            P.dma("sp", out[b, (ti - 2) * 128:(ti - 1) * 128, :], xt[:], reads=[rxt], writes=[rRES[gi]])
                P.flush()

        P.finish()
        P.flush()
    return nc


def _rope_tables():
    t = np.arange(2048)
    rows = (t // 64).astype(np.float32)
    cols = (t % 64).astype(np.float32)
    nf = 16
    inv = (np.float32(10000.0) ** (-np.arange(nf, dtype=np.float32) / nf)).astype(np.float32)
    ang = np.stack([rows[:, None] * inv, cols[:, None] * inv], axis=1)
    cos = np.cos(ang).astype(np.float32)
    sin = np.sin(ang).astype(np.float32)
    C = np.zeros((64, 2048), np.float32)
    S = np.zeros((64, 2048), np.float32)
    for a in range(2):
        for b in range(2):
            for f in range(16):
                idx = a * 32 + b * 16 + f
                C[idx] = cos[:, a, f]
                S[idx] = sin[:, a, f] * (-1.0 if b == 0 else 1.0)
    return np.concatenate([C, C], 0), np.concatenate([S, S], 0)


def _swap_perm():
    perm = np.zeros(64, np.int64)
    for a in range(2):
        for b in range(2):
            for f in range(16):
                perm[a * 32 + b * 16 + f] = a * 32 + (1 - b) * 16 + f
    return perm


def prep_shared(inp):
    f = lambda a: np.ascontiguousarray(np.asarray(a, dtype=np.float32))
    perm = _swap_perm()
    w_in = f(inp["w_in"])
    kpe = w_in[:, :, 3360:3424]
    kpes = kpe[:, :, perm]
    win = np.concatenate([w_in[:, :, :3360], kpe, kpe, kpes, kpes], axis=2)
    assert win.shape[2] == NWIN
    wqb = f(inp["w_q_b"]).reshape(L, 512, 8, 192)
    nope = wqb[:, :, :, :128].reshape(L, 512, 1024)
    rope = wqb[:, :, :, 128:]
    wq = np.concatenate([nope, rope.reshape(L, 512, 512), rope[:, :, :, perm].reshape(L, 512, 512)], axis=2)
    wkvb = f(inp["w_kv_b"]).reshape(L, 256, 8, 256)
    wkv = np.concatenate([wkvb[:, :, :, :128].reshape(L, 256, 1024), wkvb[:, :, :, 128:].reshape(L, 256, 1024)], axis=2)
    colT = lambda v, n: np.ascontiguousarray(f(v).reshape(L, n, 128).transpose(0, 2, 1))
    bc = lambda v: np.ascontiguousarray(np.broadcast_to(f(v).reshape(L, 1, -1), (L, 128, f(v).reshape(L, -1).shape[1])))
    convw = f(inp["conv_w"])
    convw_l = np.ascontiguousarray(convw.reshape(L, 3, 12, 128).transpose(0, 3, 2, 1).reshape(L, 128, 36))
    cosT, sinT = _rope_tables()
    i_ = np.arange(128)
    tri_f = (i_[:, None] <= i_[None, :]).astype(np.float32)
    tri_b = (i_[:, None] >= i_[None, :]).astype(np.float32)
    mask_f = np.where(i_[None, :] >= i_[:, None], 0.0, NEG).astype(np.float32)
    mask_b = np.where(i_[None, :] <= i_[:, None], 0.0, NEG).astype(np.float32)
    sh = {
        "ada_w": f(inp["ada_w"]),
        "ada_bT": colT(inp["ada_b"], 96),
        "g1T": colT(inp["norm1_g"], 16), "g2T": colT(inp["norm2_g"], 16),
        "win": np.ascontiguousarray(win),
        "convw": convw_l, "convb": colT(inp["conv_b"], 12),
        "dtb": bc(inp["dt_bias"]), "alog": bc(inp["a_log"]), "dskip": bc(inp["d_skip"]),
        "ssdg": bc(inp["ssd_norm_g"]),
        "qgT": colT(inp["q_norm_g"], 4), "wq": np.ascontiguousarray(wq),
        "kvgT": colT(inp["kv_norm_g"], 2), "wkv": np.ascontiguousarray(wkv),
        "wo": f(inp["w_o"]),
        "rwT": np.ascontiguousarray(f(inp["router_w"]).reshape(16, 128, 16).transpose(1, 0, 2)),
        "rb": np.ascontiguousarray(np.broadcast_to(f(inp["router_b"]).reshape(1, 16), (128, 16))),
        "wg": f(inp["w_gate"]), "wu": f(inp["w_up"]), "wd": f(inp["w_down"]),
        "fng": np.ascontiguousarray(np.broadcast_to(f(inp["final_norm_g"]).reshape(1, D), (128, D))),
        "c_ident": np.eye(128, dtype=np.float32),
        "c_tri": np.stack([tri_f, tri_b]),
        "c_mask": np.stack([np.tile(mask_f, (1, 4)), np.tile(mask_b, (1, 4))]),
        "c_cos": cosT, "c_sin": sinT,
    }
    return sh


def core_inputs(inp, sh, core):
    f = lambda a: np.ascontiguousarray(np.asarray(a, dtype=np.float32))
    b0 = core * 2
    cc = np.stack([f(inp["c"])[b0], f(inp["c"])[b0 + 1], f(inp["c_ctx"])], axis=1)
    m = dict(sh)
    m["xin"] = f(inp["x"][b0:b0 + 2])
    m["cin"] = f(inp["ctx"][b0:b0 + 2])
    m["ccT"] = np.ascontiguousarray(cc.reshape(16, 128, 3).transpose(1, 0, 2))
    return m


_NC = None
_SKIP = set()


def kernel(**inputs):
    global _NC
    if _NC is None:
        _NC = build()
    sh = prep_shared(inputs)
    in_maps = [core_inputs(inputs, sh, c) for c in range(8)]
    res = run_bass_kernel_spmd(_NC, in_maps, core_ids=list(range(8)))
    return np.concatenate([np.asarray(r["out"]) for r in res.results], axis=0).astype(np.float32)
```

```python
import numpy as np
from contextlib import ExitStack
import concourse.bass as bass
import concourse.mybir as mybir
from concourse.bass_utils import run_bass_kernel_spmd

F32 = mybir.dt.float32
BF16 = mybir.dt.bfloat16
AF = mybir.ActivationFunctionType
ALU = mybir.AluOpType
AX = mybir.AxisListType

L = 2
D = 2048
TB = 2304
T = 2 * TB
EPS = 1e-6
NWIN = 3616
ATTN_SCALE = 192.0 ** -0.5
NEG = -30000.0


class R:
    __slots__ = ("lw", "rd")

    def __init__(self):
        self.lw = None
        self.rd = {}


class Prog:
    ENGS = ("pe", "act", "dve", "pool", "sp")
    NDS = 48
    NHW = 36

    def __init__(self, nc, es):
        self.nc = nc
        self.q = {e: [] for e in self.ENGS}
        self.cnt = {e: 0 for e in self.ENGS}
        self.waited = {e: {} for e in self.ENGS}
        self.sem = {e: es.enter_context(nc.semaphore("s_" + e)) for e in self.ENGS}
        self.dsem = [es.enter_context(nc.semaphore("d%d" % i)) for i in range(self.NDS)]
        self.dcnt = [0] * self.NDS
        self.dnext = 0
        self.dnext_sw = 0

    def _semof(self, key):
        return self.sem[key[1]] if key[0] == "e" else self.dsem[key[1]]

    def _deps(self, eng, reads, writes, extra=()):
        deps = {}

        def add(d):
            if d is None:
                return
            k, v = d
            if deps.get(k, 0) < v:
                deps[k] = v

        for r in reads:
            add(r.lw)
        for w in writes:
            add(w.lw)
            for kv in w.rd.items():
                add(kv)
        for d in extra:
            add(d)
        waits = []
        wd = self.waited[eng]
        for k, v in deps.items():
            if eng == "pe" and k == ("e", "pe"):
                continue
            if wd.get(k, 0) >= v:
                continue
            wd[k] = v
            waits.append((self._semof(k), v))
        return waits

    def emit(self, eng, fn, reads=(), writes=()):
        waits = self._deps(eng, reads, writes)
        self.cnt[eng] += 1
        key = ("e", eng)
        val = self.cnt[eng]
        sem = self.sem[eng]

        def thunk(e):
            for s, v in waits:
                e.wait_ge(s, v)
            fn(e).then_inc(sem, 1)

        self.q[eng].append(thunk)
        for r in reads:
            r.rd[key] = val
        for w in writes:
            w.lw = (key, val)
            w.rd = {}

    def dma(self, queue, out, in_, reads=(), writes=(), **kw):
        if queue == "pool":
            i = self.NHW + self.dnext_sw
            self.dnext_sw = (self.dnext_sw + 1) % (self.NDS - self.NHW)
        else:
            i = self.dnext
            self.dnext = (i + 1) % self.NHW
        prev = self.dcnt[i]
        self.dcnt[i] += 16
        val = self.dcnt[i]
        key = ("d", i)
        extra = [(key, prev)] if prev > 0 else []
        waits = self._deps(queue, reads, writes, extra)
        sem = self.dsem[i]

        def thunk(e):
            for s, v in waits:
                e.wait_ge(s, v)
            e.dma_start(out=out, in_=in_, **kw).then_inc(sem, 16)

        self.q[queue].append(thunk)
        for r in reads:
            r.rd[key] = val
        for w in writes:
            w.lw = (key, val)
            w.rd = {}

    def finish(self):
        waits = []
        for i in range(self.NDS):
            if self.dcnt[i] > 0:
                waits.append((self.dsem[i], self.dcnt[i]))
        for en in self.ENGS:
            if en != "sp" and self.cnt[en] > 0:
                waits.append((self.sem[en], self.cnt[en]))

        def thunk(e):
            for s, v in waits:
                e.wait_ge(s, v)

        self.q["sp"].append(thunk)

    def barrier(self):
        for en in self.ENGS:
            waits = []
            wd = self.waited[en]
            for i in range(self.NDS):
                k = ("d", i)
                if self.dcnt[i] > wd.get(k, 0):
                    wd[k] = self.dcnt[i]
                    waits.append((self.dsem[i], self.dcnt[i]))
            for e2 in self.ENGS:
                k = ("e", e2)
                if e2 != en and self.cnt[e2] > wd.get(k, 0):
                    wd[k] = self.cnt[e2]
                    waits.append((self.sem[e2], self.cnt[e2]))

            def thunk(e, waits=waits):
                for s_, v in waits:
                    e.wait_ge(s_, v)
            self.q[en].append(thunk)

    def flush(self):
        self.barrier()
        nc = self.nc
        q = self.q
        with nc.Block() as block:
            @block.tensor
            def _(e):
                for t in q["pe"]:
                    t(e)

            @block.scalar
            def _(e):
                for t in q["act"]:
                    t(e)

            @block.vector
            def _(e):
                for t in q["dve"]:
                    t(e)

            @block.gpsimd
            def _(e):
                for t in q["pool"]:
                    t(e)

            @block.sync
            def _(e):
                for t in q["sp"]:
                    t(e)
        self.q = {e: [] for e in self.ENGS}


def build(dbg=False, nlayers=L, phases=None):
    nc = bass.Bass("TRN2", target_bir_lowering=False)

    def din(name, shape, dt=F32):
        return nc.dram_tensor(name, list(shape), dt, kind="ExternalInput").ap()

    def dscr(name, shape, dt):
        return nc.dram_tensor(name, list(shape), dt, kind=("ExternalOutput" if dbg else "Internal")).ap()

    xin = din("xin", [2, 2048, D])
    cin = din("cin", [2, 256, D])
    ccT = din("ccT", [128, 16, 3])
    ada_w = din("ada_w", [L, D, 12288])
    ada_bT = din("ada_bT", [L, 128, 96])
    g1T = din("g1T", [L, 128, 16])
    g2T = din("g2T", [L, 128, 16])
    win = din("win", [L, D, NWIN])
    convw = din("convw", [L, 128, 36])
    convb = din("convb", [L, 128, 12])
    dtb = din("dtb", [L, 128, 32])
    alog = din("alog", [L, 128, 32])
    dskip = din("dskip", [L, 128, 16])
    ssdg = din("ssdg", [L, 128, 1024])
    qgT = din("qgT", [L, 128, 4])
    wq = din("wq", [L, 512, 2048])
    kvgT = din("kvgT", [L, 128, 2])
    wkv = din("wkv", [L, 256, 2048])
    wo = din("wo", [L, D, D])
    rwT = din("rwT", [128, 16, 16])
    rb = din("rb", [128, 16])
    wg = din("wg", [L, 16, D, 512])
    wu = din("wu", [L, 16, D, 512])
    wd = din("wd", [L, 16, 512, D])
    fng = din("fng", [128, D])
    c_ident = din("c_ident", [128, 128])
    c_tri = din("c_tri", [2, 128, 128])
    c_mask = din("c_mask", [2, 128, 512])
    c_cos = din("c_cos", [128, 2048])
    c_sin = din("c_sin", [128, 2048])
    out = nc.dram_tensor("out", [2, 2048, D], F32, kind="ExternalOutput").ap()

    RES = dscr("RES", [T, D], F32)
    ZT = dscr("ZT", [T, 1024], BF16)
    DTR = dscr("DTR", [T, 32], F32)
    XBC = dscr("XBC", [1536, T], BF16)
    QN = dscr("QN", [8, 128, T], BF16)
    QR = dscr("QR", [4, 128, T], BF16)
    KN = dscr("KN", [8, 128, T], BF16)
    KR = dscr("KR", [128, T], BF16)
    VV = dscr("VV", [T, 1024], BF16)
    YF = dscr("YF", [T, 1024], F32)
    YB = dscr("YB", [T, 1024], F32)
    MIX = dscr("MIX", [D, T], BF16)
    HT = dscr("HT", [D, T], BF16)
    COMB = dscr("COMB", [T, 16], F32)
    DBGM = dscr("DBGM", [128, 96, 3], F32)
    rRES = [R() for _ in range(36)]
    rZT = [R() for _ in range(36)]
    rDTR = [R() for _ in range(36)]
    rXBC = [R() for _ in range(2)]
    rQK = [R() for _ in range(2)]
    rYF = [R() for _ in range(36)]
    rYB = [R() for _ in range(36)]
    rMIX = [R() for _ in range(36)]
    rHT = [R() for _ in range(36)]
    rCOMB = [R() for _ in range(36)]

    def resid_src(l, b, ti):
        if l == 0:
            if ti < 2:
                return cin[b, ti * 128:(ti + 1) * 128, :]
            return xin[b, (ti - 2) * 128:(ti - 1) * 128, :]
        r0 = b * TB + ti * 128
        return RES[r0:r0 + 128, :]

    with ExitStack() as es:
        P = Prog(nc, es)

        uid = [0]

        def sbt(st, name, shape, dt):
            uid[0] += 1
            return st.enter_context(nc.sbuf_tensor("%s_%d" % (name, uid[0]), list(shape), dt))

        def pst(st, name, shape, dt):
            uid[0] += 1
            return st.enter_context(nc.psum_tensor("%s_%d" % (name, uid[0]), list(shape), dt))

        identf = sbt(es, "identf", [128, 128], F32)
        identb = sbt(es, "identb", [128, 128], BF16)
        onesf = sbt(es, "onesf", [128, 128], F32)
        onesb = sbt(es, "onesb", [128, 128], BF16)
        modS = sbt(es, "modS", [128, 96, 3], F32)
        gm1 = sbt(es, "gm1", [128, 16, 3], F32)
        gm2 = sbt(es, "gm2", [128, 16, 3], F32)
        rC = R()
        rmod = R()
        P.dma("sp", identf[:], c_ident, writes=[rC])
        P.emit("dve", lambda e: e.tensor_copy(identb[:], identf[:]), reads=[rC], writes=[rC])
        P.emit("dve", lambda e: e.memset(onesf[:], 1.0), writes=[rC])
        P.emit("dve", lambda e: e.memset(onesb[:], 1.0), writes=[rC])

        def rms_rstd(eng_src, src_ap, n, ss, rstd, junk, rsrc, rtmp):
            P.emit("act", lambda e: e.activation(junk, src_ap, AF.Square, accum_out=ss), reads=[rsrc], writes=[rtmp])
            P.emit("act", lambda e: e.activation(rstd, ss, AF.Sqrt, bias=EPS, scale=1.0 / n), reads=[rtmp], writes=[rtmp])
            P.emit("dve", lambda e: e.reciprocal(rstd, rstd), reads=[rtmp], writes=[rtmp])

        for l in range(nlayers):
            last = (l == L - 1)
            tiles_l = [(b, ti) for b in range(2) for ti in range(18) if not (last and ti < 2)]

            stA1w = ExitStack()
            W_A1 = sbt(stA1w, "W", [128, 16, 2592], BF16)
            rW_A1 = R()
            for j in range(16):
                P.dma("pool", W_A1[:, j, :], win[l, j * 128:(j + 1) * 128, 0:2592], writes=[rW_A1], max_dma_last_dim=4096)
            if phases is None or 0 in phases:
              with ExitStack() as st:
                scf = sbt(st, "scf", [128, 16, 3], F32)
                scb = sbt(st, "scb", [128, 16, 3], BF16)
                abT = sbt(st, "abT", [128, 96], F32)
                g1s = sbt(st, "g1s", [128, 16], F32)
                g2s = sbt(st, "g2s", [128, 16], F32)
                awf = [sbt(st, "awf%d" % i, [128, 16, 512], F32) for i in range(2)]
                rawf = [R(), R()]
                aw = [sbt(st, "aw%d" % i, [128, 16, 512], BF16) for i in range(2)]
                raw = [R(), R()]
                pm = pst(st, "pm", [128, 96, 3], F32)
                rpm = R()
                rs = R()
                P.dma("sp", scf[:], ccT, writes=[rs])
                P.dma("sp", abT[:], ada_bT[l], writes=[rs])
                P.dma("sp", g1s[:], g1T[l], writes=[rs])
                P.dma("sp", g2s[:], g2T[l], writes=[rs])
                P.emit("act", lambda e: e.activation(scb[:], scf[:], AF.Silu), reads=[rs], writes=[rs])
                for pc in range(24):
                    af, raf = awf[pc % 2], rawf[pc % 2]
                    a, ra = aw[pc % 2], raw[pc % 2]
                    for hj in range(2):
                        P.dma("sp" if hj == 0 else "act", af[:, hj * 8:(hj + 1) * 8, :], ada_w[l, hj * 1024:(hj + 1) * 1024, pc * 512:(pc + 1) * 512].rearrange("(j p) m -> p j m", p=128), writes=[raf])
                    P.emit("dve", lambda e, a=a, af=af: e.tensor_copy(a[:, 0:6, :], af[:, 0:6, :]), reads=[raf], writes=[ra])
                    P.emit("pool", lambda e, a=a, af=af: e.tensor_copy(a[:, 6:10, :], af[:, 6:10, :]), reads=[raf], writes=[ra])
                    P.emit("act", lambda e, a=a, af=af: e.activation(a[:, 10:16, :], af[:, 10:16, :], AF.Copy), reads=[raf], writes=[ra])
                    for mc in range(4):
                        m = pc * 4 + mc

                        def f(e, a=a, m=m, mc=mc):
                            for j in range(16):
                                ins = e.matmul(pm[:, m, :], a[:, j, mc * 128:(mc + 1) * 128], scb[:, j, :], start=(j == 0), stop=(j == 15))
                            return ins
                        P.emit("pe", f, reads=[ra, rs], writes=[rpm])
                P.emit("dve", lambda e: e.tensor_tensor(modS[:], pm[:], abT[:].unsqueeze(2).broadcast_to([128, 96, 3]), ALU.add), reads=[rpm, rs, rmod], writes=[rmod])
                P.emit("dve", lambda e: e.tensor_scalar(gm1[:], modS[:, 16:32, :], 1.0, None, ALU.add), reads=[rmod], writes=[rmod])
                P.emit("dve", lambda e: e.tensor_tensor(gm1[:], gm1[:], g1s[:].unsqueeze(2).broadcast_to([128, 16, 3]), ALU.mult), reads=[rmod, rs], writes=[rmod])
                P.emit("dve", lambda e: e.tensor_scalar(gm2[:], modS[:, 64:80, :], 1.0, None, ALU.add), reads=[rmod], writes=[rmod])
                P.emit("dve", lambda e: e.tensor_tensor(gm2[:], gm2[:], g2s[:].unsqueeze(2).broadcast_to([128, 16, 3]), ALU.mult), reads=[rmod, rs], writes=[rmod])
                if dbg:
                    P.dma("sp", DBGM, modS[:], reads=[rmod])
                P.flush()

            def build_gate(st, name, j0, conds, pg=None, rpg=None, gbuf=None):
                G = {}
                dg = gbuf[2] if gbuf else sbt(st, name + "dg", [128, 128], F32)
                if pg is None:
                    pg = pst(st, name + "pg", [128, 512], F32)
                    rpg = R()
                rdg = gbuf[3] if gbuf else R()
                for c in conds:
                    if gbuf:
                        g, rg = gbuf[0], gbuf[1]
                    else:
                        g = sbt(st, name + "G%d" % c, [128, D], F32)
                        rg = R()
                    for j in range(16):
                        P.emit("dve", lambda e, j=j, c=c: e.tensor_scalar(dg[:], identf[:], modS[:, j0 + j, c:c + 1], None, ALU.mult), reads=[rmod, rC], writes=[rdg])
                        P.emit("pe", lambda e, j=j: e.matmul(pg[:, (j % 4) * 128:(j % 4 + 1) * 128], onesf[:], dg[:], start=True, stop=True), reads=[rdg, rC], writes=[rpg])
                        if j % 4 == 3:
                            P.emit("act", lambda e, j=j, g=g: e.activation(g[:, (j - 3) * 128:(j + 1) * 128], pg[:, 0:512], AF.Copy), reads=[rpg], writes=[rg])
                    G[c] = (g, rg)
                return G

            if phases is None or 1 in phases:
              with ExitStack() as st:
                NA = 2592
                W = W_A1
                rW = rW_A1
                xt = [sbt(st, "xt%d" % i, [128, D], F32) for i in range(2)]
                rxt = [R(), R()]
                junk = sbt(st, "junk", [128, D], BF16)
                ss = sbt(st, "ss", [128, 1], F32)
                rstd = sbt(st, "rstd", [128, 1], F32)
                rtmp = R()
                xns = [sbt(st, "xn%d" % i, [128, D], BF16) for i in range(2)]
                rxns = [R(), R()]
                hTs = [sbt(st, "hT%d" % i, [128, 16, 512], BF16) for i in range(2)]
                rhTs = [R(), R()]
                gcnt = 0
                pT = [pst(st, "pT%d" % i, [128, 8, 128], BF16) for i in range(2)]
                rpT = [R(), R()]
                pO = [pst(st, "pO%d" % i, [128, 512], F32) for i in range(4)]
                rpO = [R() for _ in range(4)]
                ob = [sbt(st, "ob%d" % i, [128, 512], BF16) for i in range(4)]
                rob = [R() for _ in range(4)]
                dto = sbt(st, "dto", [128, 32], F32)
                rdto = R()
                cnt = 0
                a1_tiles = [(b, t0 + i) for b in range(2) for (t0, nt) in ((0, 2), (2, 4), (6, 4), (10, 4), (14, 4)) for i in range(nt)]

                def a1_norm(k_):
                    b_, ti_ = a1_tiles[k_]
                    x_, rx = xt[k_ % 2], rxt[k_ % 2]
                    xn_, rxn_ = xns[k_ % 2], rxns[k_ % 2]
                    P.dma("sp", x_[:], resid_src(l, b_, ti_), reads=[rRES[b_ * 18 + ti_]], writes=[rx])
                    rms_rstd("act", x_[:], D, ss[:], rstd[:], junk[:], rx, rtmp)
                    P.emit("act", lambda e: e.activation(xn_[:], x_[:], AF.Copy, scale=rstd[:]), reads=[rx, rtmp], writes=[rxn_])
                for b in range(2):
                    for (t0, nt) in ((0, 2), (2, 4), (6, 4), (10, 4), (14, 4)):
                        c = 2 if t0 == 0 else b
                        n = nt * 128
                        col0 = b * TB + t0 * 128
                        hT, rhT = hTs[gcnt % 2], rhTs[gcnt % 2]
                        gcnt += 1
                        for i in range(nt):
                            ti = t0 + i
                            gi = b * 18 + ti
                            xn, rxn = xns[cnt % 2], rxns[cnt % 2]
                            if cnt == 0:
                                a1_norm(0)
                            if cnt + 1 < len(a1_tiles):
                                a1_norm(cnt + 1)
                            cnt += 1
                            for hh in range(2):
                                def f(e, hh=hh, xn=xn):
                                    for jj in range(8):
                                        j = hh * 8 + jj
                                        ins = e.transpose(pT[hh][:, jj, :], xn[:, j * 128:(j + 1) * 128], identb[:])
                                    return ins
                                P.emit("pe", f, reads=[rxn, rC], writes=[rpT[hh]])
                                for jj in range(8):
                                    j = hh * 8 + jj
                                    if jj % 2 == 0:
                                        P.emit("dve", lambda e, hh=hh, jj=jj, j=j, i=i, c=c, hT=hT: e.tensor_scalar(hT[:, j, i * 128:(i + 1) * 128], pT[hh][:, jj, :], gm1[:, j, c:c + 1], modS[:, j, c:c + 1], ALU.mult, ALU.add), reads=[rpT[hh], rmod], writes=[rhT])
                                    else:
                                        P.emit("act", lambda e, hh=hh, jj=jj, j=j, i=i, c=c, hT=hT: e.activation(hT[:, j, i * 128:(i + 1) * 128], pT[hh][:, jj, :], AF.Identity, scale=gm1[:, j, c:c + 1], bias=modS[:, j, c:c + 1]), reads=[rpT[hh], rmod], writes=[rhT])
                        P.dma("sp", HT[:, col0:col0 + n].rearrange("(j p) t -> p j t", p=128), hT[:, :, 0:n], reads=[rhT], writes=[rHT[b * 18 + t0 + i] for i in range(nt)])
                        k = 0
                        for i in range(nt):
                            ti = t0 + i
                            gi = b * 18 + ti
                            r0 = b * TB + ti * 128
                            for zb in range(2):
                                po, rpo, o_, ro = pO[k % 4], rpO[k % 4], ob[k % 4], rob[k % 4]
                                k += 1

                                def f(e, po=po, zb=zb, i=i, hT=hT):
                                    for j in range(16):
                                        ins = e.matmul(po[:], hT[:, j, i * 128:(i + 1) * 128], W[:, j, zb * 512:(zb + 1) * 512], start=(j == 0), stop=(j == 15))
                                    return ins
                                P.emit("pe", f, reads=[rhT, rW], writes=[rpo])
                                P.emit("act" if k % 2 else "dve", (lambda e, po=po, o_=o_: e.activation(o_[:], po[:], AF.Copy)) if k % 2 else (lambda e, po=po, o_=o_: e.tensor_copy(o_[:], po[:])), reads=[rpo], writes=[ro])
                                P.dma("sp", ZT[r0:r0 + 128, zb * 512:(zb + 1) * 512], o_[:], reads=[ro], writes=[rZT[gi]])
                            po, rpo = pO[k % 4], rpO[k % 4]
                            k += 1

                            def f(e, po=po, i=i, hT=hT):
                                for j in range(16):
                                    ins = e.matmul(po[:, 0:32], hT[:, j, i * 128:(i + 1) * 128], W[:, j, 2560:2592], start=(j == 0), stop=(j == 15))
                                return ins
                            P.emit("pe", f, reads=[rhT, rW], writes=[rpo])
                            P.emit("dve", lambda e, po=po: e.tensor_copy(dto[:], po[:, 0:32]), reads=[rpo], writes=[rdto])
                            P.dma("sp", DTR[r0:r0 + 128, :], dto[:], reads=[rdto], writes=[rDTR[gi]])
                        for m in range(12):
                            po, rpo, o_, ro = pO[k % 4], rpO[k % 4], ob[k % 4], rob[k % 4]
                            k += 1

                            def f(e, po=po, m=m, n=n, hT=hT):
                                for j in range(16):
                                    ins = e.matmul(po[:, 0:n], W[:, j, 1024 + m * 128:1024 + (m + 1) * 128], hT[:, j, 0:n], start=(j == 0), stop=(j == 15))
                                return ins
                            P.emit("pe", f, reads=[rhT, rW], writes=[rpo])
                            P.emit("act" if k % 2 else "dve", (lambda e, po=po, o_=o_, n=n: e.activation(o_[:, 0:n], po[:, 0:n], AF.Copy)) if k % 2 else (lambda e, po=po, o_=o_, n=n: e.tensor_copy(o_[:, 0:n], po[:, 0:n])), reads=[rpo], writes=[ro])
                            P.dma("sp", XBC[m * 128:(m + 1) * 128, col0:col0 + n], o_[:, 0:n], reads=[ro], writes=[rXBC[b]])
                P.flush()

            stA1w.close()
            if phases is None or 2 in phases:
              with ExitStack() as st:
                NB2 = NWIN - 2592
                W = sbt(st, "W2", [128, 16, NB2], BF16)
                rW = R()
                for j in range(16):
                    P.dma("pool", W[:, j, :], win[l, j * 128:(j + 1) * 128, 2592:NWIN], writes=[rW])
                WQ = sbt(st, "WQ", [128, 4, 2048], BF16)
                WKV = sbt(st, "WKV", [128, 2, 2048], BF16)
                for j in range(4):
                    P.dma("pool", WQ[:, j, :], wq[l, j * 128:(j + 1) * 128, :], writes=[rW], max_dma_last_dim=4096)
                for j in range(2):
                    P.dma("pool", WKV[:, j, :], wkv[l, j * 128:(j + 1) * 128, :], writes=[rW], max_dma_last_dim=4096)
                cosT = sbt(st, "cosT", [128, 2048], F32)
                sinT = sbt(st, "sinT", [128, 2048], F32)
                qg = sbt(st, "qg", [128, 4], F32)
                kvg = sbt(st, "kvg", [128, 2], F32)
                P.dma("sp", cosT[:], c_cos, writes=[rW])
                P.dma("sp", sinT[:], c_sin, writes=[rW])
                P.dma("sp", qg[:], qgT[l], writes=[rW])
                P.dma("sp", kvg[:], kvgT[l], writes=[rW])
                hT = [sbt(st, "hT2%d" % i, [128, 16, 512], BF16) for i in range(2)]
                rhT = [R(), R()]
                pO = [pst(st, "pO%d" % i, [128, 512], F32) for i in range(6)]
                rpO = [R() for _ in range(6)]
                pSq = [pst(st, "pSq%d" % i, [128, 512], F32) for i in range(2)]
                rpSq = [R(), R()]

                class NS:
                    pass
                nsets = {}
                for nm, nch in (("q", 4), ("kv", 2)):
                    for i in range(2):
                        s_ = NS()
                        s_.sq = sbt(st, "sq%s%d" % (nm, i), [128, nch, 512], BF16)
                        s_.qa = sbt(st, "qa%s%d" % (nm, i), [128, nch, 512], F32)
                        s_.qab = sbt(st, "qab%s%d" % (nm, i), [128, nch, 512], BF16)
                        s_.rbc = sbt(st, "rbc%s%d" % (nm, i), [128, 512], F32)
                        s_.rsq, s_.rqa, s_.rqab, s_.rrbc = R(), R(), R(), R()
                        nsets[(nm, i)] = s_
                ob = [sbt(st, "ob%d" % i, [128, 512], BF16) for i in range(6)]
                rob = [R() for _ in range(6)]
                ta = [sbt(st, "ta%d" % i, [128, 512], F32) for i in range(2)]
                tb_ = [sbt(st, "tb%d" % i, [128, 512], F32) for i in range(2)]
                rta, rtb = [R(), R()], [R(), R()]
                ov = [sbt(st, "ov%d" % i, [128, 1024], BF16) for i in range(2)]
                rov = [R(), R()]
                kst = [0, 0, 0, 0]

                def group(b, t0, nt, gidx):
                    isx = t0 > 0
                    n = nt * 128
                    col0 = b * TB + t0 * 128
                    p0 = (t0 - 2) * 128
                    h_ = hT[gidx % 2]
                    rh = rhT[gidx % 2]
                    Nq = nsets[("q", gidx % 2)]
                    Nk = nsets[("kv", gidx % 2)]
                    P.dma("sp", h_[:, :, 0:n], HT[:, col0:col0 + n].rearrange("(j p) t -> p j t", p=128), reads=[rHT[b * 18 + t0 + i] for i in range(nt)], writes=[rh])

                    def nps():
                        kst[0] += 1
                        return pO[kst[0] % 6], rpO[kst[0] % 6]

                    def nob():
                        kst[1] += 1
                        return ob[kst[1] % 6], rob[kst[1] % 6]

                    def store(dst, src, rsrc):
                        kst[3] += 1
                        P.dma("sp" if kst[3] % 2 else "act", dst, src, reads=[rsrc], writes=[rQK[b]])

                    def proj(po, c0):
                        def f(e):
                            for j in range(16):
                                ins = e.matmul(po[:, 0:n], W[:, j, c0:c0 + 128], h_[:, j, 0:n], start=(j == 0), stop=(j == 15))
                            return ins
                        return f

                    def stage1(N_, nchunk, c0, gcol):
                        for m in range(nchunk):
                            po, rpo = nps()
                            P.emit("pe", proj(po, c0 + m * 128), reads=[rh, rW], writes=[rpo])
                            P.emit("act", lambda e, po=po, m=m: e.activation(N_.qa[:, m, 0:n], po[:, 0:n], AF.Copy), reads=[rpo], writes=[N_.rqa])
                            P.emit("dve", lambda e, m=m: e.tensor_tensor(N_.sq[:, m, 0:n], N_.qa[:, m, 0:n], N_.qa[:, m, 0:n], ALU.mult), reads=[N_.rqa], writes=[N_.rsq])
                            P.emit("pool", lambda e, m=m: e.tensor_scalar(N_.qa[:, m, 0:n], N_.qa[:, m, 0:n], gcol[:, m:m + 1], 1.0, ALU.mult, ALU.mult), reads=[N_.rqa, rW, N_.rsq], writes=[N_.rqa])

                    def stage2(N_, nchunk, pS, rpS):
                        def f(e):
                            for m in range(nchunk):
                                ins = e.matmul(pS[:, 0:n], onesb[:], N_.sq[:, m, 0:n], start=(m == 0), stop=(m == nchunk - 1))
                            return ins
                        P.emit("pe", f, reads=[N_.rsq, rC], writes=[rpS])
                        P.emit("act", lambda e: e.activation(N_.rbc[:, 0:n], pS[:, 0:n], AF.Sqrt, bias=EPS, scale=1.0 / (nchunk * 128)), reads=[rpS], writes=[N_.rrbc])
                        P.emit("dve", lambda e: e.reciprocal(N_.rbc[:, 0:n], N_.rbc[:, 0:n]), reads=[N_.rrbc], writes=[N_.rrbc])
                        for m in range(nchunk):
                            P.emit("dve", lambda e, m=m: e.tensor_tensor(N_.qab[:, m, 0:n], N_.qa[:, m, 0:n], N_.rbc[:, 0:n], ALU.mult), reads=[N_.rqa, N_.rrbc], writes=[N_.rqab])

                    def rope_combine(pa, rpa, pb, rpb, o_, ro):
                        if not isx:
                            P.emit("act", lambda e: e.activation(o_[:, 0:n], pa[:, 0:n], AF.Copy), reads=[rpa], writes=[ro])
                            return
                        kst[2] += 1
                        ta_, tb2, rta_, rtb_ = ta[kst[2] % 2], tb_[kst[2] % 2], rta[kst[2] % 2], rtb[kst[2] % 2]
                        P.emit("dve", lambda e: e.tensor_tensor(ta_[:, 0:n], pa[:, 0:n], cosT[:, p0:p0 + n], ALU.mult), reads=[rpa, rW], writes=[rta_])
                        P.emit("dve", lambda e: e.tensor_tensor(tb2[:, 0:n], pb[:, 0:n], sinT[:, p0:p0 + n], ALU.mult), reads=[rpb, rW], writes=[rtb_])
                        P.emit("pool", lambda e: e.tensor_tensor(o_[:, 0:n], ta_[:, 0:n], tb2[:, 0:n], ALU.add), reads=[rta_, rtb_], writes=[ro])

                    stage1(Nq, 4, 0, qg)
                    stage1(Nk, 2, 512, kvg)
                    pa, rpa = nps()
                    pb, rpb = nps()
                    o_, ro = nob()
                    P.emit("pe", proj(pa, 768), reads=[rh, rW], writes=[rpa])
                    P.emit("pe", proj(pb, 896), reads=[rh, rW], writes=[rpb])
                    rope_combine(pa, rpa, pb, rpb, o_, ro)
                    store(KR[:, col0:col0 + n], o_[:, 0:n], ro)
                    stage2(Nq, 4, pSq[0], rpSq[0])
                    stage2(Nk, 2, pSq[1], rpSq[1])

                    def qproj(po, mcol):
                        def f(e):
                            for j in range(4):
                                ins = e.matmul(po[:, 0:n], WQ[:, j, mcol:mcol + 128], Nq.qab[:, j, 0:n], start=(j == 0), stop=(j == 3))
                            return ins
                        return f
                    for h in range(8):
                        po, rpo = nps()
                        o_, ro = nob()
                        P.emit("pe", qproj(po, h * 128), reads=[Nq.rqab, rW], writes=[rpo])
                        P.emit("act", lambda e, po=po, o_=o_: e.activation(o_[:, 0:n], po[:, 0:n], AF.Copy), reads=[rpo], writes=[ro])
                        store(QN[h, :, col0:col0 + n], o_[:, 0:n], ro)
                    for h in range(8):
                        po, rpo = nps()
                        o_, ro = nob()

                        def f(e, po=po, h=h):
                            for j in range(2):
                                ins = e.matmul(po[:, 0:n], WKV[:, j, h * 128:(h + 1) * 128], Nk.qab[:, j, 0:n], start=(j == 0), stop=(j == 1))
                            return ins
                        P.emit("pe", f, reads=[Nk.rqab, rW], writes=[rpo])
                        P.emit("dve", lambda e, po=po, o_=o_: e.tensor_copy(o_[:, 0:n], po[:, 0:n]), reads=[rpo], writes=[ro])
                        store(KN[h, :, col0:col0 + n], o_[:, 0:n], ro)
                    for pr in range(4):
                        pa, rpa = nps()
                        pb, rpb = nps()
                        o_, ro = nob()
                        P.emit("pe", qproj(pa, 1024 + pr * 128), reads=[Nq.rqab, rW], writes=[rpa])
                        P.emit("pe", qproj(pb, 1536 + pr * 128), reads=[Nq.rqab, rW], writes=[rpb])
                        rope_combine(pa, rpa, pb, rpb, o_, ro)
                        store(QR[pr, :, col0:col0 + n], o_[:, 0:n], ro)
                    for i in range(nt):
                        r0 = col0 + i * 128
                        ov_, rov_ = ov[i % 2], rov[i % 2]
                        for vb in range(2):
                            po, rpo = nps()

                            def f(e, po=po, vb=vb, i=i):
                                for j in range(2):
                                    ins = e.matmul(po[:], Nk.qab[:, j, i * 128:(i + 1) * 128], WKV[:, j, 1024 + vb * 512:1024 + (vb + 1) * 512], start=(j == 0), stop=(j == 1))
                                return ins
                            P.emit("pe", f, reads=[Nk.rqab, rW], writes=[rpo])
                            P.emit("act", lambda e, po=po, ov_=ov_, vb=vb: e.activation(ov_[:, vb * 512:(vb + 1) * 512], po[:], AF.Copy), reads=[rpo], writes=[rov_])
                        store(VV[r0:r0 + 128, :], ov_[:], rov_)

                gidx = 0
                for b in range(2):
                    for (t0, nt) in ((0, 2), (2, 4), (6, 4), (10, 4), (14, 4)):
                        group(b, t0, nt, gidx)
                        gidx += 1
                P.flush()

            if phases is None or 3 in phases:
              with ExitStack() as st:
                cw = sbt(st, "cw", [128, 36], F32)
                cb = sbt(st, "cb", [128, 12], F32)
                dtbs = sbt(st, "dtbs", [128, 32], F32)
                abc = sbt(st, "abc", [128, 32], F32)
                dsk = sbt(st, "dsk", [128, 16], F32)
                tri = [sbt(st, "tri%d" % d, [128, 128], F32) for d in range(2)]
                msk = [sbt(st, "msk%d" % d, [128, 512], F32) for d in range(2)]
                rK = R()
                P.dma("sp", cw[:], convw[l], writes=[rK])
                P.dma("sp", cb[:], convb[l], writes=[rK])
                P.dma("sp", dtbs[:], dtb[l], writes=[rK])
                P.dma("sp", abc[:], alog[l], writes=[rK])
                P.dma("sp", dsk[:], dskip[l], writes=[rK])
                for d in range(2):
                    P.dma("sp", tri[d][:], c_tri[d], writes=[rK])
                    P.dma("sp", msk[d][:], c_mask[d], writes=[rK])
                P.emit("act", lambda e: e.activation(abc[:], abc[:], AF.Exp), reads=[rK], writes=[rK])
                P.emit("dve", lambda e: e.tensor_scalar(abc[:], abc[:], -1.0, None, ALU.mult), reads=[rK], writes=[rK])
                XC = sbt(st, "XC", [128, 12, TB], BF16)
                rXC = R()
                u = [sbt(st, "u%d" % i, [128, 2048], BF16) for i in range(2)]
                ru = [R(), R()]
                acc = [sbt(st, "acc%d" % i, [128, 2048], F32) for i in range(2)]
                racc = [R(), R()]
                zs = sbt(st, "zsB", [128, 1024], F32)
                rzs = R()

                class BS:
                    pass
                sets = []
                for d in range(2):
                    s_ = BS()
                    for nm, shp, dt_t in (("S", [128, 1024], F32), ("Sb", [128, 1024], BF16), ("XS", [128, 1024], BF16), ("BT", [128, 256], BF16),
                                          ("dtr", [128, 32], F32), ("dt_", [128, 32], F32), ("dtA", [128, 32], F32),
                                          ("xdt", [128, 16, 64], BF16), ("xdd", [128, 16, 64], BF16),
                                          ("nac", [128, 16], F32), ("expA", [128, 16], F32), ("dec", [128, 16], F32), ("cd", [128, 16], F32),
                                          ("Rt", [128, 8, 128], F32), ("LM", [128, 16, 128], BF16), ("CBs", [128, 2, 128], BF16),
                                          ("MT", [128, 16, 128], BF16), ("yo", [128, 16, 64], F32), ("y", [128, 1024], F32)):
                        setattr(s_, nm, sbt(st, "%s_d%d" % (nm, d), shp, dt_t))
                    for nm in ("rS", "rSb", "rXS", "rBT", "rdtr", "rdt", "rxdt", "rxdd", "rsm", "rRt", "rLM", "rCBs", "rMT", "ryo", "ry"):
                        setattr(s_, nm, R())
                    sets.append(s_)
                bT = pst(st, "bT", [128, 8, 128], BF16)
                rbT = R()
                bA = pst(st, "bA", [128, 512], F32)
                rbA = R()
                pSs = pst(st, "pSs", [128, 1024], F32)
                rpSs = R()
                pY = pst(st, "pY", [128, 1024], F32)
                rpY = R()
                pL = pst(st, "pL", [128, 8, 128], F32)
                rpL = R()

                def chunk_pass(b, d, ti, B_):
                    base = b * TB
                    gi = b * 18 + ti
                    r0 = base + ti * 128
                    cs = slice(ti * 128, (ti + 1) * 128)
                    do_y = not (last and ti < 2)
                    dsl = slice(d * 16, (d + 1) * 16)

                    def f(e):
                        for j in range(8):
                            ins = e.transpose(bT[:, j, :], XC[:, j, cs], identb[:])
                        return ins
                    P.emit("pe", f, reads=[rXC, rC], writes=[rbT])
                    P.emit("act", lambda e: e.activation(B_.XS[:], bT[:].rearrange("p a b -> p (a b)"), AF.Copy), reads=[rbT], writes=[B_.rXS])

                    def f(e):
                        for j in range(2):
                            ins = e.transpose(bT[:, j, :], XC[:, 8 + j, cs], identb[:])
                        return ins
                    P.emit("pe", f, reads=[rXC, rC], writes=[rbT])
                    P.emit("act", lambda e: e.activation(B_.BT[:], bT[:, 0:2, :].rearrange("p a b -> p (a b)"), AF.Copy), reads=[rbT], writes=[B_.rBT])
                    P.dma("act", B_.dtr[:], DTR[r0:r0 + 128, :], reads=[rDTR[gi]], writes=[B_.rdtr])
                    P.emit("dve", lambda e: e.tensor_tensor(B_.dt_[:], B_.dtr[:], dtbs[:], ALU.add), reads=[B_.rdtr, rK], writes=[B_.rdt])
                    P.emit("act", lambda e: e.activation(B_.dt_[:], B_.dt_[:], AF.Exp), reads=[B_.rdt], writes=[B_.rdt])
                    P.emit("act", lambda e: e.activation(B_.dt_[:], B_.dt_[:], AF.Ln, bias=1.0), reads=[B_.rdt], writes=[B_.rdt])
                    P.emit("dve", lambda e: e.tensor_tensor(B_.dtA[:], B_.dt_[:], abc[:], ALU.mult), reads=[B_.rdt, rK], writes=[B_.rdt])
                    P.emit("dve", lambda e: e.tensor_tensor(B_.xdt[:], B_.XS[:].rearrange("p (h q) -> p h q", h=16), B_.dt_[:, dsl].unsqueeze(2).broadcast_to([128, 16, 64]), ALU.mult), reads=[B_.rXS, B_.rdt], writes=[B_.rxdt])
                    yield

                    def f(e):
                        e.matmul(bA[:, 0:16], tri[d][:], B_.dtA[:, dsl], start=True, stop=True)
                        return e.matmul(bA[:, 16:32], onesf[:], B_.dtA[:, dsl], start=True, stop=True)
                    P.emit("pe", f, reads=[B_.rdt, rK, rC], writes=[rbA])
                    P.emit("dve", lambda e: e.tensor_scalar(B_.nac[:], bA[:, 0:16], -1.0, None, ALU.mult), reads=[rbA], writes=[B_.rsm])
                    P.emit("act", lambda e: e.activation(B_.expA[:], bA[:, 0:16], AF.Exp), reads=[rbA], writes=[B_.rsm])
                    P.emit("act", lambda e: e.activation(B_.cd[:], bA[:, 16:32], AF.Exp), reads=[rbA], writes=[B_.rsm])
                    P.emit("dve", lambda e: e.tensor_tensor(B_.dec[:], bA[:, 16:32], B_.nac[:], ALU.add), reads=[rbA, B_.rsm], writes=[B_.rsm])
                    P.emit("act", lambda e: e.activation(B_.dec[:], B_.dec[:], AF.Exp), reads=[B_.rsm], writes=[B_.rsm])
                    P.emit("dve", lambda e: e.tensor_tensor(B_.xdd[:], B_.xdt[:], B_.dec[:].unsqueeze(2).broadcast_to([128, 16, 64]), ALU.mult), reads=[B_.rxdt, B_.rsm], writes=[B_.rxdd])
                    yield

                    def f(e):
                        for g in range(2):
                            ins = e.matmul(pSs[:, g * 512:(g + 1) * 512], B_.BT[:, g * 128:(g + 1) * 128], B_.xdd[:, g * 8:(g + 1) * 8, :].rearrange("p h q -> p (h q)"), start=True, stop=True)
                        return ins
                    P.emit("pe", f, reads=[B_.rBT, B_.rxdd], writes=[rpSs])
                    if do_y:
                        P.emit("act", lambda e: e.activation(B_.Sb[:], B_.S[:], AF.Copy), reads=[B_.rS], writes=[B_.rSb])

                        def f(e):
                            for g in range(2):
                                ins = e.matmul(pY[:, g * 512:(g + 1) * 512], XC[:, 10 + g, cs], B_.Sb[:, g * 512:(g + 1) * 512], start=True, stop=True)
                            return ins
                        P.emit("pe", f, reads=[rXC, B_.rSb], writes=[rpY])
                        P.emit("dve", lambda e: e.tensor_tensor(B_.yo[:], pY[:].rearrange("p (h q) -> p h q", h=16), B_.expA[:].unsqueeze(2).broadcast_to([128, 16, 64]), ALU.mult), reads=[rpY, B_.rsm], writes=[B_.ryo])
                    P.emit("dve", lambda e: e.tensor_tensor(B_.S[:].rearrange("p (h q) -> p h q", h=16), B_.S[:].rearrange("p (h q) -> p h q", h=16), B_.cd[:].unsqueeze(2).broadcast_to([128, 16, 64]), ALU.mult), reads=[B_.rS, B_.rsm, B_.rSb], writes=[B_.rS])
                    P.emit("dve", lambda e: e.tensor_tensor(B_.S[:], B_.S[:], pSs[:], ALU.add), reads=[B_.rS, rpSs], writes=[B_.rS])
                    yield
                    if not do_y:
                        return

                    def f(e):
                        for g in range(2):
                            ins = e.matmul(bA[:, 256 + g * 128:256 + (g + 1) * 128], XC[:, 8 + g, cs], XC[:, 10 + g, cs], start=True, stop=True)
                        return ins
                    P.emit("pe", f, reads=[rXC], writes=[rbA])
                    P.emit("act", lambda e: e.activation(B_.CBs[:].rearrange("p a b -> p (a b)"), bA[:, 256:512], AF.Copy), reads=[rbA], writes=[B_.rCBs])
                    yield
                    for hf in range(2):
                        P.emit("pool", lambda e, hf=hf: e.tensor_tensor(B_.Rt[:], tri[d][:].unsqueeze(1).broadcast_to([128, 8, 128]), B_.dtA[:, d * 16 + hf * 8:d * 16 + hf * 8 + 8].unsqueeze(2).broadcast_to([128, 8, 128]), ALU.mult), reads=[rK, B_.rdt], writes=[B_.rRt])

                        def f(e):
                            for kb in range(2):
                                e.matmul(pL[:, kb * 4:(kb + 1) * 4, :].rearrange("p a b -> p (a b)"), onesf[:], B_.Rt[:, kb * 4:(kb + 1) * 4, :].rearrange("p a b -> p (a b)"), start=True, stop=False)
                                ins = e.matmul(pL[:, kb * 4:(kb + 1) * 4, :].rearrange("p a b -> p (a b)"), identf[:], msk[d][:], start=False, stop=True)
                            return ins
                        P.emit("pe", f, reads=[B_.rRt, rK, rC], writes=[rpL])
                        for hh in range(8):
                            h = hf * 8 + hh
                            P.emit("act", lambda e, h=h, hh=hh: e.activation(B_.LM[:, h, :], pL[:, hh, :], AF.Exp, bias=B_.nac[:, h:h + 1]), reads=[rpL, B_.rsm], writes=[B_.rLM])
                        yield
                    P.emit("dve", lambda e: e.tensor_tensor(B_.MT[:].rearrange("p (g h) i -> p g h i", g=2), B_.LM[:].rearrange("p (g h) i -> p g h i", g=2), B_.CBs[:].unsqueeze(2).broadcast_to([128, 2, 8, 128]), ALU.mult), reads=[B_.rLM, B_.rCBs], writes=[B_.rMT])

                    def f(e):
                        for h in range(16):
                            ins = e.matmul(pY[:, h * 64:(h + 1) * 64], B_.MT[:, h, :], B_.xdt[:, h, :], start=True, stop=True)
                        return ins
                    P.emit("pe", f, reads=[B_.rMT, B_.rxdt, B_.ryo], writes=[rpY])
                    P.emit("dve", lambda e: e.tensor_tensor(B_.y[:], B_.yo[:].rearrange("p h q -> p (h q)"), pY[:], ALU.add), reads=[B_.ryo, rpY], writes=[B_.ry])
                    if d == 0:
                        P.emit("pool", lambda e: e.tensor_tensor(zs[:].rearrange("p (h q) -> p h q", h=16), B_.XS[:].rearrange("p (h q) -> p h q", h=16), dsk[:].unsqueeze(2).broadcast_to([128, 16, 64]), ALU.mult), reads=[B_.rXS, rK], writes=[rzs])
                        P.emit("dve", lambda e: e.tensor_tensor(B_.y[:], B_.y[:], zs[:], ALU.add), reads=[B_.ry, rzs], writes=[B_.ry])
                        P.dma("sp", YF[r0:r0 + 128, :], B_.y[:], reads=[B_.ry], writes=[rYF[gi]])
                    else:
                        P.dma("sp", YB[r0:r0 + 128, :], B_.y[:], reads=[B_.ry], writes=[rYB[gi]])

                for b in range(2):
                    base = b * TB
                    cc_ = 0
                    for j in range(12):
                        for (s0, Ls) in ((0, 256), (256, 2048)):
                            u_, ru_, a_, ra_ = u[cc_ % 2], ru[cc_ % 2], acc[cc_ % 2], racc[cc_ % 2]
                            cc_ += 1
                            P.dma("sp", u_[:, 0:Ls], XBC[j * 128:(j + 1) * 128, base + s0:base + s0 + Ls], reads=[rXBC[b]], writes=[ru_])
                            P.emit("dve", lambda e, j=j, Ls=Ls, u_=u_, a_=a_: e.tensor_scalar(a_[:, 0:Ls], u_[:, 0:Ls], cw[:, j * 3 + 1:j * 3 + 2], None, ALU.mult), reads=[ru_, rK], writes=[ra_])
                            P.emit("dve", lambda e, j=j, Ls=Ls, u_=u_, a_=a_: e.scalar_tensor_tensor(a_[:, 1:Ls], u_[:, 0:Ls - 1], cw[:, j * 3:j * 3 + 1], a_[:, 1:Ls], ALU.mult, ALU.add), reads=[ru_, rK, ra_], writes=[ra_])
                            P.emit("dve", lambda e, j=j, Ls=Ls, u_=u_, a_=a_: e.scalar_tensor_tensor(a_[:, 0:Ls - 1], u_[:, 1:Ls], cw[:, j * 3 + 2:j * 3 + 3], a_[:, 0:Ls - 1], ALU.mult, ALU.add), reads=[ru_, rK, ra_], writes=[ra_])
                            P.emit("act", lambda e, j=j, Ls=Ls, s0=s0, a_=a_: e.activation(XC[:, j, s0:s0 + Ls], a_[:, 0:Ls], AF.Silu, bias=cb[:, j:j + 1]), reads=[ra_, rK], writes=[rXC])
                    orders = [list(range(18)), [1, 0] + list(range(17, 1, -1))]
                    for d in range(2):
                        P.emit("dve", lambda e, d=d: e.memset(sets[d].S[:], 0.0), writes=[sets[d].rS])
                    for step in range(18):
                        gens = [chunk_pass(b, d, orders[d][step], sets[d]) for d in range(2)]
                        while gens:
                            for g_ in list(gens):
                                try:
                                    next(g_)
                                except StopIteration:
                                    gens.remove(g_)
                P.flush()
              with ExitStack() as st:
                sg_ = sbt(st, "ssdgs", [128, 1024], F32)
                rK = R()
                P.dma("sp", sg_[:], ssdg[l], writes=[rK])

                class MS:
                    pass
                ms = []
                for i in range(2):
                    m_ = MS()
                    for nm, shp, dt_t in (("yf", [128, 1024], F32), ("yb", [128, 1024], F32), ("zt", [128, 1024], BF16), ("zs", [128, 1024], F32),
                                          ("y16", [128, 1024], BF16), ("yT", [128, 8, 128], BF16), ("junk", [128, 1024], BF16),
                                          ("ss", [128, 1], F32), ("rstd", [128, 1], F32)):
                        setattr(m_, nm, sbt(st, "%s_m%d" % (nm, i), shp, dt_t))
                    m_.bT = pst(st, "bTm%d" % i, [128, 8, 128], BF16)
                    for nm in ("ryf", "ryb", "rzt", "rzs", "ry16", "ryT", "rtmp", "rbT"):
                        setattr(m_, nm, R())
                    ms.append(m_)
                for n_, (b, ti) in enumerate(tiles_l):
                    M_ = ms[n_ % 2]
                    gi = b * 18 + ti
                    r0 = b * TB + ti * 128

                    def mrg(M_=M_, gi=gi, r0=r0):
                        P.dma("act", M_.yf[:], YF[r0:r0 + 128, :], reads=[rYF[gi]], writes=[M_.ryf])
                        P.dma("act", M_.yb[:], YB[r0:r0 + 128, :], reads=[rYB[gi]], writes=[M_.ryb])
                        P.dma("act", M_.zt[:], ZT[r0:r0 + 128, :], reads=[rZT[gi]], writes=[M_.rzt])
                        P.emit("dve", lambda e: e.tensor_tensor(M_.yf[:], M_.yf[:], M_.yb[:], ALU.add), reads=[M_.ryf, M_.ryb], writes=[M_.ryf])
                        P.emit("act", lambda e: e.activation(M_.zs[:], M_.zt[:], AF.Silu), reads=[M_.rzt], writes=[M_.rzs])
                        P.emit("dve", lambda e: e.tensor_tensor(M_.yf[:], M_.yf[:], M_.zs[:], ALU.mult), reads=[M_.ryf, M_.rzs], writes=[M_.ryf])
                        rms_rstd("act", M_.yf[:], 1024, M_.ss[:], M_.rstd[:], M_.junk[:], M_.ryf, M_.rtmp)
                        P.emit("act", lambda e: e.activation(M_.yf[:], M_.yf[:], AF.Copy, scale=M_.rstd[:]), reads=[M_.ryf, M_.rtmp], writes=[M_.ryf])
                        P.emit("dve", lambda e: e.tensor_tensor(M_.y16[:], M_.yf[:], sg_[:], ALU.mult), reads=[M_.ryf, rK], writes=[M_.ry16])

                        def f(e):
                            for j in range(8):
                                ins = e.transpose(M_.bT[:, j, :], M_.y16[:, j * 128:(j + 1) * 128], identb[:])
                            return ins
                        P.emit("pe", f, reads=[M_.ry16, rC], writes=[M_.rbT])
                        P.emit("act", lambda e: e.activation(M_.yT[:], M_.bT[:], AF.Copy), reads=[M_.rbT], writes=[M_.ryT])
                        P.dma("sp", MIX[0:1024, r0:r0 + 128].rearrange("(j p) t -> p j t", p=128), M_.yT[:], reads=[M_.ryT], writes=[rMIX[gi]])
                    mrg()
                P.flush()

            stWO = ExitStack()
            WO_pre = sbt(stWO, "WO", [128, 16, D], BF16)
            rWO_pre = R()
            for j in range(16):
                P.dma("pool", WO_pre[:, j, :], wo[l, j * 128:(j + 1) * 128, :], writes=[rWO_pre], max_dma_last_dim=4096)
            if phases is None or 4 in phases:
              with ExitStack() as st:
                KNs = sbt(st, "KNs", [128, 8, TB], BF16)
                KRs = [sbt(st, "KRs%d" % i, [128, TB], BF16) for i in range(2)]
                Vs = sbt(st, "Vs", [128, 18, 1024], BF16)
                rKV = R()
                QNs = [sbt(st, "QNs%d" % i, [128, 8, 512], BF16) for i in range(2)]
                QRs = [sbt(st, "QRs%d" % i, [128, 4, 512], BF16) for i in range(2)]
                rQ = [R(), R()]
                pS = [pst(st, "pSc%d" % i, [128, 512], F32) for i in range(4)]
                rpS = [R() for _ in range(4)]
                pO = [pst(st, "pOc%d" % i, [128, 512], F32) for i in range(2)]
                rpO = [R(), R()]
                pL = [pst(st, "pLc%d" % i, [128, 512], F32) for i in range(2)]
                rpL = [R(), R()]
                PT = [sbt(st, "PTc%d" % i, [128, 512], BF16) for i in range(6)]
                rPT = [R() for _ in range(6)]
                rl = [sbt(st, "rlc%d" % i, [128, 512], F32) for i in range(2)]
                rrl = [R(), R()]
                ot = [sbt(st, "otc%d" % i, [128, 512], BF16) for i in range(2)]
                rot = [R(), R()]
                cnt = [0, 0]

                def block_head(b, Qn, Qr, rq, h, nq, nkt, c0, gis):
                    u = cnt[1]
                    cnt[1] += 1
                    po, rpo, pl, rpl = pO[u % 2], rpO[u % 2], pL[u % 2], rpL[u % 2]
                    tiles = []

                    def score(kt):
                        i = cnt[0]
                        cnt[0] += 1
                        ps, rps = pS[i % 4], rpS[i % 4]
                        pt, rpt = PT[i % 6], rPT[i % 6]

                        def f(e):
                            e.matmul(ps[:, 0:nq], KNs[:, h, kt * 128:(kt + 1) * 128], Qn[:, h, 0:nq], start=True, stop=False)
                            return e.matmul(ps[:, 0:nq], KRs[h % 2][:, kt * 128:(kt + 1) * 128], Qr[:, h // 2, 0:nq], start=False, stop=True)
                        P.emit("pe", f, reads=[rq, rKV], writes=[rps])
                        P.emit("act", lambda e: e.activation(pt[:, 0:nq], ps[:, 0:nq], AF.Exp, scale=ATTN_SCALE), reads=[rps], writes=[rpt])
                        tiles.append((kt, pt, rpt))

                    def pv(idx):
                        kt, pt, rpt = tiles[idx]

                        def f(e):
                            e.matmul(po[:, 0:nq], Vs[:, kt, h * 128:(h + 1) * 128], pt[:, 0:nq], start=(idx == 0), stop=(idx == nkt - 1))
                            return e.matmul(pl[:, 0:nq], onesb[:], pt[:, 0:nq], start=(idx == 0), stop=(idx == nkt - 1))
                        P.emit("pe", f, reads=[rpt, rKV, rC], writes=[rpo, rpl])
                    DEPTH = 2
                    for kt in range(nkt):
                        score(kt)
                        if kt >= DEPTH:
                            pv(kt - DEPTH)
                    for idx in range(max(0, nkt - DEPTH), nkt):
                        pv(idx)
                    r_, rr_, o_, ro_ = rl[u % 2], rrl[u % 2], ot[u % 2], rot[u % 2]
                    P.emit("dve", lambda e: e.reciprocal(r_[:, 0:nq], pl[:, 0:nq]), reads=[rpl], writes=[rr_])
                    P.emit("dve", lambda e: e.tensor_tensor(o_[:, 0:nq], po[:, 0:nq], r_[:, 0:nq], ALU.mult), reads=[rpo, rr_], writes=[ro_])
                    P.dma("sp", MIX[1024 + h * 128:1024 + (h + 1) * 128, c0:c0 + nq], o_[:, 0:nq], reads=[ro_], writes=[rMIX[g] for g in gis])

                for b in range(2):
                    base = b * TB
                    P.dma("sp", KNs[:], KN[:, :, base:base + TB].rearrange("h p t -> p h t"), reads=[rQK[b]], writes=[rKV])
                    for i_ in range(2):
                        P.emit("dve", lambda e, i_=i_: e.memset(KRs[i_][:], 0.0), writes=[rKV])
                        P.dma("act", KRs[i_][i_ * 64:(i_ + 1) * 64, :], KR[i_ * 64:(i_ + 1) * 64, base:base + TB], reads=[rQK[b]], writes=[rKV])
                    for kt in range(18):
                        P.dma("sp" if kt % 2 else "act", Vs[:, kt, :], VV[base + kt * 128:base + (kt + 1) * 128, :], reads=[rQK[b]], writes=[rKV])
                    blocks = [(2, 4), (6, 4), (10, 4), (14, 4)]
                    if not last:
                        blocks = [(0, 2)] + blocks
                    for bi, (t0, nt) in enumerate(blocks):
                        nq = nt * 128
                        nkt = 2 if t0 == 0 else 18
                        c0 = base + t0 * 128
                        Qn, Qr, rq = QNs[bi % 2], QRs[bi % 2], rQ[bi % 2]
                        P.dma("act", Qn[:, :, 0:nq], QN[:, :, c0:c0 + nq].rearrange("h p t -> p h t"), reads=[rQK[b]], writes=[rq])
                        P.dma("act", Qr[:, :, 0:nq], QR[:, :, c0:c0 + nq].rearrange("h p t -> p h t"), reads=[rQK[b]], writes=[rq])
                        gis = [b * 18 + t0 + i for i in range(nt)]
                        for h in range(8):
                            block_head(b, Qn, Qr, rq, h, nq, nkt, c0, gis)
                P.flush()

            if phases is None or 5 in phases:
              with ExitStack() as st:
                WO = WO_pre
                rW = rWO_pre
                RW = sbt(st, "RW", [128, 16, 16], F32)
                rbs = sbt(st, "rbs", [128, 16], F32)
                P.dma("sp", RW[:], rwT, writes=[rW])
                P.dma("sp", rbs[:], rb, writes=[rW])
                G1 = build_gate(st, "g1", 32, [0, 1] if last else [0, 1, 2])
                pO = pst(st, "pOd", [128, D], F32)
                rpO = R()
                pT = [pst(st, "pTd%d" % i, [128, 4, 128], F32) for i in range(2)]
                rpT = [R(), R()]
                pR = pst(st, "pRd", [128, 16], F32)
                rpR = R()

                class DS:
                    pass
                dsets = []
                for i in range(2):
                    s_ = DS()
                    for nm, shp, dt_t in (("mixT", [128, 16, 128], BF16), ("xt", [128, D], F32), ("tt", [128, D], F32), ("xs", [128, D], F32),
                                          ("junk", [128, D], BF16), ("ss", [128, 1], F32), ("rstd", [128, 1], F32),
                                          ("h2f", [128, 16, 128], F32), ("h2b", [128, 16, 128], BF16),
                                          ("sc", [128, 16], F32), ("sel", [128, 16], F32), ("pr6", [128, 4, 6], F32), ("gs", [128, 4], F32),
                                          ("gmx", [128, 1], F32), ("gmk", [128, 4], F32), ("mk", [128, 16], F32), ("m1", [128, 16], F32),
                                          ("m2", [128, 16], F32), ("t1", [128, 1], F32), ("cmb", [128, 16], F32)):
                        setattr(s_, nm, sbt(st, "%s_D%d" % (nm, i), shp, dt_t))
                    for nm in ("rmixT", "rxt", "rtt", "rxs", "rtmp", "rh2f", "rh2b", "rr"):
                        setattr(s_, nm, R())
                    dsets.append(s_)

                def dtile(S_, b, ti):
                    gi = b * 18 + ti
                    r0 = b * TB + ti * 128
                    c = 2 if ti < 2 else b
                    g1, rg1 = G1[c]
                    P.dma("act", S_.mixT[:], MIX[:, r0:r0 + 128].rearrange("(j p) t -> p j t", p=128), reads=[rMIX[gi]], writes=[S_.rmixT])
                    P.dma("act", S_.xt[:], resid_src(l, b, ti), reads=[rRES[gi]], writes=[S_.rxt])

                    def f(e):
                        for nb in range(4):
                            for j in range(16):
                                ins = e.matmul(pO[:, nb * 512:(nb + 1) * 512], S_.mixT[:, j, :], WO[:, j, nb * 512:(nb + 1) * 512], start=(j == 0), stop=(j == 15))
                        return ins
                    P.emit("pe", f, reads=[S_.rmixT, rW], writes=[rpO])

                def dtile1b(S_, b, ti):
                    gi = b * 18 + ti
                    r0 = b * TB + ti * 128
                    c = 2 if ti < 2 else b
                    g1, rg1 = G1[c]
                    P.emit("dve", lambda e: e.tensor_tensor(S_.tt[:], pO[:], g1[:], ALU.mult), reads=[rpO, rg1], writes=[S_.rtt])
                    P.emit("dve", lambda e: e.tensor_tensor(S_.xt[:], S_.xt[:], S_.tt[:], ALU.add), reads=[S_.rxt, S_.rtt], writes=[S_.rxt])
                    P.dma("sp", RES[r0:r0 + 128, :], S_.xt[:], reads=[S_.rxt], writes=[rRES[gi]])
                    rms_rstd("act", S_.xt[:], D, S_.ss[:], S_.rstd[:], S_.junk[:], S_.rxt, S_.rtmp)
                    P.emit("act", lambda e: e.activation(S_.xs[:], S_.xt[:], AF.Copy, scale=S_.rstd[:]), reads=[S_.rxt, S_.rtmp], writes=[S_.rxs])

                def dtile2(S_, b, ti):
                    gi = b * 18 + ti
                    r0 = b * TB + ti * 128
                    c = 2 if ti < 2 else b
                    for r4 in range(4):
                        p_, rp_ = pT[r4 % 2], rpT[r4 % 2]

                        def f(e, r4=r4, p_=p_):
                            for jj in range(4):
                                j = r4 * 4 + jj
                                ins = e.transpose(p_[:, jj, :], S_.xs[:, j * 128:(j + 1) * 128], identf[:])
                            return ins
                        P.emit("pe", f, reads=[S_.rxs, rC], writes=[rp_])
                        for jj in range(4):
                            j = r4 * 4 + jj
                            if jj % 2 == 0:
                                P.emit("dve", lambda e, p_=p_, jj=jj, j=j: e.tensor_scalar(S_.h2f[:, j, :], p_[:, jj, :], gm2[:, j, c:c + 1], modS[:, 48 + j, c:c + 1], ALU.mult, ALU.add), reads=[rp_, rmod], writes=[S_.rh2f])
                            else:
                                P.emit("act", lambda e, p_=p_, jj=jj, j=j: e.activation(S_.h2f[:, j, :], p_[:, jj, :], AF.Identity, scale=gm2[:, j, c:c + 1], bias=modS[:, 48 + j, c:c + 1]), reads=[rp_, rmod], writes=[S_.rh2f])
                    P.emit("dve", lambda e: e.tensor_copy(S_.h2b[:], S_.h2f[:]), reads=[S_.rh2f], writes=[S_.rh2b])
                    P.dma("sp", HT[:, r0:r0 + 128].rearrange("(j p) t -> p j t", p=128), S_.h2b[:], reads=[S_.rh2b], writes=[rHT[gi]])

                    def f(e):
                        for j in range(16):
                            ins = e.matmul(pR[:], S_.h2f[:, j, :], RW[:, j, :], start=(j == 0), stop=(j == 15))
                        return ins
                    P.emit("pe", f, reads=[S_.rh2f, rW], writes=[rpR])

                def dtile2b(S_, b, ti):
                    gi = b * 18 + ti
                    r0 = b * TB + ti * 128
                    P.emit("act", lambda e: e.activation(S_.sc[:], pR[:], AF.Sigmoid), reads=[rpR], writes=[S_.rr])
                    V = lambda fn, rd=(): P.emit("dve", fn, reads=[S_.rr] + list(rd), writes=[S_.rr])
                    sc, sel, pr6, gs, gmx, gmk, mk, m1, m2, t1, cmb = S_.sc, S_.sel, S_.pr6, S_.gs, S_.gmx, S_.gmk, S_.mk, S_.m1, S_.m2, S_.t1, S_.cmb
                    V(lambda e: e.tensor_tensor(sel[:], sc[:], rbs[:], ALU.add), [rW])
                    s4 = sel[:].rearrange("p (g k) -> p g k", g=4)
                    V(lambda e: e.tensor_tensor(pr6[:, :, 0:3], s4[:, :, 0:3], s4[:, :, 1:4], ALU.add))
                    V(lambda e: e.tensor_tensor(pr6[:, :, 3:5], s4[:, :, 0:2], s4[:, :, 2:4], ALU.add))
                    V(lambda e: e.tensor_tensor(pr6[:, :, 5:6], s4[:, :, 0:1], s4[:, :, 3:4], ALU.add))
                    V(lambda e: e.tensor_reduce(gs[:], pr6[:], AX.X, ALU.max))
                    V(lambda e: e.tensor_reduce(gmx[:], gs[:], AX.X, ALU.max))
                    V(lambda e: e.tensor_scalar(gmk[:], gs[:], gmx[:], None, ALU.is_ge))
                    V(lambda e: e.tensor_tensor(mk[:].rearrange("p (g k) -> p g k", g=4), s4, gmk[:].unsqueeze(2).broadcast_to([128, 4, 4]), ALU.mult))
                    V(lambda e: e.tensor_scalar(gmk[:], gmk[:], -1.0, 10.0, ALU.add, ALU.mult))
                    V(lambda e: e.tensor_tensor(mk[:].rearrange("p (g k) -> p g k", g=4), mk[:].rearrange("p (g k) -> p g k", g=4), gmk[:].unsqueeze(2).broadcast_to([128, 4, 4]), ALU.add))
                    V(lambda e: e.tensor_reduce(t1[:], mk[:], AX.X, ALU.max))
                    V(lambda e: e.tensor_scalar(m1[:], mk[:], t1[:], None, ALU.is_ge))
                    V(lambda e: e.scalar_tensor_tensor(mk[:], m1[:], -20.0, mk[:], ALU.mult, ALU.add))
                    V(lambda e: e.tensor_reduce(t1[:], mk[:], AX.X, ALU.max))
                    V(lambda e: e.tensor_scalar(m2[:], mk[:], t1[:], None, ALU.is_ge))
                    V(lambda e: e.tensor_tensor(m1[:], m1[:], m2[:], ALU.add))
                    V(lambda e: e.tensor_tensor(m1[:], m1[:], sc[:], ALU.mult))
                    V(lambda e: e.tensor_reduce(t1[:], m1[:], AX.X, ALU.add))
                    V(lambda e: e.reciprocal(t1[:], t1[:]))
                    V(lambda e: e.tensor_scalar(cmb[:], m1[:], t1[:], None, ALU.mult))
                    P.dma("sp", COMB[r0:r0 + 128, :], cmb[:], reads=[S_.rr], writes=[rCOMB[gi]])

                prev = None
                for n_, (b, ti) in enumerate(tiles_l):
                    cur = (dsets[n_ % 2], b, ti)
                    dtile(*cur)
                    if prev is not None:
                        dtile2(*prev)
                    dtile1b(*cur)
                    if prev is not None:
                        dtile2b(*prev)
                    prev = cur
                dtile2(*prev)
                dtile2b(*prev)
                P.flush()

            stWO.close()
            if phases is None or 6 in phases:
              with ExitStack() as st:
                xt_tiles = [(b, ti) for b in range(2) for ti in range(2, 18)]
                sblocks = [xt_tiles[i * 8:(i + 1) * 8] for i in range(4)]
                if not last:
                    sblocks.append([(0, 0), (0, 1), (1, 0), (1, 1)])
                WG = sbt(st, "WG", [128, 16, 512], BF16)
                WU = sbt(st, "WU", [128, 16, 512], BF16)
                WD = sbt(st, "WD", [128, 4, D], BF16)
                rWG, rWD = R(), R()
                h2 = sbt(st, "h2", [128, 16, 1024], BF16)
                rh2 = R()
                accm = sbt(st, "accm", [128, 8, D], F32)
                racc = [R() for _ in range(8)]
                cmb = sbt(st, "cmbm", [128, 8, 16], F32)
                rcmb = R()
                actT = sbt(st, "actT", [128, 4, 1024], BF16)
                ract = R()
                sgl = [sbt(st, "sgl%d" % i, [128, 512], F32) for i in range(2)]
                rsgl = [R(), R()]
                pGU = [pst(st, "pGU%d" % i, [128, 2, 512], F32) for i in range(2)]
                rpGU = [R(), R()]
                pY = [pst(st, "pYm%d" % i, [128, 1024], F32) for i in range(2)]
                rpY = [R(), R()]
                xts = [sbt(st, "xtm%d" % i, [128, D], F32) for i in range(2)]
                rxts = [R(), R()]
                xcnt = 0
                junk = sbt(st, "junkm", [128, D], BF16)
                ss = sbt(st, "ssm", [128, 1], F32)
                rstd = sbt(st, "rstdm", [128, 1], F32)
                rtmp = R()
                fg = sbt(st, "fg", [128, D], F32)
                rfg = R()
                if last:
                    P.dma("sp", fg[:], fng, writes=[rfg])
                g2buf = (sbt(st, "g2b", [128, D], F32), R(), sbt(st, "g2dg", [128, 128], F32), R())
                g2cond = None
                kq = 0
                for sbk in sblocks:
                    nt = len(sbk)
                    nh = nt // 4
                    for i, (b, ti) in enumerate(sbk):
                        gi = b * 18 + ti
                        r0 = b * TB + ti * 128
                        P.dma("sp", h2[:, :, i * 128:(i + 1) * 128], HT[:, r0:r0 + 128].rearrange("(j p) t -> p j t", p=128), reads=[rHT[gi]], writes=[rh2])
                        P.dma("sp", cmb[:, i, :], COMB[r0:r0 + 128, :], reads=[rCOMB[gi]], writes=[rcmb])
                    for ex in range(16):
                        P.dma("pool", WG[:], wg[l, ex].rearrange("(j p) f -> p j f", p=128), writes=[rWG])
                        P.dma("pool", WU[:], wu[l, ex].rearrange("(j p) f -> p j f", p=128), writes=[rWG])
                        for j in range(4):
                            P.dma("pool", WD[:, j, :], wd[l, ex, j * 128:(j + 1) * 128, :], writes=[rWD], max_dma_last_dim=4096)
                        for hb in range(nh):
                            for fc in range(4):
                                pg, rpg = pGU[kq % 2], rpGU[kq % 2]
                                sg, rsg = sgl[kq % 2], rsgl[kq % 2]
                                kq += 1

                                def f(e, pg=pg, fc=fc, hb=hb):
                                    for j in range(16):
                                        e.matmul(pg[:, 0, :], WG[:, j, fc * 128:(fc + 1) * 128], h2[:, j, hb * 512:(hb + 1) * 512], start=(j == 0), stop=(j == 15))
                                    for j in range(16):
                                        ins = e.matmul(pg[:, 1, :], WU[:, j, fc * 128:(fc + 1) * 128], h2[:, j, hb * 512:(hb + 1) * 512], start=(j == 0), stop=(j == 15))
                                    return ins
                                P.emit("pe", f, reads=[rWG, rh2], writes=[rpg])
                                P.emit("act", lambda e, pg=pg, sg=sg: e.activation(sg[:], pg[:, 0, :], AF.Silu), reads=[rpg], writes=[rsg])
                                P.emit("dve", lambda e, pg=pg, sg=sg, fc=fc, hb=hb: e.tensor_tensor(actT[:, fc, hb * 512:(hb + 1) * 512], sg[:], pg[:, 1, :], ALU.mult), reads=[rpg, rsg], writes=[ract])
                        for i in range(nt):
                            for dh in range(2):
                                py, rpy = pY[kq % 2], rpY[kq % 2]
                                kq += 1

                                def f(e, py=py, i=i, dh=dh):
                                    for nb in range(2):
                                        for fc in range(4):
                                            ins = e.matmul(py[:, nb * 512:(nb + 1) * 512], actT[:, fc, i * 128:(i + 1) * 128], WD[:, fc, dh * 1024 + nb * 512:dh * 1024 + (nb + 1) * 512], start=(fc == 0), stop=(fc == 3))
                                    return ins
                                P.emit("pe", f, reads=[ract, rWD], writes=[rpy])
                                a_ = accm[:, i, dh * 1024:(dh + 1) * 1024]
                                if ex == 0:
                                    P.emit("dve", lambda e, py=py, a_=a_, i=i, ex=ex: e.tensor_scalar(a_, py[:], cmb[:, i, ex:ex + 1], None, ALU.mult), reads=[rpy, rcmb], writes=[racc[i]])
                                else:
                                    P.emit("dve", lambda e, py=py, a_=a_, i=i, ex=ex: e.scalar_tensor_tensor(a_, py[:], cmb[:, i, ex:ex + 1], a_, ALU.mult, ALU.add), reads=[rpy, rcmb, racc[i]], writes=[racc[i]])
                    for i, (b, ti) in enumerate(sbk):
                        gi = b * 18 + ti
                        r0 = b * TB + ti * 128
                        c = 2 if ti < 2 else b
                        if g2cond != c:
                            build_gate(st, "g2", 80, [c], pg=pY[0], rpg=rpY[0], gbuf=g2buf)
                            g2cond = c
                        g2, rg2 = g2buf[0], g2buf[1]
                        xt, rxt = xts[xcnt % 2], rxts[xcnt % 2]
                        xcnt += 1
                        P.dma("act", xt[:], RES[r0:r0 + 128, :], reads=[rRES[gi]], writes=[rxt])
                        P.emit("dve", lambda e, i=i, g2=g2: e.tensor_tensor(accm[:, i, :], accm[:, i, :], g2[:], ALU.mult), reads=[racc[i], rg2], writes=[racc[i]])
                        P.emit("dve", lambda e, i=i, xt=xt: e.tensor_tensor(xt[:], xt[:], accm[:, i, :], ALU.add), reads=[racc[i], rxt], writes=[rxt])
                        if not last:
                            P.dma("sp", RES[r0:r0 + 128, :], xt[:], reads=[rxt], writes=[rRES[gi]])
                        else:
                            rms_rstd("act", xt[:], D, ss[:], rstd[:], junk[:], rxt, rtmp)
                            P.emit("act", lambda e, xt=xt: e.activation(xt[:], xt[:], AF.Copy, scale=rstd[:]), reads=[rxt, rtmp], writes=[rxt])
                            P.emit("dve", lambda e, xt=xt: e.tensor_tensor(xt[:], xt[:], fg[:], ALU.mult), reads=[rxt, rfg], writes=[rxt])
                            P.dma("sp", out[b, (ti - 2) * 128:(ti - 1) * 128, :], xt[:], reads=[rxt], writes=[rRES[gi]])
                P.flush()

        P.finish()
        P.flush()
    return nc


def _rope_tables():
    t = np.arange(2048)
    rows = (t // 64).astype(np.float32)
    cols = (t % 64).astype(np.float32)
    nf = 16
    inv = (np.float32(10000.0) ** (-np.arange(nf, dtype=np.float32) / nf)).astype(np.float32)
    ang = np.stack([rows[:, None] * inv, cols[:, None] * inv], axis=1)
    cos = np.cos(ang).astype(np.float32)
    sin = np.sin(ang).astype(np.float32)
    C = np.zeros((64, 2048), np.float32)
    S = np.zeros((64, 2048), np.float32)
    for a in range(2):
        for b in range(2):
            for f in range(16):
                idx = a * 32 + b * 16 + f
                C[idx] = cos[:, a, f]
                S[idx] = sin[:, a, f] * (-1.0 if b == 0 else 1.0)
    return np.concatenate([C, C], 0), np.concatenate([S, S], 0)


def _swap_perm():
    perm = np.zeros(64, np.int64)
    for a in range(2):
        for b in range(2):
            for f in range(16):
                perm[a * 32 + b * 16 + f] = a * 32 + (1 - b) * 16 + f
    return perm


def prep_shared(inp):
    f = lambda a: np.ascontiguousarray(np.asarray(a, dtype=np.float32))
    perm = _swap_perm()
    w_in = f(inp["w_in"])
    kpe = w_in[:, :, 3360:3424]
    kpes = kpe[:, :, perm]
    win = np.concatenate([w_in[:, :, :3360], kpe, kpe, kpes, kpes], axis=2)
    assert win.shape[2] == NWIN
    wqb = f(inp["w_q_b"]).reshape(L, 512, 8, 192)
    nope = wqb[:, :, :, :128].reshape(L, 512, 1024)
    rope = wqb[:, :, :, 128:]
    wq = np.concatenate([nope, rope.reshape(L, 512, 512), rope[:, :, :, perm].reshape(L, 512, 512)], axis=2)
    wkvb = f(inp["w_kv_b"]).reshape(L, 256, 8, 256)
    wkv = np.concatenate([wkvb[:, :, :, :128].reshape(L, 256, 1024), wkvb[:, :, :, 128:].reshape(L, 256, 1024)], axis=2)
    colT = lambda v, n: np.ascontiguousarray(f(v).reshape(L, n, 128).transpose(0, 2, 1))
    bc = lambda v: np.ascontiguousarray(np.broadcast_to(f(v).reshape(L, 1, -1), (L, 128, f(v).reshape(L, -1).shape[1])))
    convw = f(inp["conv_w"])
    convw_l = np.ascontiguousarray(convw.reshape(L, 3, 12, 128).transpose(0, 3, 2, 1).reshape(L, 128, 36))
    cosT, sinT = _rope_tables()
    i_ = np.arange(128)
    tri_f = (i_[:, None] <= i_[None, :]).astype(np.float32)
    tri_b = (i_[:, None] >= i_[None, :]).astype(np.float32)
    mask_f = np.where(i_[None, :] >= i_[:, None], 0.0, NEG).astype(np.float32)
    mask_b = np.where(i_[None, :] <= i_[:, None], 0.0, NEG).astype(np.float32)
    sh = {
        "ada_w": f(inp["ada_w"]),
        "ada_bT": colT(inp["ada_b"], 96),
        "g1T": colT(inp["norm1_g"], 16), "g2T": colT(inp["norm2_g"], 16),
        "win": np.ascontiguousarray(win),
        "convw": convw_l, "convb": colT(inp["conv_b"], 12),
        "dtb": bc(inp["dt_bias"]), "alog": bc(inp["a_log"]), "dskip": bc(inp["d_skip"]),
        "ssdg": bc(inp["ssd_norm_g"]),
        "qgT": colT(inp["q_norm_g"], 4), "wq": np.ascontiguousarray(wq),
        "kvgT": colT(inp["kv_norm_g"], 2), "wkv": np.ascontiguousarray(wkv),
        "wo": f(inp["w_o"]),
        "rwT": np.ascontiguousarray(f(inp["router_w"]).reshape(16, 128, 16).transpose(1, 0, 2)),
        "rb": np.ascontiguousarray(np.broadcast_to(f(inp["router_b"]).reshape(1, 16), (128, 16))),
        "wg": f(inp["w_gate"]), "wu": f(inp["w_up"]), "wd": f(inp["w_down"]),
        "fng": np.ascontiguousarray(np.broadcast_to(f(inp["final_norm_g"]).reshape(1, D), (128, D))),
        "c_ident": np.eye(128, dtype=np.float32),
        "c_tri": np.stack([tri_f, tri_b]),
        "c_mask": np.stack([np.tile(mask_f, (1, 4)), np.tile(mask_b, (1, 4))]),
        "c_cos": cosT, "c_sin": sinT,
    }
    return sh


def core_inputs(inp, sh, core):
    f = lambda a: np.ascontiguousarray(np.asarray(a, dtype=np.float32))
    b0 = core * 2
    cc = np.stack([f(inp["c"])[b0], f(inp["c"])[b0 + 1], f(inp["c_ctx"])], axis=1)
    m = dict(sh)
    m["xin"] = f(inp["x"][b0:b0 + 2])
    m["cin"] = f(inp["ctx"][b0:b0 + 2])
    m["ccT"] = np.ascontiguousarray(cc.reshape(16, 128, 3).transpose(1, 0, 2))
    return m


_NC = None
_SKIP = set()


def kernel(**inputs):
    global _NC
    if _NC is None:
        _NC = build()
    sh = prep_shared(inputs)
    in_maps = [core_inputs(inputs, sh, c) for c in range(8)]
    res = run_bass_kernel_spmd(_NC, in_maps, core_ids=list(range(8)))
    return np.concatenate([np.asarray(r["out"]) for r in res.results], axis=0).astype(np.float32)
```

```python
import numpy as np
from contextlib import ExitStack
import concourse.bass as bass
import concourse.mybir as mybir
from concourse.bass_utils import run_bass_kernel_spmd

F32 = mybir.dt.float32
BF16 = mybir.dt.bfloat16
AF = mybir.ActivationFunctionType
ALU = mybir.AluOpType
AX = mybir.AxisListType

L = 2
D = 2048
TB = 2304
T = 2 * TB
EPS = 1e-6
NWIN = 3616
ATTN_SCALE = 192.0 ** -0.5
NEG = -30000.0


class R:
    __slots__ = ("lw", "rd")

    def __init__(self):
        self.lw = None
        self.rd = {}


class Prog:
    ENGS = ("pe", "act", "dve", "pool", "sp")
    NDS = 48
    NHW = 36

    def __init__(self, nc, es):
        self.nc = nc
        self.q = {e: [] for e in self.ENGS}
        self.cnt = {e: 0 for e in self.ENGS}
        self.waited = {e: {} for e in self.ENGS}
        self.sem = {e: es.enter_context(nc.semaphore("s_" + e)) for e in self.ENGS}
        self.dsem = [es.enter_context(nc.semaphore("d%d" % i)) for i in range(self.NDS)]
        self.dcnt = [0] * self.NDS
        self.dnext = 0
        self.dnext_sw = 0

    def _semof(self, key):
        return self.sem[key[1]] if key[0] == "e" else self.dsem[key[1]]

    def _deps(self, eng, reads, writes, extra=()):
        deps = {}

        def add(d):
            if d is None:
                return
            k, v = d
            if deps.get(k, 0) < v:
                deps[k] = v

        for r in reads:
            add(r.lw)
        for w in writes:
            add(w.lw)
            for kv in w.rd.items():
                add(kv)
        for d in extra:
            add(d)
        waits = []
        wd = self.waited[eng]
        for k, v in deps.items():
            if eng == "pe" and k == ("e", "pe"):
                continue
            if wd.get(k, 0) >= v:
                continue
            wd[k] = v
            waits.append((self._semof(k), v))
        return waits

    def emit(self, eng, fn, reads=(), writes=()):
        waits = self._deps(eng, reads, writes)
        self.cnt[eng] += 1
        key = ("e", eng)
        val = self.cnt[eng]
        sem = self.sem[eng]

        def thunk(e):
            for s, v in waits:
                e.wait_ge(s, v)
            fn(e).then_inc(sem, 1)

        self.q[eng].append(thunk)
        for r in reads:
            r.rd[key] = val
        for w in writes:
            w.lw = (key, val)
            w.rd = {}

    def dma(self, queue, out, in_, reads=(), writes=(), **kw):
        if queue == "pool":
            i = self.NHW + self.dnext_sw
            self.dnext_sw = (self.dnext_sw + 1) % (self.NDS - self.NHW)
        else:
            i = self.dnext
            self.dnext = (i + 1) % self.NHW
        prev = self.dcnt[i]
        self.dcnt[i] += 16
        val = self.dcnt[i]
        key = ("d", i)
        extra = [(key, prev)] if prev > 0 else []
        waits = self._deps(queue, reads, writes, extra)
        sem = self.dsem[i]

        def thunk(e):
            for s, v in waits:
                e.wait_ge(s, v)
            e.dma_start(out=out, in_=in_, **kw).then_inc(sem, 16)

        self.q[queue].append(thunk)
        for r in reads:
            r.rd[key] = val
        for w in writes:
            w.lw = (key, val)
            w.rd = {}

    def finish(self):
        waits = []
        for i in range(self.NDS):
            if self.dcnt[i] > 0:
                waits.append((self.dsem[i], self.dcnt[i]))
        for en in self.ENGS:
            if en != "sp" and self.cnt[en] > 0:
                waits.append((self.sem[en], self.cnt[en]))

        def thunk(e):
            for s, v in waits:
                e.wait_ge(s, v)

        self.q["sp"].append(thunk)

    def barrier(self):
        for en in self.ENGS:
            waits = []
            wd = self.waited[en]
            for i in range(self.NDS):
                k = ("d", i)
                if self.dcnt[i] > wd.get(k, 0):
                    wd[k] = self.dcnt[i]
                    waits.append((self.dsem[i], self.dcnt[i]))
            for e2 in self.ENGS:
                k = ("e", e2)
                if e2 != en and self.cnt[e2] > wd.get(k, 0):
                    wd[k] = self.cnt[e2]
                    waits.append((self.sem[e2], self.cnt[e2]))

            def thunk(e, waits=waits):
                for s_, v in waits:
                    e.wait_ge(s_, v)
            self.q[en].append(thunk)

    def flush(self):
        self.barrier()
        nc = self.nc
        q = self.q
        with nc.Block() as block:
            @block.tensor
            def _(e):
                for t in q["pe"]:
                    t(e)

            @block.scalar
            def _(e):
                for t in q["act"]:
                    t(e)

            @block.vector
            def _(e):
                for t in q["dve"]:
                    t(e)

            @block.gpsimd
            def _(e):
                for t in q["pool"]:
                    t(e)

            @block.sync
            def _(e):
                for t in q["sp"]:
                    t(e)
        self.q = {e: [] for e in self.ENGS}


def build(dbg=False, nlayers=L, phases=None):
    nc = bass.Bass("TRN2", target_bir_lowering=False)

    def din(name, shape, dt=F32):
        return nc.dram_tensor(name, list(shape), dt, kind="ExternalInput").ap()

    def dscr(name, shape, dt):
        return nc.dram_tensor(name, list(shape), dt, kind=("ExternalOutput" if dbg else "Internal")).ap()

    xin = din("xin", [2, 2048, D])
    cin = din("cin", [2, 256, D])
    ccT = din("ccT", [128, 16, 3])
    ada_w = din("ada_w", [L, D, 12288])
    ada_bT = din("ada_bT", [L, 128, 96])
    g1T = din("g1T", [L, 128, 16])
    g2T = din("g2T", [L, 128, 16])
    win = din("win", [L, D, NWIN])
    convw = din("convw", [L, 128, 36])
    convb = din("convb", [L, 128, 12])
    dtb = din("dtb", [L, 128, 32])
    alog = din("alog", [L, 128, 32])
    dskip = din("dskip", [L, 128, 16])
    ssdg = din("ssdg", [L, 128, 1024])
    qgT = din("qgT", [L, 128, 4])
    wq = din("wq", [L, 512, 2048])
    kvgT = din("kvgT", [L, 128, 2])
    wkv = din("wkv", [L, 256, 2048])
    wo = din("wo", [L, D, D])
    rwT = din("rwT", [128, 16, 16])
    rb = din("rb", [128, 16])
    wg = din("wg", [L, 16, D, 512])
    wu = din("wu", [L, 16, D, 512])
    wd = din("wd", [L, 16, 512, D])
    fng = din("fng", [128, D])
    c_ident = din("c_ident", [128, 128])
    c_tri = din("c_tri", [2, 128, 128])
    c_mask = din("c_mask", [2, 128, 512])
    c_cos = din("c_cos", [128, 2048])
    c_sin = din("c_sin", [128, 2048])
    out = nc.dram_tensor("out", [2, 2048, D], F32, kind="ExternalOutput").ap()

    RES = dscr("RES", [T, D], F32)
    ZT = dscr("ZT", [T, 1024], BF16)
    DTR = dscr("DTR", [T, 32], F32)
    XBC = dscr("XBC", [1536, T], BF16)
    QN = dscr("QN", [8, 128, T], BF16)
    QR = dscr("QR", [4, 128, T], BF16)
    KN = dscr("KN", [8, 128, T], BF16)
    KR = dscr("KR", [128, T], BF16)
    VV = dscr("VV", [T, 1024], BF16)
    YF = dscr("YF", [T, 1024], F32)
    YB = dscr("YB", [T, 1024], F32)
    MIX = dscr("MIX", [D, T], BF16)
    HT = dscr("HT", [D, T], BF16)
    COMB = dscr("COMB", [T, 16], F32)
    DBGM = dscr("DBGM", [128, 96, 3], F32)
    rRES = [R() for _ in range(36)]
    rZT = [R() for _ in range(36)]
    rDTR = [R() for _ in range(36)]
    rXBC = [R() for _ in range(2)]
    rQK = [R() for _ in range(2)]
    rYF = [R() for _ in range(36)]
    rYB = [R() for _ in range(36)]
    rMIX = [R() for _ in range(36)]
    rHT = [R() for _ in range(36)]
    rCOMB = [R() for _ in range(36)]

    def resid_src(l, b, ti):
        if l == 0:
            if ti < 2:
                return cin[b, ti * 128:(ti + 1) * 128, :]
            return xin[b, (ti - 2) * 128:(ti - 1) * 128, :]
        r0 = b * TB + ti * 128
        return RES[r0:r0 + 128, :]

    with ExitStack() as es:
        P = Prog(nc, es)

        uid = [0]

        def sbt(st, name, shape, dt):
            uid[0] += 1
            return st.enter_context(nc.sbuf_tensor("%s_%d" % (name, uid[0]), list(shape), dt))

        def pst(st, name, shape, dt):
            uid[0] += 1
            return st.enter_context(nc.psum_tensor("%s_%d" % (name, uid[0]), list(shape), dt))

        identf = sbt(es, "identf", [128, 128], F32)
        identb = sbt(es, "identb", [128, 128], BF16)
        onesf = sbt(es, "onesf", [128, 128], F32)
        onesb = sbt(es, "onesb", [128, 128], BF16)
        modS = sbt(es, "modS", [128, 96, 3], F32)
        gm1 = sbt(es, "gm1", [128, 16, 3], F32)
        gm2 = sbt(es, "gm2", [128, 16, 3], F32)
        rC = R()
        rmod = R()
        P.dma("sp", identf[:], c_ident, writes=[rC])
        P.emit("dve", lambda e: e.tensor_copy(identb[:], identf[:]), reads=[rC], writes=[rC])
        P.emit("dve", lambda e: e.memset(onesf[:], 1.0), writes=[rC])
        P.emit("dve", lambda e: e.memset(onesb[:], 1.0), writes=[rC])

        def rms_rstd(eng_src, src_ap, n, ss, rstd, junk, rsrc, rtmp):
            P.emit("act", lambda e: e.activation(junk, src_ap, AF.Square, accum_out=ss), reads=[rsrc], writes=[rtmp])
            P.emit("act", lambda e: e.activation(rstd, ss, AF.Sqrt, bias=EPS, scale=1.0 / n), reads=[rtmp], writes=[rtmp])
            P.emit("dve", lambda e: e.reciprocal(rstd, rstd), reads=[rtmp], writes=[rtmp])

        for l in range(nlayers):
            last = (l == L - 1)
            tiles_l = [(b, ti) for b in range(2) for ti in range(18) if not (last and ti < 2)]

            stA1w = ExitStack()
            W_A1 = sbt(stA1w, "W", [128, 16, 2592], BF16)
            rW_A1 = R()
            for j in range(16):
                P.dma("pool", W_A1[:, j, :], win[l, j * 128:(j + 1) * 128, 0:2592], writes=[rW_A1], max_dma_last_dim=4096)
            if phases is None or 0 in phases:
              with ExitStack() as st:
                scf = sbt(st, "scf", [128, 16, 3], F32)
                scb = sbt(st, "scb", [128, 16, 3], BF16)
                abT = sbt(st, "abT", [128, 96], F32)
                g1s = sbt(st, "g1s", [128, 16], F32)
                g2s = sbt(st, "g2s", [128, 16], F32)
                awf = [sbt(st, "awf%d" % i, [128, 16, 512], F32) for i in range(2)]
                rawf = [R(), R()]
                aw = [sbt(st, "aw%d" % i, [128, 16, 512], BF16) for i in range(2)]
                raw = [R(), R()]
                pm = pst(st, "pm", [128, 96, 3], F32)
                rpm = R()
                rs = R()
                P.dma("sp", scf[:], ccT, writes=[rs])
                P.dma("sp", abT[:], ada_bT[l], writes=[rs])
                P.dma("sp", g1s[:], g1T[l], writes=[rs])
                P.dma("sp", g2s[:], g2T[l], writes=[rs])
                P.emit("act", lambda e: e.activation(scb[:], scf[:], AF.Silu), reads=[rs], writes=[rs])
                for pc in range(24):
                    af, raf = awf[pc % 2], rawf[pc % 2]
                    a, ra = aw[pc % 2], raw[pc % 2]
                    for hj in range(2):
                        P.dma("sp" if hj == 0 else "act", af[:, hj * 8:(hj + 1) * 8, :], ada_w[l, hj * 1024:(hj + 1) * 1024, pc * 512:(pc + 1) * 512].rearrange("(j p) m -> p j m", p=128), writes=[raf])
                    P.emit("dve", lambda e, a=a, af=af: e.tensor_copy(a[:, 0:6, :], af[:, 0:6, :]), reads=[raf], writes=[ra])
                    P.emit("pool", lambda e, a=a, af=af: e.tensor_copy(a[:, 6:10, :], af[:, 6:10, :]), reads=[raf], writes=[ra])
                    P.emit("act", lambda e, a=a, af=af: e.activation(a[:, 10:16, :], af[:, 10:16, :], AF.Copy), reads=[raf], writes=[ra])
                    for mc in range(4):
                        m = pc * 4 + mc

                        def f(e, a=a, m=m, mc=mc):
                            for j in range(16):
                                ins = e.matmul(pm[:, m, :], a[:, j, mc * 128:(mc + 1) * 128], scb[:, j, :], start=(j == 0), stop=(j == 15))
                            return ins
                        P.emit("pe", f, reads=[ra, rs], writes=[rpm])
                P.emit("dve", lambda e: e.tensor_tensor(modS[:], pm[:], abT[:].unsqueeze(2).broadcast_to([128, 96, 3]), ALU.add), reads=[rpm, rs, rmod], writes=[rmod])
                P.emit("dve", lambda e: e.tensor_scalar(gm1[:], modS[:, 16:32, :], 1.0, None, ALU.add), reads=[rmod], writes=[rmod])
                P.emit("dve", lambda e: e.tensor_tensor(gm1[:], gm1[:], g1s[:].unsqueeze(2).broadcast_to([128, 16, 3]), ALU.mult), reads=[rmod, rs], writes=[rmod])
                P.emit("dve", lambda e: e.tensor_scalar(gm2[:], modS[:, 64:80, :], 1.0, None, ALU.add), reads=[rmod], writes=[rmod])
                P.emit("dve", lambda e: e.tensor_tensor(gm2[:], gm2[:], g2s[:].unsqueeze(2).broadcast_to([128, 16, 3]), ALU.mult), reads=[rmod, rs], writes=[rmod])
                if dbg:
                    P.dma("sp", DBGM, modS[:], reads=[rmod])
                P.flush()

            def build_gate(st, name, j0, conds, pg=None, rpg=None, gbuf=None):
                G = {}
                dg = gbuf[2] if gbuf else sbt(st, name + "dg", [128, 128], F32)
                if pg is None:
                    pg = pst(st, name + "pg", [128, 512], F32)
                    rpg = R()
                rdg = gbuf[3] if gbuf else R()
                for c in conds:
                    if gbuf:
                        g, rg = gbuf[0], gbuf[1]
                    else:
                        g = sbt(st, name + "G%d" % c, [128, D], F32)
                        rg = R()
                    for j in range(16):
                        P.emit("dve", lambda e, j=j, c=c: e.tensor_scalar(dg[:], identf[:], modS[:, j0 + j, c:c + 1], None, ALU.mult), reads=[rmod, rC], writes=[rdg])
                        P.emit("pe", lambda e, j=j: e.matmul(pg[:, (j % 4) * 128:(j % 4 + 1) * 128], onesf[:], dg[:], start=True, stop=True), reads=[rdg, rC], writes=[rpg])
                        if j % 4 == 3:
                            P.emit("act", lambda e, j=j, g=g: e.activation(g[:, (j - 3) * 128:(j + 1) * 128], pg[:, 0:512], AF.Copy), reads=[rpg], writes=[rg])
                    G[c] = (g, rg)
                return G

            if phases is None or 1 in phases:
              with ExitStack() as st:
                NA = 2592
                W = W_A1
                rW = rW_A1
                xt = [sbt(st, "xt%d" % i, [128, D], F32) for i in range(2)]
                rxt = [R(), R()]
                junk = sbt(st, "junk", [128, D], BF16)
                ss = sbt(st, "ss", [128, 1], F32)
                rstd = sbt(st, "rstd", [128, 1], F32)
                rtmp = R()
                xns = [sbt(st, "xn%d" % i, [128, D], BF16) for i in range(2)]
                rxns = [R(), R()]
                hTs = [sbt(st, "hT%d" % i, [128, 16, 512], BF16) for i in range(2)]
                rhTs = [R(), R()]
                gcnt = 0
                pT = [pst(st, "pT%d" % i, [128, 8, 128], BF16) for i in range(2)]
                rpT = [R(), R()]
                pO = [pst(st, "pO%d" % i, [128, 512], F32) for i in range(4)]
                rpO = [R() for _ in range(4)]
                ob = [sbt(st, "ob%d" % i, [128, 512], BF16) for i in range(4)]
                rob = [R() for _ in range(4)]
                dto = sbt(st, "dto", [128, 32], F32)
                rdto = R()
                cnt = 0
                a1_tiles = [(b, t0 + i) for b in range(2) for (t0, nt) in ((0, 2), (2, 4), (6, 4), (10, 4), (14, 4)) for i in range(nt)]

                def a1_norm(k_):
                    b_, ti_ = a1_tiles[k_]
                    x_, rx = xt[k_ % 2], rxt[k_ % 2]
                    xn_, rxn_ = xns[k_ % 2], rxns[k_ % 2]
                    P.dma("sp", x_[:], resid_src(l, b_, ti_), reads=[rRES[b_ * 18 + ti_]], writes=[rx])
                    rms_rstd("act", x_[:], D, ss[:], rstd[:], junk[:], rx, rtmp)
                    P.emit("act", lambda e: e.activation(xn_[:], x_[:], AF.Copy, scale=rstd[:]), reads=[rx, rtmp], writes=[rxn_])
                for b in range(2):
                    for (t0, nt) in ((0, 2), (2, 4), (6, 4), (10, 4), (14, 4)):
                        c = 2 if t0 == 0 else b
                        n = nt * 128
                        col0 = b * TB + t0 * 128
                        hT, rhT = hTs[gcnt % 2], rhTs[gcnt % 2]
                        gcnt += 1
                        for i in range(nt):
                            ti = t0 + i
                            gi = b * 18 + ti
                            xn, rxn = xns[cnt % 2], rxns[cnt % 2]
                            if cnt == 0:
                                a1_norm(0)
                            if cnt + 1 < len(a1_tiles):
                                a1_norm(cnt + 1)
                            cnt += 1
                            for hh in range(2):
                                def f(e, hh=hh, xn=xn):
                                    for jj in range(8):
                                        j = hh * 8 + jj
                                        ins = e.transpose(pT[hh][:, jj, :], xn[:, j * 128:(j + 1) * 128], identb[:])
                                    return ins
                                P.emit("pe", f, reads=[rxn, rC], writes=[rpT[hh]])
                                for jj in range(8):
                                    j = hh * 8 + jj
                                    if jj % 2 == 0:
                                        P.emit("dve", lambda e, hh=hh, jj=jj, j=j, i=i, c=c, hT=hT: e.tensor_scalar(hT[:, j, i * 128:(i + 1) * 128], pT[hh][:, jj, :], gm1[:, j, c:c + 1], modS[:, j, c:c + 1], ALU.mult, ALU.add), reads=[rpT[hh], rmod], writes=[rhT])
                                    else:
                                        P.emit("act", lambda e, hh=hh, jj=jj, j=j, i=i, c=c, hT=hT: e.activation(hT[:, j, i * 128:(i + 1) * 128], pT[hh][:, jj, :], AF.Identity, scale=gm1[:, j, c:c + 1], bias=modS[:, j, c:c + 1]), reads=[rpT[hh], rmod], writes=[rhT])
                        P.dma("sp", HT[:, col0:col0 + n].rearrange("(j p) t -> p j t", p=128), hT[:, :, 0:n], reads=[rhT], writes=[rHT[b * 18 + t0 + i] for i in range(nt)])
                        k = 0
                        for i in range(nt):
                            ti = t0 + i
                            gi = b * 18 + ti
                            r0 = b * TB + ti * 128
                            for zb in range(2):
                                po, rpo, o_, ro = pO[k % 4], rpO[k % 4], ob[k % 4], rob[k % 4]
                                k += 1

                                def f(e, po=po, zb=zb, i=i, hT=hT):
                                    for j in range(16):
                                        ins = e.matmul(po[:], hT[:, j, i * 128:(i + 1) * 128], W[:, j, zb * 512:(zb + 1) * 512], start=(j == 0), stop=(j == 15))
                                    return ins
                                P.emit("pe", f, reads=[rhT, rW], writes=[rpo])
                                P.emit("act" if k % 2 else "dve", (lambda e, po=po, o_=o_: e.activation(o_[:], po[:], AF.Copy)) if k % 2 else (lambda e, po=po, o_=o_: e.tensor_copy(o_[:], po[:])), reads=[rpo], writes=[ro])
                                P.dma("sp", ZT[r0:r0 + 128, zb * 512:(zb + 1) * 512], o_[:], reads=[ro], writes=[rZT[gi]])
                            po, rpo = pO[k % 4], rpO[k % 4]
                            k += 1

                            def f(e, po=po, i=i, hT=hT):
                                for j in range(16):
                                    ins = e.matmul(po[:, 0:32], hT[:, j, i * 128:(i + 1) * 128], W[:, j, 2560:2592], start=(j == 0), stop=(j == 15))
                                return ins
                            P.emit("pe", f, reads=[rhT, rW], writes=[rpo])
                            P.emit("dve", lambda e, po=po: e.tensor_copy(dto[:], po[:, 0:32]), reads=[rpo], writes=[rdto])
                            P.dma("sp", DTR[r0:r0 + 128, :], dto[:], reads=[rdto], writes=[rDTR[gi]])
                        for m in range(12):
                            po, rpo, o_, ro = pO[k % 4], rpO[k % 4], ob[k % 4], rob[k % 4]
                            k += 1

                            def f(e, po=po, m=m, n=n, hT=hT):
                                for j in range(16):
                                    ins = e.matmul(po[:, 0:n], W[:, j, 1024 + m * 128:1024 + (m + 1) * 128], hT[:, j, 0:n], start=(j == 0), stop=(j == 15))
                                return ins
                            P.emit("pe", f, reads=[rhT, rW], writes=[rpo])
                            P.emit("act" if k % 2 else "dve", (lambda e, po=po, o_=o_, n=n: e.activation(o_[:, 0:n], po[:, 0:n], AF.Copy)) if k % 2 else (lambda e, po=po, o_=o_, n=n: e.tensor_copy(o_[:, 0:n], po[:, 0:n])), reads=[rpo], writes=[ro])
                            P.dma("sp", XBC[m * 128:(m + 1) * 128, col0:col0 + n], o_[:, 0:n], reads=[ro], writes=[rXBC[b]])
                P.flush()

            stA1w.close()
            if phases is None or 2 in phases:
              with ExitStack() as st:
                NB2 = NWIN - 2592
                W = sbt(st, "W2", [128, 16, NB2], BF16)
                rW = R()
                for j in range(16):
                    P.dma("pool", W[:, j, :], win[l, j * 128:(j + 1) * 128, 2592:NWIN], writes=[rW])
                WQ = sbt(st, "WQ", [128, 4, 2048], BF16)
                WKV = sbt(st, "WKV", [128, 2, 2048], BF16)
                for j in range(4):
                    P.dma("pool", WQ[:, j, :], wq[l, j * 128:(j + 1) * 128, :], writes=[rW], max_dma_last_dim=4096)
                for j in range(2):
                    P.dma("pool", WKV[:, j, :], wkv[l, j * 128:(j + 1) * 128, :], writes=[rW], max_dma_last_dim=4096)
                cosT = sbt(st, "cosT", [128, 2048], F32)
                sinT = sbt(st, "sinT", [128, 2048], F32)
                qg = sbt(st, "qg", [128, 4], F32)
                kvg = sbt(st, "kvg", [128, 2], F32)
                P.dma("sp", cosT[:], c_cos, writes=[rW])
                P.dma("sp", sinT[:], c_sin, writes=[rW])
                P.dma("sp", qg[:], qgT[l], writes=[rW])
                P.dma("sp", kvg[:], kvgT[l], writes=[rW])
                hT = [sbt(st, "hT2%d" % i, [128, 16, 512], BF16) for i in range(2)]
                rhT = [R(), R()]
                pO = [pst(st, "pO%d" % i, [128, 512], F32) for i in range(6)]
                rpO = [R() for _ in range(6)]
                pSq = [pst(st, "pSq%d" % i, [128, 512], F32) for i in range(2)]
                rpSq = [R(), R()]

                class NS:
                    pass
                nsets = {}
                for nm, nch in (("q", 4), ("kv", 2)):
                    for i in range(2):
                        s_ = NS()
                        s_.sq = sbt(st, "sq%s%d" % (nm, i), [128, nch, 512], BF16)
                        s_.qa = sbt(st, "qa%s%d" % (nm, i), [128, nch, 512], F32)
                        s_.qab = sbt(st, "qab%s%d" % (nm, i), [128, nch, 512], BF16)
                        s_.rbc = sbt(st, "rbc%s%d" % (nm, i), [128, 512], F32)
                        s_.rsq, s_.rqa, s_.rqab, s_.rrbc = R(), R(), R(), R()
                        nsets[(nm, i)] = s_
                ob = [sbt(st, "ob%d" % i, [128, 512], BF16) for i in range(6)]
                rob = [R() for _ in range(6)]
                ta = [sbt(st, "ta%d" % i, [128, 512], F32) for i in range(2)]
                tb_ = [sbt(st, "tb%d" % i, [128, 512], F32) for i in range(2)]
                rta, rtb = [R(), R()], [R(), R()]
                ov = [sbt(st, "ov%d" % i, [128, 1024], BF16) for i in range(2)]
                rov = [R(), R()]
                kst = [0, 0, 0, 0]

                def group(b, t0, nt, gidx):
                    isx = t0 > 0
                    n = nt * 128
                    col0 = b * TB + t0 * 128
                    p0 = (t0 - 2) * 128
                    h_ = hT[gidx % 2]
                    rh = rhT[gidx % 2]
                    Nq = nsets[("q", gidx % 2)]
                    Nk = nsets[("kv", gidx % 2)]
                    P.dma("sp", h_[:, :, 0:n], HT[:, col0:col0 + n].rearrange("(j p) t -> p j t", p=128), reads=[rHT[b * 18 + t0 + i] for i in range(nt)], writes=[rh])

                    def nps():
                        kst[0] += 1
                        return pO[kst[0] % 6], rpO[kst[0] % 6]

                    def nob():
                        kst[1] += 1
                        return ob[kst[1] % 6], rob[kst[1] % 6]

                    def store(dst, src, rsrc):
                        kst[3] += 1
                        P.dma("sp" if kst[3] % 2 else "act", dst, src, reads=[rsrc], writes=[rQK[b]])

                    def proj(po, c0):
                        def f(e):
                            for j in range(16):
                                ins = e.matmul(po[:, 0:n], W[:, j, c0:c0 + 128], h_[:, j, 0:n], start=(j == 0), stop=(j == 15))
                            return ins
                        return f

                    def stage1(N_, nchunk, c0, gcol):
                        for m in range(nchunk):
                            po, rpo = nps()
                            P.emit("pe", proj(po, c0 + m * 128), reads=[rh, rW], writes=[rpo])
                            P.emit("act", lambda e, po=po, m=m: e.activation(N_.qa[:, m, 0:n], po[:, 0:n], AF.Copy), reads=[rpo], writes=[N_.rqa])
                            P.emit("dve", lambda e, m=m: e.tensor_tensor(N_.sq[:, m, 0:n], N_.qa[:, m, 0:n], N_.qa[:, m, 0:n], ALU.mult), reads=[N_.rqa], writes=[N_.rsq])
                            P.emit("pool", lambda e, m=m: e.tensor_scalar(N_.qa[:, m, 0:n], N_.qa[:, m, 0:n], gcol[:, m:m + 1], 1.0, ALU.mult, ALU.mult), reads=[N_.rqa, rW, N_.rsq], writes=[N_.rqa])

                    def stage2(N_, nchunk, pS, rpS):
                        def f(e):
                            for m in range(nchunk):
                                ins = e.matmul(pS[:, 0:n], onesb[:], N_.sq[:, m, 0:n], start=(m == 0), stop=(m == nchunk - 1))
                            return ins
                        P.emit("pe", f, reads=[N_.rsq, rC], writes=[rpS])
                        P.emit("act", lambda e: e.activation(N_.rbc[:, 0:n], pS[:, 0:n], AF.Sqrt, bias=EPS, scale=1.0 / (nchunk * 128)), reads=[rpS], writes=[N_.rrbc])
                        P.emit("dve", lambda e: e.reciprocal(N_.rbc[:, 0:n], N_.rbc[:, 0:n]), reads=[N_.rrbc], writes=[N_.rrbc])
                        for m in range(nchunk):
                            P.emit("dve", lambda e, m=m: e.tensor_tensor(N_.qab[:, m, 0:n], N_.qa[:, m, 0:n], N_.rbc[:, 0:n], ALU.mult), reads=[N_.rqa, N_.rrbc], writes=[N_.rqab])

                    def rope_combine(pa, rpa, pb, rpb, o_, ro):
                        if not isx:
                            P.emit("act", lambda e: e.activation(o_[:, 0:n], pa[:, 0:n], AF.Copy), reads=[rpa], writes=[ro])
                            return
                        kst[2] += 1
                        ta_, tb2, rta_, rtb_ = ta[kst[2] % 2], tb_[kst[2] % 2], rta[kst[2] % 2], rtb[kst[2] % 2]
                        P.emit("dve", lambda e: e.tensor_tensor(ta_[:, 0:n], pa[:, 0:n], cosT[:, p0:p0 + n], ALU.mult), reads=[rpa, rW], writes=[rta_])
                        P.emit("dve", lambda e: e.tensor_tensor(tb2[:, 0:n], pb[:, 0:n], sinT[:, p0:p0 + n], ALU.mult), reads=[rpb, rW], writes=[rtb_])
                        P.emit("pool", lambda e: e.tensor_tensor(o_[:, 0:n], ta_[:, 0:n], tb2[:, 0:n], ALU.add), reads=[rta_, rtb_], writes=[ro])

                    stage1(Nq, 4, 0, qg)
                    stage1(Nk, 2, 512, kvg)
                    pa, rpa = nps()
                    pb, rpb = nps()
                    o_, ro = nob()
                    P.emit("pe", proj(pa, 768), reads=[rh, rW], writes=[rpa])
                    P.emit("pe", proj(pb, 896), reads=[rh, rW], writes=[rpb])
                    rope_combine(pa, rpa, pb, rpb, o_, ro)
                    store(KR[:, col0:col0 + n], o_[:, 0:n], ro)
                    stage2(Nq, 4, pSq[0], rpSq[0])
                    stage2(Nk, 2, pSq[1], rpSq[1])

                    def qproj(po, mcol):
                        def f(e):
                            for j in range(4):
                                ins = e.matmul(po[:, 0:n], WQ[:, j, mcol:mcol + 128], Nq.qab[:, j, 0:n], start=(j == 0), stop=(j == 3))
                            return ins
                        return f
                    for h in range(8):
                        po, rpo = nps()
                        o_, ro = nob()
                        P.emit("pe", qproj(po, h * 128), reads=[Nq.rqab, rW], writes=[rpo])
                        P.emit("act", lambda e, po=po, o_=o_: e.activation(o_[:, 0:n], po[:, 0:n], AF.Copy), reads=[rpo], writes=[ro])
                        store(QN[h, :, col0:col0 + n], o_[:, 0:n], ro)
                    for h in range(8):
                        po, rpo = nps()
                        o_, ro = nob()

                        def f(e, po=po, h=h):
                            for j in range(2):
                                ins = e.matmul(po[:, 0:n], WKV[:, j, h * 128:(h + 1) * 128], Nk.qab[:, j, 0:n], start=(j == 0), stop=(j == 1))
                            return ins
                        P.emit("pe", f, reads=[Nk.rqab, rW], writes=[rpo])
                        P.emit("dve", lambda e, po=po, o_=o_: e.tensor_copy(o_[:, 0:n], po[:, 0:n]), reads=[rpo], writes=[ro])
                        store(KN[h, :, col0:col0 + n], o_[:, 0:n], ro)
                    for pr in range(4):
                        pa, rpa = nps()
                        pb, rpb = nps()
                        o_, ro = nob()
                        P.emit("pe", qproj(pa, 1024 + pr * 128), reads=[Nq.rqab, rW], writes=[rpa])
                        P.emit("pe", qproj(pb, 1536 + pr * 128), reads=[Nq.rqab, rW], writes=[rpb])
                        rope_combine(pa, rpa, pb, rpb, o_, ro)
                        store(QR[pr, :, col0:col0 + n], o_[:, 0:n], ro)
                    for i in range(nt):
                        r0 = col0 + i * 128
                        ov_, rov_ = ov[i % 2], rov[i % 2]
                        for vb in range(2):
                            po, rpo = nps()

                            def f(e, po=po, vb=vb, i=i):
                                for j in range(2):
                                    ins = e.matmul(po[:], Nk.qab[:, j, i * 128:(i + 1) * 128], WKV[:, j, 1024 + vb * 512:1024 + (vb + 1) * 512], start=(j == 0), stop=(j == 1))
                                return ins
                            P.emit("pe", f, reads=[Nk.rqab, rW], writes=[rpo])
                            P.emit("act", lambda e, po=po, ov_=ov_, vb=vb: e.activation(ov_[:, vb * 512:(vb + 1) * 512], po[:], AF.Copy), reads=[rpo], writes=[rov_])
                        store(VV[r0:r0 + 128, :], ov_[:], rov_)

                gidx = 0
                for b in range(2):
                    for (t0, nt) in ((0, 2), (2, 4), (6, 4), (10, 4), (14, 4)):
                        group(b, t0, nt, gidx)
                        gidx += 1
                P.flush()

            if phases is None or 3 in phases:
              with ExitStack() as st:
                cw = sbt(st, "cw", [128, 36], F32)
                cb = sbt(st, "cb", [128, 12], F32)
                dtbs = sbt(st, "dtbs", [128, 32], F32)
                abc = sbt(st, "abc", [128, 32], F32)
                dsk = sbt(st, "dsk", [128, 16], F32)
                tri = [sbt(st, "tri%d" % d, [128, 128], F32) for d in range(2)]
                msk = [sbt(st, "msk%d" % d, [128, 512], F32) for d in range(2)]
                rK = R()
                P.dma("sp", cw[:], convw[l], writes=[rK])
                P.dma("sp", cb[:], convb[l], writes=[rK])
                P.dma("sp", dtbs[:], dtb[l], writes=[rK])
                P.dma("sp", abc[:], alog[l], writes=[rK])
                P.dma("sp", dsk[:], dskip[l], writes=[rK])
                for d in range(2):
                    P.dma("sp", tri[d][:], c_tri[d], writes=[rK])
                    P.dma("sp", msk[d][:], c_mask[d], writes=[rK])
                P.emit("act", lambda e: e.activation(abc[:], abc[:], AF.Exp), reads=[rK], writes=[rK])
                P.emit("dve", lambda e: e.tensor_scalar(abc[:], abc[:], -1.0, None, ALU.mult), reads=[rK], writes=[rK])
                XC = sbt(st, "XC", [128, 12, TB], BF16)
                rXC = R()
                u = [sbt(st, "u%d" % i, [128, 2048], BF16) for i in range(2)]
                ru = [R(), R()]
                acc = [sbt(st, "acc%d" % i, [128, 2048], F32) for i in range(2)]
                racc = [R(), R()]
                zs = sbt(st, "zsB", [128, 1024], F32)
                rzs = R()

                class BS:
                    pass
                sets = []
                for d in range(2):
                    s_ = BS()
                    for nm, shp, dt_t in (("S", [128, 1024], F32), ("Sb", [128, 1024], BF16), ("XS", [128, 1024], BF16), ("BT", [128, 256], BF16),
                                          ("dtr", [128, 32], F32), ("dt_", [128, 32], F32), ("dtA", [128, 32], F32),
                                          ("xdt", [128, 16, 64], BF16), ("xdd", [128, 16, 64], BF16),
                                          ("nac", [128, 16], F32), ("expA", [128, 16], F32), ("dec", [128, 16], F32), ("cd", [128, 16], F32),
                                          ("Rt", [128, 8, 128], F32), ("LM", [128, 16, 128], BF16), ("CBs", [128, 2, 128], BF16),
                                          ("MT", [128, 16, 128], BF16), ("yo", [128, 16, 64], F32), ("y", [128, 1024], F32)):
                        setattr(s_, nm, sbt(st, "%s_d%d" % (nm, d), shp, dt_t))
                    for nm in ("rS", "rSb", "rXS", "rBT", "rdtr", "rdt", "rxdt", "rxdd", "rsm", "rRt", "rLM", "rCBs", "rMT", "ryo", "ry"):
                        setattr(s_, nm, R())
                    sets.append(s_)
                dtr_all = sbt(st, "dtr_all", [128, 18, 32], F32)
                dt_all = sbt(st, "dt_all", [128, 18, 32], F32)
                dtA_all = sbt(st, "dtA_all", [128, 18, 32], F32)
                nac_all = sbt(st, "nac_all", [128, 18, 32], F32)
                expA_all = sbt(st, "expA_all", [128, 18, 32], F32)
                cd_all = sbt(st, "cd_all", [128, 18, 32], F32)
                dec_all = sbt(st, "dec_all", [128, 18, 32], F32)
                rpre = R()
                bT = pst(st, "bT", [128, 8, 128], BF16)
                rbT = R()
                bA = pst(st, "bA", [128, 512], F32)
                rbA = R()
                pSs = pst(st, "pSs", [128, 1024], F32)
                rpSs = R()
                pY = pst(st, "pY", [128, 1024], F32)
                rpY = R()
                pL = pst(st, "pL", [128, 8, 128], F32)
                rpL = R()

                def chunk_pass(b, d, ti, B_):
                    base = b * TB
                    gi = b * 18 + ti
                    r0 = base + ti * 128
                    cs = slice(ti * 128, (ti + 1) * 128)
                    do_y = not (last and ti < 2)
                    dsl = slice(d * 16, (d + 1) * 16)

                    def f(e):
                        for j in range(8):
                            ins = e.transpose(bT[:, j, :], XC[:, j, cs], identb[:])
                        return ins
                    P.emit("pe", f, reads=[rXC, rC], writes=[rbT])
                    P.emit("act", lambda e: e.activation(B_.XS[:], bT[:].rearrange("p a b -> p (a b)"), AF.Copy), reads=[rbT], writes=[B_.rXS])

                    def f(e):
                        for j in range(2):
                            ins = e.transpose(bT[:, j, :], XC[:, 8 + j, cs], identb[:])
                        return ins
                    P.emit("pe", f, reads=[rXC, rC], writes=[rbT])
                    P.emit("act", lambda e: e.activation(B_.BT[:], bT[:, 0:2, :].rearrange("p a b -> p (a b)"), AF.Copy), reads=[rbT], writes=[B_.rBT])
                    P.emit("dve", lambda e: e.tensor_tensor(B_.xdt[:], B_.XS[:].rearrange("p (h q) -> p h q", h=16), dt_all[:, ti, dsl].unsqueeze(2).broadcast_to([128, 16, 64]), ALU.mult), reads=[B_.rXS, rpre], writes=[B_.rxdt])
                    yield

                    P.emit("dve", lambda e: e.tensor_tensor(B_.xdd[:], B_.xdt[:], dec_all[:, ti, dsl].unsqueeze(2).broadcast_to([128, 16, 64]), ALU.mult), reads=[B_.rxdt, rpre], writes=[B_.rxdd])
                    yield

                    def f(e):
                        for g in range(2):
                            ins = e.matmul(pSs[:, g * 512:(g + 1) * 512], B_.BT[:, g * 128:(g + 1) * 128], B_.xdd[:, g * 8:(g + 1) * 8, :].rearrange("p h q -> p (h q)"), start=True, stop=True)
                        return ins
                    P.emit("pe", f, reads=[B_.rBT, B_.rxdd], writes=[rpSs])
                    if do_y:
                        P.emit("act", lambda e: e.activation(B_.Sb[:], B_.S[:], AF.Copy), reads=[B_.rS], writes=[B_.rSb])

                        def f(e):
                            for g in range(2):
                                ins = e.matmul(pY[:, g * 512:(g + 1) * 512], XC[:, 10 + g, cs], B_.Sb[:, g * 512:(g + 1) * 512], start=True, stop=True)
                            return ins
                        P.emit("pe", f, reads=[rXC, B_.rSb], writes=[rpY])
                        P.emit("dve", lambda e: e.tensor_tensor(B_.yo[:], pY[:].rearrange("p (h q) -> p h q", h=16), expA_all[:, ti, dsl].unsqueeze(2).broadcast_to([128, 16, 64]), ALU.mult), reads=[rpY, rpre], writes=[B_.ryo])
                    P.emit("dve", lambda e: e.tensor_tensor(B_.S[:].rearrange("p (h q) -> p h q", h=16), B_.S[:].rearrange("p (h q) -> p h q", h=16), cd_all[:, ti, dsl].unsqueeze(2).broadcast_to([128, 16, 64]), ALU.mult), reads=[B_.rS, rpre, B_.rSb], writes=[B_.rS])
                    P.emit("dve", lambda e: e.tensor_tensor(B_.S[:], B_.S[:], pSs[:], ALU.add), reads=[B_.rS, rpSs], writes=[B_.rS])
                    yield
                    if not do_y:
                        return

                    def f(e):
                        for g in range(2):
                            ins = e.matmul(bA[:, 256 + g * 128:256 + (g + 1) * 128], XC[:, 8 + g, cs], XC[:, 10 + g, cs], start=True, stop=True)
                        return ins
                    P.emit("pe", f, reads=[rXC], writes=[rbA])
                    P.emit("act", lambda e: e.activation(B_.CBs[:].rearrange("p a b -> p (a b)"), bA[:, 256:512], AF.Copy), reads=[rbA], writes=[B_.rCBs])
                    yield
                    for hf in range(2):
                        P.emit("pool", lambda e, hf=hf: e.tensor_tensor(B_.Rt[:], tri[d][:].unsqueeze(1).broadcast_to([128, 8, 128]), dtA_all[:, ti, d * 16 + hf * 8:d * 16 + hf * 8 + 8].unsqueeze(2).broadcast_to([128, 8, 128]), ALU.mult), reads=[rK, rpre], writes=[B_.rRt])

                        def f(e):
                            for kb in range(2):
                                e.matmul(pL[:, kb * 4:(kb + 1) * 4, :].rearrange("p a b -> p (a b)"), onesf[:], B_.Rt[:, kb * 4:(kb + 1) * 4, :].rearrange("p a b -> p (a b)"), start=True, stop=False)
                                ins = e.matmul(pL[:, kb * 4:(kb + 1) * 4, :].rearrange("p a b -> p (a b)"), identf[:], msk[d][:], start=False, stop=True)
                            return ins
                        P.emit("pe", f, reads=[B_.rRt, rK, rC], writes=[rpL])
                        for hh in range(8):
                            h = hf * 8 + hh
                            P.emit("act", lambda e, h=h, hh=hh: e.activation(B_.LM[:, h, :], pL[:, hh, :], AF.Exp, bias=nac_all[:, ti, d * 16 + h:d * 16 + h + 1]), reads=[rpL, rpre], writes=[B_.rLM])
                        yield
                    P.emit("dve", lambda e: e.tensor_tensor(B_.MT[:].rearrange("p (g h) i -> p g h i", g=2), B_.LM[:].rearrange("p (g h) i -> p g h i", g=2), B_.CBs[:].unsqueeze(2).broadcast_to([128, 2, 8, 128]), ALU.mult), reads=[B_.rLM, B_.rCBs], writes=[B_.rMT])

                    def f(e):
                        for h in range(16):
                            ins = e.matmul(pY[:, h * 64:(h + 1) * 64], B_.MT[:, h, :], B_.xdt[:, h, :], start=True, stop=True)
                        return ins
                    P.emit("pe", f, reads=[B_.rMT, B_.rxdt, B_.ryo], writes=[rpY])
                    P.emit("dve", lambda e: e.tensor_tensor(B_.y[:], B_.yo[:].rearrange("p h q -> p (h q)"), pY[:], ALU.add), reads=[B_.ryo, rpY], writes=[B_.ry])
                    if d == 0:
                        P.emit("pool", lambda e: e.tensor_tensor(zs[:].rearrange("p (h q) -> p h q", h=16), B_.XS[:].rearrange("p (h q) -> p h q", h=16), dsk[:].unsqueeze(2).broadcast_to([128, 16, 64]), ALU.mult), reads=[B_.rXS, rK], writes=[rzs])
                        P.emit("dve", lambda e: e.tensor_tensor(B_.y[:], B_.y[:], zs[:], ALU.add), reads=[B_.ry, rzs], writes=[B_.ry])
                        P.dma("sp", YF[r0:r0 + 128, :], B_.y[:], reads=[B_.ry], writes=[rYF[gi]])
                    else:
                        P.dma("sp", YB[r0:r0 + 128, :], B_.y[:], reads=[B_.ry], writes=[rYB[gi]])

                for b in range(2):
                    base = b * TB
                    cc_ = 0
                    for j in range(12):
                        for (s0, Ls) in ((0, 256), (256, 2048)):
                            u_, ru_, a_, ra_ = u[cc_ % 2], ru[cc_ % 2], acc[cc_ % 2], racc[cc_ % 2]
                            cc_ += 1
                            P.dma("sp", u_[:, 0:Ls], XBC[j * 128:(j + 1) * 128, base + s0:base + s0 + Ls], reads=[rXBC[b]], writes=[ru_])
                            P.emit("dve", lambda e, j=j, Ls=Ls, u_=u_, a_=a_: e.tensor_scalar(a_[:, 0:Ls], u_[:, 0:Ls], cw[:, j * 3 + 1:j * 3 + 2], None, ALU.mult), reads=[ru_, rK], writes=[ra_])
                            P.emit("dve", lambda e, j=j, Ls=Ls, u_=u_, a_=a_: e.scalar_tensor_tensor(a_[:, 1:Ls], u_[:, 0:Ls - 1], cw[:, j * 3:j * 3 + 1], a_[:, 1:Ls], ALU.mult, ALU.add), reads=[ru_, rK, ra_], writes=[ra_])
                            P.emit("dve", lambda e, j=j, Ls=Ls, u_=u_, a_=a_: e.scalar_tensor_tensor(a_[:, 0:Ls - 1], u_[:, 1:Ls], cw[:, j * 3 + 2:j * 3 + 3], a_[:, 0:Ls - 1], ALU.mult, ALU.add), reads=[ru_, rK, ra_], writes=[ra_])
                            P.emit("act", lambda e, j=j, Ls=Ls, s0=s0, a_=a_: e.activation(XC[:, j, s0:s0 + Ls], a_[:, 0:Ls], AF.Silu, bias=cb[:, j:j + 1]), reads=[ra_, rK], writes=[rXC])
                    P.dma("act", dtr_all[:], DTR[base:base + TB, :].rearrange("(c p) f -> p c f", p=128), reads=[rDTR[b * 18 + i] for i in range(18)], writes=[rpre])
                    P.emit("dve", lambda e: e.tensor_tensor(dt_all[:], dtr_all[:], dtbs[:].unsqueeze(1).broadcast_to([128, 18, 32]), ALU.add), reads=[rpre, rK], writes=[rpre])
                    P.emit("act", lambda e: e.activation(dt_all[:], dt_all[:], AF.Exp), reads=[rpre], writes=[rpre])
                    P.emit("act", lambda e: e.activation(dt_all[:], dt_all[:], AF.Ln, bias=1.0), reads=[rpre], writes=[rpre])
                    P.emit("dve", lambda e: e.tensor_tensor(dtA_all[:], dt_all[:], abc[:].unsqueeze(1).broadcast_to([128, 18, 32]), ALU.mult), reads=[rpre, rK], writes=[rpre])

                    def f(e):
                        for d in range(2):
                            rhs = dtA_all[:, :, d * 16:(d + 1) * 16]
                            e.matmul(pSs[:, d * 512:d * 512 + 288].rearrange("p (c h) -> p c h", c=18), tri[d][:], rhs, start=True, stop=True)
                            ins = e.matmul(pY[:, d * 512:d * 512 + 288].rearrange("p (c h) -> p c h", c=18), onesf[:], rhs, start=True, stop=True)
                        return ins
                    P.emit("pe", f, reads=[rpre, rK, rC], writes=[rpSs, rpY])
                    for d in range(2):
                        cum = pSs[:, d * 512:d * 512 + 288].rearrange("p (c h) -> p c h", c=18)
                        tot = pY[:, d * 512:d * 512 + 288].rearrange("p (c h) -> p c h", c=18)
                        sl_ = slice(d * 16, (d + 1) * 16)
                        P.emit("dve", lambda e, cum=cum, sl_=sl_: e.tensor_scalar(nac_all[:, :, sl_], cum, -1.0, None, ALU.mult), reads=[rpSs], writes=[rpre])
                        P.emit("act", lambda e, cum=cum, sl_=sl_: e.activation(expA_all[:, :, sl_], cum, AF.Exp), reads=[rpSs], writes=[rpre])
                        P.emit("act", lambda e, tot=tot, sl_=sl_: e.activation(cd_all[:, :, sl_], tot, AF.Exp), reads=[rpY], writes=[rpre])
                        P.emit("dve", lambda e, tot=tot, sl_=sl_: e.tensor_tensor(dec_all[:, :, sl_], tot, nac_all[:, :, sl_], ALU.add), reads=[rpY, rpre], writes=[rpre])
                    P.emit("act", lambda e: e.activation(dec_all[:], dec_all[:], AF.Exp), reads=[rpre], writes=[rpre])
                    orders = [list(range(18)), [1, 0] + list(range(17, 1, -1))]
                    for d in range(2):
                        P.emit("dve", lambda e, d=d: e.memset(sets[d].S[:], 0.0), writes=[sets[d].rS])
                    for step in range(18):
                        gens = [chunk_pass(b, d, orders[d][step], sets[d]) for d in range(2)]
                        while gens:
                            for g_ in list(gens):
                                try:
                                    next(g_)
                                except StopIteration:
                                    gens.remove(g_)
                P.flush()
              with ExitStack() as st:
                sg_ = sbt(st, "ssdgs", [128, 1024], F32)
                rK = R()
                P.dma("sp", sg_[:], ssdg[l], writes=[rK])

                class MS:
                    pass
                ms = []
                for i in range(2):
                    m_ = MS()
                    for nm, shp, dt_t in (("yf", [128, 1024], F32), ("yb", [128, 1024], F32), ("zt", [128, 1024], BF16), ("zs", [128, 1024], F32),
                                          ("y16", [128, 1024], BF16), ("yT", [128, 8, 128], BF16), ("junk", [128, 1024], BF16),
                                          ("ss", [128, 1], F32), ("rstd", [128, 1], F32)):
                        setattr(m_, nm, sbt(st, "%s_m%d" % (nm, i), shp, dt_t))
                    m_.bT = pst(st, "bTm%d" % i, [128, 8, 128], BF16)
                    for nm in ("ryf", "ryb", "rzt", "rzs", "ry16", "ryT", "rtmp", "rbT"):
                        setattr(m_, nm, R())
                    ms.append(m_)
                for n_, (b, ti) in enumerate(tiles_l):
                    M_ = ms[n_ % 2]
                    gi = b * 18 + ti
                    r0 = b * TB + ti * 128

                    def mrg(M_=M_, gi=gi, r0=r0):
                        P.dma("act", M_.yf[:], YF[r0:r0 + 128, :], reads=[rYF[gi]], writes=[M_.ryf])
                        P.dma("act", M_.yb[:], YB[r0:r0 + 128, :], reads=[rYB[gi]], writes=[M_.ryb])
                        P.dma("act", M_.zt[:], ZT[r0:r0 + 128, :], reads=[rZT[gi]], writes=[M_.rzt])
                        P.emit("dve", lambda e: e.tensor_tensor(M_.yf[:], M_.yf[:], M_.yb[:], ALU.add), reads=[M_.ryf, M_.ryb], writes=[M_.ryf])
                        P.emit("act", lambda e: e.activation(M_.zs[:], M_.zt[:], AF.Silu), reads=[M_.rzt], writes=[M_.rzs])
                        P.emit("dve", lambda e: e.tensor_tensor(M_.yf[:], M_.yf[:], M_.zs[:], ALU.mult), reads=[M_.ryf, M_.rzs], writes=[M_.ryf])
                        rms_rstd("act", M_.yf[:], 1024, M_.ss[:], M_.rstd[:], M_.junk[:], M_.ryf, M_.rtmp)
                        P.emit("act", lambda e: e.activation(M_.yf[:], M_.yf[:], AF.Copy, scale=M_.rstd[:]), reads=[M_.ryf, M_.rtmp], writes=[M_.ryf])
                        P.emit("dve", lambda e: e.tensor_tensor(M_.y16[:], M_.yf[:], sg_[:], ALU.mult), reads=[M_.ryf, rK], writes=[M_.ry16])

                        def f(e):
                            for j in range(8):
                                ins = e.transpose(M_.bT[:, j, :], M_.y16[:, j * 128:(j + 1) * 128], identb[:])
                            return ins
                        P.emit("pe", f, reads=[M_.ry16, rC], writes=[M_.rbT])
                        P.emit("act", lambda e: e.activation(M_.yT[:], M_.bT[:], AF.Copy), reads=[M_.rbT], writes=[M_.ryT])
                        P.dma("sp", MIX[0:1024, r0:r0 + 128].rearrange("(j p) t -> p j t", p=128), M_.yT[:], reads=[M_.ryT], writes=[rMIX[gi]])
                    mrg()
                P.flush()

            stWO = ExitStack()
            WO_pre = sbt(stWO, "WO", [128, 16, D], BF16)
            rWO_pre = R()
            for j in range(16):
                P.dma("pool", WO_pre[:, j, :], wo[l, j * 128:(j + 1) * 128, :], writes=[rWO_pre], max_dma_last_dim=4096)
            if phases is None or 4 in phases:
              with ExitStack() as st:
                KNs = sbt(st, "KNs", [128, 8, TB], BF16)
                KRs = [sbt(st, "KRs%d" % i, [128, TB], BF16) for i in range(2)]
                Vs = sbt(st, "Vs", [128, 18, 1024], BF16)
                rKV = R()
                QNs = [sbt(st, "QNs%d" % i, [128, 8, 512], BF16) for i in range(2)]
                QRs = [sbt(st, "QRs%d" % i, [128, 4, 512], BF16) for i in range(2)]
                rQ = [R(), R()]
                pS = [pst(st, "pSc%d" % i, [128, 512], F32) for i in range(4)]
                rpS = [R() for _ in range(4)]
                pO = [pst(st, "pOc%d" % i, [128, 512], F32) for i in range(2)]
                rpO = [R(), R()]
                pL = [pst(st, "pLc%d" % i, [128, 512], F32) for i in range(2)]
                rpL = [R(), R()]
                PT = [sbt(st, "PTc%d" % i, [128, 512], BF16) for i in range(6)]
                rPT = [R() for _ in range(6)]
                rl = [sbt(st, "rlc%d" % i, [128, 512], F32) for i in range(2)]
                rrl = [R(), R()]
                ot = [sbt(st, "otc%d" % i, [128, 512], BF16) for i in range(2)]
                rot = [R(), R()]
                cnt = [0, 0]

                def block_head(b, Qn, Qr, rq, h, nq, nkt, c0, gis):
                    u = cnt[1]
                    cnt[1] += 1
                    po, rpo, pl, rpl = pO[u % 2], rpO[u % 2], pL[u % 2], rpL[u % 2]
                    tiles = []

                    def score(kt):
                        i = cnt[0]
                        cnt[0] += 1
                        ps, rps = pS[i % 4], rpS[i % 4]
                        pt, rpt = PT[i % 6], rPT[i % 6]

                        def f(e):
                            e.matmul(ps[:, 0:nq], KNs[:, h, kt * 128:(kt + 1) * 128], Qn[:, h, 0:nq], start=True, stop=False)
                            return e.matmul(ps[:, 0:nq], KRs[h % 2][:, kt * 128:(kt + 1) * 128], Qr[:, h // 2, 0:nq], start=False, stop=True)
                        P.emit("pe", f, reads=[rq, rKV], writes=[rps])
                        P.emit("act", lambda e: e.activation(pt[:, 0:nq], ps[:, 0:nq], AF.Exp, scale=ATTN_SCALE), reads=[rps], writes=[rpt])
                        tiles.append((kt, pt, rpt))

                    def pv(idx):
                        kt, pt, rpt = tiles[idx]

                        def f(e):
                            e.matmul(po[:, 0:nq], Vs[:, kt, h * 128:(h + 1) * 128], pt[:, 0:nq], start=(idx == 0), stop=(idx == nkt - 1))
                            return e.matmul(pl[:, 0:nq], onesb[:], pt[:, 0:nq], start=(idx == 0), stop=(idx == nkt - 1))
                        P.emit("pe", f, reads=[rpt, rKV, rC], writes=[rpo, rpl])
                    DEPTH = 2
                    for kt in range(nkt):
                        score(kt)
                        if kt >= DEPTH:
                            pv(kt - DEPTH)
                    for idx in range(max(0, nkt - DEPTH), nkt):
                        pv(idx)
                    r_, rr_, o_, ro_ = rl[u % 2], rrl[u % 2], ot[u % 2], rot[u % 2]
                    P.emit("dve", lambda e: e.reciprocal(r_[:, 0:nq], pl[:, 0:nq]), reads=[rpl], writes=[rr_])
                    P.emit("dve", lambda e: e.tensor_tensor(o_[:, 0:nq], po[:, 0:nq], r_[:, 0:nq], ALU.mult), reads=[rpo, rr_], writes=[ro_])
                    P.dma("sp", MIX[1024 + h * 128:1024 + (h + 1) * 128, c0:c0 + nq], o_[:, 0:nq], reads=[ro_], writes=[rMIX[g] for g in gis])

                for b in range(2):
                    base = b * TB
                    P.dma("sp", KNs[:], KN[:, :, base:base + TB].rearrange("h p t -> p h t"), reads=[rQK[b]], writes=[rKV])
                    for i_ in range(2):
                        P.emit("dve", lambda e, i_=i_: e.memset(KRs[i_][:], 0.0), writes=[rKV])
                        P.dma("act", KRs[i_][i_ * 64:(i_ + 1) * 64, :], KR[i_ * 64:(i_ + 1) * 64, base:base + TB], reads=[rQK[b]], writes=[rKV])
                    for kt in range(18):
                        P.dma("sp" if kt % 2 else "act", Vs[:, kt, :], VV[base + kt * 128:base + (kt + 1) * 128, :], reads=[rQK[b]], writes=[rKV])
                    blocks = [(2, 4), (6, 4), (10, 4), (14, 4)]
                    if not last:
                        blocks = [(0, 2)] + blocks
                    for bi, (t0, nt) in enumerate(blocks):
                        nq = nt * 128
                        nkt = 2 if t0 == 0 else 18
                        c0 = base + t0 * 128
                        Qn, Qr, rq = QNs[bi % 2], QRs[bi % 2], rQ[bi % 2]
                        P.dma("act", Qn[:, :, 0:nq], QN[:, :, c0:c0 + nq].rearrange("h p t -> p h t"), reads=[rQK[b]], writes=[rq])
                        P.dma("act", Qr[:, :, 0:nq], QR[:, :, c0:c0 + nq].rearrange("h p t -> p h t"), reads=[rQK[b]], writes=[rq])
                        gis = [b * 18 + t0 + i for i in range(nt)]
                        for h in range(8):
                            block_head(b, Qn, Qr, rq, h, nq, nkt, c0, gis)
                P.flush()

            if phases is None or 5 in phases:
              with ExitStack() as st:
                WO = WO_pre
                rW = rWO_pre
                RW = sbt(st, "RW", [128, 16, 16], F32)
                rbs = sbt(st, "rbs", [128, 16], F32)
                P.dma("sp", RW[:], rwT, writes=[rW])
                P.dma("sp", rbs[:], rb, writes=[rW])
                G1 = build_gate(st, "g1", 32, [0, 1] if last else [0, 1, 2])
                pO = pst(st, "pOd", [128, D], F32)
                rpO = R()
                pT = [pst(st, "pTd%d" % i, [128, 4, 128], F32) for i in range(2)]
                rpT = [R(), R()]
                pR = pst(st, "pRd", [128, 16], F32)
                rpR = R()

                class DS:
                    pass
                dsets = []
                for i in range(2):
                    s_ = DS()
                    for nm, shp, dt_t in (("mixT", [128, 16, 128], BF16), ("xt", [128, D], F32), ("tt", [128, D], F32), ("xs", [128, D], F32),
                                          ("junk", [128, D], BF16), ("ss", [128, 1], F32), ("rstd", [128, 1], F32),
                                          ("h2f", [128, 16, 128], F32), ("h2b", [128, 16, 128], BF16),
                                          ("sc", [128, 16], F32), ("sel", [128, 16], F32), ("pr6", [128, 4, 6], F32), ("gs", [128, 4], F32),
                                          ("gmx", [128, 1], F32), ("gmk", [128, 4], F32), ("mk", [128, 16], F32), ("m1", [128, 16], F32),
                                          ("m2", [128, 16], F32), ("t1", [128, 1], F32), ("cmb", [128, 16], F32)):
                        setattr(s_, nm, sbt(st, "%s_D%d" % (nm, i), shp, dt_t))
                    for nm in ("rmixT", "rxt", "rtt", "rxs", "rtmp", "rh2f", "rh2b", "rr"):
                        setattr(s_, nm, R())
                    dsets.append(s_)

                def dtile(S_, b, ti):
                    gi = b * 18 + ti
                    r0 = b * TB + ti * 128
                    c = 2 if ti < 2 else b
                    g1, rg1 = G1[c]
                    P.dma("act", S_.mixT[:], MIX[:, r0:r0 + 128].rearrange("(j p) t -> p j t", p=128), reads=[rMIX[gi]], writes=[S_.rmixT])
                    P.dma("act", S_.xt[:], resid_src(l, b, ti), reads=[rRES[gi]], writes=[S_.rxt])

                    def f(e):
                        for nb in range(4):
                            for j in range(16):
                                ins = e.matmul(pO[:, nb * 512:(nb + 1) * 512], S_.mixT[:, j, :], WO[:, j, nb * 512:(nb + 1) * 512], start=(j == 0), stop=(j == 15))
                        return ins
                    P.emit("pe", f, reads=[S_.rmixT, rW], writes=[rpO])

                def dtile1b(S_, b, ti):
                    gi = b * 18 + ti
                    r0 = b * TB + ti * 128
                    c = 2 if ti < 2 else b
                    g1, rg1 = G1[c]
                    P.emit("dve", lambda e: e.tensor_tensor(S_.tt[:], pO[:], g1[:], ALU.mult), reads=[rpO, rg1], writes=[S_.rtt])
                    P.emit("dve", lambda e: e.tensor_tensor(S_.xt[:], S_.xt[:], S_.tt[:], ALU.add), reads=[S_.rxt, S_.rtt], writes=[S_.rxt])
                    P.dma("sp", RES[r0:r0 + 128, :], S_.xt[:], reads=[S_.rxt], writes=[rRES[gi]])
                    rms_rstd("act", S_.xt[:], D, S_.ss[:], S_.rstd[:], S_.junk[:], S_.rxt, S_.rtmp)
                    P.emit("act", lambda e: e.activation(S_.xs[:], S_.xt[:], AF.Copy, scale=S_.rstd[:]), reads=[S_.rxt, S_.rtmp], writes=[S_.rxs])

                def dtile2(S_, b, ti):
                    gi = b * 18 + ti
                    r0 = b * TB + ti * 128
                    c = 2 if ti < 2 else b
                    for r4 in range(4):
                        p_, rp_ = pT[r4 % 2], rpT[r4 % 2]

                        def f(e, r4=r4, p_=p_):
                            for jj in range(4):
                                j = r4 * 4 + jj
                                ins = e.transpose(p_[:, jj, :], S_.xs[:, j * 128:(j + 1) * 128], identf[:])
                            return ins
                        P.emit("pe", f, reads=[S_.rxs, rC], writes=[rp_])
                        for jj in range(4):
                            j = r4 * 4 + jj
                            if jj % 2 == 0:
                                P.emit("dve", lambda e, p_=p_, jj=jj, j=j: e.tensor_scalar(S_.h2f[:, j, :], p_[:, jj, :], gm2[:, j, c:c + 1], modS[:, 48 + j, c:c + 1], ALU.mult, ALU.add), reads=[rp_, rmod], writes=[S_.rh2f])
                            else:
                                P.emit("act", lambda e, p_=p_, jj=jj, j=j: e.activation(S_.h2f[:, j, :], p_[:, jj, :], AF.Identity, scale=gm2[:, j, c:c + 1], bias=modS[:, 48 + j, c:c + 1]), reads=[rp_, rmod], writes=[S_.rh2f])
                    P.emit("dve", lambda e: e.tensor_copy(S_.h2b[:], S_.h2f[:]), reads=[S_.rh2f], writes=[S_.rh2b])
                    P.dma("sp", HT[:, r0:r0 + 128].rearrange("(j p) t -> p j t", p=128), S_.h2b[:], reads=[S_.rh2b], writes=[rHT[gi]])

                    def f(e):
                        for j in range(16):
                            ins = e.matmul(pR[:], S_.h2f[:, j, :], RW[:, j, :], start=(j == 0), stop=(j == 15))
                        return ins
                    P.emit("pe", f, reads=[S_.rh2f, rW], writes=[rpR])

                def dtile2b(S_, b, ti):
                    gi = b * 18 + ti
                    r0 = b * TB + ti * 128
                    P.emit("act", lambda e: e.activation(S_.sc[:], pR[:], AF.Sigmoid), reads=[rpR], writes=[S_.rr])
                    V = lambda fn, rd=(): P.emit("dve", fn, reads=[S_.rr] + list(rd), writes=[S_.rr])
                    sc, sel, pr6, gs, gmx, gmk, mk, m1, m2, t1, cmb = S_.sc, S_.sel, S_.pr6, S_.gs, S_.gmx, S_.gmk, S_.mk, S_.m1, S_.m2, S_.t1, S_.cmb
                    V(lambda e: e.tensor_tensor(sel[:], sc[:], rbs[:], ALU.add), [rW])
                    s4 = sel[:].rearrange("p (g k) -> p g k", g=4)
                    V(lambda e: e.tensor_tensor(pr6[:, :, 0:3], s4[:, :, 0:3], s4[:, :, 1:4], ALU.add))
                    V(lambda e: e.tensor_tensor(pr6[:, :, 3:5], s4[:, :, 0:2], s4[:, :, 2:4], ALU.add))
                    V(lambda e: e.tensor_tensor(pr6[:, :, 5:6], s4[:, :, 0:1], s4[:, :, 3:4], ALU.add))
                    V(lambda e: e.tensor_reduce(gs[:], pr6[:], AX.X, ALU.max))
                    V(lambda e: e.tensor_reduce(gmx[:], gs[:], AX.X, ALU.max))
                    V(lambda e: e.tensor_scalar(gmk[:], gs[:], gmx[:], None, ALU.is_ge))
                    V(lambda e: e.tensor_tensor(mk[:].rearrange("p (g k) -> p g k", g=4), s4, gmk[:].unsqueeze(2).broadcast_to([128, 4, 4]), ALU.mult))
                    V(lambda e: e.tensor_scalar(gmk[:], gmk[:], -1.0, 10.0, ALU.add, ALU.mult))
                    V(lambda e: e.tensor_tensor(mk[:].rearrange("p (g k) -> p g k", g=4), mk[:].rearrange("p (g k) -> p g k", g=4), gmk[:].unsqueeze(2).broadcast_to([128, 4, 4]), ALU.add))
                    V(lambda e: e.tensor_reduce(t1[:], mk[:], AX.X, ALU.max))
                    V(lambda e: e.tensor_scalar(m1[:], mk[:], t1[:], None, ALU.is_ge))
                    V(lambda e: e.scalar_tensor_tensor(mk[:], m1[:], -20.0, mk[:], ALU.mult, ALU.add))
                    V(lambda e: e.tensor_reduce(t1[:], mk[:], AX.X, ALU.max))
                    V(lambda e: e.tensor_scalar(m2[:], mk[:], t1[:], None, ALU.is_ge))
                    V(lambda e: e.tensor_tensor(m1[:], m1[:], m2[:], ALU.add))
                    V(lambda e: e.tensor_tensor(m1[:], m1[:], sc[:], ALU.mult))
                    V(lambda e: e.tensor_reduce(t1[:], m1[:], AX.X, ALU.add))
                    V(lambda e: e.reciprocal(t1[:], t1[:]))
                    V(lambda e: e.tensor_scalar(cmb[:], m1[:], t1[:], None, ALU.mult))
                    P.dma("sp", COMB[r0:r0 + 128, :], cmb[:], reads=[S_.rr], writes=[rCOMB[gi]])

                prev = None
                for n_, (b, ti) in enumerate(tiles_l):
                    cur = (dsets[n_ % 2], b, ti)
                    dtile(*cur)
                    if prev is not None:
                        dtile2(*prev)
                    dtile1b(*cur)
                    if prev is not None:
                        dtile2b(*prev)
                    prev = cur
                dtile2(*prev)
                dtile2b(*prev)
                P.flush()

            stWO.close()
            if phases is None or 6 in phases:
              with ExitStack() as st:
                xt_tiles = [(b, ti) for b in range(2) for ti in range(2, 18)]
                sblocks = [xt_tiles[i * 8:(i + 1) * 8] for i in range(4)]
                if not last:
                    sblocks.append([(0, 0), (0, 1), (1, 0), (1, 1)])
                WG = sbt(st, "WG", [128, 16, 512], BF16)
                WU = sbt(st, "WU", [128, 16, 512], BF16)
                WD = sbt(st, "WD", [128, 4, D], BF16)
                rWG, rWD = R(), R()
                h2 = sbt(st, "h2", [128, 16, 1024], BF16)
                rh2 = R()
                accm = sbt(st, "accm", [128, 8, D], F32)
                racc = [R() for _ in range(8)]
                cmb = sbt(st, "cmbm", [128, 8, 16], F32)
                rcmb = R()
                actT = sbt(st, "actT", [128, 4, 1024], BF16)
                ract = R()
                sgl = [sbt(st, "sgl%d" % i, [128, 512], F32) for i in range(2)]
                rsgl = [R(), R()]
                pGU = [pst(st, "pGU%d" % i, [128, 2, 512], F32) for i in range(2)]
                rpGU = [R(), R()]
                pY = [pst(st, "pYm%d" % i, [128, 1024], F32) for i in range(2)]
                rpY = [R(), R()]
                xts = [sbt(st, "xtm%d" % i, [128, D], F32) for i in range(2)]
                rxts = [R(), R()]
                xcnt = 0
                junk = sbt(st, "junkm", [128, D], BF16)
                ss = sbt(st, "ssm", [128, 1], F32)
                rstd = sbt(st, "rstdm", [128, 1], F32)
                rtmp = R()
                fg = sbt(st, "fg", [128, D], F32)
                rfg = R()
                if last:
                    P.dma("sp", fg[:], fng, writes=[rfg])
                g2buf = (sbt(st, "g2b", [128, D], F32), R(), sbt(st, "g2dg", [128, 128], F32), R())
                g2cond = None
                kq = 0
                for sbk in sblocks:
                    nt = len(sbk)
                    nh = nt // 4
                    for i, (b, ti) in enumerate(sbk):
                        gi = b * 18 + ti
                        r0 = b * TB + ti * 128
                        P.dma("sp", h2[:, :, i * 128:(i + 1) * 128], HT[:, r0:r0 + 128].rearrange("(j p) t -> p j t", p=128), reads=[rHT[gi]], writes=[rh2])
                        P.dma("sp", cmb[:, i, :], COMB[r0:r0 + 128, :], reads=[rCOMB[gi]], writes=[rcmb])
                    for ex in range(16):
                        P.dma("pool", WG[:], wg[l, ex].rearrange("(j p) f -> p j f", p=128), writes=[rWG])
                        P.dma("pool", WU[:], wu[l, ex].rearrange("(j p) f -> p j f", p=128), writes=[rWG])
                        for j in range(4):
                            P.dma("pool", WD[:, j, :], wd[l, ex, j * 128:(j + 1) * 128, :], writes=[rWD], max_dma_last_dim=4096)
                        for hb in range(nh):
                            for fc in range(4):
                                pg, rpg = pGU[kq % 2], rpGU[kq % 2]
                                sg, rsg = sgl[kq % 2], rsgl[kq % 2]
                                kq += 1

                                def f(e, pg=pg, fc=fc, hb=hb):
                                    for j in range(16):
                                        e.matmul(pg[:, 0, :], WG[:, j, fc * 128:(fc + 1) * 128], h2[:, j, hb * 512:(hb + 1) * 512], start=(j == 0), stop=(j == 15))
                                    for j in range(16):
                                        ins = e.matmul(pg[:, 1, :], WU[:, j, fc * 128:(fc + 1) * 128], h2[:, j, hb * 512:(hb + 1) * 512], start=(j == 0), stop=(j == 15))
                                    return ins
                                P.emit("pe", f, reads=[rWG, rh2], writes=[rpg])
                                P.emit("act", lambda e, pg=pg, sg=sg: e.activation(sg[:], pg[:, 0, :], AF.Silu), reads=[rpg], writes=[rsg])
                                P.emit("dve", lambda e, pg=pg, sg=sg, fc=fc, hb=hb: e.tensor_tensor(actT[:, fc, hb * 512:(hb + 1) * 512], sg[:], pg[:, 1, :], ALU.mult), reads=[rpg, rsg], writes=[ract])
                        for i in range(nt):
                            for dh in range(2):
                                py, rpy = pY[kq % 2], rpY[kq % 2]
                                kq += 1

                                def f(e, py=py, i=i, dh=dh):
                                    for nb in range(2):
                                        for fc in range(4):
                                            ins = e.matmul(py[:, nb * 512:(nb + 1) * 512], actT[:, fc, i * 128:(i + 1) * 128], WD[:, fc, dh * 1024 + nb * 512:dh * 1024 + (nb + 1) * 512], start=(fc == 0), stop=(fc == 3))
                                    return ins
                                P.emit("pe", f, reads=[ract, rWD], writes=[rpy])
                                a_ = accm[:, i, dh * 1024:(dh + 1) * 1024]
                                if ex == 0:
                                    P.emit("dve", lambda e, py=py, a_=a_, i=i, ex=ex: e.tensor_scalar(a_, py[:], cmb[:, i, ex:ex + 1], None, ALU.mult), reads=[rpy, rcmb], writes=[racc[i]])
                                else:
                                    P.emit("dve", lambda e, py=py, a_=a_, i=i, ex=ex: e.scalar_tensor_tensor(a_, py[:], cmb[:, i, ex:ex + 1], a_, ALU.mult, ALU.add), reads=[rpy, rcmb, racc[i]], writes=[racc[i]])
                    for i, (b, ti) in enumerate(sbk):
                        gi = b * 18 + ti
                        r0 = b * TB + ti * 128
                        c = 2 if ti < 2 else b
                        if g2cond != c:
                            build_gate(st, "g2", 80, [c], pg=pY[0], rpg=rpY[0], gbuf=g2buf)
                            g2cond = c
                        g2, rg2 = g2buf[0], g2buf[1]
                        xt, rxt = xts[xcnt % 2], rxts[xcnt % 2]
                        xcnt += 1
                        P.dma("act", xt[:], RES[r0:r0 + 128, :], reads=[rRES[gi]], writes=[rxt])
                        P.emit("dve", lambda e, i=i, g2=g2: e.tensor_tensor(accm[:, i, :], accm[:, i, :], g2[:], ALU.mult), reads=[racc[i], rg2], writes=[racc[i]])
                        P.emit("dve", lambda e, i=i, xt=xt: e.tensor_tensor(xt[:], xt[:], accm[:, i, :], ALU.add), reads=[racc[i], rxt], writes=[rxt])
                        if not last:
                            P.dma("sp", RES[r0:r0 + 128, :], xt[:], reads=[rxt], writes=[rRES[gi]])
                        else:
                            rms_rstd("act", xt[:], D, ss[:], rstd[:], junk[:], rxt, rtmp)
                            P.emit("act", lambda e, xt=xt: e.activation(xt[:], xt[:], AF.Copy, scale=rstd[:]), reads=[rxt, rtmp], writes=[rxt])
                            P.emit("dve", lambda e, xt=xt: e.tensor_tensor(xt[:], xt[:], fg[:], ALU.mult), reads=[rxt, rfg], writes=[rxt])
                            P.dma("sp", out[b, (ti - 2) * 128:(ti - 1) * 128, :], xt[:], reads=[rxt], writes=[rRES[gi]])
                P.flush()

        P.finish()
        P.flush()
    return nc


def _rope_tables():
    t = np.arange(2048)
    rows = (t // 64).astype(np.float32)
    cols = (t % 64).astype(np.float32)
    nf = 16
    inv = (np.float32(10000.0) ** (-np.arange(nf, dtype=np.float32) / nf)).astype(np.float32)
    ang = np.stack([rows[:, None] * inv, cols[:, None] * inv], axis=1)
    cos = np.cos(ang).astype(np.float32)
    sin = np.sin(ang).astype(np.float32)
    C = np.zeros((64, 2048), np.float32)
    S = np.zeros((64, 2048), np.float32)
    for a in range(2):
        for b in range(2):
            for f in range(16):
                idx = a * 32 + b * 16 + f
                C[idx] = cos[:, a, f]
                S[idx] = sin[:, a, f] * (-1.0 if b == 0 else 1.0)
    return np.concatenate([C, C], 0), np.concatenate([S, S], 0)


def _swap_perm():
    perm = np.zeros(64, np.int64)
    for a in range(2):
        for b in range(2):
            for f in range(16):
                perm[a * 32 + b * 16 + f] = a * 32 + (1 - b) * 16 + f
    return perm


def prep_shared(inp):
    f = lambda a: np.ascontiguousarray(np.asarray(a, dtype=np.float32))
    perm = _swap_perm()
    w_in = f(inp["w_in"])
    kpe = w_in[:, :, 3360:3424]
    kpes = kpe[:, :, perm]
    win = np.concatenate([w_in[:, :, :3360], kpe, kpe, kpes, kpes], axis=2)
    assert win.shape[2] == NWIN
    wqb = f(inp["w_q_b"]).reshape(L, 512, 8, 192)
    nope = wqb[:, :, :, :128].reshape(L, 512, 1024)
    rope = wqb[:, :, :, 128:]
    wq = np.concatenate([nope, rope.reshape(L, 512, 512), rope[:, :, :, perm].reshape(L, 512, 512)], axis=2)
    wkvb = f(inp["w_kv_b"]).reshape(L, 256, 8, 256)
    wkv = np.concatenate([wkvb[:, :, :, :128].reshape(L, 256, 1024), wkvb[:, :, :, 128:].reshape(L, 256, 1024)], axis=2)
    colT = lambda v, n: np.ascontiguousarray(f(v).reshape(L, n, 128).transpose(0, 2, 1))
    bc = lambda v: np.ascontiguousarray(np.broadcast_to(f(v).reshape(L, 1, -1), (L, 128, f(v).reshape(L, -1).shape[1])))
    convw = f(inp["conv_w"])
    convw_l = np.ascontiguousarray(convw.reshape(L, 3, 12, 128).transpose(0, 3, 2, 1).reshape(L, 128, 36))
    cosT, sinT = _rope_tables()
    i_ = np.arange(128)
    tri_f = (i_[:, None] <= i_[None, :]).astype(np.float32)
    tri_b = (i_[:, None] >= i_[None, :]).astype(np.float32)
    mask_f = np.where(i_[None, :] >= i_[:, None], 0.0, NEG).astype(np.float32)
    mask_b = np.where(i_[None, :] <= i_[:, None], 0.0, NEG).astype(np.float32)
    sh = {
        "ada_w": f(inp["ada_w"]),
        "ada_bT": colT(inp["ada_b"], 96),
        "g1T": colT(inp["norm1_g"], 16), "g2T": colT(inp["norm2_g"], 16),
        "win": np.ascontiguousarray(win),
        "convw": convw_l, "convb": colT(inp["conv_b"], 12),
        "dtb": bc(inp["dt_bias"]), "alog": bc(inp["a_log"]), "dskip": bc(inp["d_skip"]),
        "ssdg": bc(inp["ssd_norm_g"]),
        "qgT": colT(inp["q_norm_g"], 4), "wq": np.ascontiguousarray(wq),
        "kvgT": colT(inp["kv_norm_g"], 2), "wkv": np.ascontiguousarray(wkv),
        "wo": f(inp["w_o"]),
        "rwT": np.ascontiguousarray(f(inp["router_w"]).reshape(16, 128, 16).transpose(1, 0, 2)),
        "rb": np.ascontiguousarray(np.broadcast_to(f(inp["router_b"]).reshape(1, 16), (128, 16))),
        "wg": f(inp["w_gate"]), "wu": f(inp["w_up"]), "wd": f(inp["w_down"]),
        "fng": np.ascontiguousarray(np.broadcast_to(f(inp["final_norm_g"]).reshape(1, D), (128, D))),
        "c_ident": np.eye(128, dtype=np.float32),
        "c_tri": np.stack([tri_f, tri_b]),
        "c_mask": np.stack([np.tile(mask_f, (1, 4)), np.tile(mask_b, (1, 4))]),
        "c_cos": cosT, "c_sin": sinT,
    }
    return sh


def core_inputs(inp, sh, core):
    f = lambda a: np.ascontiguousarray(np.asarray(a, dtype=np.float32))
    b0 = core * 2
    cc = np.stack([f(inp["c"])[b0], f(inp["c"])[b0 + 1], f(inp["c_ctx"])], axis=1)
    m = dict(sh)
    m["xin"] = f(inp["x"][b0:b0 + 2])
    m["cin"] = f(inp["ctx"][b0:b0 + 2])
    m["ccT"] = np.ascontiguousarray(cc.reshape(16, 128, 3).transpose(1, 0, 2))
    return m


_NC = None
_SKIP = set()


def kernel(**inputs):
    global _NC
    if _NC is None:
        _NC = build()
    sh = prep_shared(inputs)
    in_maps = [core_inputs(inputs, sh, c) for c in range(8)]
    res = run_bass_kernel_spmd(_NC, in_maps, core_ids=list(range(8)))
    return np.concatenate([np.asarray(r["out"]) for r in res.results], axis=0).astype(np.float32)
```

```python
import numpy as np
from contextlib import ExitStack
import concourse.bass as bass
import concourse.mybir as mybir
from concourse.bass_utils import run_bass_kernel_spmd

F32 = mybir.dt.float32
BF16 = mybir.dt.bfloat16
AF = mybir.ActivationFunctionType
ALU = mybir.AluOpType
AX = mybir.AxisListType

L = 2
D = 2048
TB = 2304
T = 2 * TB
EPS = 1e-6
NWIN = 3616
ATTN_SCALE = 192.0 ** -0.5
NEG = -30000.0


class R:
    __slots__ = ("lw", "rd")

    def __init__(self):
        self.lw = None
        self.rd = {}


class Prog:
    ENGS = ("pe", "act", "dve", "pool", "sp")
    NDS = 48
    NHW = 36

    def __init__(self, nc, es):
        self.nc = nc
        self.q = {e: [] for e in self.ENGS}
        self.cnt = {e: 0 for e in self.ENGS}
        self.waited = {e: {} for e in self.ENGS}
        self.sem = {e: es.enter_context(nc.semaphore("s_" + e)) for e in self.ENGS}
        self.dsem = [es.enter_context(nc.semaphore("d%d" % i)) for i in range(self.NDS)]
        self.dcnt = [0] * self.NDS
        self.dnext = 0
        self.dnext_sw = 0

    def _semof(self, key):
        return self.sem[key[1]] if key[0] == "e" else self.dsem[key[1]]

    def _deps(self, eng, reads, writes, extra=()):
        deps = {}

        def add(d):
            if d is None:
                return
            k, v = d
            if deps.get(k, 0) < v:
                deps[k] = v

        for r in reads:
            add(r.lw)
        for w in writes:
            add(w.lw)
            for kv in w.rd.items():
                add(kv)
        for d in extra:
            add(d)
        waits = []
        wd = self.waited[eng]
        for k, v in deps.items():
            if eng == "pe" and k == ("e", "pe"):
                continue
            if wd.get(k, 0) >= v:
                continue
            wd[k] = v
            waits.append((self._semof(k), v))
        return waits

    def emit(self, eng, fn, reads=(), writes=()):
        waits = self._deps(eng, reads, writes)
        self.cnt[eng] += 1
        key = ("e", eng)
        val = self.cnt[eng]
        sem = self.sem[eng]

        def thunk(e):
            for s, v in waits:
                e.wait_ge(s, v)
            fn(e).then_inc(sem, 1)

        self.q[eng].append(thunk)
        for r in reads:
            r.rd[key] = val
        for w in writes:
            w.lw = (key, val)
            w.rd = {}

    def dma(self, queue, out, in_, reads=(), writes=(), **kw):
        if queue == "pool":
            i = self.NHW + self.dnext_sw
            self.dnext_sw = (self.dnext_sw + 1) % (self.NDS - self.NHW)
        else:
            i = self.dnext
            self.dnext = (i + 1) % self.NHW
        prev = self.dcnt[i]
        self.dcnt[i] += 16
        val = self.dcnt[i]
        key = ("d", i)
        extra = [(key, prev)] if prev > 0 else []
        waits = self._deps(queue, reads, writes, extra)
        sem = self.dsem[i]

        def thunk(e):
            for s, v in waits:
                e.wait_ge(s, v)
            e.dma_start(out=out, in_=in_, **kw).then_inc(sem, 16)

        self.q[queue].append(thunk)
        for r in reads:
            r.rd[key] = val
        for w in writes:
            w.lw = (key, val)
            w.rd = {}

    def finish(self):
        waits = []
        for i in range(self.NDS):
            if self.dcnt[i] > 0:
                waits.append((self.dsem[i], self.dcnt[i]))
        for en in self.ENGS:
            if en != "sp" and self.cnt[en] > 0:
                waits.append((self.sem[en], self.cnt[en]))

        def thunk(e):
            for s, v in waits:
                e.wait_ge(s, v)

        self.q["sp"].append(thunk)

    def barrier(self):
        for en in self.ENGS:
            waits = []
            wd = self.waited[en]
            for i in range(self.NDS):
                k = ("d", i)
                if self.dcnt[i] > wd.get(k, 0):
                    wd[k] = self.dcnt[i]
                    waits.append((self.dsem[i], self.dcnt[i]))
            for e2 in self.ENGS:
                k = ("e", e2)
                if e2 != en and self.cnt[e2] > wd.get(k, 0):
                    wd[k] = self.cnt[e2]
                    waits.append((self.sem[e2], self.cnt[e2]))

            def thunk(e, waits=waits):
                for s_, v in waits:
                    e.wait_ge(s_, v)
            self.q[en].append(thunk)

    def flush(self):
        self.barrier()
        nc = self.nc
        q = self.q
        with nc.Block() as block:
            @block.tensor
            def _(e):
                for t in q["pe"]:
                    t(e)

            @block.scalar
            def _(e):
                for t in q["act"]:
                    t(e)

            @block.vector
            def _(e):
                for t in q["dve"]:
                    t(e)

            @block.gpsimd
            def _(e):
                for t in q["pool"]:
                    t(e)

            @block.sync
            def _(e):
                for t in q["sp"]:
                    t(e)
        self.q = {e: [] for e in self.ENGS}


def build(dbg=False, nlayers=L, phases=None):
    nc = bass.Bass("TRN2", target_bir_lowering=False)

    def din(name, shape, dt=F32):
        return nc.dram_tensor(name, list(shape), dt, kind="ExternalInput").ap()

    def dscr(name, shape, dt):
        return nc.dram_tensor(name, list(shape), dt, kind=("ExternalOutput" if dbg else "Internal")).ap()

    xin = din("xin", [2, 2048, D])
    cin = din("cin", [2, 256, D])
    ccT = din("ccT", [128, 16, 3])
    ada_w = din("ada_w", [L, D, 12288])
    ada_bT = din("ada_bT", [L, 128, 96])
    g1T = din("g1T", [L, 128, 16])
    g2T = din("g2T", [L, 128, 16])
    win = din("win", [L, D, NWIN])
    convw = din("convw", [L, 128, 36])
    convb = din("convb", [L, 128, 12])
    dtb = din("dtb", [L, 128, 32])
    alog = din("alog", [L, 128, 32])
    dskip = din("dskip", [L, 128, 16])
    ssdg = din("ssdg", [L, 128, 1024])
    qgT = din("qgT", [L, 128, 4])
    wq = din("wq", [L, 512, 2048])
    kvgT = din("kvgT", [L, 128, 2])
    wkv = din("wkv", [L, 256, 2048])
    wo = din("wo", [L, D, D])
    rwT = din("rwT", [128, 16, 16])
    rb = din("rb", [128, 16])
    wg = din("wg", [L, 16, D, 512])
    wu = din("wu", [L, 16, D, 512])
    wd = din("wd", [L, 16, 512, D])
    fng = din("fng", [128, D])
    c_ident = din("c_ident", [128, 128])
    c_tri = din("c_tri", [2, 128, 128])
    c_mask = din("c_mask", [2, 128, 512])
    c_cos = din("c_cos", [128, 2048])
    c_sin = din("c_sin", [128, 2048])
    out = nc.dram_tensor("out", [2, 2048, D], F32, kind="ExternalOutput").ap()

    RES = dscr("RES", [T, D], F32)
    ZT = dscr("ZT", [T, 1024], BF16)
    DTR = dscr("DTR", [T, 32], F32)
    XBC = dscr("XBC", [1536, T], BF16)
    QN = dscr("QN", [8, 128, T], BF16)
    QR = dscr("QR", [4, 128, T], BF16)
    KN = dscr("KN", [8, 128, T], BF16)
    KR = dscr("KR", [128, T], BF16)
    VV = dscr("VV", [T, 1024], BF16)
    YF = dscr("YF", [T, 1024], F32)
    YB = dscr("YB", [T, 1024], F32)
    MIX = dscr("MIX", [D, T], BF16)
    HT = dscr("HT", [D, T], BF16)
    COMB = dscr("COMB", [T, 16], F32)
    DBGM = dscr("DBGM", [128, 96, 3], F32)
    rRES = [R() for _ in range(36)]
    rZT = [R() for _ in range(36)]
    rDTR = [R() for _ in range(36)]
    rXBC = [R() for _ in range(2)]
    rQK = [R() for _ in range(2)]
    rYF = [R() for _ in range(36)]
    rYB = [R() for _ in range(36)]
    rMIX = [R() for _ in range(36)]
    rHT = [R() for _ in range(36)]
    rCOMB = [R() for _ in range(36)]

    def resid_src(l, b, ti):
        if l == 0:
            if ti < 2:
                return cin[b, ti * 128:(ti + 1) * 128, :]
            return xin[b, (ti - 2) * 128:(ti - 1) * 128, :]
        r0 = b * TB + ti * 128
        return RES[r0:r0 + 128, :]

    with ExitStack() as es:
        P = Prog(nc, es)

        uid = [0]

        def sbt(st, name, shape, dt):
            uid[0] += 1
            return st.enter_context(nc.sbuf_tensor("%s_%d" % (name, uid[0]), list(shape), dt))

        def pst(st, name, shape, dt):
            uid[0] += 1
            return st.enter_context(nc.psum_tensor("%s_%d" % (name, uid[0]), list(shape), dt))

        identf = sbt(es, "identf", [128, 128], F32)
        identb = sbt(es, "identb", [128, 128], BF16)
        onesf = sbt(es, "onesf", [128, 128], F32)
        onesb = sbt(es, "onesb", [128, 128], BF16)
        modS = sbt(es, "modS", [128, 96, 3], F32)
        gm1 = sbt(es, "gm1", [128, 16, 3], F32)
        gm2 = sbt(es, "gm2", [128, 16, 3], F32)
        rC = R()
        rmod = R()
        P.dma("sp", identf[:], c_ident, writes=[rC])
        P.emit("dve", lambda e: e.tensor_copy(identb[:], identf[:]), reads=[rC], writes=[rC])
        P.emit("dve", lambda e: e.memset(onesf[:], 1.0), writes=[rC])
        P.emit("dve", lambda e: e.memset(onesb[:], 1.0), writes=[rC])

        def rms_rstd(eng_src, src_ap, n, ss, rstd, junk, rsrc, rtmp):
            P.emit("act", lambda e: e.activation(junk, src_ap, AF.Square, accum_out=ss), reads=[rsrc], writes=[rtmp])
            P.emit("act", lambda e: e.activation(rstd, ss, AF.Sqrt, bias=EPS, scale=1.0 / n), reads=[rtmp], writes=[rtmp])
            P.emit("dve", lambda e: e.reciprocal(rstd, rstd), reads=[rtmp], writes=[rtmp])

        for l in range(nlayers):
            last = (l == L - 1)
            tiles_l = [(b, ti) for b in range(2) for ti in range(18) if not (last and ti < 2)]

            stA1w = ExitStack()
            W_A1 = sbt(stA1w, "W", [128, 16, 2592], BF16)
            rW_A1 = R()
            for j in range(16):
                P.dma("pool", W_A1[:, j, :], win[l, j * 128:(j + 1) * 128, 0:2592], writes=[rW_A1], max_dma_last_dim=4096)
            if phases is None or 0 in phases:
              with ExitStack() as st:
                scf = sbt(st, "scf", [128, 16, 3], F32)
                scb = sbt(st, "scb", [128, 16, 3], BF16)
                abT = sbt(st, "abT", [128, 96], F32)
                g1s = sbt(st, "g1s", [128, 16], F32)
                g2s = sbt(st, "g2s", [128, 16], F32)
                awf = [sbt(st, "awf%d" % i, [128, 16, 512], F32) for i in range(2)]
                rawf = [R(), R()]
                aw = [sbt(st, "aw%d" % i, [128, 16, 512], BF16) for i in range(2)]
                raw = [R(), R()]
                pm = pst(st, "pm", [128, 96, 3], F32)
                rpm = R()
                rs = R()
                P.dma("sp", scf[:], ccT, writes=[rs])
                P.dma("sp", abT[:], ada_bT[l], writes=[rs])
                P.dma("sp", g1s[:], g1T[l], writes=[rs])
                P.dma("sp", g2s[:], g2T[l], writes=[rs])
                P.emit("act", lambda e: e.activation(scb[:], scf[:], AF.Silu), reads=[rs], writes=[rs])
                for pc in range(24):
                    af, raf = awf[pc % 2], rawf[pc % 2]
                    a, ra = aw[pc % 2], raw[pc % 2]
                    for hj in range(2):
                        P.dma("sp" if hj == 0 else "act", af[:, hj * 8:(hj + 1) * 8, :], ada_w[l, hj * 1024:(hj + 1) * 1024, pc * 512:(pc + 1) * 512].rearrange("(j p) m -> p j m", p=128), writes=[raf])
                    P.emit("dve", lambda e, a=a, af=af: e.tensor_copy(a[:, 0:6, :], af[:, 0:6, :]), reads=[raf], writes=[ra])
                    P.emit("pool", lambda e, a=a, af=af: e.tensor_copy(a[:, 6:10, :], af[:, 6:10, :]), reads=[raf], writes=[ra])
                    P.emit("act", lambda e, a=a, af=af: e.activation(a[:, 10:16, :], af[:, 10:16, :], AF.Copy), reads=[raf], writes=[ra])
                    for mc in range(4):
                        m = pc * 4 + mc

                        def f(e, a=a, m=m, mc=mc):
                            for j in range(16):
                                ins = e.matmul(pm[:, m, :], a[:, j, mc * 128:(mc + 1) * 128], scb[:, j, :], start=(j == 0), stop=(j == 15))
                            return ins
                        P.emit("pe", f, reads=[ra, rs], writes=[rpm])
                P.emit("dve", lambda e: e.tensor_tensor(modS[:], pm[:], abT[:].unsqueeze(2).broadcast_to([128, 96, 3]), ALU.add), reads=[rpm, rs, rmod], writes=[rmod])
                P.emit("dve", lambda e: e.tensor_scalar(gm1[:], modS[:, 16:32, :], 1.0, None, ALU.add), reads=[rmod], writes=[rmod])
                P.emit("dve", lambda e: e.tensor_tensor(gm1[:], gm1[:], g1s[:].unsqueeze(2).broadcast_to([128, 16, 3]), ALU.mult), reads=[rmod, rs], writes=[rmod])
                P.emit("dve", lambda e: e.tensor_scalar(gm2[:], modS[:, 64:80, :], 1.0, None, ALU.add), reads=[rmod], writes=[rmod])
                P.emit("dve", lambda e: e.tensor_tensor(gm2[:], gm2[:], g2s[:].unsqueeze(2).broadcast_to([128, 16, 3]), ALU.mult), reads=[rmod, rs], writes=[rmod])
                if dbg:
                    P.dma("sp", DBGM, modS[:], reads=[rmod])
                P.flush()

            def build_gate(st, name, j0, conds, pg=None, rpg=None, gbuf=None):
                G = {}
                dg = gbuf[2] if gbuf else sbt(st, name + "dg", [128, 128], F32)
                if pg is None:
                    pg = pst(st, name + "pg", [128, 512], F32)
                    rpg = R()
                rdg = gbuf[3] if gbuf else R()
                for c in conds:
                    if gbuf:
                        g, rg = gbuf[0], gbuf[1]
                    else:
                        g = sbt(st, name + "G%d" % c, [128, D], F32)
                        rg = R()
                    for j in range(16):
                        P.emit("dve", lambda e, j=j, c=c: e.tensor_scalar(dg[:], identf[:], modS[:, j0 + j, c:c + 1], None, ALU.mult), reads=[rmod, rC], writes=[rdg])
                        P.emit("pe", lambda e, j=j: e.matmul(pg[:, (j % 4) * 128:(j % 4 + 1) * 128], onesf[:], dg[:], start=True, stop=True), reads=[rdg, rC], writes=[rpg])
                        if j % 4 == 3:
                            P.emit("act", lambda e, j=j, g=g: e.activation(g[:, (j - 3) * 128:(j + 1) * 128], pg[:, 0:512], AF.Copy), reads=[rpg], writes=[rg])
                    G[c] = (g, rg)
                return G

            if phases is None or 1 in phases:
              with ExitStack() as st:
                NA = 2592
                W = W_A1
                rW = rW_A1
                xt = [sbt(st, "xt%d" % i, [128, D], F32) for i in range(2)]
                rxt = [R(), R()]
                junk = sbt(st, "junk", [128, D], BF16)
                ss = sbt(st, "ss", [128, 1], F32)
                rstd = sbt(st, "rstd", [128, 1], F32)
                rtmp = R()
                xns = [sbt(st, "xn%d" % i, [128, D], BF16) for i in range(2)]
                rxns = [R(), R()]
                hTs = [sbt(st, "hT%d" % i, [128, 16, 512], BF16) for i in range(2)]
                rhTs = [R(), R()]
                gcnt = 0
                pT = [pst(st, "pT%d" % i, [128, 8, 128], BF16) for i in range(2)]
                rpT = [R(), R()]
                pO = [pst(st, "pO%d" % i, [128, 512], F32) for i in range(4)]
                rpO = [R() for _ in range(4)]
                ob = [sbt(st, "ob%d" % i, [128, 512], BF16) for i in range(4)]
                rob = [R() for _ in range(4)]
                dto = sbt(st, "dto", [128, 32], F32)
                rdto = R()
                cnt = 0
                a1_tiles = [(b, t0 + i) for b in range(2) for (t0, nt) in ((0, 2), (2, 4), (6, 4), (10, 4), (14, 4)) for i in range(nt)]

                def a1_norm(k_):
                    b_, ti_ = a1_tiles[k_]
                    x_, rx = xt[k_ % 2], rxt[k_ % 2]
                    xn_, rxn_ = xns[k_ % 2], rxns[k_ % 2]
                    P.dma("sp", x_[:], resid_src(l, b_, ti_), reads=[rRES[b_ * 18 + ti_]], writes=[rx])
                    rms_rstd("act", x_[:], D, ss[:], rstd[:], junk[:], rx, rtmp)
                    P.emit("act", lambda e: e.activation(xn_[:], x_[:], AF.Copy, scale=rstd[:]), reads=[rx, rtmp], writes=[rxn_])
                for b in range(2):
                    for (t0, nt) in ((0, 2), (2, 4), (6, 4), (10, 4), (14, 4)):
                        c = 2 if t0 == 0 else b
                        n = nt * 128
                        col0 = b * TB + t0 * 128
                        hT, rhT = hTs[gcnt % 2], rhTs[gcnt % 2]
                        gcnt += 1
                        for i in range(nt):
                            ti = t0 + i
                            gi = b * 18 + ti
                            xn, rxn = xns[cnt % 2], rxns[cnt % 2]
                            if cnt == 0:
                                a1_norm(0)
                            if cnt + 1 < len(a1_tiles):
                                a1_norm(cnt + 1)
                            cnt += 1
                            for hh in range(2):
                                def f(e, hh=hh, xn=xn):
                                    for jj in range(8):
                                        j = hh * 8 + jj
                                        ins = e.transpose(pT[hh][:, jj, :], xn[:, j * 128:(j + 1) * 128], identb[:])
                                    return ins
                                P.emit("pe", f, reads=[rxn, rC], writes=[rpT[hh]])
                                for jj in range(8):
                                    j = hh * 8 + jj
                                    if jj % 2 == 0:
                                        P.emit("dve", lambda e, hh=hh, jj=jj, j=j, i=i, c=c, hT=hT: e.tensor_scalar(hT[:, j, i * 128:(i + 1) * 128], pT[hh][:, jj, :], gm1[:, j, c:c + 1], modS[:, j, c:c + 1], ALU.mult, ALU.add), reads=[rpT[hh], rmod], writes=[rhT])
                                    else:
                                        P.emit("act", lambda e, hh=hh, jj=jj, j=j, i=i, c=c, hT=hT: e.activation(hT[:, j, i * 128:(i + 1) * 128], pT[hh][:, jj, :], AF.Identity, scale=gm1[:, j, c:c + 1], bias=modS[:, j, c:c + 1]), reads=[rpT[hh], rmod], writes=[rhT])
                        P.dma("sp", HT[:, col0:col0 + n].rearrange("(j p) t -> p j t", p=128), hT[:, :, 0:n], reads=[rhT], writes=[rHT[b * 18 + t0 + i] for i in range(nt)])
                        k = 0
                        for i in range(nt):
                            ti = t0 + i
                            gi = b * 18 + ti
                            r0 = b * TB + ti * 128
                            for zb in range(2):
                                po, rpo, o_, ro = pO[k % 4], rpO[k % 4], ob[k % 4], rob[k % 4]
                                k += 1

                                def f(e, po=po, zb=zb, i=i, hT=hT):
                                    for j in range(16):
                                        ins = e.matmul(po[:], hT[:, j, i * 128:(i + 1) * 128], W[:, j, zb * 512:(zb + 1) * 512], start=(j == 0), stop=(j == 15))
                                    return ins
                                P.emit("pe", f, reads=[rhT, rW], writes=[rpo])
                                P.emit("act", lambda e, po=po, o_=o_: e.activation(o_[:], po[:], AF.Silu), reads=[rpo], writes=[ro])
                                P.dma("sp", ZT[r0:r0 + 128, zb * 512:(zb + 1) * 512], o_[:], reads=[ro], writes=[rZT[gi]])
                            po, rpo = pO[k % 4], rpO[k % 4]
                            k += 1

                            def f(e, po=po, i=i, hT=hT):
                                for j in range(16):
                                    ins = e.matmul(po[:, 0:32], hT[:, j, i * 128:(i + 1) * 128], W[:, j, 2560:2592], start=(j == 0), stop=(j == 15))
                                return ins
                            P.emit("pe", f, reads=[rhT, rW], writes=[rpo])
                            P.emit("dve", lambda e, po=po: e.tensor_copy(dto[:], po[:, 0:32]), reads=[rpo], writes=[rdto])
                            P.dma("sp", DTR[r0:r0 + 128, :], dto[:], reads=[rdto], writes=[rDTR[gi]])
                        for m in range(12):
                            po, rpo, o_, ro = pO[k % 4], rpO[k % 4], ob[k % 4], rob[k % 4]
                            k += 1

                            def f(e, po=po, m=m, n=n, hT=hT):
                                for j in range(16):
                                    ins = e.matmul(po[:, 0:n], W[:, j, 1024 + m * 128:1024 + (m + 1) * 128], hT[:, j, 0:n], start=(j == 0), stop=(j == 15))
                                return ins
                            P.emit("pe", f, reads=[rhT, rW], writes=[rpo])
                            P.emit("act" if k % 2 else "dve", (lambda e, po=po, o_=o_, n=n: e.activation(o_[:, 0:n], po[:, 0:n], AF.Copy)) if k % 2 else (lambda e, po=po, o_=o_, n=n: e.tensor_copy(o_[:, 0:n], po[:, 0:n])), reads=[rpo], writes=[ro])
                            P.dma("sp", XBC[m * 128:(m + 1) * 128, col0:col0 + n], o_[:, 0:n], reads=[ro], writes=[rXBC[b]])
                P.flush()

            stA1w.close()
            if phases is None or 2 in phases:
              with ExitStack() as st:
                NB2 = NWIN - 2592
                W = sbt(st, "W2", [128, 16, NB2], BF16)
                rW = R()
                for j in range(16):
                    P.dma("pool", W[:, j, :], win[l, j * 128:(j + 1) * 128, 2592:NWIN], writes=[rW])
                WQ = sbt(st, "WQ", [128, 4, 2048], BF16)
                WKV = sbt(st, "WKV", [128, 2, 2048], BF16)
                for j in range(4):
                    P.dma("pool", WQ[:, j, :], wq[l, j * 128:(j + 1) * 128, :], writes=[rW], max_dma_last_dim=4096)
                for j in range(2):
                    P.dma("pool", WKV[:, j, :], wkv[l, j * 128:(j + 1) * 128, :], writes=[rW], max_dma_last_dim=4096)
                cosT = sbt(st, "cosT", [128, 2048], F32)
                sinT = sbt(st, "sinT", [128, 2048], F32)
                qg = sbt(st, "qg", [128, 4], F32)
                kvg = sbt(st, "kvg", [128, 2], F32)
                P.dma("sp", cosT[:], c_cos, writes=[rW])
                P.dma("sp", sinT[:], c_sin, writes=[rW])
                P.dma("sp", qg[:], qgT[l], writes=[rW])
                P.dma("sp", kvg[:], kvgT[l], writes=[rW])
                hT = [sbt(st, "hT2%d" % i, [128, 16, 512], BF16) for i in range(2)]
                rhT = [R(), R()]
                pO = [pst(st, "pO%d" % i, [128, 512], F32) for i in range(6)]
                rpO = [R() for _ in range(6)]
                pSq = [pst(st, "pSq%d" % i, [128, 512], F32) for i in range(2)]
                rpSq = [R(), R()]

                class NS:
                    pass
                nsets = {}
                for nm, nch in (("q", 4), ("kv", 2)):
                    for i in range(2):
                        s_ = NS()
                        s_.sq = sbt(st, "sq%s%d" % (nm, i), [128, nch, 512], BF16)
                        s_.qa = sbt(st, "qa%s%d" % (nm, i), [128, nch, 512], F32)
                        s_.qab = sbt(st, "qab%s%d" % (nm, i), [128, nch, 512], BF16)
                        s_.rbc = sbt(st, "rbc%s%d" % (nm, i), [128, 512], F32)
                        s_.rsq, s_.rqa, s_.rqab, s_.rrbc = R(), R(), R(), R()
                        nsets[(nm, i)] = s_
                ob = [sbt(st, "ob%d" % i, [128, 512], BF16) for i in range(6)]
                rob = [R() for _ in range(6)]
                ta = [sbt(st, "ta%d" % i, [128, 512], F32) for i in range(2)]
                tb_ = [sbt(st, "tb%d" % i, [128, 512], F32) for i in range(2)]
                rta, rtb = [R(), R()], [R(), R()]
                ov = [sbt(st, "ov%d" % i, [128, 1024], BF16) for i in range(2)]
                rov = [R(), R()]
                kst = [0, 0, 0, 0]

                def group(b, t0, nt, gidx):
                    isx = t0 > 0
                    n = nt * 128
                    col0 = b * TB + t0 * 128
                    p0 = (t0 - 2) * 128
                    h_ = hT[gidx % 2]
                    rh = rhT[gidx % 2]
                    Nq = nsets[("q", gidx % 2)]
                    Nk = nsets[("kv", gidx % 2)]
                    P.dma("sp", h_[:, :, 0:n], HT[:, col0:col0 + n].rearrange("(j p) t -> p j t", p=128), reads=[rHT[b * 18 + t0 + i] for i in range(nt)], writes=[rh])

                    def nps():
                        kst[0] += 1
                        return pO[kst[0] % 6], rpO[kst[0] % 6]

                    def nob():
                        kst[1] += 1
                        return ob[kst[1] % 6], rob[kst[1] % 6]

                    def store(dst, src, rsrc):
                        kst[3] += 1
                        P.dma("sp" if kst[3] % 2 else "act", dst, src, reads=[rsrc], writes=[rQK[b]])

                    def proj(po, c0):
                        def f(e):
                            for j in range(16):
                                ins = e.matmul(po[:, 0:n], W[:, j, c0:c0 + 128], h_[:, j, 0:n], start=(j == 0), stop=(j == 15))
                            return ins
                        return f

                    def stage1(N_, nchunk, c0, gcol):
                        for m in range(nchunk):
                            po, rpo = nps()
                            P.emit("pe", proj(po, c0 + m * 128), reads=[rh, rW], writes=[rpo])
                            P.emit("act", lambda e, po=po, m=m: e.activation(N_.qa[:, m, 0:n], po[:, 0:n], AF.Copy), reads=[rpo], writes=[N_.rqa])
                            P.emit("dve", lambda e, m=m: e.tensor_tensor(N_.sq[:, m, 0:n], N_.qa[:, m, 0:n], N_.qa[:, m, 0:n], ALU.mult), reads=[N_.rqa], writes=[N_.rsq])
                            P.emit("pool", lambda e, m=m: e.tensor_scalar(N_.qa[:, m, 0:n], N_.qa[:, m, 0:n], gcol[:, m:m + 1], 1.0, ALU.mult, ALU.mult), reads=[N_.rqa, rW, N_.rsq], writes=[N_.rqa])

                    def stage2(N_, nchunk, pS, rpS):
                        def f(e):
                            for m in range(nchunk):
                                ins = e.matmul(pS[:, 0:n], onesb[:], N_.sq[:, m, 0:n], start=(m == 0), stop=(m == nchunk - 1))
                            return ins
                        P.emit("pe", f, reads=[N_.rsq, rC], writes=[rpS])
                        P.emit("act", lambda e: e.activation(N_.rbc[:, 0:n], pS[:, 0:n], AF.Sqrt, bias=EPS, scale=1.0 / (nchunk * 128)), reads=[rpS], writes=[N_.rrbc])
                        P.emit("dve", lambda e: e.reciprocal(N_.rbc[:, 0:n], N_.rbc[:, 0:n]), reads=[N_.rrbc], writes=[N_.rrbc])
                        for m in range(nchunk):
                            P.emit("dve", lambda e, m=m: e.tensor_tensor(N_.qab[:, m, 0:n], N_.qa[:, m, 0:n], N_.rbc[:, 0:n], ALU.mult), reads=[N_.rqa, N_.rrbc], writes=[N_.rqab])

                    def rope_combine(pa, rpa, pb, rpb, o_, ro):
                        if not isx:
                            P.emit("act", lambda e: e.activation(o_[:, 0:n], pa[:, 0:n], AF.Copy), reads=[rpa], writes=[ro])
                            return
                        kst[2] += 1
                        ta_, tb2, rta_, rtb_ = ta[kst[2] % 2], tb_[kst[2] % 2], rta[kst[2] % 2], rtb[kst[2] % 2]
                        P.emit("dve", lambda e: e.tensor_tensor(ta_[:, 0:n], pa[:, 0:n], cosT[:, p0:p0 + n], ALU.mult), reads=[rpa, rW], writes=[rta_])
                        P.emit("dve", lambda e: e.tensor_tensor(tb2[:, 0:n], pb[:, 0:n], sinT[:, p0:p0 + n], ALU.mult), reads=[rpb, rW], writes=[rtb_])
                        P.emit("pool", lambda e: e.tensor_tensor(o_[:, 0:n], ta_[:, 0:n], tb2[:, 0:n], ALU.add), reads=[rta_, rtb_], writes=[ro])

                    stage1(Nq, 4, 0, qg)
                    stage1(Nk, 2, 512, kvg)
                    pa, rpa = nps()
                    pb, rpb = nps()
                    o_, ro = nob()
                    P.emit("pe", proj(pa, 768), reads=[rh, rW], writes=[rpa])
                    P.emit("pe", proj(pb, 896), reads=[rh, rW], writes=[rpb])
                    rope_combine(pa, rpa, pb, rpb, o_, ro)
                    store(KR[:, col0:col0 + n], o_[:, 0:n], ro)
                    stage2(Nq, 4, pSq[0], rpSq[0])
                    stage2(Nk, 2, pSq[1], rpSq[1])

                    def qproj(po, mcol):
                        def f(e):
                            for j in range(4):
                                ins = e.matmul(po[:, 0:n], WQ[:, j, mcol:mcol + 128], Nq.qab[:, j, 0:n], start=(j == 0), stop=(j == 3))
                            return ins
                        return f
                    for h in range(8):
                        po, rpo = nps()
                        o_, ro = nob()
                        P.emit("pe", qproj(po, h * 128), reads=[Nq.rqab, rW], writes=[rpo])
                        P.emit("act", lambda e, po=po, o_=o_: e.activation(o_[:, 0:n], po[:, 0:n], AF.Copy), reads=[rpo], writes=[ro])
                        store(QN[h, :, col0:col0 + n], o_[:, 0:n], ro)
                    for h in range(8):
                        po, rpo = nps()
                        o_, ro = nob()

                        def f(e, po=po, h=h):
                            for j in range(2):
                                ins = e.matmul(po[:, 0:n], WKV[:, j, h * 128:(h + 1) * 128], Nk.qab[:, j, 0:n], start=(j == 0), stop=(j == 1))
                            return ins
                        P.emit("pe", f, reads=[Nk.rqab, rW], writes=[rpo])
                        P.emit("dve", lambda e, po=po, o_=o_: e.tensor_copy(o_[:, 0:n], po[:, 0:n]), reads=[rpo], writes=[ro])
                        store(KN[h, :, col0:col0 + n], o_[:, 0:n], ro)
                    for pr in range(4):
                        pa, rpa = nps()
                        pb, rpb = nps()
                        o_, ro = nob()
                        P.emit("pe", qproj(pa, 1024 + pr * 128), reads=[Nq.rqab, rW], writes=[rpa])
                        P.emit("pe", qproj(pb, 1536 + pr * 128), reads=[Nq.rqab, rW], writes=[rpb])
                        rope_combine(pa, rpa, pb, rpb, o_, ro)
                        store(QR[pr, :, col0:col0 + n], o_[:, 0:n], ro)
                    for i in range(nt):
                        r0 = col0 + i * 128
                        ov_, rov_ = ov[i % 2], rov[i % 2]
                        for vb in range(2):
                            po, rpo = nps()

                            def f(e, po=po, vb=vb, i=i):
                                for j in range(2):
                                    ins = e.matmul(po[:], Nk.qab[:, j, i * 128:(i + 1) * 128], WKV[:, j, 1024 + vb * 512:1024 + (vb + 1) * 512], start=(j == 0), stop=(j == 1))
                                return ins
                            P.emit("pe", f, reads=[Nk.rqab, rW], writes=[rpo])
                            P.emit("act", lambda e, po=po, ov_=ov_, vb=vb: e.activation(ov_[:, vb * 512:(vb + 1) * 512], po[:], AF.Copy), reads=[rpo], writes=[rov_])
                        store(VV[r0:r0 + 128, :], ov_[:], rov_)

                gidx = 0
                for b in range(2):
                    for (t0, nt) in ((0, 2), (2, 4), (6, 4), (10, 4), (14, 4)):
                        group(b, t0, nt, gidx)
                        gidx += 1
                P.flush()

            if phases is None or 3 in phases:
              with ExitStack() as st:
                cw = sbt(st, "cw", [128, 36], F32)
                cb = sbt(st, "cb", [128, 12], F32)
                dtbs = sbt(st, "dtbs", [128, 32], F32)
                abc = sbt(st, "abc", [128, 32], F32)
                dsk = sbt(st, "dsk", [128, 16], F32)
                tri = [sbt(st, "tri%d" % d, [128, 128], F32) for d in range(2)]
                msk = [sbt(st, "msk%d" % d, [128, 512], F32) for d in range(2)]
                rK = R()
                P.dma("sp", cw[:], convw[l], writes=[rK])
                P.dma("sp", cb[:], convb[l], writes=[rK])
                P.dma("sp", dtbs[:], dtb[l], writes=[rK])
                P.dma("sp", abc[:], alog[l], writes=[rK])
                P.dma("sp", dsk[:], dskip[l], writes=[rK])
                for d in range(2):
                    P.dma("sp", tri[d][:], c_tri[d], writes=[rK])
                    P.dma("sp", msk[d][:], c_mask[d], writes=[rK])
                P.emit("act", lambda e: e.activation(abc[:], abc[:], AF.Exp), reads=[rK], writes=[rK])
                P.emit("dve", lambda e: e.tensor_scalar(abc[:], abc[:], -1.0, None, ALU.mult), reads=[rK], writes=[rK])
                XC = sbt(st, "XC", [128, 12, TB], BF16)
                rXC = R()
                u = [sbt(st, "u%d" % i, [128, 2048], BF16) for i in range(2)]
                ru = [R(), R()]
                acc = [sbt(st, "acc%d" % i, [128, 2048], F32) for i in range(2)]
                racc = [R(), R()]
                zs = sbt(st, "zsB", [128, 1024], F32)
                rzs = R()

                class BS:
                    pass
                sets = []
                for d in range(2):
                    s_ = BS()
                    for nm, shp, dt_t in (("S", [128, 1024], F32), ("Sb", [128, 1024], BF16), ("XS", [128, 1024], BF16), ("BT", [128, 256], BF16),
                                          ("dtr", [128, 32], F32), ("dt_", [128, 32], F32), ("dtA", [128, 32], F32),
                                          ("xdt", [128, 16, 64], BF16), ("xdd", [128, 16, 64], BF16),
                                          ("nac", [128, 16], F32), ("expA", [128, 16], F32), ("dec", [128, 16], F32), ("cd", [128, 16], F32),
                                          ("Rt", [128, 8, 128], F32), ("LM", [128, 16, 128], BF16), ("CBs", [128, 2, 128], BF16),
                                          ("MT", [128, 16, 128], BF16), ("yo", [128, 16, 64], F32), ("y", [128, 1024], F32)):
                        setattr(s_, nm, sbt(st, "%s_d%d" % (nm, d), shp, dt_t))
                    for nm in ("rS", "rSb", "rXS", "rBT", "rdtr", "rdt", "rxdt", "rxdd", "rsm", "rRt", "rLM", "rCBs", "rMT", "ryo", "ry"):
                        setattr(s_, nm, R())
                    sets.append(s_)
                dtr_all = sbt(st, "dtr_all", [128, 18, 32], F32)
                dt_all = sbt(st, "dt_all", [128, 18, 32], F32)
                dtA_all = sbt(st, "dtA_all", [128, 18, 32], F32)
                nac_all = sbt(st, "nac_all", [128, 18, 32], F32)
                expA_all = sbt(st, "expA_all", [128, 18, 32], F32)
                cd_all = sbt(st, "cd_all", [128, 18, 32], F32)
                dec_all = sbt(st, "dec_all", [128, 18, 32], F32)
                rpre = R()
                bT = pst(st, "bT", [128, 8, 128], BF16)
                rbT = R()
                bA = pst(st, "bA", [128, 512], F32)
                rbA = R()
                pSs = pst(st, "pSs", [128, 1024], F32)
                rpSs = R()
                pY = pst(st, "pY", [128, 1024], F32)
                rpY = R()
                pL = pst(st, "pL", [128, 8, 128], F32)
                rpL = R()

                def chunk_pass(b, d, ti, B_):
                    base = b * TB
                    gi = b * 18 + ti
                    r0 = base + ti * 128
                    cs = slice(ti * 128, (ti + 1) * 128)
                    do_y = not (last and ti < 2)
                    dsl = slice(d * 16, (d + 1) * 16)

                    def f(e):
                        for j in range(8):
                            ins = e.transpose(bT[:, j, :], XC[:, j, cs], identb[:])
                        return ins
                    P.emit("pe", f, reads=[rXC, rC], writes=[rbT])
                    P.emit("act", lambda e: e.activation(B_.XS[:], bT[:].rearrange("p a b -> p (a b)"), AF.Copy), reads=[rbT], writes=[B_.rXS])

                    def f(e):
                        for j in range(2):
                            ins = e.transpose(bT[:, j, :], XC[:, 8 + j, cs], identb[:])
                        return ins
                    P.emit("pe", f, reads=[rXC, rC], writes=[rbT])
                    P.emit("act", lambda e: e.activation(B_.BT[:], bT[:, 0:2, :].rearrange("p a b -> p (a b)"), AF.Copy), reads=[rbT], writes=[B_.rBT])
                    P.emit("dve", lambda e: e.tensor_tensor(B_.xdt[:], B_.XS[:].rearrange("p (h q) -> p h q", h=16), dt_all[:, ti, dsl].unsqueeze(2).broadcast_to([128, 16, 64]), ALU.mult), reads=[B_.rXS, rpre], writes=[B_.rxdt])
                    yield

                    P.emit("dve", lambda e: e.tensor_tensor(B_.xdd[:], B_.XS[:].rearrange("p (h q) -> p h q", h=16), dec_all[:, ti, dsl].unsqueeze(2).broadcast_to([128, 16, 64]), ALU.mult), reads=[B_.rXS, rpre], writes=[B_.rxdd])
                    yield

                    def f(e):
                        for g in range(2):
                            ins = e.matmul(pSs[:, g * 512:(g + 1) * 512], B_.BT[:, g * 128:(g + 1) * 128], B_.xdd[:, g * 8:(g + 1) * 8, :].rearrange("p h q -> p (h q)"), start=True, stop=True)
                        return ins
                    P.emit("pe", f, reads=[B_.rBT, B_.rxdd], writes=[rpSs])
                    if do_y:
                        P.emit("act", lambda e: e.activation(B_.Sb[:], B_.S[:], AF.Copy), reads=[B_.rS], writes=[B_.rSb])

                        def f(e):
                            for g in range(2):
                                ins = e.matmul(pY[:, g * 512:(g + 1) * 512], XC[:, 10 + g, cs], B_.Sb[:, g * 512:(g + 1) * 512], start=True, stop=True)
                            return ins
                        P.emit("pe", f, reads=[rXC, B_.rSb], writes=[rpY])
                        P.emit("dve", lambda e: e.tensor_tensor(B_.yo[:], pY[:].rearrange("p (h q) -> p h q", h=16), expA_all[:, ti, dsl].unsqueeze(2).broadcast_to([128, 16, 64]), ALU.mult), reads=[rpY, rpre], writes=[B_.ryo])
                    P.emit("dve", lambda e: e.tensor_tensor(B_.S[:].rearrange("p (h q) -> p h q", h=16), B_.S[:].rearrange("p (h q) -> p h q", h=16), cd_all[:, ti, dsl].unsqueeze(2).broadcast_to([128, 16, 64]), ALU.mult), reads=[B_.rS, rpre, B_.rSb], writes=[B_.rS])
                    P.emit("dve", lambda e: e.tensor_tensor(B_.S[:], B_.S[:], pSs[:], ALU.add), reads=[B_.rS, rpSs], writes=[B_.rS])
                    yield
                    if not do_y:
                        return

                    def f(e):
                        for g in range(2):
                            ins = e.matmul(bA[:, 256 + g * 128:256 + (g + 1) * 128], XC[:, 8 + g, cs], XC[:, 10 + g, cs], start=True, stop=True)
                        return ins
                    P.emit("pe", f, reads=[rXC], writes=[rbA])
                    P.emit("act", lambda e: e.activation(B_.CBs[:].rearrange("p a b -> p (a b)"), bA[:, 256:512], AF.Copy), reads=[rbA], writes=[B_.rCBs])
                    yield
                    for hf in range(2):
                        P.emit("pool", lambda e, hf=hf: e.tensor_tensor(B_.Rt[:], tri[d][:].unsqueeze(1).broadcast_to([128, 8, 128]), dtA_all[:, ti, d * 16 + hf * 8:d * 16 + hf * 8 + 8].unsqueeze(2).broadcast_to([128, 8, 128]), ALU.mult), reads=[rK, rpre], writes=[B_.rRt])

                        def f(e):
                            for kb in range(2):
                                e.matmul(pL[:, kb * 4:(kb + 1) * 4, :].rearrange("p a b -> p (a b)"), onesf[:], B_.Rt[:, kb * 4:(kb + 1) * 4, :].rearrange("p a b -> p (a b)"), start=True, stop=False)
                                ins = e.matmul(pL[:, kb * 4:(kb + 1) * 4, :].rearrange("p a b -> p (a b)"), identf[:], msk[d][:], start=False, stop=True)
                            return ins
                        P.emit("pe", f, reads=[B_.rRt, rK, rC], writes=[rpL])
                        for hh in range(8):
                            h = hf * 8 + hh
                            P.emit("act", lambda e, h=h, hh=hh: e.activation(B_.LM[:, h, :], pL[:, hh, :], AF.Exp, bias=nac_all[:, ti, d * 16 + h:d * 16 + h + 1]), reads=[rpL, rpre], writes=[B_.rLM])
                        yield
                    P.emit("dve", lambda e: e.tensor_tensor(B_.MT[:].rearrange("p (g h) i -> p g h i", g=2), B_.LM[:].rearrange("p (g h) i -> p g h i", g=2), B_.CBs[:].unsqueeze(2).broadcast_to([128, 2, 8, 128]), ALU.mult), reads=[B_.rLM, B_.rCBs], writes=[B_.rMT])

                    def f(e):
                        for h in range(16):
                            ins = e.matmul(pY[:, h * 64:(h + 1) * 64], B_.MT[:, h, :], B_.xdt[:, h, :], start=True, stop=True)
                        return ins
                    P.emit("pe", f, reads=[B_.rMT, B_.rxdt, B_.ryo], writes=[rpY])
                    P.emit("dve", lambda e: e.tensor_tensor(B_.y[:], B_.yo[:].rearrange("p h q -> p (h q)"), pY[:], ALU.add), reads=[B_.ryo, rpY], writes=[B_.ry])
                    if d == 0:
                        P.emit("pool", lambda e: e.tensor_tensor(zs[:].rearrange("p (h q) -> p h q", h=16), B_.XS[:].rearrange("p (h q) -> p h q", h=16), dsk[:].unsqueeze(2).broadcast_to([128, 16, 64]), ALU.mult), reads=[B_.rXS, rK], writes=[rzs])
                        P.emit("dve", lambda e: e.tensor_tensor(B_.y[:], B_.y[:], zs[:], ALU.add), reads=[B_.ry, rzs], writes=[B_.ry])
                        P.dma("sp", YF[r0:r0 + 128, :], B_.y[:], reads=[B_.ry], writes=[rYF[gi]])
                    else:
                        P.dma("sp", YB[r0:r0 + 128, :], B_.y[:], reads=[B_.ry], writes=[rYB[gi]])

                for b in range(2):
                    base = b * TB
                    cc_ = 0
                    for j in range(12):
                        for (s0, Ls) in ((0, 256), (256, 2048)):
                            u_, ru_, a_, ra_ = u[cc_ % 2], ru[cc_ % 2], acc[cc_ % 2], racc[cc_ % 2]
                            cc_ += 1
                            P.dma("sp", u_[:, 0:Ls], XBC[j * 128:(j + 1) * 128, base + s0:base + s0 + Ls], reads=[rXBC[b]], writes=[ru_])
                            P.emit("dve", lambda e, j=j, Ls=Ls, u_=u_, a_=a_: e.tensor_scalar(a_[:, 0:Ls], u_[:, 0:Ls], cw[:, j * 3 + 1:j * 3 + 2], None, ALU.mult), reads=[ru_, rK], writes=[ra_])
                            P.emit("dve", lambda e, j=j, Ls=Ls, u_=u_, a_=a_: e.scalar_tensor_tensor(a_[:, 1:Ls], u_[:, 0:Ls - 1], cw[:, j * 3:j * 3 + 1], a_[:, 1:Ls], ALU.mult, ALU.add), reads=[ru_, rK, ra_], writes=[ra_])
                            P.emit("dve", lambda e, j=j, Ls=Ls, u_=u_, a_=a_: e.scalar_tensor_tensor(a_[:, 0:Ls - 1], u_[:, 1:Ls], cw[:, j * 3 + 2:j * 3 + 3], a_[:, 0:Ls - 1], ALU.mult, ALU.add), reads=[ru_, rK, ra_], writes=[ra_])
                            P.emit("act", lambda e, j=j, Ls=Ls, s0=s0, a_=a_: e.activation(XC[:, j, s0:s0 + Ls], a_[:, 0:Ls], AF.Silu, bias=cb[:, j:j + 1]), reads=[ra_, rK], writes=[rXC])
                    P.dma("act", dtr_all[:], DTR[base:base + TB, :].rearrange("(c p) f -> p c f", p=128), reads=[rDTR[b * 18 + i] for i in range(18)], writes=[rpre])
                    P.emit("dve", lambda e: e.tensor_tensor(dt_all[:], dtr_all[:], dtbs[:].unsqueeze(1).broadcast_to([128, 18, 32]), ALU.add), reads=[rpre, rK], writes=[rpre])
                    P.emit("act", lambda e: e.activation(dt_all[:], dt_all[:], AF.Exp), reads=[rpre], writes=[rpre])
                    P.emit("act", lambda e: e.activation(dt_all[:], dt_all[:], AF.Ln, bias=1.0), reads=[rpre], writes=[rpre])
                    P.emit("dve", lambda e: e.tensor_tensor(dtA_all[:], dt_all[:], abc[:].unsqueeze(1).broadcast_to([128, 18, 32]), ALU.mult), reads=[rpre, rK], writes=[rpre])

                    def f(e):
                        for d in range(2):
                            rhs = dtA_all[:, :, d * 16:(d + 1) * 16]
                            e.matmul(pSs[:, d * 512:d * 512 + 288].rearrange("p (c h) -> p c h", c=18), tri[d][:], rhs, start=True, stop=True)
                            ins = e.matmul(pY[:, d * 512:d * 512 + 288].rearrange("p (c h) -> p c h", c=18), onesf[:], rhs, start=True, stop=True)
                        return ins
                    P.emit("pe", f, reads=[rpre, rK, rC], writes=[rpSs, rpY])
                    for d in range(2):
                        cum = pSs[:, d * 512:d * 512 + 288].rearrange("p (c h) -> p c h", c=18)
                        tot = pY[:, d * 512:d * 512 + 288].rearrange("p (c h) -> p c h", c=18)
                        sl_ = slice(d * 16, (d + 1) * 16)
                        P.emit("dve", lambda e, cum=cum, sl_=sl_: e.tensor_scalar(nac_all[:, :, sl_], cum, -1.0, None, ALU.mult), reads=[rpSs], writes=[rpre])
                        P.emit("act", lambda e, cum=cum, sl_=sl_: e.activation(expA_all[:, :, sl_], cum, AF.Exp), reads=[rpSs], writes=[rpre])
                        P.emit("act", lambda e, tot=tot, sl_=sl_: e.activation(cd_all[:, :, sl_], tot, AF.Exp), reads=[rpY], writes=[rpre])
                        P.emit("dve", lambda e, tot=tot, sl_=sl_: e.tensor_tensor(dec_all[:, :, sl_], tot, nac_all[:, :, sl_], ALU.add), reads=[rpY, rpre], writes=[rpre])
                    P.emit("act", lambda e: e.activation(dec_all[:], dec_all[:], AF.Exp), reads=[rpre], writes=[rpre])
                    P.emit("dve", lambda e: e.tensor_tensor(dec_all[:], dec_all[:], dt_all[:], ALU.mult), reads=[rpre], writes=[rpre])
                    orders = [list(range(18)), [1, 0] + list(range(17, 1, -1))]
                    for d in range(2):
                        P.emit("dve", lambda e, d=d: e.memset(sets[d].S[:], 0.0), writes=[sets[d].rS])
                    for step in range(18):
                        gens = [chunk_pass(b, d, orders[d][step], sets[d]) for d in range(2)]
                        while gens:
                            for g_ in list(gens):
                                try:
                                    next(g_)
                                except StopIteration:
                                    gens.remove(g_)
                P.flush()
              with ExitStack() as st:
                sg_ = sbt(st, "ssdgs", [128, 1024], F32)
                rK = R()
                P.dma("sp", sg_[:], ssdg[l], writes=[rK])

                class MS:
                    pass
                ms = []
                for i in range(2):
                    m_ = MS()
                    for nm, shp, dt_t in (("yf", [128, 1024], F32), ("yb", [128, 1024], F32), ("zt", [128, 1024], BF16), ("zs", [128, 1024], F32),
                                          ("y16", [128, 1024], BF16), ("yT", [128, 8, 128], BF16), ("junk", [128, 1024], BF16),
                                          ("ss", [128, 1], F32), ("rstd", [128, 1], F32)):
                        setattr(m_, nm, sbt(st, "%s_m%d" % (nm, i), shp, dt_t))
                    m_.bT = pst(st, "bTm%d" % i, [128, 8, 128], BF16)
                    for nm in ("ryf", "ryb", "rzt", "rzs", "ry16", "ryT", "rtmp", "rbT"):
                        setattr(m_, nm, R())
                    ms.append(m_)
                for n_, (b, ti) in enumerate(tiles_l):
                    M_ = ms[n_ % 2]
                    gi = b * 18 + ti
                    r0 = b * TB + ti * 128

                    def mrg(M_=M_, gi=gi, r0=r0):
                        P.dma("act", M_.yf[:], YF[r0:r0 + 128, :], reads=[rYF[gi]], writes=[M_.ryf])
                        P.dma("act", M_.yb[:], YB[r0:r0 + 128, :], reads=[rYB[gi]], writes=[M_.ryb])
                        P.dma("act", M_.zt[:], ZT[r0:r0 + 128, :], reads=[rZT[gi]], writes=[M_.rzt])
                        P.emit("dve", lambda e: e.tensor_tensor(M_.yf[:], M_.yf[:], M_.yb[:], ALU.add), reads=[M_.ryf, M_.ryb], writes=[M_.ryf])
                        P.emit("dve", lambda e: e.tensor_tensor(M_.yf[:], M_.yf[:], M_.zt[:], ALU.mult), reads=[M_.ryf, M_.rzt], writes=[M_.ryf])
                        rms_rstd("act", M_.yf[:], 1024, M_.ss[:], M_.rstd[:], M_.junk[:], M_.ryf, M_.rtmp)
                        P.emit("act", lambda e: e.activation(M_.yf[:], M_.yf[:], AF.Copy, scale=M_.rstd[:]), reads=[M_.ryf, M_.rtmp], writes=[M_.ryf])
                        P.emit("dve", lambda e: e.tensor_tensor(M_.y16[:], M_.yf[:], sg_[:], ALU.mult), reads=[M_.ryf, rK], writes=[M_.ry16])

                        def f(e):
                            for j in range(8):
                                ins = e.transpose(M_.bT[:, j, :], M_.y16[:, j * 128:(j + 1) * 128], identb[:])
                            return ins
                        P.emit("pe", f, reads=[M_.ry16, rC], writes=[M_.rbT])
                        P.emit("act", lambda e: e.activation(M_.yT[:], M_.bT[:], AF.Copy), reads=[M_.rbT], writes=[M_.ryT])
                        P.dma("sp", MIX[0:1024, r0:r0 + 128].rearrange("(j p) t -> p j t", p=128), M_.yT[:], reads=[M_.ryT], writes=[rMIX[gi]])
                    mrg()
                P.flush()

            stWO = ExitStack()
            WO_pre = sbt(stWO, "WO", [128, 16, D], BF16)
            rWO_pre = R()
            for j in range(16):
                P.dma("pool", WO_pre[:, j, :], wo[l, j * 128:(j + 1) * 128, :], writes=[rWO_pre], max_dma_last_dim=4096)
            if phases is None or 4 in phases:
              with ExitStack() as st:
                KNs = sbt(st, "KNs", [128, 8, TB], BF16)
                KRs = [sbt(st, "KRs%d" % i, [128, TB], BF16) for i in range(2)]
                Vs = sbt(st, "Vs", [128, 18, 1024], BF16)
                rKV = R()
                QNs = [sbt(st, "QNs%d" % i, [128, 8, 512], BF16) for i in range(2)]
                QRs = [sbt(st, "QRs%d" % i, [128, 4, 512], BF16) for i in range(2)]
                rQ = [R(), R()]
                pS = [pst(st, "pSc%d" % i, [128, 512], F32) for i in range(4)]
                rpS = [R() for _ in range(4)]
                pO = [pst(st, "pOc%d" % i, [128, 512], F32) for i in range(2)]
                rpO = [R(), R()]
                pL = [pst(st, "pLc%d" % i, [128, 512], F32) for i in range(2)]
                rpL = [R(), R()]
                PT = [sbt(st, "PTc%d" % i, [128, 512], BF16) for i in range(6)]
                rPT = [R() for _ in range(6)]
                rl = [sbt(st, "rlc%d" % i, [128, 512], F32) for i in range(2)]
                rrl = [R(), R()]
                ot = [sbt(st, "otc%d" % i, [128, 512], BF16) for i in range(2)]
                rot = [R(), R()]
                cnt = [0, 0]

                def block_head(b, Qn, Qr, rq, h, nq, nkt, c0, gis):
                    u = cnt[1]
                    cnt[1] += 1
                    po, rpo, pl, rpl = pO[u % 2], rpO[u % 2], pL[u % 2], rpL[u % 2]
                    tiles = []

                    def score(kt):
                        i = cnt[0]
                        cnt[0] += 1
                        ps, rps = pS[i % 4], rpS[i % 4]
                        pt, rpt = PT[i % 6], rPT[i % 6]

                        def f(e):
                            e.matmul(ps[:, 0:nq], KNs[:, h, kt * 128:(kt + 1) * 128], Qn[:, h, 0:nq], start=True, stop=False)
                            return e.matmul(ps[:, 0:nq], KRs[h % 2][:, kt * 128:(kt + 1) * 128], Qr[:, h // 2, 0:nq], start=False, stop=True)
                        P.emit("pe", f, reads=[rq, rKV], writes=[rps])
                        P.emit("act", lambda e: e.activation(pt[:, 0:nq], ps[:, 0:nq], AF.Exp, scale=ATTN_SCALE), reads=[rps], writes=[rpt])
                        tiles.append((kt, pt, rpt))

                    def pv(idx):
                        kt, pt, rpt = tiles[idx]

                        def f(e):
                            e.matmul(po[:, 0:nq], Vs[:, kt, h * 128:(h + 1) * 128], pt[:, 0:nq], start=(idx == 0), stop=(idx == nkt - 1))
                            return e.matmul(pl[:, 0:nq], onesb[:], pt[:, 0:nq], start=(idx == 0), stop=(idx == nkt - 1))
                        P.emit("pe", f, reads=[rpt, rKV, rC], writes=[rpo, rpl])
                    DEPTH = 2
                    for kt in range(nkt):
                        score(kt)
                        if kt >= DEPTH:
                            pv(kt - DEPTH)
                    for idx in range(max(0, nkt - DEPTH), nkt):
                        pv(idx)
                    r_, rr_, o_, ro_ = rl[u % 2], rrl[u % 2], ot[u % 2], rot[u % 2]
                    P.emit("dve", lambda e: e.reciprocal(r_[:, 0:nq], pl[:, 0:nq]), reads=[rpl], writes=[rr_])
                    P.emit("dve", lambda e: e.tensor_tensor(o_[:, 0:nq], po[:, 0:nq], r_[:, 0:nq], ALU.mult), reads=[rpo, rr_], writes=[ro_])
                    P.dma("sp", MIX[1024 + h * 128:1024 + (h + 1) * 128, c0:c0 + nq], o_[:, 0:nq], reads=[ro_], writes=[rMIX[g] for g in gis])

                for b in range(2):
                    base = b * TB
                    P.dma("sp", KNs[:], KN[:, :, base:base + TB].rearrange("h p t -> p h t"), reads=[rQK[b]], writes=[rKV])
                    for i_ in range(2):
                        P.emit("dve", lambda e, i_=i_: e.memset(KRs[i_][:], 0.0), writes=[rKV])
                        P.dma("act", KRs[i_][i_ * 64:(i_ + 1) * 64, :], KR[i_ * 64:(i_ + 1) * 64, base:base + TB], reads=[rQK[b]], writes=[rKV])
                    for kt in range(18):
                        P.dma("sp" if kt % 2 else "act", Vs[:, kt, :], VV[base + kt * 128:base + (kt + 1) * 128, :], reads=[rQK[b]], writes=[rKV])
                    blocks = [(2, 4), (6, 4), (10, 4), (14, 4)]
                    if not last:
                        blocks = [(0, 2)] + blocks
                    for bi, (t0, nt) in enumerate(blocks):
                        nq = nt * 128
                        nkt = 2 if t0 == 0 else 18
                        c0 = base + t0 * 128
                        Qn, Qr, rq = QNs[bi % 2], QRs[bi % 2], rQ[bi % 2]
                        P.dma("act", Qn[:, :, 0:nq], QN[:, :, c0:c0 + nq].rearrange("h p t -> p h t"), reads=[rQK[b]], writes=[rq])
                        P.dma("act", Qr[:, :, 0:nq], QR[:, :, c0:c0 + nq].rearrange("h p t -> p h t"), reads=[rQK[b]], writes=[rq])
                        gis = [b * 18 + t0 + i for i in range(nt)]
                        for h in range(8):
                            block_head(b, Qn, Qr, rq, h, nq, nkt, c0, gis)
                P.flush()

            if phases is None or 5 in phases:
              with ExitStack() as st:
                WO = WO_pre
                rW = rWO_pre
                RW = sbt(st, "RW", [128, 16, 16], F32)
                rbs = sbt(st, "rbs", [128, 16], F32)
                P.dma("sp", RW[:], rwT, writes=[rW])
                P.dma("sp", rbs[:], rb, writes=[rW])
                G1 = build_gate(st, "g1", 32, [0, 1] if last else [0, 1, 2])
                pO = pst(st, "pOd", [128, D], F32)
                rpO = R()
                pT = [pst(st, "pTd%d" % i, [128, 4, 128], F32) for i in range(2)]
                rpT = [R(), R()]
                pR = pst(st, "pRd", [128, 16], F32)
                rpR = R()

                class DS:
                    pass
                dsets = []
                for i in range(2):
                    s_ = DS()
                    for nm, shp, dt_t in (("mixT", [128, 16, 128], BF16), ("xt", [128, D], F32), ("tt", [128, D], F32), ("xs", [128, D], F32),
                                          ("junk", [128, D], BF16), ("ss", [128, 1], F32), ("rstd", [128, 1], F32),
                                          ("h2f", [128, 16, 128], F32), ("h2b", [128, 16, 128], BF16),
                                          ("sc", [128, 16], F32), ("sel", [128, 16], F32), ("pr6", [128, 4, 6], F32), ("gs", [128, 4], F32),
                                          ("gmx", [128, 1], F32), ("gmk", [128, 4], F32), ("mk", [128, 16], F32), ("m1", [128, 16], F32),
                                          ("m2", [128, 16], F32), ("t1", [128, 1], F32), ("cmb", [128, 16], F32)):
                        setattr(s_, nm, sbt(st, "%s_D%d" % (nm, i), shp, dt_t))
                    for nm in ("rmixT", "rxt", "rtt", "rxs", "rtmp", "rh2f", "rh2b", "rr"):
                        setattr(s_, nm, R())
                    dsets.append(s_)

                def dtile(S_, b, ti):
                    gi = b * 18 + ti
                    r0 = b * TB + ti * 128
                    c = 2 if ti < 2 else b
                    g1, rg1 = G1[c]
                    P.dma("act", S_.mixT[:], MIX[:, r0:r0 + 128].rearrange("(j p) t -> p j t", p=128), reads=[rMIX[gi]], writes=[S_.rmixT])
                    P.dma("act", S_.xt[:], resid_src(l, b, ti), reads=[rRES[gi]], writes=[S_.rxt])

                    def f(e):
                        for nb in range(4):
                            for j in range(16):
                                ins = e.matmul(pO[:, nb * 512:(nb + 1) * 512], S_.mixT[:, j, :], WO[:, j, nb * 512:(nb + 1) * 512], start=(j == 0), stop=(j == 15))
                        return ins
                    P.emit("pe", f, reads=[S_.rmixT, rW], writes=[rpO])

                def dtile1b(S_, b, ti):
                    gi = b * 18 + ti
                    r0 = b * TB + ti * 128
                    c = 2 if ti < 2 else b
                    g1, rg1 = G1[c]
                    P.emit("dve", lambda e: e.tensor_tensor(S_.tt[:], pO[:], g1[:], ALU.mult), reads=[rpO, rg1], writes=[S_.rtt])
                    P.emit("dve", lambda e: e.tensor_tensor(S_.xt[:], S_.xt[:], S_.tt[:], ALU.add), reads=[S_.rxt, S_.rtt], writes=[S_.rxt])
                    P.dma("sp", RES[r0:r0 + 128, :], S_.xt[:], reads=[S_.rxt], writes=[rRES[gi]])
                    rms_rstd("act", S_.xt[:], D, S_.ss[:], S_.rstd[:], S_.junk[:], S_.rxt, S_.rtmp)
                    P.emit("act", lambda e: e.activation(S_.xs[:], S_.xt[:], AF.Copy, scale=S_.rstd[:]), reads=[S_.rxt, S_.rtmp], writes=[S_.rxs])

                def dtile2(S_, b, ti):
                    gi = b * 18 + ti
                    r0 = b * TB + ti * 128
                    c = 2 if ti < 2 else b
                    for r4 in range(4):
                        p_, rp_ = pT[r4 % 2], rpT[r4 % 2]

                        def f(e, r4=r4, p_=p_):
                            for jj in range(4):
                                j = r4 * 4 + jj
                                ins = e.transpose(p_[:, jj, :], S_.xs[:, j * 128:(j + 1) * 128], identf[:])
                            return ins
                        P.emit("pe", f, reads=[S_.rxs, rC], writes=[rp_])
                        for jj in range(4):
                            j = r4 * 4 + jj
                            if jj % 2 == 0:
                                P.emit("dve", lambda e, p_=p_, jj=jj, j=j: e.tensor_scalar(S_.h2f[:, j, :], p_[:, jj, :], gm2[:, j, c:c + 1], modS[:, 48 + j, c:c + 1], ALU.mult, ALU.add), reads=[rp_, rmod], writes=[S_.rh2f])
                            else:
                                P.emit("act", lambda e, p_=p_, jj=jj, j=j: e.activation(S_.h2f[:, j, :], p_[:, jj, :], AF.Identity, scale=gm2[:, j, c:c + 1], bias=modS[:, 48 + j, c:c + 1]), reads=[rp_, rmod], writes=[S_.rh2f])
                    P.emit("dve", lambda e: e.tensor_copy(S_.h2b[:], S_.h2f[:]), reads=[S_.rh2f], writes=[S_.rh2b])
                    P.dma("sp", HT[:, r0:r0 + 128].rearrange("(j p) t -> p j t", p=128), S_.h2b[:], reads=[S_.rh2b], writes=[rHT[gi]])

                    def f(e):
                        for j in range(16):
                            ins = e.matmul(pR[:], S_.h2f[:, j, :], RW[:, j, :], start=(j == 0), stop=(j == 15))
                        return ins
                    P.emit("pe", f, reads=[S_.rh2f, rW], writes=[rpR])

                def dtile2b(S_, b, ti):
                    gi = b * 18 + ti
                    r0 = b * TB + ti * 128
                    P.emit("act", lambda e: e.activation(S_.sc[:], pR[:], AF.Sigmoid), reads=[rpR], writes=[S_.rr])
                    V = lambda fn, rd=(): P.emit("dve", fn, reads=[S_.rr] + list(rd), writes=[S_.rr])
                    sc, sel, pr6, gs, gmx, gmk, mk, m1, m2, t1, cmb = S_.sc, S_.sel, S_.pr6, S_.gs, S_.gmx, S_.gmk, S_.mk, S_.m1, S_.m2, S_.t1, S_.cmb
                    V(lambda e: e.tensor_tensor(sel[:], sc[:], rbs[:], ALU.add), [rW])
                    s4 = sel[:].rearrange("p (g k) -> p g k", g=4)
                    V(lambda e: e.tensor_tensor(pr6[:, :, 0:3], s4[:, :, 0:3], s4[:, :, 1:4], ALU.add))
                    V(lambda e: e.tensor_tensor(pr6[:, :, 3:5], s4[:, :, 0:2], s4[:, :, 2:4], ALU.add))
                    V(lambda e: e.tensor_tensor(pr6[:, :, 5:6], s4[:, :, 0:1], s4[:, :, 3:4], ALU.add))
                    V(lambda e: e.tensor_reduce(gs[:], pr6[:], AX.X, ALU.max))
                    V(lambda e: e.tensor_reduce(gmx[:], gs[:], AX.X, ALU.max))
                    V(lambda e: e.tensor_scalar(gmk[:], gs[:], gmx[:], None, ALU.is_ge))
                    V(lambda e: e.tensor_tensor(mk[:].rearrange("p (g k) -> p g k", g=4), s4, gmk[:].unsqueeze(2).broadcast_to([128, 4, 4]), ALU.mult))
                    V(lambda e: e.tensor_scalar(gmk[:], gmk[:], -1.0, 10.0, ALU.add, ALU.mult))
                    V(lambda e: e.tensor_tensor(mk[:].rearrange("p (g k) -> p g k", g=4), mk[:].rearrange("p (g k) -> p g k", g=4), gmk[:].unsqueeze(2).broadcast_to([128, 4, 4]), ALU.add))
                    V(lambda e: e.tensor_reduce(t1[:], mk[:], AX.X, ALU.max))
                    V(lambda e: e.tensor_scalar(m1[:], mk[:], t1[:], None, ALU.is_ge))
                    V(lambda e: e.scalar_tensor_tensor(mk[:], m1[:], -20.0, mk[:], ALU.mult, ALU.add))
                    V(lambda e: e.tensor_reduce(t1[:], mk[:], AX.X, ALU.max))
                    V(lambda e: e.tensor_scalar(m2[:], mk[:], t1[:], None, ALU.is_ge))
                    V(lambda e: e.tensor_tensor(m1[:], m1[:], m2[:], ALU.add))
                    V(lambda e: e.tensor_tensor(m1[:], m1[:], sc[:], ALU.mult))
                    V(lambda e: e.tensor_reduce(t1[:], m1[:], AX.X, ALU.add))
                    V(lambda e: e.reciprocal(t1[:], t1[:]))
                    V(lambda e: e.tensor_scalar(cmb[:], m1[:], t1[:], None, ALU.mult))
                    P.dma("sp", COMB[r0:r0 + 128, :], cmb[:], reads=[S_.rr], writes=[rCOMB[gi]])

                prev = None
                for n_, (b, ti) in enumerate(tiles_l):
                    cur = (dsets[n_ % 2], b, ti)
                    dtile(*cur)
                    if prev is not None:
                        dtile2(*prev)
                    dtile1b(*cur)
                    if prev is not None:
                        dtile2b(*prev)
                    prev = cur
                dtile2(*prev)
                dtile2b(*prev)
                P.flush()

            stWO.close()
            if phases is None or 6 in phases:
              with ExitStack() as st:
                xt_tiles = [(b, ti) for b in range(2) for ti in range(2, 18)]
                sblocks = [xt_tiles[i * 8:(i + 1) * 8] for i in range(4)]
                if not last:
                    sblocks.append([(0, 0), (0, 1), (1, 0), (1, 1)])
                WG = sbt(st, "WG", [128, 16, 512], BF16)
                WU = sbt(st, "WU", [128, 16, 512], BF16)
                WD = sbt(st, "WD", [128, 4, D], BF16)
                rWG, rWD = R(), R()
                h2 = sbt(st, "h2", [128, 16, 1024], BF16)
                rh2 = R()
                accm = sbt(st, "accm", [128, 8, D], F32)
                racc = [R() for _ in range(8)]
                cmb = sbt(st, "cmbm", [128, 8, 16], F32)
                rcmb = R()
                actT = sbt(st, "actT", [128, 4, 1024], BF16)
                ract = R()
                sgl = [sbt(st, "sgl%d" % i, [128, 512], F32) for i in range(2)]
                rsgl = [R(), R()]
                pGU = [pst(st, "pGU%d" % i, [128, 2, 512], F32) for i in range(2)]
                rpGU = [R(), R()]
                pY = [pst(st, "pYm%d" % i, [128, 1024], F32) for i in range(2)]
                rpY = [R(), R()]
                xts = [sbt(st, "xtm%d" % i, [128, D], F32) for i in range(2)]
                rxts = [R(), R()]
                xcnt = 0
                junk = sbt(st, "junkm", [128, D], BF16)
                ss = sbt(st, "ssm", [128, 1], F32)
                rstd = sbt(st, "rstdm", [128, 1], F32)
                rtmp = R()
                fg = sbt(st, "fg", [128, D], F32)
                rfg = R()
                if last:
                    P.dma("sp", fg[:], fng, writes=[rfg])
                g2buf = (sbt(st, "g2b", [128, D], F32), R(), sbt(st, "g2dg", [128, 128], F32), R())
                g2cond = None
                kq = 0
                for sbk in sblocks:
                    nt = len(sbk)
                    nh = nt // 4
                    for i, (b, ti) in enumerate(sbk):
                        gi = b * 18 + ti
                        r0 = b * TB + ti * 128
                        P.dma("sp", h2[:, :, i * 128:(i + 1) * 128], HT[:, r0:r0 + 128].rearrange("(j p) t -> p j t", p=128), reads=[rHT[gi]], writes=[rh2])
                        P.dma("sp", cmb[:, i, :], COMB[r0:r0 + 128, :], reads=[rCOMB[gi]], writes=[rcmb])
                    for ex in range(16):
                        P.dma("pool", WG[:], wg[l, ex].rearrange("(j p) f -> p j f", p=128), writes=[rWG])
                        P.dma("pool", WU[:], wu[l, ex].rearrange("(j p) f -> p j f", p=128), writes=[rWG])
                        for j in range(4):
                            P.dma("pool", WD[:, j, :], wd[l, ex, j * 128:(j + 1) * 128, :], writes=[rWD], max_dma_last_dim=4096)
                        for hb in range(nh):
                            for fc in range(4):
                                pg, rpg = pGU[kq % 2], rpGU[kq % 2]
                                sg, rsg = sgl[kq % 2], rsgl[kq % 2]
                                kq += 1

                                def f(e, pg=pg, fc=fc, hb=hb):
                                    for j in range(16):
                                        e.matmul(pg[:, 0, :], WG[:, j, fc * 128:(fc + 1) * 128], h2[:, j, hb * 512:(hb + 1) * 512], start=(j == 0), stop=(j == 15))
                                    for j in range(16):
                                        ins = e.matmul(pg[:, 1, :], WU[:, j, fc * 128:(fc + 1) * 128], h2[:, j, hb * 512:(hb + 1) * 512], start=(j == 0), stop=(j == 15))
                                    return ins
                                P.emit("pe", f, reads=[rWG, rh2], writes=[rpg])
                                P.emit("act", lambda e, pg=pg, sg=sg: e.activation(sg[:], pg[:, 0, :], AF.Silu), reads=[rpg], writes=[rsg])
                                P.emit("dve", lambda e, pg=pg, sg=sg, fc=fc, hb=hb: e.tensor_tensor(actT[:, fc, hb * 512:(hb + 1) * 512], sg[:], pg[:, 1, :], ALU.mult), reads=[rpg, rsg], writes=[ract])
                        for i in range(nt):
                            for dh in range(2):
                                py, rpy = pY[kq % 2], rpY[kq % 2]
                                kq += 1

                                def f(e, py=py, i=i, dh=dh):
                                    for nb in range(2):
                                        for fc in range(4):
                                            ins = e.matmul(py[:, nb * 512:(nb + 1) * 512], actT[:, fc, i * 128:(i + 1) * 128], WD[:, fc, dh * 1024 + nb * 512:dh * 1024 + (nb + 1) * 512], start=(fc == 0), stop=(fc == 3))
                                    return ins
                                P.emit("pe", f, reads=[ract, rWD], writes=[rpy])
                                a_ = accm[:, i, dh * 1024:(dh + 1) * 1024]
                                if ex == 0:
                                    P.emit("dve", lambda e, py=py, a_=a_, i=i, ex=ex: e.tensor_scalar(a_, py[:], cmb[:, i, ex:ex + 1], None, ALU.mult), reads=[rpy, rcmb], writes=[racc[i]])
                                else:
                                    P.emit("dve", lambda e, py=py, a_=a_, i=i, ex=ex: e.scalar_tensor_tensor(a_, py[:], cmb[:, i, ex:ex + 1], a_, ALU.mult, ALU.add), reads=[rpy, rcmb, racc[i]], writes=[racc[i]])
                    for i, (b, ti) in enumerate(sbk):
                        gi = b * 18 + ti
                        r0 = b * TB + ti * 128
                        c = 2 if ti < 2 else b
                        if g2cond != c:
                            build_gate(st, "g2", 80, [c], pg=pY[0], rpg=rpY[0], gbuf=g2buf)
                            g2cond = c
                        g2, rg2 = g2buf[0], g2buf[1]
                        xt, rxt = xts[xcnt % 2], rxts[xcnt % 2]
                        xcnt += 1
                        P.dma("act", xt[:], RES[r0:r0 + 128, :], reads=[rRES[gi]], writes=[rxt])
                        P.emit("dve", lambda e, i=i, g2=g2: e.tensor_tensor(accm[:, i, :], accm[:, i, :], g2[:], ALU.mult), reads=[racc[i], rg2], writes=[racc[i]])
                        P.emit("dve", lambda e, i=i, xt=xt: e.tensor_tensor(xt[:], xt[:], accm[:, i, :], ALU.add), reads=[racc[i], rxt], writes=[rxt])
                        if not last:
                            P.dma("sp", RES[r0:r0 + 128, :], xt[:], reads=[rxt], writes=[rRES[gi]])
                        else:
                            rms_rstd("act", xt[:], D, ss[:], rstd[:], junk[:], rxt, rtmp)
                            P.emit("act", lambda e, xt=xt: e.activation(xt[:], xt[:], AF.Copy, scale=rstd[:]), reads=[rxt, rtmp], writes=[rxt])
                            P.emit("dve", lambda e, xt=xt: e.tensor_tensor(xt[:], xt[:], fg[:], ALU.mult), reads=[rxt, rfg], writes=[rxt])
                            P.dma("sp", out[b, (ti - 2) * 128:(ti - 1) * 128, :], xt[:], reads=[rxt], writes=[rRES[gi]])
                P.flush()

        P.finish()
        P.flush()
    return nc


def _rope_tables():
    t = np.arange(2048)
    rows = (t // 64).astype(np.float32)
    cols = (t % 64).astype(np.float32)
    nf = 16
    inv = (np.float32(10000.0) ** (-np.arange(nf, dtype=np.float32) / nf)).astype(np.float32)
    ang = np.stack([rows[:, None] * inv, cols[:, None] * inv], axis=1)
    cos = np.cos(ang).astype(np.float32)
    sin = np.sin(ang).astype(np.float32)
    C = np.zeros((64, 2048), np.float32)
    S = np.zeros((64, 2048), np.float32)
    for a in range(2):
        for b in range(2):
            for f in range(16):
                idx = a * 32 + b * 16 + f
                C[idx] = cos[:, a, f]
                S[idx] = sin[:, a, f] * (-1.0 if b == 0 else 1.0)
    return np.concatenate([C, C], 0), np.concatenate([S, S], 0)


def _swap_perm():
    perm = np.zeros(64, np.int64)
    for a in range(2):
        for b in range(2):
            for f in range(16):
                perm[a * 32 + b * 16 + f] = a * 32 + (1 - b) * 16 + f
    return perm


def prep_shared(inp):
    f = lambda a: np.ascontiguousarray(np.asarray(a, dtype=np.float32))
    perm = _swap_perm()
    w_in = f(inp["w_in"])
    kpe = w_in[:, :, 3360:3424]
    kpes = kpe[:, :, perm]
    win = np.concatenate([w_in[:, :, :3360], kpe, kpe, kpes, kpes], axis=2)
    assert win.shape[2] == NWIN
    wqb = f(inp["w_q_b"]).reshape(L, 512, 8, 192)
    nope = wqb[:, :, :, :128].reshape(L, 512, 1024)
    rope = wqb[:, :, :, 128:]
    wq = np.concatenate([nope, rope.reshape(L, 512, 512), rope[:, :, :, perm].reshape(L, 512, 512)], axis=2)
    wkvb = f(inp["w_kv_b"]).reshape(L, 256, 8, 256)
    wkv = np.concatenate([wkvb[:, :, :, :128].reshape(L, 256, 1024), wkvb[:, :, :, 128:].reshape(L, 256, 1024)], axis=2)
    colT = lambda v, n: np.ascontiguousarray(f(v).reshape(L, n, 128).transpose(0, 2, 1))
    bc = lambda v: np.ascontiguousarray(np.broadcast_to(f(v).reshape(L, 1, -1), (L, 128, f(v).reshape(L, -1).shape[1])))
    convw = f(inp["conv_w"])
    convw_l = np.ascontiguousarray(convw.reshape(L, 3, 12, 128).transpose(0, 3, 2, 1).reshape(L, 128, 36))
    cosT, sinT = _rope_tables()
    i_ = np.arange(128)
    tri_f = (i_[:, None] <= i_[None, :]).astype(np.float32)
    tri_b = (i_[:, None] >= i_[None, :]).astype(np.float32)
    mask_f = np.where(i_[None, :] >= i_[:, None], 0.0, NEG).astype(np.float32)
    mask_b = np.where(i_[None, :] <= i_[:, None], 0.0, NEG).astype(np.float32)
    sh = {
        "ada_w": f(inp["ada_w"]),
        "ada_bT": colT(inp["ada_b"], 96),
        "g1T": colT(inp["norm1_g"], 16), "g2T": colT(inp["norm2_g"], 16),
        "win": np.ascontiguousarray(win),
        "convw": convw_l, "convb": colT(inp["conv_b"], 12),
        "dtb": bc(inp["dt_bias"]), "alog": bc(inp["a_log"]), "dskip": bc(inp["d_skip"]),
        "ssdg": bc(inp["ssd_norm_g"]),
        "qgT": colT(inp["q_norm_g"], 4), "wq": np.ascontiguousarray(wq),
        "kvgT": colT(inp["kv_norm_g"], 2), "wkv": np.ascontiguousarray(wkv),
        "wo": f(inp["w_o"]),
        "rwT": np.ascontiguousarray(f(inp["router_w"]).reshape(16, 128, 16).transpose(1, 0, 2)),
        "rb": np.ascontiguousarray(np.broadcast_to(f(inp["router_b"]).reshape(1, 16), (128, 16))),
        "wg": f(inp["w_gate"]), "wu": f(inp["w_up"]), "wd": f(inp["w_down"]),
        "fng": np.ascontiguousarray(np.broadcast_to(f(inp["final_norm_g"]).reshape(1, D), (128, D))),
        "c_ident": np.eye(128, dtype=np.float32),
        "c_tri": np.stack([tri_f, tri_b]),
        "c_mask": np.stack([np.tile(mask_f, (1, 4)), np.tile(mask_b, (1, 4))]),
        "c_cos": cosT, "c_sin": sinT,
    }
    return sh


def core_inputs(inp, sh, core):
    f = lambda a: np.ascontiguousarray(np.asarray(a, dtype=np.float32))
    b0 = core * 2
    cc = np.stack([f(inp["c"])[b0], f(inp["c"])[b0 + 1], f(inp["c_ctx"])], axis=1)
    m = dict(sh)
    m["xin"] = f(inp["x"][b0:b0 + 2])
    m["cin"] = f(inp["ctx"][b0:b0 + 2])
    m["ccT"] = np.ascontiguousarray(cc.reshape(16, 128, 3).transpose(1, 0, 2))
    return m


_NC = None
_SKIP = set()


def kernel(**inputs):
    global _NC
    if _NC is None:
        _NC = build()
    sh = prep_shared(inputs)
    in_maps = [core_inputs(inputs, sh, c) for c in range(8)]
    res = run_bass_kernel_spmd(_NC, in_maps, core_ids=list(range(8)))
    return np.concatenate([np.asarray(r["out"]) for r in res.results], axis=0).astype(np.float32)
```
